# Optimizing a Trainium2 kernel written in Bass

```python
import math
import jax, jax.numpy as jnp
from jax import lax
import numpy as np

D_MODEL = 2048
BATCH = 4
SEQ = 4096
DEPTH = 1

CHUNK = 64
D_MIX = D_MODEL
NORM_EPS = 1e-6
DSA_WIDTH = D_MIX // 2
DSA_HEAD_DIM = 128
DSA_HEADS = DSA_WIDTH // DSA_HEAD_DIM
IDX_HEADS = 16
IDX_DIM = 64
TOPK_MAX = 256
ROPE_THETA = 10000.0
RWKV_WIDTH = D_MIX - DSA_WIDTH
RWKV_HEAD_DIM = 64
RWKV_HEADS = RWKV_WIDTH // RWKV_HEAD_DIM
DECAY_LORA = 64
AICL_LORA = 64
GATE_LORA = 160
GN_EPS = 64e-5
N_GROUPS = 4
EXPERTS_PER_GROUP = 4
N_EXPERTS = N_GROUPS * EXPERTS_PER_GROUP
TOP_K_IN_GROUP = 2
D_EXPERT = 512
DSA_SPLITS = (DSA_WIDTH, DSA_WIDTH, DSA_WIDTH, IDX_HEADS * IDX_DIM, IDX_DIM, IDX_HEADS)
RWKV_SPLITS = (RWKV_WIDTH, DECAY_LORA, RWKV_WIDTH, RWKV_WIDTH, AICL_LORA, GATE_LORA)
DSA_COLS = sum(DSA_SPLITS)
RWKV_COLS = sum(RWKV_SPLITS)
IN_COLS = DSA_COLS + RWKV_COLS

kernel_name = 'hybrid_dsa_rwkv7_hmoe_block'


def split_cols(p, sizes):
    cuts = np.cumsum(sizes)[:-1].tolist()
    return jnp.split(p, cuts, axis=-1)


def rms_norm(x, g, eps=NORM_EPS):
    xf = x.astype(jnp.float32)
    y = xf * lax.rsqrt(jnp.mean(xf * xf, axis=-1, keepdims=True) + eps)
    return (y * g.astype(jnp.float32)).astype(x.dtype)


def rope(x, pos):
    d = x.shape[-1]
    half = d // 2
    inv = ROPE_THETA ** (-jnp.arange(half, dtype=jnp.float32) * (2.0 / d))
    ang = pos.astype(jnp.float32)[:, None] * inv[None, :]
    cos = jnp.cos(ang)[None, :, None, :]
    sin = jnp.sin(ang)[None, :, None, :]
    xf = x.astype(jnp.float32)
    x1, x2 = xf[..., :half], xf[..., half:]
    out = jnp.concatenate([x1 * cos - x2 * sin, x2 * cos + x1 * sin], axis=-1)
    return out.astype(x.dtype)


def token_shift_mix(p, mu):
    prev = jnp.pad(p, ((0, 0), (1, 0), (0, 0)))[:, :-1]
    return p + (prev - p) * mu


def dsa_mixer(q, k, v, q_idx, k_idx, w_idx, q_gain, k_gain):
    B, S = q.shape[0], q.shape[1]
    f32 = jnp.float32
    pos = jnp.arange(S)
    q = rope(rms_norm(q.reshape(B, S, DSA_HEADS, DSA_HEAD_DIM), q_gain), pos)
    k = rope(rms_norm(k.reshape(B, S, DSA_HEADS, DSA_HEAD_DIM), k_gain), pos)
    v = v.reshape(B, S, DSA_HEADS, DSA_HEAD_DIM)
    q_idx = rope(q_idx.reshape(B, S, IDX_HEADS, IDX_DIM), pos)
    k_idx = rope(k_idx[:, :, None, :], pos)[:, :, 0]
    w_idx = w_idx.astype(f32) * (IDX_HEADS ** -0.5 * IDX_DIM ** -0.5)
    top_k = min(TOPK_MAX, S // 4)
    n_blocks = S // CHUNK
    key_pos = jnp.arange(S)
    scale = DSA_HEAD_DIM ** -0.5

    def block(i):
        start = i * CHUNK
        qi_idx = lax.dynamic_slice_in_dim(q_idx, start, CHUNK, axis=1)
        wi = lax.dynamic_slice_in_dim(w_idx, start, CHUNK, axis=1)
        qi = lax.dynamic_slice_in_dim(q, start, CHUNK, axis=1)
        iscore = jnp.einsum('bqhd,bsd->bqhs', qi_idx, k_idx, preferred_element_type=f32)
        iscore = jnp.einsum('bqhs,bqh->bqs', jax.nn.relu(iscore), wi)
        admissible = key_pos < start + CHUNK
        iscore = jnp.where(admissible[None, None, :], iscore, -jnp.inf)
        sel_score, sel = lax.top_k(iscore, top_k)
        valid = jnp.isfinite(sel_score)
        k_sel = jax.vmap(lambda kb, ib: kb[ib])(k, sel)
        v_sel = jax.vmap(lambda vb, ib: vb[ib])(v, sel)
        logits = jnp.einsum('bqhd,bqkhd->bhqk', qi, k_sel, preferred_element_type=f32) * scale
        logits = jnp.where(valid[:, None, :, :], logits, -jnp.inf)
        probs = jax.nn.softmax(logits, axis=-1)
        return jnp.einsum('bhqk,bqkhd->bqhd', probs.astype(v.dtype), v_sel)

    out = lax.map(block, jnp.arange(n_blocks))
    return out.transpose(1, 0, 2, 3, 4).reshape(B, S, DSA_WIDTH)


def rwkv7_mixer(r, d_w, k, v, d_a, d_g, w0, w_decay_up, a0, w_aicl_up, w_gate_lora_up,
                k_k, k_a, r_k, ln_x_w, ln_x_b, out_dtype):
    B, S = r.shape[0], r.shape[1]
    H, N = RWKV_HEADS, RWKV_HEAD_DIM
    f32 = jnp.float32
    w = -jax.nn.softplus(-(w0 + jnp.tanh(d_w) @ w_decay_up)) - 0.5
    decay = jnp.exp(-jnp.exp(w.astype(f32)))
    a = jax.nn.sigmoid((a0 + d_a @ w_aicl_up).astype(f32))
    g = (jax.nn.sigmoid(d_g) @ w_gate_lora_up).astype(f32)
    heads = lambda t: t.astype(f32).reshape(B, S, H, N)
    kk = heads(k * k_k)
    kk = kk / jnp.maximum(jnp.sqrt(jnp.sum(kk * kk, axis=-1, keepdims=True)), 1e-12)
    k = k.astype(f32) * (1.0 + (a - 1.0) * k_a.astype(f32))
    r_h, k_h, v_h, w_h, a_h = heads(r), heads(k), heads(v), heads(decay), heads(a)
    b_h = kk * a_h

    def step(state, inp):
        r_t, w_t, k_t, v_t, kk_t, b_t = inp
        sa = jnp.einsum('bhvk,bhk->bhv', state, -kk_t)
        state = (state * w_t[:, :, None, :] + sa[..., None] * b_t[:, :, None, :]
                 + v_t[..., None] * k_t[:, :, None, :])
        return state, jnp.einsum('bhvk,bhk->bhv', state, r_t)

    xs = tuple(t.transpose(1, 0, 2, 3) for t in (r_h, w_h, k_h, v_h, kk, b_h))
    _, ys = lax.scan(step, jnp.zeros((B, H, N, N), f32), xs)
    y = ys.transpose(1, 0, 2, 3)
    mu = jnp.mean(y, axis=-1, keepdims=True)
    var = jnp.mean(jnp.square(y - mu), axis=-1, keepdims=True)
    y = ((y - mu) * lax.rsqrt(var + GN_EPS)).reshape(B, S, RWKV_WIDTH)
    y = y * ln_x_w.astype(f32) + ln_x_b.astype(f32)
    bonus = jnp.sum(r_h * k_h * r_k.astype(f32), axis=-1, keepdims=True) * v_h
    y = (y + bonus.reshape(B, S, RWKV_WIDTH)) * g
    return y.astype(out_dtype)


def hier_moe(h, w_route_group, b_route_group, w_route_expert, b_route_expert,
             w_e_gate, w_e_up, w_e_down):
    B, S, D = h.shape
    f32 = jnp.float32
    t = h.reshape(B * S, D)
    grp_prob = jax.nn.softmax((t @ w_route_group).astype(f32) + b_route_group.astype(f32), axis=-1)
    p_grp, g_sel = lax.top_k(grp_prob, 1)
    exp_logits = ((t @ w_route_expert).astype(f32) + b_route_expert.astype(f32))
    exp_logits = exp_logits.reshape(-1, N_GROUPS, EXPERTS_PER_GROUP)
    exp_logits = jnp.take_along_axis(exp_logits, g_sel[:, :, None], axis=1)[:, 0]
    p_exp, e_sel = lax.top_k(jax.nn.softmax(exp_logits, axis=-1), TOP_K_IN_GROUP)
    p_exp = p_exp / jnp.sum(p_exp, axis=-1, keepdims=True)
    gate_w = p_grp * p_exp
    expert_id = g_sel * EXPERTS_PER_GROUP + e_sel
    combine = jnp.sum(jax.nn.one_hot(expert_id, N_EXPERTS, dtype=f32) * gate_w[..., None], axis=1)
    y = jnp.zeros((B * S, D), f32)
    for e in range(N_EXPERTS):
        he = jax.nn.silu(t @ w_e_gate[e]) * (t @ w_e_up[e])
        y = y + combine[:, e:e + 1] * (he @ w_e_down[e]).astype(f32)
    return y.reshape(B, S, D).astype(h.dtype)


def setup_inputs(seed: int = 0) -> dict:
    key = jax.random.key(seed)
    ks = jax.random.split(key, 32)
    f32 = jnp.float32
    nrm = lambda k, shape, s: jax.random.normal(k, shape, f32) * s
    L, D = DEPTH, D_MODEL
    return {
        'x': jax.random.normal(ks[0], (BATCH, SEQ, D), f32),
        'g_mix': 1.0 + nrm(ks[1], (L, D), 0.02),
        'w_in': nrm(ks[2], (L, D, IN_COLS), D ** -0.5),
        'rwkv_shift_mix': jax.random.uniform(ks[3], (L, RWKV_COLS), f32),
        'q_gain': 1.0 + nrm(ks[4], (L, DSA_HEAD_DIM), 0.02),
        'k_gain': 1.0 + nrm(ks[5], (L, DSA_HEAD_DIM), 0.02),
        'w0': jax.random.uniform(ks[6], (L, RWKV_WIDTH), f32, -6.0, -1.0),
        'w_decay_up': nrm(ks[7], (L, DECAY_LORA, RWKV_WIDTH), 0.1),
        'a0': nrm(ks[8], (L, RWKV_WIDTH), 0.1),
        'w_aicl_up': nrm(ks[9], (L, AICL_LORA, RWKV_WIDTH), 0.5 * AICL_LORA ** -0.5),
        'w_gate_lora_up': nrm(ks[10], (L, GATE_LORA, RWKV_WIDTH), GATE_LORA ** -0.5),
        'k_k': 0.85 + nrm(ks[11], (L, RWKV_WIDTH), 0.02),
        'k_a': 1.0 + nrm(ks[12], (L, RWKV_WIDTH), 0.02),
        'r_k': nrm(ks[13], (L, RWKV_HEADS, RWKV_HEAD_DIM), 0.1),
        'ln_x_w': 1.0 + nrm(ks[14], (L, RWKV_WIDTH), 0.02),
        'ln_x_b': nrm(ks[15], (L, RWKV_WIDTH), 0.01),
        'w_out': nrm(ks[16], (L, D_MIX, D), D_MIX ** -0.5),
        'g_ffn': 1.0 + nrm(ks[17], (L, D), 0.02),
        'w_route_group': nrm(ks[18], (L, D, N_GROUPS), D ** -0.5),
        'b_route_group': nrm(ks[19], (L, N_GROUPS), 0.01),
        'w_route_expert': nrm(ks[20], (L, D, N_EXPERTS), D ** -0.5),
        'b_route_expert': nrm(ks[21], (L, N_EXPERTS), 0.01),
        'w_e_gate': nrm(ks[22], (L, N_EXPERTS, D, D_EXPERT), D ** -0.5),
        'w_e_up': nrm(ks[23], (L, N_EXPERTS, D, D_EXPERT), D ** -0.5),
        'w_e_down': nrm(ks[24], (L, N_EXPERTS, D_EXPERT, D), D_EXPERT ** -0.5),
    }


def reference(x, g_mix, w_in, rwkv_shift_mix, q_gain, k_gain, w0, w_decay_up, a0, w_aicl_up,
              w_gate_lora_up, k_k, k_a, r_k, ln_x_w, ln_x_b, w_out, g_ffn, w_route_group,
              b_route_group, w_route_expert, b_route_expert, w_e_gate, w_e_up, w_e_down):
    for l in range(DEPTH):
        h = rms_norm(x, g_mix[l])
        proj = h @ w_in[l]
        p_dsa, p_rwkv = proj[..., :DSA_COLS], proj[..., DSA_COLS:]
        q, k, v, q_idx, k_idx, w_idx = split_cols(p_dsa, DSA_SPLITS)
        y_dsa = dsa_mixer(q, k, v, q_idx, k_idx, w_idx, q_gain[l], k_gain[l])
        p_rwkv = token_shift_mix(p_rwkv, rwkv_shift_mix[l])
        r, d_w, kr, vr, d_a, d_g = split_cols(p_rwkv, RWKV_SPLITS)
        y_rwkv = rwkv7_mixer(r, d_w, kr, vr, d_a, d_g, w0[l], w_decay_up[l], a0[l], w_aicl_up[l],
                             w_gate_lora_up[l], k_k[l], k_a[l], r_k[l], ln_x_w[l], ln_x_b[l],
                             x.dtype)
        x = x + jnp.concatenate([y_dsa.astype(x.dtype), y_rwkv], axis=-1) @ w_out[l]
        x = x + hier_moe(rms_norm(x, g_ffn[l]), w_route_group[l], b_route_group[l],
                         w_route_expert[l], b_route_expert[l], w_e_gate[l], w_e_up[l],
                         w_e_down[l])
    return x
```

```python
import os
import numpy as np
import ml_dtypes
from contextlib import ExitStack

import concourse.bass as bass
import concourse.mybir as mybir
from concourse.bass_utils import run_bass_kernel_spmd

F32 = mybir.dt.float32
BF16 = mybir.dt.bfloat16
ALU = mybir.AluOpType
AF = mybir.ActivationFunctionType
AX = mybir.AxisListType

D = 2048
KT = 16
DSA_W = 1024
NH = 8
HD = 128
IH = 16
IDD = 64
RW = 1024
RH = 16
RN = 64
NEXP = 16
DEXP = 512
DSA_COLS = 4176
RWKV_COLS = 3360
IN_COLS = 7536
NORM_EPS = 1e-6
GN_EPS = 64e-5
CHUNK = 64

ENGS = ("pe", "act", "dve", "pool", "sp")
DMAQ = ("sp", "act", "pool")
NSLOT = 24
FUSE_WAIT = True


class Tok:
    __slots__ = ("name", "w", "r")

    def __init__(self, name=""):
        self.name = name
        self.w = None
        self.r = []


class Sched:
    def __init__(self, nc, es):
        self.nc = nc
        self.esem = {e: es.enter_context(nc.semaphore("es_" + e)) for e in ENGS}
        self.ebase = {e: 0 for e in ENGS}
        self.dsem = {q: [es.enter_context(nc.semaphore("ds_%s_%d" % (q, i))) for i in range(NSLOT)]
                     for q in DMAQ}
        self.dcnt = {q: [0] * NSLOT for q in DMAQ}
        self.dn = {q: 0 for q in DMAQ}
        self.waited = {e: {} for e in ENGS}
        self.ops = {e: [] for e in ENGS}
        self.phase = 0
        self.stop = None

    def _deps(self, eng, reads, writes, is_dma):
        raw, other = set(), set()
        for t in reads:
            if t.w is not None:
                raw.add(t.w)
        for t in writes:
            if t.w is not None:
                other.add(t.w)
            for r in t.r:
                other.add(r)
        deps = set()
        for ev in raw:
            if ev[0] == "e" and ev[3] != self.phase:
                continue
            if ev[0] == "e" and ev[1] == eng and eng == "pe" and not is_dma:
                continue
            deps.add(ev)
        for ev in other:
            if ev[0] == "e" and ev[3] != self.phase:
                continue
            if ev[0] == "e" and ev[1] == eng and eng == "pe" and not is_dma:
                continue
            deps.add(ev)
        return deps

    def op(self, eng, fn, reads=(), writes=()):
        idx = len(self.ops[eng])
        deps = self._deps(eng, reads, writes, False)
        ev = ("e", eng, idx, self.phase)
        for t in reads:
            t.r.append(ev)
        for t in writes:
            t.w = ev
            t.r = []
        self.ops[eng].append(dict(fn=fn, deps=deps, ms=False, dma=None))

    def dma(self, q, fn, reads=(), writes=()):
        deps = self._deps(q, reads, writes, True)
        n = self.dn[q]
        self.dn[q] += 1
        slot = n % NSLOT
        if self.dcnt[q][slot] > 0:
            deps.add(("d", q, slot, self.dcnt[q][slot]))
        self.dcnt[q][slot] += 16
        ev = ("d", q, slot, self.dcnt[q][slot])
        for t in reads:
            t.r.append(ev)
        for t in writes:
            t.w = ev
            t.r = []
        self.ops[q].append(dict(fn=fn, deps=deps, ms=False, dma=(q, slot)))

    def barrier(self):
        evs = []
        for e in ENGS:
            for i in range(len(self.ops[e]) - 1, -1, -1):
                if self.ops[e][i]["dma"] is None and self.ops[e][i]["fn"] is not None:
                    evs.append(("e", e, i, self.phase))
                    break
        for q in DMAQ:
            for s in range(NSLOT):
                if self.dcnt[q][s] > 0:
                    evs.append(("d", q, s, self.dcnt[q][s]))
        for e in ENGS:
            deps = set(ev for ev in evs if not (ev[0] == "e" and ev[1] == e))
            self.ops[e].append(dict(fn=None, deps=deps, ms=False, dma=None))

    def emit(self):
        nc = self.nc
        if self.stop is not None and self.phase >= self.stop:
            self.ops = {e: [] for e in ENGS}
            self.phase += 1
            return
        for e in ENGS:
            for o in self.ops[e]:
                for ev in o["deps"]:
                    if ev[0] == "e":
                        self.ops[ev[1]][ev[2]]["ms"] = True
        msv = {}
        for e in ENGS:
            c = self.ebase[e]
            arr = []
            for o in self.ops[e]:
                if o["ms"]:
                    c += 1
                arr.append(c)
            msv[e] = arr
            self.ebase[e] = c
        handles = {"pe": "tensor", "act": "scalar", "dve": "vector", "pool": "gpsimd", "sp": "sync"}

        def replay(e, eng):
            wd = self.waited[e]
            for i, o in enumerate(self.ops[e]):
                need = {}
                for ev in o["deps"]:
                    if ev[0] == "e":
                        sem, val, key = self.esem[ev[1]], msv[ev[1]][ev[2]], ("e", ev[1])
                    else:
                        sem, val, key = self.dsem[ev[1]][ev[2]], ev[3], ("d", ev[1], ev[2])
                    if wd.get(key, 0) >= val:
                        continue
                    if key not in need or need[key][1] < val:
                        need[key] = (sem, val)
                waits = [need[k] for k in sorted(need, key=str)]
                for k in need:
                    wd[k] = need[k][1]
                fused = None
                if FUSE_WAIT and o["fn"] is not None and waits:
                    fused = waits.pop()
                for sem, val in waits:
                    eng.wait_ge(sem, val)
                if o["fn"] is None:
                    continue
                ins = o["fn"](eng)
                if fused is not None:
                    ins._wait_ge(fused[0], fused[1])
                if o["dma"] is not None:
                    ins.then_inc(self.dsem[o["dma"][0]][o["dma"][1]], 16)
                elif o["ms"]:
                    ins.then_inc(self.esem[e], 1)

        with nc.Block() as blk:
            @blk.tensor
            def _(eng):
                replay("pe", eng)

            @blk.scalar
            def _(eng):
                replay("act", eng)

            @blk.vector
            def _(eng):
                replay("dve", eng)

            @blk.gpsimd
            def _(eng):
                replay("pool", eng)

            @blk.sync
            def _(eng):
                replay("sp", eng)
        self.ops = {e: [] for e in ENGS}
        self.phase += 1


class Ctx:
    pass


def build(NP, NO, TOPK, dbg=False, stop_after=None):
    NT = NP + NO
    nc = bass.Bass("TRN2", target_bir_lowering=False)
    g = Ctx()
    kind_s = "ExternalOutput" if dbg else "Internal"

    def din(name, shape, dt=F32):
        return nc.dram_tensor(name, list(shape), dt, kind="ExternalInput").ap()

    def dscr(name, shape, dt=BF16):
        return nc.dram_tensor(name, list(shape), dt, kind=kind_s).ap()

    x = din("x", [NT, D])
    g_mix = din("g_mix", [1, D])
    w_in = din("w_in", [D, IN_COLS])
    rope_tab = din("rope_tab", [NT, 192])
    ident_in = din("ident_in", [128, 128])
    q_gain = din("q_gain", [1, HD])
    k_gain = din("k_gain", [1, HD])
    pcols_in = din("pcols", [128, 84])
    wdu_in = din("w_decay_up", [64, RW])
    wau_in = din("w_aicl_up", [64, RW])
    wgu_in = din("w_gate_lora_up", [160, RW])
    cmats_in = din("cmats", [4, 128, 128])
    lnrow_in = din("lnrow", [2, RW])
    NG = NT // 128
    CMs = dscr("s_CM", [NG, 64, 16, 4, 128])
    TMs = dscr("s_TM", [NG, 128, 4, RW])
    WCs = dscr("s_WC", [64, 16, NT // 64], F32)
    GTs = dscr("s_GT", [RW, NO])
    BNs = dscr("s_BN", [RW, NO])
    w_out = din("w_out", [D, D])
    g_ffn = din("g_ffn", [1, D])
    wr_in = din("wr", [D, 20])
    br_in = din("br", [1, 20])
    weg = din("w_e_gate", [NEXP, D, DEXP])
    weu = din("w_e_up", [NEXP, D, DEXP])
    wed = din("w_e_down", [NEXP, DEXP, D])
    X1 = dscr("s_X1", [NO, D], F32)
    H2T = dscr("s_H2T", [D, NO])
    CMB = dscr("s_CMB", [NO, 16], F32)
    cmask_in = din("cmask", [128, 128])
    pbias_in = din("pbias", [128, 1])
    out = nc.dram_tensor("out", [NO, D], F32, kind="ExternalOutput").ap()
    YT = dscr("s_YT", [D, NO])
    if dbg:
        DBGM = dscr("dbg_mask", [NO // 128, 128, NT])
        DBGI = dscr("dbg_isc", [NO // 128, 128, NT], F32)
        DBGS = dscr("dbg_sm", [NO // 128, 128, 8], F32)

    QT = dscr("s_QT", [NH * HD, NO])
    KTs = dscr("s_KT", [NH * HD, NT])
    VS = dscr("s_V", [NT, DSA_W])
    QI = dscr("s_QI", [IH * IDD, NO])
    KI = dscr("s_KI", [128, NT])
    WI = dscr("s_WI", [NO, IH], F32)

    es = ExitStack()
    S = Sched(nc, es)
    S.stop = stop_after

    uniq = [0]

    def sb(st, name, shape, dt):
        uniq[0] += 1
        return st.enter_context(nc.sbuf_tensor("%s_%d" % (name, uniq[0]), list(shape), dt))

    def ps(st, name, shape, dt=F32):
        uniq[0] += 1
        return st.enter_context(nc.psum_tensor("%s_%d" % (name, uniq[0]), list(shape), dt))

    ident_f = sb(es, "ident_f", [128, 128], F32)
    ident = sb(es, "ident", [128, 128], BF16)
    t_ident = Tok("ident")
    S.dma("sp", lambda e: e.dma_start(out=ident_f[:], in_=ident_in), writes=[t_ident])
    S.op("dve", lambda e: e.tensor_copy(out=ident[:], in_=ident_f[:]), reads=[t_ident], writes=[t_ident])

    def phase_norm(st, hT, t_hT, tok0, ntok):
        gm = sb(st, "gm", [128, D], F32)
        t_gm = Tok()
        S.dma("sp", lambda e: e.dma_start(out=gm[:], in_=g_mix.broadcast_to([128, D])), writes=[t_gm])
        xt = [sb(st, "xt%d" % i, [128, D], F32) for i in range(2)]
        xn = [sb(st, "xn%d" % i, [128, D], BF16) for i in range(2)]
        junk = sb(st, "junk", [128, D], BF16)
        ss = [sb(st, "ss%d" % i, [128, 2], F32) for i in range(2)]
        pst = [ps(st, "pst%d" % i, [128, D], BF16) for i in range(2)]
        t_xt = [Tok() for _ in range(2)]
        t_xn = [Tok() for _ in range(2)]
        t_ss = [Tok() for _ in range(2)]
        t_ps = [Tok() for _ in range(2)]
        t_junk = Tok()
        for it in range(ntok // 128):
            b = it % 2
            r0 = tok0 + it * 128
            S.dma("sp", lambda e, b=b, r0=r0: e.dma_start(out=xt[b][:], in_=x[r0:r0 + 128, :]), writes=[t_xt[b]])
            S.op("act", lambda e, b=b: e.activation(out=junk[:], in_=xt[b][:], func=AF.Square,
                                                   accum_out=ss[b][:, 0:1]),
                 reads=[t_xt[b]], writes=[t_junk, t_ss[b]])
            S.op("act", lambda e, b=b: e.activation(out=ss[b][:, 1:2], in_=ss[b][:, 0:1], func=AF.Sqrt,
                                                   scale=1.0 / D, bias=NORM_EPS),
                 reads=[t_ss[b]], writes=[t_ss[b]])
            S.op("dve", lambda e, b=b: e.reciprocal(out=ss[b][:, 1:2], in_=ss[b][:, 1:2]),
                 reads=[t_ss[b]], writes=[t_ss[b]])
            S.op("dve", lambda e, b=b: e.scalar_tensor_tensor(out=xn[b][:], in0=xt[b][:], scalar=ss[b][:, 1:2],
                                                             in1=gm[:], op0=ALU.mult, op1=ALU.mult),
                 reads=[t_xt[b], t_ss[b], t_gm], writes=[t_xn[b]])
            for kt in range(KT):
                S.op("pe", lambda e, b=b, kt=kt: e.transpose(out=pst[b][:, kt * 128:(kt + 1) * 128],
                                                            in_=xn[b][:, kt * 128:(kt + 1) * 128],
                                                            identity=ident[:]),
                     reads=[t_xn[b], t_ident], writes=[t_ps[b]])
            c0 = it * 128
            S.op("act", lambda e, b=b, c0=c0: e.copy(out=hT[:, :, c0:c0 + 128],
                                                    in_=pst[b][:].rearrange("p (k t) -> p k t", k=KT)),
                 reads=[t_ps[b]], writes=[t_hT])

    def load_w(st_bufs, cols0, ncols):
        wst, t_wst, wbfs, t_wbfs, cnt = st_bufs
        i = cnt[0]
        cnt[0] += 1
        wb, tw = wbfs[i % 2], t_wbfs[i % 2]
        src = w_in[:, cols0:cols0 + ncols].rearrange("(k p) c -> p k c", p=128)
        S.dma("sp", lambda e: e.dma_start(out=wst[:, :, 0:ncols], in_=src), writes=[t_wst])
        eng = "pool" if i % 2 == 0 else "dve"
        S.op(eng, lambda e: e.tensor_copy(out=wb[:, :, 0:ncols], in_=wst[:, :, 0:ncols]),
             reads=[t_wst], writes=[tw])
        return wb, tw

    def phase_dsa_proj(st, hT, t_hT, tok0, ntok, own):
        wst = sb(st, "wst", [128, KT, 512], F32)
        wbfs = [sb(st, "wbf%d" % i, [128, KT, 512], BF16) for i in range(2)]
        wb = (wst, Tok(), wbfs, [Tok(), Tok()], [0])
        gq = sb(st, "gq", [128, HD], F32)
        gk = sb(st, "gk", [128, HD], F32)
        t_g = Tok()
        S.dma("sp", lambda e: e.dma_start(out=gq[:], in_=q_gain.broadcast_to([128, HD])), writes=[t_g])
        S.dma("sp", lambda e: e.dma_start(out=gk[:], in_=k_gain.broadcast_to([128, HD])), writes=[t_g])
        ntt = ntok // 128
        tabs = sb(st, "tabs", [128, ntt, 192], F32)
        t_tabs = Tok()
        S.dma("sp", lambda e: e.dma_start(out=tabs[:], in_=rope_tab[tok0:tok0 + ntok, :].rearrange(
            "(n p) c -> p n c", p=128)), writes=[t_tabs])
        pp = [ps(st, "pp%d" % i, [128, 512], F32) for i in range(2)]
        t_pp = [Tok(), Tok()]
        ptr_full = [ps(st, "ptr%d" % i, [128, 1024], BF16) for i in range(2)]
        ptr = [t_[:, 0:512] for t_ in ptr_full]
        t_ptr = [Tok(), Tok()]
        sq = sb(st, "sq", [128, 512], F32)
        xn = sb(st, "xnq", [128, 512], F32)
        ro = sb(st, "ro", [128, 512], BF16)
        tmp1 = sb(st, "tmp1", [128, 256], F32)
        tmp2 = sb(st, "tmp2", [128, 256], F32)
        ssq = sb(st, "ssq", [128, 8], F32)
        t_sq, t_xn, t_ro, t_t1, t_t2, t_ssq = Tok(), Tok(), Tok(), Tok(), Tok(), Tok()
        stg = [sb(st, "stg%d" % i, [128, 4, 512], BF16) for i in range(2)]
        t_stg = [Tok(), Tok()]
        vst = [sb(st, "vst%d" % i, [128, 512], BF16) for i in range(2)]
        t_vst = [Tok(), Tok()]
        wis = sb(st, "wis", [128, IH], F32)
        t_wis = Tok()
        cnt = [0]
        nstg = [0]

        def mm_tok(wtile, tw, ncols, it):
            i = cnt[0]
            cnt[0] += 1
            p, tp = pp[i % 2], t_pp[i % 2]
            for kt in range(KT):
                S.op("pe", lambda e, kt=kt, p=p: e.matmul(p[:, 0:ncols], lhsT=hT[:, kt, it * 128:(it + 1) * 128],
                                                       rhs=wtile[:, kt, 0:ncols], start=(kt == 0),
                                                       stop=(kt == KT - 1)),
                     reads=[t_hT, tw], writes=[tp])
            return p, tp

        def rope_ops(src, t_src, nh, hd, cos, sin, dst, t_dst):
            hf = hd // 2
            s3 = src.rearrange("p (h d) -> p h d", h=nh)
            d3 = dst.rearrange("p (h d) -> p h d", h=nh)
            cb = cos.unsqueeze(1).broadcast_to([128, nh, hf])
            sn = sin.unsqueeze(1).broadcast_to([128, nh, hf])
            a = tmp1[:, 0:nh * hf].rearrange("p (h d) -> p h d", h=nh)
            b = tmp2[:, 0:nh * hf].rearrange("p (h d) -> p h d", h=nh)
            x1, x2 = s3[:, :, 0:hf], s3[:, :, hf:hd]
            S.op("dve", lambda e: e.tensor_tensor(out=a, in0=x1, in1=cb, op=ALU.mult),
                 reads=[t_src, t_tabs], writes=[t_t1])
            S.op("pool", lambda e: e.tensor_tensor(out=b, in0=x2, in1=sn, op=ALU.mult),
                 reads=[t_src, t_tabs], writes=[t_t2])
            S.op("dve", lambda e: e.tensor_tensor(out=d3[:, :, 0:hf], in0=a, in1=b, op=ALU.subtract),
                 reads=[t_t1, t_t2], writes=[t_dst])
            S.op("dve", lambda e: e.tensor_tensor(out=a, in0=x2, in1=cb, op=ALU.mult),
                 reads=[t_src, t_tabs, t_dst], writes=[t_t1])
            S.op("pool", lambda e: e.tensor_tensor(out=b, in0=x1, in1=sn, op=ALU.mult),
                 reads=[t_src, t_tabs, t_dst], writes=[t_t2])
            S.op("dve", lambda e: e.tensor_tensor(out=d3[:, :, hf:hd], in0=a, in1=b, op=ALU.add),
                 reads=[t_t1, t_t2], writes=[t_dst])

        def qk_group(col0, gain, dstT, is_q):
            for half in range(2):
                wtile, tw = load_w(wb, col0 + half * 512, 512)
                for it in range(ntt):
                    p, tp = mm_tok(wtile, tw, 512, it)
                    S.op("act", lambda e, p=p: e.activation(out=sq[:], in_=p[:], func=AF.Square),
                         reads=[tp], writes=[t_sq])
                    S.op("dve", lambda e: e.tensor_reduce(out=ssq[:, 0:4], in_=sq[:].rearrange(
                        "p (h d) -> p h d", h=4), axis=AX.X, op=ALU.add), reads=[t_sq], writes=[t_ssq])
                    S.op("act", lambda e: e.activation(out=ssq[:, 4:8], in_=ssq[:, 0:4], func=AF.Sqrt,
                                                       scale=1.0 / HD, bias=NORM_EPS),
                         reads=[t_ssq], writes=[t_ssq])
                    S.op("dve", lambda e: e.reciprocal(out=ssq[:, 4:8], in_=ssq[:, 4:8]),
                         reads=[t_ssq], writes=[t_ssq])
                    S.op("dve", lambda e, p=p: e.tensor_tensor(
                        out=xn[:].rearrange("p (h d) -> p h d", h=4), in0=p[:].rearrange("p (h d) -> p h d", h=4),
                        in1=ssq[:, 4:8].unsqueeze(2).broadcast_to([128, 4, HD]), op=ALU.mult),
                         reads=[tp, t_ssq], writes=[t_xn])
                    S.op("pool", lambda e: e.tensor_tensor(
                        out=xn[:].rearrange("p (h d) -> p h d", h=4), in0=xn[:].rearrange("p (h d) -> p h d", h=4),
                        in1=gain[:].unsqueeze(1).broadcast_to([128, 4, HD]), op=ALU.mult),
                         reads=[t_xn, t_g], writes=[t_xn])
                    rope_ops(xn[:], t_xn, 4, HD, tabs[:, it, 0:64], tabs[:, it, 64:128], ro[:], t_ro)
                    j = nstg[0] // 4
                    sl = nstg[0] % 4
                    nstg[0] += 1
                    pt, tpt = ptr[it % 2], t_ptr[it % 2]
                    for h in range(4):
                        S.op("pe", lambda e, h=h, pt=pt: e.transpose(out=pt[:, h * 128:(h + 1) * 128],
                                                                  in_=ro[:, h * 128:(h + 1) * 128],
                                                                  identity=ident[:]),
                             reads=[t_ro, t_ident], writes=[tpt])
                    sg, tsg = stg[j % 2], t_stg[j % 2]
                    S.op("act", lambda e, pt=pt, sg=sg, sl=sl: e.copy(
                        out=sg[:, :, sl * 128:(sl + 1) * 128], in_=pt.rearrange("p (h t) -> p h t", h=4)),
                         reads=[tpt], writes=[tsg])
                    if sl == 3:
                        t0 = (it - 3) * 128 + (0 if is_q else tok0)
                        h0 = half * 4
                        dst = dstT.rearrange("(h d) t -> d h t", d=HD)[:, h0:h0 + 4, t0:t0 + 512]
                        S.dma("sp", lambda e, sg=sg, dst=dst: e.dma_start(out=dst, in_=sg[:]),
                              reads=[tsg], writes=[])

        qk_group(DSA_W, gk, KTs, False)
        if own:
            qk_group(0, gq, QT, True)
        for half in range(2):
            wtile, tw = load_w(wb, 2 * DSA_W + half * 512, 512)
            for it in range(ntt):
                p, tp = mm_tok(wtile, tw, 512, it)
                v, tv = vst[it % 2], t_vst[it % 2]
                S.op("act", lambda e, p=p, v=v: e.copy(out=v[:], in_=p[:]), reads=[tp], writes=[tv])
                r0 = tok0 + it * 128
                S.dma("sp", lambda e, v=v, r0=r0, half=half: e.dma_start(
                    out=VS[r0:r0 + 128, half * 512:(half + 1) * 512], in_=v[:]), reads=[tv], writes=[])
        wtile, tw = load_w(wb, 3 * DSA_W + IH * IDD, IDD + IH)
        for it in range(ntt):
            p, tp = mm_tok(wtile, tw, IDD + IH, it)
            S.op("act", lambda e, p=p: e.copy(out=xn[:, 0:IDD + IH], in_=p[:, 0:IDD + IH]), reads=[tp], writes=[t_xn])
            rope_ops(xn[:, 0:IDD], t_xn, 1, IDD, tabs[:, it, 128:160], tabs[:, it, 160:192], ro[:, 0:IDD], t_ro)
            S.op("pool", lambda e: e.tensor_copy(out=ro[:, IDD:2 * IDD], in_=ro[:, 0:IDD]),
                 reads=[t_ro], writes=[t_ro])
            if own:
                S.op("act", lambda e: e.mul(out=wis[:], in_=xn[:, IDD:IDD + IH], mul=1.0 / 32.0),
                     reads=[t_xn], writes=[t_wis])
                S.dma("sp", lambda e, it=it: e.dma_start(out=WI[it * 128:(it + 1) * 128, :], in_=wis[:]),
                      reads=[t_wis], writes=[])
            pt, tpt = ptr[it % 2], t_ptr[it % 2]
            S.op("pe", lambda e, pt=pt: e.transpose(out=pt[:, 0:128], in_=ro[:, 0:128], identity=ident[:]),
                 reads=[t_ro, t_ident], writes=[tpt])
            j = nstg[0] // 4
            sl = nstg[0] % 4
            nstg[0] += 1
            sg, tsg = stg[j % 2], t_stg[j % 2]
            S.op("act", lambda e, pt=pt, sg=sg, sl=sl: e.copy(out=sg[:, 0, sl * 128:(sl + 1) * 128],
                                                            in_=pt[:, 0:128]), reads=[tpt], writes=[tsg])
            if sl == 3:
                t0 = tok0 + (it - 3) * 128
                S.dma("sp", lambda e, sg=sg, t0=t0: e.dma_start(out=KI[:, t0:t0 + 512], in_=sg[:, 0, :]),
                      reads=[tsg], writes=[])
        if own:
            for half in range(2):
                wtile, tw = load_w(wb, 3 * DSA_W + half * 512, 512)
                for it in range(ntt):
                    p, tp = mm_tok(wtile, tw, 512, it)
                    S.op("act", lambda e, p=p: e.copy(out=xn[:], in_=p[:]), reads=[tp], writes=[t_xn])
                    rope_ops(xn[:], t_xn, 8, IDD, tabs[:, it, 128:160], tabs[:, it, 160:192], ro[:], t_ro)
                    pt, tpt = ptr[it % 2], t_ptr[it % 2]
                    for h in range(4):
                        S.op("pe", lambda e, h=h, pt=pt: e.transpose(out=pt[:, h * 128:(h + 1) * 128],
                                                                  in_=ro[:, h * 128:(h + 1) * 128],
                                                                  identity=ident[:]),
                             reads=[t_ro, t_ident], writes=[tpt])
                    j = nstg[0] // 4
                    sl = nstg[0] % 4
                    nstg[0] += 1
                    sg, tsg = stg[j % 2], t_stg[j % 2]
                    S.op("act", lambda e, pt=pt, sg=sg, sl=sl: e.copy(
                        out=sg[:, :, sl * 128:(sl + 1) * 128], in_=pt.rearrange("p (h t) -> p h t", h=4)),
                         reads=[tpt], writes=[tsg])
                    if sl == 3:
                        t0 = (it - 3) * 128
                        dst = QI.rearrange("(h d) t -> d h t", d=128)[:, half * 4:half * 4 + 4, t0:t0 + 512]
                        S.dma("sp", lambda e, sg=sg, dst=dst: e.dma_start(out=dst, in_=sg[:]),
                              reads=[tsg], writes=[])


    NQT = NO // 128
    mt_base = []
    acc_ = 0
    for qt in range(NQT):
        mt_base.append(acc_)
        acc_ += (NP + (qt + 1) * 128) // 128
    MT_TILES = acc_
    NBIS = 20

    def phase_index(st, maskT, t_maskT):
        qi = sb(st, "qi", [128, 8, NO], BF16)
        ki = sb(st, "ki", [128, NT], BF16)
        wi = sb(st, "wi", [128, NQT, IH], F32)
        cm = sb(st, "cm", [128, 128], F32)
        pb = sb(st, "pb", [128, 1], F32)
        t_in = Tok()
        S.dma("sp", lambda e: e.dma_start(out=qi[:], in_=QI.rearrange("(h d) t -> d h t", d=128)), writes=[t_in])
        S.dma("sp", lambda e: e.dma_start(out=ki[:], in_=KI), writes=[t_in])
        S.dma("sp", lambda e: e.dma_start(out=wi[:], in_=WI.rearrange("(n p) h -> p n h", p=128)), writes=[t_in])
        S.dma("sp", lambda e: e.dma_start(out=cm[:], in_=cmask_in), writes=[t_in])
        S.dma("sp", lambda e: e.dma_start(out=pb[:], in_=pbias_in), writes=[t_in])
        isc = sb(st, "isc", [128, NT], F32)
        junk = sb(st, "junkm", [128, NT], BF16)
        rr = [sb(st, "rr%d" % i, [128, 512], F32) for i in range(4)]
        t_rr = [Tok() for _ in range(4)]
        iscB = [sb(st, "iscB%d" % i, [128, 512], F32) for i in range(2)]
        t_iscB = [Tok(), Tok()]
        kbc = [0]
        pp = [ps(st, "pi%d" % i, [128, 512], F32) for i in range(4)]
        t_pp = [Tok() for _ in range(4)]
        pT_full = [ps(st, "pT%d" % i, [128, 1024], BF16) for i in range(2)]
        pT = [t_[:, 0:512] for t_ in pT_full]
        t_pT = [Tok(), Tok()]
        sm = sb(st, "sm", [128, 8], F32)
        t_isc, t_junk, t_sm = Tok(), Tok(), Tok()
        cnt = [0]
        for qt in range(NQT):
            nk = NP + (qt + 1) * 128
            nkt = nk // 128
            nblk = (nk + 511) // 512
            for kb in range(nblk):
                w = min(512, nk - kb * 512)
                for h in range(IH):
                    i = cnt[0]
                    cnt[0] += 1
                    p, tp = pp[i % 4], t_pp[i % 4]
                    r, tr = rr[i % 4], t_rr[i % 4]
                    pr = (h % 2) * 64
                    S.op("pe", lambda e, p=p, h=h, pr=pr, w=w, kb=kb, qt=qt: e.matmul(
                        p[:, 0:w], lhsT=qi[pr:pr + 64, h // 2, qt * 128:(qt + 1) * 128],
                        rhs=ki[pr:pr + 64, kb * 512:kb * 512 + w], start=True, stop=True),
                         reads=[t_in], writes=[tp])
                    S.op("act", lambda e, p=p, r=r, w=w: e.activation(out=r[:, 0:w], in_=p[:, 0:w], func=AF.Relu),
                         reads=[tp], writes=[tr])
                    dst = isc[:, kb * 512:kb * 512 + w]
                    NDV = 12
                    if h == 0:
                        S.op("dve", lambda e, r=r, w=w, dst=dst, qt=qt, h=h: e.tensor_scalar(
                            out=dst, in0=r[:, 0:w], scalar1=wi[:, qt, h:h + 1], scalar2=None, op0=ALU.mult),
                             reads=[tr, t_in], writes=[t_isc])
                    elif h < NDV:
                        S.op("dve", lambda e, r=r, w=w, dst=dst, qt=qt, h=h: e.scalar_tensor_tensor(
                            out=dst, in0=r[:, 0:w], scalar=wi[:, qt, h:h + 1], in1=dst, op0=ALU.mult, op1=ALU.add),
                             reads=[tr, t_in, t_isc], writes=[t_isc])
                    else:
                        ib, tib = iscB[kbc[0] % 2], t_iscB[kbc[0] % 2]
                        if h == NDV:
                            S.op("pool", lambda e, r=r, w=w, ib=ib, qt=qt, h=h: e.tensor_scalar(
                                out=ib[:, 0:w], in0=r[:, 0:w], scalar1=wi[:, qt, h:h + 1], scalar2=None, op0=ALU.mult),
                                 reads=[tr, t_in], writes=[tib])
                        else:
                            S.op("pool", lambda e, r=r, w=w, qt=qt, h=h: e.tensor_scalar(
                                out=r[:, 0:w], in0=r[:, 0:w], scalar1=wi[:, qt, h:h + 1], scalar2=None, op0=ALU.mult),
                                 reads=[tr, t_in], writes=[tr])
                            S.op("pool", lambda e, r=r, w=w, ib=ib: e.tensor_tensor(out=ib[:, 0:w], in0=ib[:, 0:w], in1=r[:, 0:w], op=ALU.add),
                                 reads=[tr, tib], writes=[tib])
                        if h == IH - 1:
                            S.op("dve", lambda e, w=w, ib=ib, dst=dst: e.tensor_tensor(out=dst, in0=dst, in1=ib[:, 0:w], op=ALU.add),
                                 reads=[tib, t_isc], writes=[t_isc])
                            kbc[0] += 1
            S.op("dve", lambda e, nk=nk: e.tensor_reduce(out=sm[:, 5:6], in_=isc[:, 0:nk], axis=AX.X, op=ALU.max),
                 reads=[t_isc], writes=[t_sm])
            S.op("dve", lambda e, nk=nk: e.tensor_reduce(out=sm[:, 0:1], in_=isc[:, 0:nk], axis=AX.X, op=ALU.min),
                 reads=[t_isc, t_sm], writes=[t_sm])
            S.op("dve", lambda e: e.tensor_tensor(out=sm[:, 1:2], in0=sm[:, 5:6], in1=sm[:, 0:1], op=ALU.subtract),
                 reads=[t_sm], writes=[t_sm])
            S.op("dve", lambda e, nk=nk: e.tensor_tensor(out=isc[:, nk - 128:nk], in0=isc[:, nk - 128:nk],
                                                        in1=cm[:], op=ALU.add),
                 reads=[t_isc, t_in], writes=[t_isc])
            if NP > 0:
                S.op("dve", lambda e: e.tensor_scalar(out=isc[:, 0:NP], in0=isc[:, 0:NP], scalar1=pb[:, 0:1],
                                                      scalar2=None, op0=ALU.add),
                     reads=[t_isc, t_in], writes=[t_isc])
            S.op("dve", lambda e: e.tensor_scalar(out=sm[:, 1:2], in0=sm[:, 1:2], scalar1=1e-20, scalar2=None, op0=ALU.max),
                 reads=[t_sm], writes=[t_sm])
            S.op("dve", lambda e: e.reciprocal(out=sm[:, 4:5], in_=sm[:, 1:2]), reads=[t_sm], writes=[t_sm])
            S.op("dve", lambda e, nk=nk: e.tensor_scalar(out=isc[:, 0:nk], in0=isc[:, 0:nk], scalar1=sm[:, 0:1],
                                                        scalar2=sm[:, 4:5], op0=ALU.subtract, op1=ALU.mult),
                 reads=[t_isc, t_sm], writes=[t_isc])
            S.op("dve", lambda e: e.memset(sm[:, 2:3], 0.5), reads=[t_sm], writes=[t_sm])
            for it in range(NBIS):
                last = (it == NBIS - 1)
                f = 0.5 ** (it + 1)
                S.op("dve", lambda e, nk=nk: e.tensor_scalar(out=junk[:, 0:nk], in0=isc[:, 0:nk],
                                                            scalar1=sm[:, 2:3], scalar2=None, op0=ALU.is_ge,
                                                            op1=ALU.add, accum_out=sm[:, 3:4]),
                     reads=[t_isc, t_sm, t_junk], writes=[t_junk, t_sm])
                S.op("dve", lambda e, last=last: e.tensor_scalar(out=sm[:, 5:6], in0=sm[:, 3:4], scalar1=float(TOPK) - 0.5,
                                                                scalar2=(-1.0 if last else -0.5), op0=ALU.is_ge, op1=ALU.add),
                     reads=[t_sm], writes=[t_sm])
                S.op("dve", lambda e, f=f, last=last: e.scalar_tensor_tensor(
                    out=(sm[:, 0:1] if last else sm[:, 2:3]), in0=sm[:, 5:6], scalar=f, in1=sm[:, 2:3], op0=ALU.mult, op1=ALU.add),
                     reads=[t_sm], writes=[t_sm])
            S.op("dve", lambda e, nk=nk: e.tensor_scalar(out=junk[:, 0:nk], in0=isc[:, 0:nk], scalar1=sm[:, 0:1],
                                                        scalar2=None, op0=ALU.is_ge),
                 reads=[t_isc, t_sm, t_junk], writes=[t_junk])
            if dbg:
                S.dma("sp", lambda e, qt=qt, nk=nk: e.dma_start(out=DBGM[qt, :, 0:nk], in_=junk[:, 0:nk]), reads=[t_junk])
                S.dma("sp", lambda e, qt=qt, nk=nk: e.dma_start(out=DBGI[qt, :, 0:nk], in_=isc[:, 0:nk]), reads=[t_isc])
                S.dma("sp", lambda e, qt=qt: e.dma_start(out=DBGS[qt, :, 0:6], in_=sm[:, 0:6]), reads=[t_sm])
            k0 = 0
            g_ = 0
            while k0 < nkt:
                n = min(4, nkt - k0)
                p, tp = pT[g_ % 2], t_pT[g_ % 2]
                g_ += 1
                for j in range(n):
                    S.op("pe", lambda e, p=p, j=j, k0=k0: e.transpose(
                        out=p[:, j * 128:(j + 1) * 128], in_=junk[:, (k0 + j) * 128:(k0 + j + 1) * 128],
                        identity=ident[:]), reads=[t_junk, t_ident], writes=[tp])
                b0 = (mt_base[qt] + k0) * 128
                S.op("act", lambda e, p=p, n=n, b0=b0: e.copy(out=maskT[:, b0:b0 + n * 128], in_=p[:, 0:n * 128]),
                     reads=[tp], writes=[t_maskT])
                k0 += n

    def phase_attn(st, maskT, t_maskT):
        ones = sb(st, "ones", [128, 128], BF16)
        t_ones = Tok()
        S.op("dve", lambda e: e.memset(ones[:], 1.0), writes=[t_ones])
        NKT = NT // 128
        kth = [sb(st, "kth%d" % i, [128, NT], BF16) for i in range(2)]
        vh = [sb(st, "vh%d" % i, [128, NKT, 128], BF16) for i in range(2)]
        qth = [sb(st, "qth%d" % i, [128, NO], BF16) for i in range(2)]
        yth = [sb(st, "yth%d" % i, [128, NO], BF16) for i in range(2)]
        t_k = [Tok(), Tok()]
        t_y = [Tok(), Tok()]
        pS = [ps(st, "pS%d" % i, [128, 512], F32) for i in range(2)]
        t_pS = [Tok(), Tok()]
        pO = [ps(st, "pO%d" % i, [128, 512], F32) for i in range(2)]
        pD = [ps(st, "pD%d" % i, [128, 512], F32) for i in range(2)]
        t_pO = [Tok(), Tok()]
        P = [sb(st, "P%d" % i, [128, 512], BF16) for i in range(2)]
        Pm = [sb(st, "Pm%d" % i, [128, 512], BF16) for i in range(2)]
        t_P = [Tok(), Tok()]
        t_Pm = [Tok(), Tok()]
        rec = sb(st, "rec", [128, 128], F32)
        t_rec = Tok()
        g_ = [0]
        scale = float(HD) ** -0.5
        for h in range(NH):
            b = h % 2
            S.dma("sp", lambda e, b=b, h=h: e.dma_start(out=kth[b][:], in_=KTs[h * 128:(h + 1) * 128, :]),
                  writes=[t_k[b]])
            S.dma("sp", lambda e, b=b, h=h: e.dma_start(out=vh[b][:], in_=VS[:, h * 128:(h + 1) * 128].rearrange(
                "(k p) d -> p k d", p=128)), writes=[t_k[b]])
            S.dma("sp", lambda e, b=b, h=h: e.dma_start(out=qth[b][:], in_=QT[h * 128:(h + 1) * 128, :]),
                  writes=[t_k[b]])
            for qt in range(NQT):
                nkt = (NP + (qt + 1) * 128) // 128
                po, tpo = pO[qt % 2], t_pO[qt % 2]
                pd = pD[qt % 2]
                k0 = 0
                while k0 < nkt:
                    n = min(4, nkt - k0)
                    i = g_[0]
                    g_[0] += 1
                    p_s, tps = pS[i % 2], t_pS[i % 2]
                    for j in range(n):
                        S.op("pe", lambda e, p_s=p_s, j=j, k0=k0, b=b, qt=qt: e.matmul(
                            p_s[:, j * 128:(j + 1) * 128], lhsT=kth[b][:, (k0 + j) * 128:(k0 + j + 1) * 128],
                            rhs=qth[b][:, qt * 128:(qt + 1) * 128], start=True, stop=True),
                             reads=[t_k[b]], writes=[tps])
                    S.op("act", lambda e, p_s=p_s, n=n, i=i: e.activation(
                        out=P[i % 2][:, 0:n * 128], in_=p_s[:, 0:n * 128], func=AF.Exp, scale=scale),
                         reads=[tps], writes=[t_P[i % 2]])
                    b0 = (mt_base[qt] + k0) * 128
                    S.op("pool", lambda e, n=n, i=i, b0=b0: e.tensor_tensor(
                        out=Pm[i % 2][:, 0:n * 128], in0=P[i % 2][:, 0:n * 128], in1=maskT[:, b0:b0 + n * 128],
                        op=ALU.mult), reads=[t_P[i % 2], t_maskT], writes=[t_Pm[i % 2]])
                    for j in range(n):
                        first = (k0 + j == 0)
                        last = (k0 + j == nkt - 1)
                        S.op("pe", lambda e, po=po, j=j, k0=k0, b=b, i=i, first=first, last=last: e.matmul(
                            po[:, 0:128], lhsT=vh[b][:, k0 + j, :], rhs=Pm[i % 2][:, j * 128:(j + 1) * 128],
                            start=first, stop=last), reads=[t_k[b], t_Pm[i % 2]], writes=[tpo])
                        S.op("pe", lambda e, pd=pd, j=j, i=i, first=first, last=last: e.matmul(
                            pd[:, 0:128], lhsT=ones[:], rhs=Pm[i % 2][:, j * 128:(j + 1) * 128],
                            start=first, stop=last), reads=[t_ones, t_Pm[i % 2]], writes=[tpo])
                    k0 += n
                S.op("dve", lambda e, pd=pd: e.reciprocal(out=rec[:], in_=pd[:, 0:128]),
                     reads=[tpo], writes=[t_rec])
                S.op("dve", lambda e, po=po, b=b, qt=qt: e.tensor_tensor(
                    out=yth[b][:, qt * 128:(qt + 1) * 128], in0=po[:, 0:128], in1=rec[:], op=ALU.mult),
                     reads=[tpo, t_rec], writes=[t_y[b]])
            S.dma("sp", lambda e, b=b, h=h: e.dma_start(out=YT[h * 128:(h + 1) * 128, :], in_=yth[b][:]),
                  reads=[t_y[b]], writes=[])


    RO = DSA_COLS
    R_R, R_DW, R_K, R_V, R_DA, R_DG = RO, RO + 1024, RO + 1088, RO + 2112, RO + 3136, RO + 3200
    carry = sb(es, "carry", [128, 32], F32)
    t_carry = Tok()
    S.op("dve", lambda e: e.memset(carry[:], 0.0), writes=[t_carry])

    def phase_rwkv_prep(st, hT, t_hT, tok0, ntok, own):
        nb = ntok // 512
        wst = sb(st, "wstr", [128, KT, 512], F32)
        wbfs = [sb(st, "wbfr%d" % i, [128, KT, 512], BF16) for i in range(2)]
        wb = (wst, Tok(), wbfs, [Tok(), Tok()], [0])
        pc = sb(st, "pc", [128, 84], F32)
        t_pc = Tok()
        S.dma("sp", lambda e: e.dma_start(out=pc[:], in_=pcols_in), writes=[t_pc])
        lst = sb(st, "lst", [128, 1, RW], F32)
        wdu = sb(st, "wdu", [64, RW], BF16)
        wau = sb(st, "wau", [64, RW], BF16)
        wgu0 = sb(st, "wgu0", [128, RW], BF16)
        wgu1 = sb(st, "wgu1", [32, RW], BF16)
        cst = sb(st, "cst", [128, 128], F32)
        bones = sb(st, "bones", [128, 128], BF16)
        t_lw = Tok()
        S.dma("sp", lambda e: e.dma_start(out=cst[:], in_=cmats_in[0]), writes=[t_lw])
        S.op("dve", lambda e: e.tensor_copy(out=bones[:], in_=cst[:]), reads=[t_lw], writes=[t_lw])
        S.dma("sp", lambda e: e.dma_start(out=lst[0:64, 0, :], in_=wdu_in), reads=[t_lw], writes=[t_lw])
        S.op("dve", lambda e: e.tensor_copy(out=wdu[:], in_=lst[0:64, 0, :]), reads=[t_lw], writes=[t_lw])
        S.dma("sp", lambda e: e.dma_start(out=lst[0:64, 0, :], in_=wau_in), reads=[t_lw], writes=[t_lw])
        S.op("dve", lambda e: e.tensor_copy(out=wau[:], in_=lst[0:64, 0, :]), reads=[t_lw], writes=[t_lw])
        S.dma("sp", lambda e: e.dma_start(out=lst[:, 0, :], in_=wgu_in[0:128, :]), reads=[t_lw], writes=[t_lw])
        S.op("dve", lambda e: e.tensor_copy(out=wgu0[:], in_=lst[:, 0, :]), reads=[t_lw], writes=[t_lw])
        S.dma("sp", lambda e: e.dma_start(out=lst[0:32, 0, :], in_=wgu_in[128:160, :]), reads=[t_lw], writes=[t_lw])
        S.op("dve", lambda e: e.tensor_copy(out=wgu1[:], in_=lst[0:32, 0, :]), reads=[t_lw], writes=[t_lw])
        th_all = sb(st, "th_all", [64, ntok], BF16)
        da_all = sb(st, "da_all", [64, ntok], BF16)
        dg0_all = sb(st, "dg0_all", [128, ntok], BF16)
        dg1_all = sb(st, "dg1_all", [32, ntok], BF16)
        t_lora = Tok()
        pm = [ps(st, "pm%d" % i, [128, 512], F32) for i in range(3)]
        t_pm = [Tok() for _ in range(3)]
        px = [ps(st, "px%d" % i, [128, 512], F32) for i in range(2)]
        t_px = [Tok(), Tok()]
        ptT_full = ps(st, "ptT", [128, 1024], BF16)
        ptT = ptT_full[:, 0:512]
        t_ptT = Tok()
        nbuf = ["raw", "dsh", "r", "k", "v", "lw", "a", "kk", "kkn", "k2", "b", "cA", "cB", "e", "tmp"]
        B = {n: sb(st, "rb_" + n, [128, 512], F32) for n in nbuf}
        T = {n: Tok() for n in nbuf}
        hb = {n: sb(st, "rh_" + n, [128, 512], BF16) for n in ("sqb", "rkb", "tmk", "tmb", "vb", "gt", "bn")}
        TH = {n: Tok() for n in hb}
        cm = sb(st, "cmt", [128, 4, 512], BF16)
        t_cm = Tok()
        tmt = sb(st, "tmt", [128, 4, 128], BF16)
        t_tmt = Tok()
        wc = sb(st, "wc", [128, 8], F32)
        t_wc = Tok()
        mmc = [0]

        def mm_ch(wtile, tw, c0, ncol, tb):
            i = mmc[0]
            mmc[0] += 1
            p, tp = pm[i % 3], t_pm[i % 3]
            for kt in range(KT):
                S.op("pe", lambda e, kt=kt, p=p: e.matmul(p[0:ncol, :], lhsT=wtile[:, kt, c0:c0 + ncol],
                                                       rhs=hT[:, kt, tb * 512:(tb + 1) * 512], start=(kt == 0),
                                                       stop=(kt == KT - 1)), reads=[t_hT, tw], writes=[tp])
            return p, tp

        def shift(p, tp, n_, mu_col, cidx, dst, t_dst):
            raw, dsh = B["raw"], B["dsh"]
            S.op("act", lambda e: e.copy(out=raw[0:n_, :], in_=p[0:n_, :]), reads=[tp], writes=[T["raw"]])
            S.op("dve", lambda e: e.tensor_tensor(out=dsh[0:n_, 1:512], in0=raw[0:n_, 0:511], in1=raw[0:n_, 1:512],
                                                  op=ALU.subtract), reads=[T["raw"]], writes=[T["dsh"]])
            S.op("dve", lambda e: e.tensor_tensor(out=dsh[0:n_, 0:1], in0=carry[0:n_, cidx:cidx + 1],
                                                  in1=raw[0:n_, 0:1], op=ALU.subtract),
                 reads=[T["raw"], t_carry], writes=[T["dsh"]])
            S.op("pool", lambda e: e.tensor_copy(out=carry[0:n_, cidx:cidx + 1], in_=raw[0:n_, 511:512]),
                 reads=[T["raw"], T["dsh"]], writes=[t_carry])
            S.op("dve", lambda e: e.scalar_tensor_tensor(out=dst, in0=dsh[0:n_, :], scalar=pc[0:n_, mu_col:mu_col + 1],
                                                         in1=raw[0:n_, :], op0=ALU.mult, op1=ALU.add),
                 reads=[T["dsh"], T["raw"], t_pc], writes=[t_dst])

        wtile, tw = load_w(wb, R_DW, 64)
        for tb in range(nb):
            p, tp = mm_ch(wtile, tw, 0, 64, tb)
            shift(p, tp, 64, 80, 0, B["tmp"][0:64, :], T["tmp"])
            S.op("act", lambda e, tb=tb: e.activation(out=th_all[:, tb * 512:(tb + 1) * 512], in_=B["tmp"][0:64, :],
                                                     func=AF.Tanh), reads=[T["tmp"]], writes=[t_lora])
        wtile, tw = load_w(wb, R_DA, 64 + 160)
        for tb in range(nb):
            p, tp = mm_ch(wtile, tw, 0, 64, tb)
            shift(p, tp, 64, 81, 1, B["tmp"][0:64, :], T["tmp"])
            S.op("act", lambda e, tb=tb: e.copy(out=da_all[:, tb * 512:(tb + 1) * 512], in_=B["tmp"][0:64, :]),
                 reads=[T["tmp"]], writes=[t_lora])
            p, tp = mm_ch(wtile, tw, 64, 128, tb)
            shift(p, tp, 128, 82, 2, B["tmp"][:, :], T["tmp"])
            S.op("act", lambda e, tb=tb: e.activation(out=dg0_all[:, tb * 512:(tb + 1) * 512], in_=B["tmp"][:, :],
                                                     func=AF.Sigmoid), reads=[T["tmp"]], writes=[t_lora])
            p, tp = mm_ch(wtile, tw, 192, 32, tb)
            shift(p, tp, 32, 83, 3, B["tmp"][0:32, :], T["tmp"])
            S.op("act", lambda e, tb=tb: e.activation(out=dg1_all[:, tb * 512:(tb + 1) * 512], in_=B["tmp"][0:32, :],
                                                     func=AF.Sigmoid), reads=[T["tmp"]], writes=[t_lora])

        def v3(ap):
            return ap.rearrange("p (c t) -> p c t", t=64)

        for ct in range(8):
            wst_, t_wst, wbfs_, t_wbfs, cnt_ = wb
            i = cnt_[0]
            cnt_[0] += 1
            wtile, tw = wbfs_[i % 2], t_wbfs[i % 2]
            for j, c0 in enumerate((R_R, R_K, R_V)):
                src = w_in[:, c0 + ct * 128:c0 + (ct + 1) * 128].rearrange("(k p) c -> p k c", p=128)
                S.dma("sp", lambda e, src=src, j=j: e.dma_start(out=wst_[:, :, j * 128:(j + 1) * 128], in_=src),
                      writes=[t_wst])
            S.op("pool", lambda e, wtile=wtile: e.tensor_copy(out=wtile[:, :, 0:384], in_=wst_[:, :, 0:384]),
                 reads=[t_wst], writes=[tw])
            for tb in range(nb):
                t0 = tb * 512
                for j, nm in enumerate(("r", "k", "v")):
                    p, tp = mm_ch(wtile, tw, j * 128, 128, tb)
                    shift(p, tp, 128, j * 8 + ct, 4 + j * 8 + ct, B[nm][:], T[nm])
                cs = slice(ct * 128, (ct + 1) * 128)
                pz, tpz = px[0], t_px[0]
                S.op("pe", lambda e, pz=pz, cs=cs, t0=t0: e.matmul(pz[:], lhsT=wdu[:, cs], rhs=th_all[:, t0:t0 + 512],
                                                             start=True, stop=True), reads=[t_lw, t_lora], writes=[tpz])
                S.op("act", lambda e, pz=pz, ct=ct: e.activation(out=B["lw"][:], in_=pz[:], func=AF.Sigmoid,
                                                               bias=pc[:, 24 + ct:25 + ct]),
                     reads=[tpz, t_pc], writes=[T["lw"]])
                S.op("pool", lambda e: e.tensor_scalar(out=B["lw"][:], in0=B["lw"][:], scalar1=-0.6065306597126334,
                                                      scalar2=None, op0=ALU.mult), reads=[T["lw"]], writes=[T["lw"]])
                pa, tpa = px[1], t_px[1]
                S.op("pe", lambda e, pa=pa, cs=cs, t0=t0: e.matmul(pa[:], lhsT=wau[:, cs], rhs=da_all[:, t0:t0 + 512],
                                                             start=True, stop=True), reads=[t_lw, t_lora], writes=[tpa])
                S.op("act", lambda e, pa=pa, ct=ct: e.activation(out=B["a"][:], in_=pa[:], func=AF.Sigmoid,
                                                               bias=pc[:, 32 + ct:33 + ct]),
                     reads=[tpa, t_pc], writes=[T["a"]])
                if own:
                    pg, tpg = px[0], t_px[0]
                    S.op("pe", lambda e, pg=pg, cs=cs, t0=t0: e.matmul(pg[:], lhsT=wgu0[:, cs], rhs=dg0_all[:, t0:t0 + 512],
                                                                 start=True, stop=False), reads=[t_lw, t_lora], writes=[tpg])
                    S.op("pe", lambda e, pg=pg, cs=cs, t0=t0: e.matmul(pg[:], lhsT=wgu1[:, cs], rhs=dg1_all[:, t0:t0 + 512],
                                                                 start=False, stop=True), reads=[t_lw, t_lora], writes=[tpg])
                    S.op("act", lambda e, pg=pg: e.copy(out=hb["gt"][:], in_=pg[:]), reads=[tpg], writes=[TH["gt"]])
                    S.dma("sp", lambda e, cs=cs, t0=t0: e.dma_start(out=GTs[cs, t0:t0 + 512], in_=hb["gt"][:]),
                          reads=[TH["gt"]])
                S.op("dve", lambda e, ct=ct: e.tensor_scalar(out=B["kk"][:], in0=B["k"][:], scalar1=pc[:, 40 + ct:41 + ct],
                                                            scalar2=None, op0=ALU.mult),
                     reads=[T["k"], t_pc], writes=[T["kk"]])
                S.op("pool", lambda e: e.tensor_tensor(out=hb["sqb"][:], in0=B["kk"][:], in1=B["kk"][:], op=ALU.mult),
                     reads=[T["kk"]], writes=[TH["sqb"]])
                pss, tpss = px[1], t_px[1]
                S.op("pe", lambda e, pss=pss: e.matmul(pss[:], lhsT=bones[:], rhs=hb["sqb"][:], start=True, stop=True),
                     reads=[t_lw, TH["sqb"]], writes=[tpss])
                S.op("act", lambda e, pss=pss: e.activation(out=B["tmp"][:], in_=pss[:], func=AF.Sqrt),
                     reads=[tpss], writes=[T["tmp"]])
                S.op("dve", lambda e: e.tensor_scalar(out=B["tmp"][:], in0=B["tmp"][:], scalar1=1e-12, scalar2=None,
                                                      op0=ALU.max), reads=[T["tmp"]], writes=[T["tmp"]])
                S.op("dve", lambda e: e.reciprocal(out=B["tmp"][:], in_=B["tmp"][:]), reads=[T["tmp"]], writes=[T["tmp"]])
                S.op("dve", lambda e: e.tensor_tensor(out=B["kkn"][:], in0=B["kk"][:], in1=B["tmp"][:], op=ALU.mult),
                     reads=[T["kk"], T["tmp"]], writes=[T["kkn"]])
                S.op("pool", lambda e, ct=ct: e.tensor_scalar(out=B["k2"][:], in0=B["a"][:], scalar1=-1.0,
                                                             scalar2=pc[:, 48 + ct:49 + ct], op0=ALU.add, op1=ALU.mult),
                     reads=[T["a"], t_pc], writes=[T["k2"]])
                S.op("pool", lambda e: e.tensor_scalar(out=B["k2"][:], in0=B["k2"][:], scalar1=1.0, scalar2=None, op0=ALU.add),
                     reads=[T["k2"]], writes=[T["k2"]])
                S.op("pool", lambda e: e.tensor_tensor(out=B["k2"][:], in0=B["k2"][:], in1=B["k"][:], op=ALU.mult),
                     reads=[T["k2"], T["k"]], writes=[T["k2"]])
                S.op("pool", lambda e: e.tensor_tensor(out=B["b"][:], in0=B["kkn"][:], in1=B["a"][:], op=ALU.mult),
                     reads=[T["kkn"], T["a"]], writes=[T["b"]])
                if own:
                    S.op("dve", lambda e, ct=ct: e.scalar_tensor_tensor(out=hb["rkb"][:], in0=B["r"][:],
                                                                       scalar=pc[:, 56 + ct:57 + ct], in1=B["k2"][:],
                                                                       op0=ALU.mult, op1=ALU.mult),
                         reads=[T["r"], T["k2"], t_pc], writes=[TH["rkb"]])
                    prk, tprk = px[0], t_px[0]
                    S.op("pe", lambda e, prk=prk: e.matmul(prk[:], lhsT=bones[:], rhs=hb["rkb"][:], start=True, stop=True),
                         reads=[t_lw, TH["rkb"]], writes=[tprk])
                    S.op("dve", lambda e, prk=prk: e.tensor_tensor(out=hb["bn"][:], in0=prk[:], in1=B["v"][:], op=ALU.mult),
                         reads=[tprk, T["v"]], writes=[TH["bn"]])
                    S.dma("sp", lambda e, cs=cs, t0=t0: e.dma_start(out=BNs[cs, t0:t0 + 512], in_=hb["bn"][:]),
                          reads=[TH["bn"]])
                src, t_src = B["lw"], T["lw"]
                for si, sft in enumerate((1, 2, 4, 8, 16, 32)):
                    dn = "cA" if si % 2 == 0 else "cB"
                    dst_, t_dst_ = B[dn], T[dn]
                    S.op("pool", lambda e, src=src, dst_=dst_, sft=sft: e.tensor_copy(
                        out=v3(dst_[:])[:, :, 0:sft], in_=v3(src[:])[:, :, 0:sft]), reads=[t_src], writes=[t_dst_])
                    S.op("dve", lambda e, src=src, dst_=dst_, sft=sft: e.tensor_tensor(
                        out=v3(dst_[:])[:, :, sft:64], in0=v3(src[:])[:, :, sft:64], in1=v3(src[:])[:, :, 0:64 - sft],
                        op=ALU.add), reads=[t_src], writes=[t_dst_])
                    src, t_src = dst_, t_dst_
                cl, t_cl = src, t_src
                S.op("act", lambda e, cl=cl: e.activation(out=B["e"][:], in_=cl[:], func=AF.Exp), reads=[t_cl], writes=[T["e"]])
                S.op("dve", lambda e: e.tensor_tensor(out=cm[:, 3, :], in0=B["r"][:], in1=B["e"][:], op=ALU.mult),
                     reads=[T["r"], T["e"]], writes=[t_cm])
                S.op("pool", lambda e, cl=cl: e.tensor_tensor(out=B["tmp"][:], in0=cl[:], in1=B["lw"][:], op=ALU.subtract),
                     reads=[t_cl, T["lw"]], writes=[T["tmp"]])
                S.op("act", lambda e: e.activation(out=B["e"][:], in_=B["tmp"][:], func=AF.Exp),
                     reads=[T["tmp"]], writes=[T["e"]])
                S.op("dve", lambda e: e.tensor_tensor(out=cm[:, 2, :], in0=B["kkn"][:], in1=B["e"][:], op=ALU.mult),
                     reads=[T["kkn"], T["e"]], writes=[t_cm])
                S.op("act", lambda e, cl=cl: e.activation(out=B["e"][:], in_=cl[:], func=AF.Exp, scale=-1.0),
                     reads=[t_cl], writes=[T["e"]])
                S.op("dve", lambda e: e.tensor_tensor(out=cm[:, 1, :], in0=B["k2"][:], in1=B["e"][:], op=ALU.mult),
                     reads=[T["k2"], T["e"]], writes=[t_cm])
                S.op("pool", lambda e: e.tensor_tensor(out=cm[:, 0, :], in0=B["b"][:], in1=B["e"][:], op=ALU.mult),
                     reads=[T["b"], T["e"]], writes=[t_cm])
                S.op("dve", lambda e, cl=cl: e.tensor_tensor(out=v3(B["tmp"][:]), in0=v3(cl[:])[:, :, 63:64].broadcast_to([128, 8, 64]),
                                                            in1=v3(cl[:]), op=ALU.subtract), reads=[t_cl], writes=[T["tmp"]])
                S.op("act", lambda e: e.activation(out=B["e"][:], in_=B["tmp"][:], func=AF.Exp),
                     reads=[T["tmp"]], writes=[T["e"]])
                S.op("dve", lambda e: e.tensor_tensor(out=hb["tmk"][:], in0=B["k2"][:], in1=B["e"][:], op=ALU.mult),
                     reads=[T["k2"], T["e"]], writes=[TH["tmk"]])
                S.op("pool", lambda e: e.tensor_tensor(out=hb["tmb"][:], in0=B["b"][:], in1=B["e"][:], op=ALU.mult),
                     reads=[T["b"], T["e"]], writes=[TH["tmb"]])
                S.op("act", lambda e: e.copy(out=hb["vb"][:], in_=B["v"][:]), reads=[T["v"]], writes=[TH["vb"]])
                S.op("act", lambda e, cl=cl: e.activation(out=wc[:].unsqueeze(2), in_=v3(cl[:])[:, :, 63:64], func=AF.Exp),
                     reads=[t_cl], writes=[t_wc])
                g0 = (tok0 + t0) // 128
                for hh in range(2):
                    hd_ = 2 * ct + hh
                    for j in range(4):
                        dst = CMs[g0 + j, :, hd_, :, :]
                        srcap = cm[hh * 64:(hh + 1) * 64, :, j * 128:(j + 1) * 128]
                        S.dma("sp", lambda e, dst=dst, srcap=srcap: e.dma_start(out=dst, in_=srcap), reads=[t_cm])
                    c0_ = (tok0 + t0) // 64
                    S.dma("sp", lambda e, hh=hh, hd_=hd_, c0_=c0_: e.dma_start(
                        out=WCs[:, hd_, c0_:c0_ + 8], in_=wc[hh * 64:(hh + 1) * 64, :]), reads=[t_wc])
                for j in range(4):
                    srcs = (cm[:, 2, j * 128:(j + 1) * 128], hb["tmb"][:, j * 128:(j + 1) * 128],
                            hb["tmk"][:, j * 128:(j + 1) * 128], hb["vb"][:, j * 128:(j + 1) * 128])
                    for x_, sa in enumerate(srcs):
                        S.op("pe", lambda e, x_=x_, sa=sa: e.transpose(out=ptT[:, x_ * 128:(x_ + 1) * 128], in_=sa,
                                                                     identity=ident[:]),
                             reads=[t_cm, TH["tmb"], TH["tmk"], TH["vb"], t_ident], writes=[t_ptT])
                    S.op("act", lambda e: e.copy(out=tmt[:], in_=ptT.rearrange("p (x c) -> p x c", x=4)),
                         reads=[t_ptT], writes=[t_tmt])
                    S.dma("sp", lambda e, g0=g0, j=j, cs=cs: e.dma_start(out=TMs[g0 + j, :, :, cs], in_=tmt[:]),
                          reads=[t_tmt])


    NGP = NP // 128

    def phase_rwkv_scan(st):
        mk = sb(st, "mk", [128, 3, 128], F32)
        pc = sb(st, "pc2", [128, 84], F32)
        t_c = Tok()
        S.dma("sp", lambda e: e.dma_start(out=mk[:], in_=cmats_in[1:4].rearrange("m p c -> p m c")), writes=[t_c])
        S.dma("sp", lambda e: e.dma_start(out=pc[:], in_=pcols_in), writes=[t_c])
        cmg = [sb(st, "cmg%d" % i, [64, 16, 4, 128], BF16) for i in range(2)]
        tmg = [sb(st, "tmg%d" % i, [128, 4, RW], BF16) for i in range(2)]
        wcg = [sb(st, "wcg%d" % i, [64, 16, 2], F32) for i in range(2)]
        t_ld = [Tok(), Tok()]
        H = sb(st, "H", [64, 16, 64], BF16)
        t_H = [Tok() for _ in range(16)]
        S.op("dve", lambda e: e.memset(H[:], 0.0), writes=t_H)
        PB = [ps(st, "pb%d" % i, [128, 512], F32) for i in range(7)]
        t_PB = [Tok() for _ in range(7)]
        pTp = ps(st, "pTp", [128, 8, 128], BF16)
        t_pTp = Tok()
        pbc = [0]

        def bank():
            i = pbc[0] % 7
            pbc[0] += 1
            return PB[i], t_PB[i]
        names = ["LL", "G2", "Rb", "P0", "PT0", "P1", "PT1", "QpT", "AiT", "Kp", "M0", "M1"]
        NBUF = 4
        hk = [sb(st, "hk%d" % i, [64, 64], F32) for i in range(NBUF)]
        t_hk = [Tok() for _ in range(NBUF)]
        W = {n: [sb(st, "sw_%s%d" % (n, i), [128, 256], BF16) for i in range(NBUF)] for n in names}
        TW = {n: [Tok() for _ in range(NBUF)] for n in names}
        lqk = [sb(st, "lqk%d" % i, [128, 128], F32) for i in range(NBUF)]
        t_lqk = [Tok() for _ in range(NBUF)]
        ysb = sb(st, "ysb", [64, 2, RW], F32)
        t_ysb = Tok()
        ynb = sb(st, "ynb", [64, 2, RW], BF16)
        st1 = sb(st, "st1", [64, 2, 16], F32)
        st2 = sb(st, "st2", [64, 2, 16], F32)
        dd = sb(st, "dd", [64, 2, RW], F32)
        sq2 = sb(st, "sq2", [64, 2, RW], F32)
        t_post = Tok()
        bng = sb(st, "bng", [128, 8, 128], BF16)
        gtg = sb(st, "gtg", [128, 8, 128], BF16)
        t_bg = Tok()
        fin = sb(st, "fin", [128, 8, 128], F32)
        yrt = sb(st, "yrt", [128, 8, 128], BF16)
        t_fin, t_yrt = Tok(), Tok()
        engs = ("dve", "act")
        for g_ in range(NG):
            b = g_ % 2
            own_g = g_ >= NGP
            S.dma("sp", lambda e, b=b, g_=g_: e.dma_start(out=cmg[b][:], in_=CMs[g_]), writes=[t_ld[b]])
            S.dma("sp", lambda e, b=b, g_=g_: e.dma_start(out=tmg[b][:], in_=TMs[g_]), writes=[t_ld[b]])
            S.dma("sp", lambda e, b=b, g_=g_: e.dma_start(out=wcg[b][:], in_=WCs[:, :, 2 * g_:2 * g_ + 2]), writes=[t_ld[b]])
            LVL = int(os.environ.get("SCAN_LEVEL", "9"))

            def head_gen(h, b=b, g_=g_, own_g=own_g):
                u = h % NBUF
                hs = slice(h * 64, (h + 1) * 64)
                bT, kT, kkT, qT = (cmg[b][:, h, x_, :] for x_ in range(4))
                KKt, Bh, Kh, V = (tmg[b][:, x_, hs] for x_ in range(4))
                LL, G2, Rb = W["LL"][u], W["G2"][u], W["Rb"][u]
                p1, tp1 = bank()
                S.op("pe", lambda e, p1=p1, kkT=kkT, b=b, h=h: e.matmul(p1[:, 0:256], lhsT=kkT,
                     rhs=cmg[b][:, h, 0:2, :].rearrange("p x t -> p (x t)"), start=True, stop=True), reads=[t_ld[b]], writes=[tp1])
                S.op("dve", lambda e, p1=p1, LL=LL: e.tensor_tensor(out=LL[:].rearrange("p (x c) -> p x c", x=2),
                     in0=p1[:, 0:256].rearrange("p (x c) -> p x c", x=2),
                     in1=mk[:, 0, :].unsqueeze(1).broadcast_to([128, 2, 128]), op=ALU.mult), reads=[tp1, t_c], writes=[TW["LL"][u]])
                p2, tp2 = bank()
                S.op("pe", lambda e, p2=p2, bT=bT, b=b, h=h: e.matmul(p2[:, 0:256], lhsT=bT,
                     rhs=cmg[b][:, h, 2:4, :].rearrange("p x t -> p (x t)"), start=True, stop=True), reads=[t_ld[b]], writes=[tp2])
                S.op("dve", lambda e, p2=p2, G2=G2: e.tensor_tensor(out=G2[:].rearrange("p (x c) -> p x c", x=2),
                     in0=p2[:, 0:256].rearrange("p (x c) -> p x c", x=2), in1=mk[:, 1:3, :], op=ALU.mult),
                     reads=[tp2, t_c], writes=[TW["G2"][u]])
                p3, tp3 = bank()
                S.op("pe", lambda e, p3=p3, kT=kT, qT=qT: e.matmul(p3[:, 0:128], lhsT=kT, rhs=qT, start=True, stop=True),
                     reads=[t_ld[b]], writes=[tp3])
                S.op("dve", lambda e, p3=p3, u=u: e.tensor_tensor(out=lqk[u][:], in0=p3[:, 0:128], in1=mk[:, 2, :], op=ALU.mult),
                     reads=[tp3, t_c], writes=[t_lqk[u]])
                yield
                S.op("act", lambda e, Rb=Rb, KKt=KKt: e.copy(out=Rb[:, 0:64], in_=KKt), reads=[t_ld[b]], writes=[TW["Rb"][u]])
                S.op("act", lambda e, Rb=Rb, LL=LL: e.copy(out=Rb[:, 64:192], in_=LL[:, 128:256]), reads=[TW["LL"][u]], writes=[TW["Rb"][u]])
                Pc, tPc = LL[:, 0:128], TW["LL"][u]
                PTc, tPTc = G2[:, 0:128], TW["G2"][u]
                if LVL < 3:
                    return
                for k_ in range(6):
                    yield
                    if k_ > 0:
                        nP, tnP = W["P%d" % (k_ % 2)][u], TW["P%d" % (k_ % 2)][u]
                        nPT, tnPT = W["PT%d" % (k_ % 2)][u], TW["PT%d" % (k_ % 2)][u]
                        pa, tpa = bank()
                        pb_, tpb = bank()
                        S.op("pe", lambda e, pa=pa, PTc=PTc, Pc=Pc: e.matmul(pa[:, 0:128], lhsT=PTc, rhs=Pc, start=True, stop=True),
                             reads=[tPc, tPTc], writes=[tpa])
                        S.op("pe", lambda e, pb_=pb_, PTc=PTc, Pc=Pc: e.matmul(pb_[:, 0:128], lhsT=Pc, rhs=PTc, start=True, stop=True),
                             reads=[tPc, tPTc], writes=[tpb])
                        S.op("act", lambda e, pa=pa, nP=nP: e.copy(out=nP[:, 0:128], in_=pa[:, 0:128]), reads=[tpa], writes=[tnP])
                        S.op("dve", lambda e, pb_=pb_, nPT=nPT: e.tensor_copy(out=nPT[:, 0:128], in_=pb_[:, 0:128]), reads=[tpb], writes=[tnPT])
                        Pc, tPc, PTc, tPTc = nP[:, 0:128], tnP, nPT[:, 0:128], tnPT
                    pr_, tpr = bank()
                    S.op("pe", lambda e, pr_=pr_, PTc=PTc, Rb=Rb: e.matmul(pr_[:, 0:192], lhsT=PTc, rhs=Rb[:, 0:192], start=True, stop=True),
                         reads=[tPTc, TW["Rb"][u]], writes=[tpr])
                    S.op("dve", lambda e, pr_=pr_, Rb=Rb, k_=k_: e.tensor_tensor(out=Rb[:, 0:192], in0=Rb[:, 0:192], in1=pr_[:, 0:192],
                         op=(ALU.subtract if k_ == 0 else ALU.add)), reads=[tpr, TW["Rb"][u]], writes=[TW["Rb"][u]])
                if LVL < 4:
                    return
                yield
                E, F = Rb[:, 0:64], Rb[:, 64:192]
                LqbT = G2[:, 128:256]
                QpT, AiT, Kp, M0, M1 = W["QpT"][u], W["AiT"][u], W["Kp"][u], W["M0"][u], W["M1"][u]
                pq, tpq = bank()
                S.op("pe", lambda e, pq=pq, E=E, LqbT=LqbT: e.matmul(pq[0:64, 0:128], lhsT=E, rhs=LqbT, start=True, stop=True),
                     reads=[TW["Rb"][u], TW["G2"][u]], writes=[tpq])
                S.op("dve", lambda e, pq=pq, QpT=QpT, qT=qT: e.tensor_tensor(out=QpT[0:64, 0:128], in0=qT, in1=pq[0:64, 0:128], op=ALU.subtract),
                     reads=[tpq, t_ld[b]], writes=[TW["QpT"][u]])
                yield
                pa2, tpa2 = bank()
                S.op("pe", lambda e, pa2=pa2, F=F, LqbT=LqbT: e.matmul(pa2[:, 0:128], lhsT=F, rhs=LqbT, start=True, stop=True),
                     reads=[TW["Rb"][u], TW["G2"][u]], writes=[tpa2])
                S.op("dve", lambda e, pa2=pa2, AiT=AiT, u=u: e.tensor_tensor(out=AiT[:, 0:128], in0=lqk[u][:], in1=pa2[:, 0:128], op=ALU.subtract),
                     reads=[tpa2, t_lqk[u]], writes=[TW["AiT"][u]])
                yield
                pk, tpk = bank()
                S.op("pe", lambda e, pk=pk, F=F, Bh=Bh: e.matmul(pk[:, 0:64], lhsT=F, rhs=Bh, start=True, stop=True),
                     reads=[TW["Rb"][u], t_ld[b]], writes=[tpk])
                S.op("dve", lambda e, pk=pk, Kp=Kp, Kh=Kh: e.tensor_tensor(out=Kp[:, 0:64], in0=Kh, in1=pk[:, 0:64], op=ALU.subtract),
                     reads=[tpk, t_ld[b]], writes=[TW["Kp"][u]])
                yield
                for c in range(2):
                    Mc, tMc = (M0, TW["M0"][u]) if c == 0 else (M1, TW["M1"][u])
                    rs = slice(c * 64, (c + 1) * 64)
                    pm_, tpm = bank()
                    S.op("pe", lambda e, pm_=pm_, rs=rs, Bh=Bh, Rb=Rb, b=b, h=h: e.matmul(pm_[0:64, 0:64], lhsT=Rb[rs, 0:64],
                         rhs=tmg[b][rs, 1, h * 64:(h + 1) * 64], start=True, stop=True), reads=[TW["Rb"][u], t_ld[b]], writes=[tpm])
                    S.op("dve", lambda e, pm_=pm_, Mc=Mc, c=c, b=b, h=h: e.scalar_tensor_tensor(out=Mc[0:64, 0:64], in0=ident_f[0:64, 0:64],
                         scalar=wcg[b][:, h, c:c + 1], in1=pm_[0:64, 0:64], op0=ALU.mult, op1=ALU.subtract),
                         reads=[tpm, t_ld[b], t_ident], writes=[tMc])
                if LVL < 5:
                    return
                for c in range(2):
                    Mc, tMc = (M0, TW["M0"][u]) if c == 0 else (M1, TW["M1"][u])
                    rs = slice(c * 64, (c + 1) * 64)
                    yield
                    if own_g:
                        py, tpy = bank()
                        py2, tpy2 = bank()
                        S.op("pe", lambda e, py=py, QpT=QpT, rs=rs, h=h: e.matmul(py[0:64, 0:64], lhsT=QpT[0:64, rs], rhs=H[:, h, :],
                             start=True, stop=True), reads=[TW["QpT"][u], t_H[h]], writes=[tpy])
                        S.op("pe", lambda e, py2=py2, AiT=AiT, rs=rs, b=b, h=h: e.matmul(py2[0:64, 0:64], lhsT=AiT[rs, rs],
                             rhs=tmg[b][rs, 3, h * 64:(h + 1) * 64], start=True, stop=True), reads=[TW["AiT"][u], t_ld[b]], writes=[tpy2])
                        S.op("act", lambda e, py=py, c=c, hs=hs: e.copy(out=ysb[:, c, hs], in_=py[0:64, 0:64]), reads=[tpy], writes=[t_ysb])
                        S.op("dve", lambda e, py2=py2, c=c, hs=hs: e.tensor_tensor(out=ysb[:, c, hs], in0=ysb[:, c, hs], in1=py2[0:64, 0:64],
                                                                                  op=ALU.add), reads=[tpy2, t_ysb], writes=[t_ysb])
                    ph, tph = bank()
                    ph2, tph2 = bank()
                    S.op("pe", lambda e, ph2=ph2, Kp=Kp, rs=rs, b=b, h=h: e.matmul(ph2[0:64, 0:64], lhsT=Kp[rs, 0:64],
                         rhs=tmg[b][rs, 3, h * 64:(h + 1) * 64], start=True, stop=True), reads=[TW["Kp"][u], t_ld[b]], writes=[tph2])
                    S.op("pe", lambda e, ph=ph, Mc=Mc, h=h: e.matmul(ph[0:64, 0:64], lhsT=Mc[0:64, 0:64], rhs=H[:, h, :], start=True, stop=True),
                         reads=[tMc, t_H[h]], writes=[tph])
                    S.op("act", lambda e, ph2=ph2, u=u: e.copy(out=hk[u][:], in_=ph2[0:64, 0:64]), reads=[tph2], writes=[t_hk[u]])
                    S.op("dve", lambda e, ph=ph, h=h, u=u: e.tensor_tensor(out=H[:, h, :], in0=hk[u][:], in1=ph[0:64, 0:64], op=ALU.add),
                         reads=[tph, t_hk[u]], writes=[t_H[h]])

            if LVL >= 2:
                gens = [head_gen(h) for h in range(16)]
                active = []
                nxt = 0
                while nxt < 16 or active:
                    while nxt < 16 and len(active) < NBUF:
                        active.append(gens[nxt])
                        nxt += 1
                    for gi in list(active):
                        try:
                            next(gi)
                        except StopIteration:
                            active.remove(gi)
            if own_g and LVL >= 6:
                go = g_ - NGP
                y4 = lambda ap: ap.rearrange("p c (h v) -> p c h v", v=64)
                S.dma("sp", lambda e, go=go: e.dma_start(out=bng[:], in_=BNs[:, go * 128:(go + 1) * 128].rearrange("(c p) t -> p c t", p=128)), writes=[t_bg])
                S.dma("sp", lambda e, go=go: e.dma_start(out=gtg[:], in_=GTs[:, go * 128:(go + 1) * 128].rearrange("(c p) t -> p c t", p=128)), writes=[t_bg])
                S.op("dve", lambda e: e.tensor_reduce(out=st1[:], in_=y4(ysb[:]), axis=AX.X, op=ALU.add), reads=[t_ysb], writes=[t_post])
                S.op("dve", lambda e: e.tensor_scalar(out=st1[:], in0=st1[:], scalar1=1.0 / 64, scalar2=None, op0=ALU.mult), reads=[t_post], writes=[t_post])
                S.op("dve", lambda e: e.tensor_tensor(out=y4(dd[:]), in0=y4(ysb[:]), in1=st1[:].unsqueeze(3).broadcast_to([64, 2, 16, 64]),
                                                      op=ALU.subtract), reads=[t_ysb, t_post], writes=[t_post])
                S.op("pool", lambda e: e.tensor_tensor(out=sq2[:], in0=dd[:], in1=dd[:], op=ALU.mult), reads=[t_post], writes=[t_post])
                S.op("dve", lambda e: e.tensor_reduce(out=st2[:], in_=y4(sq2[:]), axis=AX.X, op=ALU.add), reads=[t_post], writes=[t_post])
                S.op("act", lambda e: e.activation(out=st2[:], in_=st2[:], func=AF.Sqrt, scale=1.0 / 64, bias=GN_EPS), reads=[t_post], writes=[t_post])
                S.op("dve", lambda e: e.reciprocal(out=st2[:], in_=st2[:]), reads=[t_post], writes=[t_post])
                S.op("dve", lambda e: e.tensor_tensor(out=y4(ynb[:]), in0=y4(dd[:]), in1=st2[:].unsqueeze(3).broadcast_to([64, 2, 16, 64]),
                                                      op=ALU.mult), reads=[t_post], writes=[t_post])
                for c in range(2):
                    for ct in range(8):
                        S.op("pe", lambda e, c=c, ct=ct: e.transpose(out=pTp[:, ct, c * 64:(c + 1) * 64], in_=ynb[:, c, ct * 128:(ct + 1) * 128],
                                                                  identity=ident[0:64, 0:64]), reads=[t_post, t_ident], writes=[t_pTp])
                S.op("dve", lambda e: e.tensor_tensor(out=fin[:], in0=pTp[:], in1=pc[:, 64:72].unsqueeze(2).broadcast_to([128, 8, 128]), op=ALU.mult),
                     reads=[t_pTp, t_c], writes=[t_fin])
                S.op("pool", lambda e: e.tensor_tensor(out=fin[:], in0=fin[:], in1=pc[:, 72:80].unsqueeze(2).broadcast_to([128, 8, 128]), op=ALU.add),
                     reads=[t_fin, t_c], writes=[t_fin])
                S.op("pool", lambda e: e.tensor_tensor(out=fin[:], in0=fin[:], in1=bng[:], op=ALU.add), reads=[t_fin, t_bg], writes=[t_fin])
                S.op("dve", lambda e: e.tensor_tensor(out=yrt[:], in0=fin[:], in1=gtg[:], op=ALU.mult), reads=[t_fin, t_bg], writes=[t_yrt])
                S.dma("sp", lambda e, go=go: e.dma_start(out=YT[1024:2048, go * 128:(go + 1) * 128].rearrange("(c p) t -> p c t", p=128), in_=yrt[:]),
                      reads=[t_yrt])


    def phase_outproj(st):
        yT = sb(st, "yT", [128, KT, NO], BF16)
        t_yT = Tok()
        S.dma("sp", lambda e: e.dma_start(out=yT[:], in_=YT.rearrange("(k p) t -> p k t", p=128)), writes=[t_yT])
        wo = sb(st, "wo", [128, KT, D], BF16)
        wst = sb(st, "wsto", [128, KT, 512], F32)
        t_wst, t_wo = Tok(), Tok()
        for cb in range(4):
            S.dma("sp", lambda e, cb=cb: e.dma_start(out=wst[:], in_=w_out[:, cb * 512:(cb + 1) * 512].rearrange(
                "(k p) c -> p k c", p=128)), writes=[t_wst])
            S.op("pool" if cb % 2 else "dve", lambda e, cb=cb: e.tensor_copy(out=wo[:, :, cb * 512:(cb + 1) * 512], in_=wst[:]),
                 reads=[t_wst], writes=[t_wo])
        wrs = sb(st, "wrs", [128, KT, 20], F32)
        wrb = sb(st, "wrb", [128, KT, 20], BF16)
        brb = sb(st, "brb", [128, 20], F32)
        gf = sb(st, "gf", [128, D], F32)
        t_r = Tok()
        S.dma("sp", lambda e: e.dma_start(out=wrs[:], in_=wr_in.rearrange("(k p) c -> p k c", p=128)), writes=[t_r])
        S.dma("sp", lambda e: e.dma_start(out=brb[:], in_=br_in.broadcast_to([128, 20])), writes=[t_r])
        S.dma("sp", lambda e: e.dma_start(out=gf[:], in_=g_ffn.broadcast_to([128, D])), writes=[t_r])
        S.op("dve", lambda e: e.tensor_copy(out=wrb[:], in_=wrs[:]), reads=[t_r], writes=[t_r])
        _xt = sb(st, "xo", [128, D], F32)
        _x1 = sb(st, "x1", [128, D], F32)
        xt = [_xt, _xt]
        x1 = [_x1, _x1]
        xn = sb(st, "xno", [128, D], BF16)
        junk = xn
        h2t = sb(st, "h2t", [128, KT, 128], BF16)
        ss = sb(st, "sso", [128, 2], F32)
        _a, _b = Tok(), Tok()
        t_xt, t_x1 = [_a, _a], [_b, _b]
        t_xn, t_h2t, t_ss = Tok(), Tok(), Tok()
        t_junk = t_xn
        po = [ps(st, "po%d" % i, [128, 512], F32) for i in range(4)]
        t_po = [Tok() for _ in range(4)]
        pst = ps(st, "psto", [128, D], BF16)
        t_pst = Tok()
        pl = ps(st, "pl", [128, 512], F32)
        t_pl = Tok()
        R = sb(st, "rt", [128, 96], F32)
        t_R = Tok()
        for it in range(NO // 128):
            b = it % 2
            r0 = NP + it * 128
            S.dma("sp", lambda e, b=b, r0=r0: e.dma_start(out=xt[b][:], in_=x[r0:r0 + 128, :]), writes=[t_xt[b]])
            for cb in range(4):
                for kt in range(KT):
                    S.op("pe", lambda e, cb=cb, kt=kt, it=it: e.matmul(po[cb][:], lhsT=yT[:, kt, it * 128:(it + 1) * 128],
                                                                       rhs=wo[:, kt, cb * 512:(cb + 1) * 512], start=(kt == 0),
                                                                       stop=(kt == KT - 1)), reads=[t_yT, t_wo], writes=[t_po[cb]])
                S.op("dve", lambda e, cb=cb, b=b: e.tensor_tensor(out=x1[b][:, cb * 512:(cb + 1) * 512], in0=po[cb][:],
                                                                 in1=xt[b][:, cb * 512:(cb + 1) * 512], op=ALU.add),
                     reads=[t_po[cb], t_xt[b]], writes=[t_x1[b]])
            S.dma("sp", lambda e, b=b, it=it: e.dma_start(out=X1[it * 128:(it + 1) * 128, :], in_=x1[b][:]), reads=[t_x1[b]])
            S.op("act", lambda e, b=b: e.activation(out=junk[:], in_=x1[b][:], func=AF.Square, accum_out=ss[:, 0:1]),
                 reads=[t_x1[b]], writes=[t_junk, t_ss])
            S.op("act", lambda e: e.activation(out=ss[:, 1:2], in_=ss[:, 0:1], func=AF.Sqrt, scale=1.0 / D, bias=NORM_EPS),
                 reads=[t_ss], writes=[t_ss])
            S.op("dve", lambda e: e.reciprocal(out=ss[:, 1:2], in_=ss[:, 1:2]), reads=[t_ss], writes=[t_ss])
            S.op("dve", lambda e, b=b: e.scalar_tensor_tensor(out=xn[:], in0=x1[b][:], scalar=ss[:, 1:2], in1=gf[:],
                                                             op0=ALU.mult, op1=ALU.mult), reads=[t_x1[b], t_ss, t_r], writes=[t_xn])
            for kt in range(KT):
                S.op("pe", lambda e, kt=kt: e.transpose(out=pst[:, kt * 128:(kt + 1) * 128], in_=xn[:, kt * 128:(kt + 1) * 128],
                                                       identity=ident[:]), reads=[t_xn, t_ident], writes=[t_pst])
            S.op("act", lambda e: e.copy(out=h2t[:], in_=pst[:].rearrange("p (k t) -> p k t", k=KT)), reads=[t_pst], writes=[t_h2t])
            S.dma("sp", lambda e, it=it: e.dma_start(out=H2T[:, it * 128:(it + 1) * 128].rearrange("(k p) t -> p k t", p=128),
                                                        in_=h2t[:]), reads=[t_h2t])
            for kt in range(KT):
                S.op("pe", lambda e, kt=kt: e.matmul(pl[:, 0:20], lhsT=h2t[:, kt, :], rhs=wrb[:, kt, :], start=(kt == 0),
                                                     stop=(kt == KT - 1)), reads=[t_h2t, t_r], writes=[t_pl])
            lg, gmx, ge, gs, gm = R[:, 0:20], R[:, 20:21], R[:, 21:25], R[:, 25:26], R[:, 26:30]
            tmp16, sel, m1, ee, t4 = R[:, 30:46], R[:, 46:50], R[:, 50:51], R[:, 51:55], R[:, 55:59]
            m2, mk2, ws, cmb = R[:, 59:60], R[:, 60:64], R[:, 64:65], R[:, 65:81]
            ngm, nm1 = R[:, 81:82], R[:, 82:83]
            def rop(eng, fn):
                S.op(eng, fn, reads=[t_R, t_pl, t_r], writes=[t_R])
            rop("dve", lambda e: e.tensor_tensor(out=lg, in0=pl[:, 0:20], in1=brb[:], op=ALU.add))
            rop("dve", lambda e: e.tensor_reduce(out=gmx, in_=R[:, 0:4], axis=AX.X, op=ALU.max))
            rop("dve", lambda e: e.tensor_scalar(out=ngm, in0=gmx, scalar1=-1.0, scalar2=None, op0=ALU.mult))
            rop("act", lambda e: e.activation(out=ge, in_=R[:, 0:4], func=AF.Exp, bias=ngm, accum_out=gs))
            rop("dve", lambda e: e.tensor_scalar(out=gm, in0=R[:, 0:4], scalar1=gmx, scalar2=None, op0=ALU.is_ge))
            rop("dve", lambda e: e.tensor_tensor(out=tmp16.rearrange("p (g j) -> p g j", g=4),
                                                 in0=R[:, 4:20].rearrange("p (g j) -> p g j", g=4),
                                                 in1=gm.unsqueeze(2).broadcast_to([128, 4, 4]), op=ALU.mult))
            rop("dve", lambda e: e.tensor_reduce(out=sel, in_=tmp16.rearrange("p (g j) -> p j g", g=4), axis=AX.X, op=ALU.add))
            rop("dve", lambda e: e.tensor_reduce(out=m1, in_=sel, axis=AX.X, op=ALU.max))
            rop("dve", lambda e: e.tensor_scalar(out=nm1, in0=m1, scalar1=-1.0, scalar2=None, op0=ALU.mult))
            rop("act", lambda e: e.activation(out=ee, in_=sel, func=AF.Exp, bias=nm1))
            rop("dve", lambda e: e.tensor_scalar(out=t4, in0=sel, scalar1=m1, scalar2=-1e30, op0=ALU.is_ge, op1=ALU.mult))
            rop("dve", lambda e: e.tensor_tensor(out=t4, in0=t4, in1=sel, op=ALU.add))
            rop("dve", lambda e: e.tensor_reduce(out=m2, in_=t4, axis=AX.X, op=ALU.max))
            rop("dve", lambda e: e.tensor_scalar(out=mk2, in0=sel, scalar1=m2, scalar2=None, op0=ALU.is_ge))
            rop("dve", lambda e: e.tensor_tensor(out=ee, in0=ee, in1=mk2, op=ALU.mult))
            rop("dve", lambda e: e.tensor_reduce(out=ws, in_=ee, axis=AX.X, op=ALU.add))
            rop("dve", lambda e: e.tensor_tensor(out=ws, in0=ws, in1=gs, op=ALU.mult))
            rop("dve", lambda e: e.reciprocal(out=ws, in_=ws))
            rop("dve", lambda e: e.tensor_scalar(out=ee, in0=ee, scalar1=ws, scalar2=None, op0=ALU.mult))
            rop("dve", lambda e: e.tensor_tensor(out=cmb.rearrange("p (g j) -> p g j", g=4),
                                                 in0=gm.unsqueeze(2).broadcast_to([128, 4, 4]),
                                                 in1=ee.unsqueeze(1).broadcast_to([128, 4, 4]), op=ALU.mult))
            S.dma("sp", lambda e, it=it: e.dma_start(out=CMB[it * 128:(it + 1) * 128, :], in_=cmb), reads=[t_R])

    def phase_moe(st):
        NB = NO // 512
        wg = [sb(st, "wg%d" % i, [128, KT, 512], BF16) for i in range(2)]
        wu = [sb(st, "wu%d" % i, [128, KT, 512], BF16) for i in range(2)]
        wd = [sb(st, "wd%d" % i, [128, 4, D], BF16) for i in range(2)]
        t_w = [[Tok(), Tok()] for _ in range(3)]
        h2 = sb(st, "h2m", [128, KT, 512], BF16)
        t_h2 = Tok()
        acc = sb(st, "accm", [128, 4, D], F32)
        t_acc = Tok()
        cmb = sb(st, "cmbm", [128, 4, 16], F32)
        t_cmb = Tok()
        xres = sb(st, "xres", [128, D], F32)
        t_xres = Tok()
        he = [sb(st, "he%d" % i, [128, 512], BF16) for i in range(4)]
        t_he = [Tok() for _ in range(4)]
        sg = [sb(st, "sgm%d" % i, [128, 512], F32) for i in range(2)]
        t_sg = [Tok(), Tok()]
        pg = [ps(st, "pg%d" % i, [128, 512], F32) for i in range(2)]
        pu = [ps(st, "pu%d" % i, [128, 512], F32) for i in range(2)]
        py = [ps(st, "py%d" % i, [128, 512], F32) for i in range(4)]
        t_pg, t_pu, t_py = [Tok(), Tok()], [Tok(), Tok()], [Tok() for _ in range(4)]
        seq = [(blk, ex) for blk in range(NB) for ex in range(NEXP)]

        def load(i):
            b = i % 2
            ex = seq[i][1]
            S.dma("pool", lambda e: e.dma_start(out=wg[b][:], in_=weg[ex].rearrange("(k p) c -> p k c", p=128)),
                  writes=[t_w[0][b]])
            S.dma("pool", lambda e: e.dma_start(out=wu[b][:], in_=weu[ex].rearrange("(k p) c -> p k c", p=128)),
                  writes=[t_w[1][b]])
            S.dma("pool", lambda e: e.dma_start(out=wd[b][:], in_=wed[ex].rearrange("(k p) c -> p k c", p=128)),
                  writes=[t_w[2][b]])
        load(0)
        for i, (blk, ex) in enumerate(seq):
            b = i % 2
            if ex == 0:
                S.dma("sp", lambda e, blk=blk: e.dma_start(out=h2[:], in_=H2T[:, blk * 512:(blk + 1) * 512].rearrange(
                    "(k p) t -> p k t", p=128)), writes=[t_h2])
                S.dma("sp", lambda e, blk=blk: e.dma_start(out=cmb[:], in_=CMB[blk * 512:(blk + 1) * 512, :].rearrange(
                    "(n p) c -> p n c", p=128)), writes=[t_cmb])
            if i + 1 < len(seq):
                load(i + 1)
            for ft in range(4):
                fb = ft % 2
                for kt in range(KT):
                    S.op("pe", lambda e, kt=kt, ft=ft, fb=fb, b=b: e.matmul(pg[fb][:], lhsT=wg[b][:, kt, ft * 128:(ft + 1) * 128],
                                                                           rhs=h2[:, kt, :], start=(kt == 0), stop=(kt == KT - 1)),
                         reads=[t_w[0][b], t_h2], writes=[t_pg[fb]])
                for kt in range(KT):
                    S.op("pe", lambda e, kt=kt, ft=ft, fb=fb, b=b: e.matmul(pu[fb][:], lhsT=wu[b][:, kt, ft * 128:(ft + 1) * 128],
                                                                           rhs=h2[:, kt, :], start=(kt == 0), stop=(kt == KT - 1)),
                         reads=[t_w[1][b], t_h2], writes=[t_pu[fb]])
                S.op("act", lambda e, fb=fb: e.activation(out=sg[fb][:], in_=pg[fb][:], func=AF.Silu), reads=[t_pg[fb]], writes=[t_sg[fb]])
                S.op("dve", lambda e, fb=fb, ft=ft: e.tensor_tensor(out=he[ft][:], in0=sg[fb][:], in1=pu[fb][:], op=ALU.mult),
                     reads=[t_sg[fb], t_pu[fb]], writes=[t_he[ft]])
            for tt in range(4):
                for cb in range(4):
                    for ft in range(4):
                        S.op("pe", lambda e, tt=tt, cb=cb, ft=ft, b=b: e.matmul(py[cb][:], lhsT=he[ft][:, tt * 128:(tt + 1) * 128],
                                                                               rhs=wd[b][:, ft, cb * 512:(cb + 1) * 512],
                                                                               start=(ft == 0), stop=(ft == 3)),
                             reads=[t_he[ft], t_w[2][b]], writes=[t_py[cb]])
                    dst = acc[:, tt, cb * 512:(cb + 1) * 512]
                    if ex == 0:
                        S.op("dve", lambda e, dst=dst, cb=cb, tt=tt, ex=ex: e.tensor_scalar(
                            out=dst, in0=py[cb][:], scalar1=cmb[:, tt, ex:ex + 1], scalar2=None, op0=ALU.mult),
                             reads=[t_py[cb], t_cmb], writes=[t_acc])
                    else:
                        S.op("dve", lambda e, dst=dst, cb=cb, tt=tt, ex=ex: e.scalar_tensor_tensor(
                            out=dst, in0=py[cb][:], scalar=cmb[:, tt, ex:ex + 1], in1=dst, op0=ALU.mult, op1=ALU.add),
                             reads=[t_py[cb], t_cmb, t_acc], writes=[t_acc])
            if ex == NEXP - 1:
                for tt in range(4):
                    r0 = blk * 512 + tt * 128
                    S.dma("sp", lambda e, r0=r0: e.dma_start(out=xres[:], in_=X1[r0:r0 + 128, :]), writes=[t_xres])
                    S.op("dve", lambda e, tt=tt: e.tensor_tensor(out=acc[:, tt, :], in0=acc[:, tt, :], in1=xres[:], op=ALU.add),
                         reads=[t_xres, t_acc], writes=[t_acc])
                    S.dma("sp", lambda e, r0=r0, tt=tt: e.dma_start(out=out[r0:r0 + 128, :], in_=acc[:, tt, :]), reads=[t_acc])

    for (tok0, ntok, own) in ((0, NP, False), (NP, NO, True)):
        with ExitStack() as st:
            hT = sb(st, "hT", [128, KT, ntok], BF16)
            t_hT = Tok("hT")
            with ExitStack() as st2:
                phase_norm(st2, hT, t_hT, tok0, ntok)
                S.barrier()
                S.emit()
            with ExitStack() as st2:
                phase_dsa_proj(st2, hT, t_hT, tok0, ntok, own)
                S.barrier()
                S.emit()
            with ExitStack() as st2:
                phase_rwkv_prep(st2, hT, t_hT, tok0, ntok, own)
                S.barrier()
                S.emit()

    with ExitStack() as st:
        phase_rwkv_scan(st)
        S.barrier()
        S.emit()
    with ExitStack() as st:
        maskT = sb(st, "maskT", [128, MT_TILES * 128], BF16)
        t_maskT = Tok()
        with ExitStack() as st2:
            phase_index(st2, maskT, t_maskT)
            S.barrier()
            S.emit()
        with ExitStack() as st2:
            phase_attn(st2, maskT, t_maskT)
            S.barrier()
            S.emit()

    with ExitStack() as st:
        phase_outproj(st)
        S.barrier()
        S.emit()
    with ExitStack() as st:
        phase_moe(st)
        S.barrier()
        S.emit()
    es.close()
    return nc


def rope_tables(pos):
    pos = pos.astype(np.float32)
    tabs = np.zeros((pos.shape[0], 192), np.float32)
    inv128 = (10000.0 ** (-np.arange(64, dtype=np.float32) * (2.0 / 128))).astype(np.float32)
    inv64 = (10000.0 ** (-np.arange(32, dtype=np.float32) * (2.0 / 64))).astype(np.float32)
    a = pos[:, None] * inv128[None, :]
    tabs[:, 0:64] = np.cos(a)
    tabs[:, 64:128] = np.sin(a)
    a = pos[:, None] * inv64[None, :]
    tabs[:, 128:160] = np.cos(a)
    tabs[:, 160:192] = np.sin(a)
    return tabs


def pcols_np(inp):
    pc = np.zeros((128, 84), np.float32)
    sm = inp["rwkv_shift_mix"].reshape(-1)
    vecs = [sm[0:1024], sm[1088:2112], sm[2112:3136], inp["w0"].reshape(-1), inp["a0"].reshape(-1),
            inp["k_k"].reshape(-1), inp["k_a"].reshape(-1), inp["r_k"].reshape(-1), inp["ln_x_w"].reshape(-1),
            inp["ln_x_b"].reshape(-1)]
    for j, v in enumerate(vecs):
        pc[:, j * 8:(j + 1) * 8] = v.reshape(8, 128).T
    pc[0:64, 80] = sm[1024:1088]
    pc[0:64, 81] = sm[3136:3200]
    pc[:, 82] = sm[3200:3328]
    pc[0:32, 83] = sm[3328:3360]
    return pc


def cmats_np():
    m = np.zeros((4, 128, 128), np.float32)
    for blk in range(2):
        o = blk * 64
        m[0, o:o + 64, o:o + 64] = 1.0
        m[1, o:o + 64, o:o + 64] = np.tril(np.ones((64, 64), np.float32), -1)
        m[2, o:o + 64, o:o + 64] = np.triu(np.ones((64, 64), np.float32), 1)
        m[3, o:o + 64, o:o + 64] = np.triu(np.ones((64, 64), np.float32), 0)
    return m


def cmask_np():
    m = np.zeros((128, 128), np.float32)
    m[0:64, 64:128] = -1e30
    return m


def kernel(**inputs):
    NP_, NO_ = 2048, 2048
    inp = {k: np.asarray(v) for k, v in inputs.items()}
    xfull = inp["x"]
    B = xfull.shape[0]
    nc = build(NP_, NO_, 256)
    p0 = {k: v[0] for k, v in inp.items() if k != "x"}
    shared = dict(g_mix=inp["g_mix"], w_in=p0["w_in"], ident_in=np.eye(128, dtype=np.float32), q_gain=inp["q_gain"],
                  k_gain=inp["k_gain"], cmask=cmask_np(), pcols=pcols_np(p0), cmats=cmats_np(),
                  lnrow=np.stack([p0["ln_x_w"], p0["ln_x_b"]]), w_decay_up=p0["w_decay_up"], w_aicl_up=p0["w_aicl_up"],
                  w_gate_lora_up=p0["w_gate_lora_up"], w_out=p0["w_out"], g_ffn=inp["g_ffn"],
                  wr=np.ascontiguousarray(np.concatenate([p0["w_route_group"], p0["w_route_expert"]], axis=1)),
                  br=np.ascontiguousarray(np.concatenate([inp["b_route_group"], inp["b_route_expert"]], axis=1)),
                  w_e_gate=p0["w_e_gate"], w_e_up=p0["w_e_up"], w_e_down=p0["w_e_down"])
    in_maps = []
    for c in range(2 * B):
        b, s_ = c // 2, c % 2
        if s_ == 0:
            xs = np.concatenate([np.zeros((NP_, D), np.float32), xfull[b, :NO_]], 0)
            pos = np.concatenate([np.zeros(NP_), np.arange(NO_)])
            pb = np.full((128, 1), -1e30, np.float32)
        else:
            xs = np.ascontiguousarray(xfull[b])
            pos = np.arange(NP_ + NO_)
            pb = np.zeros((128, 1), np.float32)
        m = dict(shared)
        m.update(x=xs, rope_tab=rope_tables(pos), pbias=pb)
        in_maps.append(m)
    res = run_bass_kernel_spmd(nc, in_maps, core_ids=list(range(2 * B)))
    outp = np.zeros_like(xfull)
    for c in range(2 * B):
        b, s_ = c // 2, c % 2
        outp[b, s_ * NO_:(s_ + 1) * NO_] = res.results[c]["out"]
    return outp
```

```python
import os
import numpy as np
import ml_dtypes
from contextlib import ExitStack

import concourse.bass as bass
import concourse.mybir as mybir
from concourse.bass_utils import run_bass_kernel_spmd

F32 = mybir.dt.float32
BF16 = mybir.dt.bfloat16
ALU = mybir.AluOpType
AF = mybir.ActivationFunctionType
AX = mybir.AxisListType

D = 2048
KT = 16
DSA_W = 1024
NH = 8
HD = 128
IH = 16
IDD = 64
RW = 1024
RH = 16
RN = 64
NEXP = 16
DEXP = 512
DSA_COLS = 4176
RWKV_COLS = 3360
IN_COLS = 7536
NORM_EPS = 1e-6
GN_EPS = 64e-5
CHUNK = 64

ENGS = ("pe", "act", "dve", "pool", "sp")
DMAQ = ("sp", "act", "pool")
NSLOT = 24
FUSE_WAIT = True


class Tok:
    __slots__ = ("name", "w", "r")

    def __init__(self, name=""):
        self.name = name
        self.w = None
        self.r = []


class Sched:
    def __init__(self, nc, es):
        self.nc = nc
        self.esem = {e: es.enter_context(nc.semaphore("es_" + e)) for e in ENGS}
        self.ebase = {e: 0 for e in ENGS}
        self.dsem = {q: [es.enter_context(nc.semaphore("ds_%s_%d" % (q, i))) for i in range(NSLOT)]
                     for q in DMAQ}
        self.dcnt = {q: [0] * NSLOT for q in DMAQ}
        self.dn = {q: 0 for q in DMAQ}
        self.waited = {e: {} for e in ENGS}
        self.ops = {e: [] for e in ENGS}
        self.phase = 0
        self.stop = None

    def _deps(self, eng, reads, writes, is_dma):
        raw, other = set(), set()
        for t in reads:
            if t.w is not None:
                raw.add(t.w)
        for t in writes:
            if t.w is not None:
                other.add(t.w)
            for r in t.r:
                other.add(r)
        deps = set()
        for ev in raw:
            if ev[0] == "e" and ev[3] != self.phase:
                continue
            if ev[0] == "e" and ev[1] == eng and eng == "pe" and not is_dma:
                continue
            deps.add(ev)
        for ev in other:
            if ev[0] == "e" and ev[3] != self.phase:
                continue
            if ev[0] == "e" and ev[1] == eng and eng == "pe" and not is_dma:
                continue
            deps.add(ev)
        return deps

    def op(self, eng, fn, reads=(), writes=()):
        idx = len(self.ops[eng])
        deps = self._deps(eng, reads, writes, False)
        ev = ("e", eng, idx, self.phase)
        for t in reads:
            t.r.append(ev)
        for t in writes:
            t.w = ev
            t.r = []
        self.ops[eng].append(dict(fn=fn, deps=deps, ms=False, dma=None))

    def dma(self, q, fn, reads=(), writes=()):
        deps = self._deps(q, reads, writes, True)
        n = self.dn[q]
        self.dn[q] += 1
        slot = n % NSLOT
        if self.dcnt[q][slot] > 0:
            deps.add(("d", q, slot, self.dcnt[q][slot]))
        self.dcnt[q][slot] += 16
        ev = ("d", q, slot, self.dcnt[q][slot])
        for t in reads:
            t.r.append(ev)
        for t in writes:
            t.w = ev
            t.r = []
        self.ops[q].append(dict(fn=fn, deps=deps, ms=False, dma=(q, slot)))

    def barrier(self):
        evs = []
        for e in ENGS:
            for i in range(len(self.ops[e]) - 1, -1, -1):
                if self.ops[e][i]["dma"] is None and self.ops[e][i]["fn"] is not None:
                    evs.append(("e", e, i, self.phase))
                    break
        for q in DMAQ:
            for s in range(NSLOT):
                if self.dcnt[q][s] > 0:
                    evs.append(("d", q, s, self.dcnt[q][s]))
        for e in ENGS:
            deps = set(ev for ev in evs if not (ev[0] == "e" and ev[1] == e))
            self.ops[e].append(dict(fn=None, deps=deps, ms=False, dma=None))

    def emit(self):
        nc = self.nc
        if self.stop is not None and self.phase >= self.stop:
            self.ops = {e: [] for e in ENGS}
            self.phase += 1
            return
        for e in ENGS:
            for o in self.ops[e]:
                for ev in o["deps"]:
                    if ev[0] == "e":
                        self.ops[ev[1]][ev[2]]["ms"] = True
        msv = {}
        for e in ENGS:
            c = self.ebase[e]
            arr = []
            for o in self.ops[e]:
                if o["ms"]:
                    c += 1
                arr.append(c)
            msv[e] = arr
            self.ebase[e] = c
        handles = {"pe": "tensor", "act": "scalar", "dve": "vector", "pool": "gpsimd", "sp": "sync"}

        def replay(e, eng):
            wd = self.waited[e]
            for i, o in enumerate(self.ops[e]):
                need = {}
                for ev in o["deps"]:
                    if ev[0] == "e":
                        sem, val, key = self.esem[ev[1]], msv[ev[1]][ev[2]], ("e", ev[1])
                    else:
                        sem, val, key = self.dsem[ev[1]][ev[2]], ev[3], ("d", ev[1], ev[2])
                    if wd.get(key, 0) >= val:
                        continue
                    if key not in need or need[key][1] < val:
                        need[key] = (sem, val)
                waits = [need[k] for k in sorted(need, key=str)]
                for k in need:
                    wd[k] = need[k][1]
                fused = None
                if FUSE_WAIT and o["fn"] is not None and waits:
                    fused = waits.pop()
                for sem, val in waits:
                    eng.wait_ge(sem, val)
                if o["fn"] is None:
                    continue
                ins = o["fn"](eng)
                if fused is not None:
                    ins._wait_ge(fused[0], fused[1])
                if o["dma"] is not None:
                    ins.then_inc(self.dsem[o["dma"][0]][o["dma"][1]], 16)
                elif o["ms"]:
                    ins.then_inc(self.esem[e], 1)

        with nc.Block() as blk:
            @blk.tensor
            def _(eng):
                replay("pe", eng)

            @blk.scalar
            def _(eng):
                replay("act", eng)

            @blk.vector
            def _(eng):
                replay("dve", eng)

            @blk.gpsimd
            def _(eng):
                replay("pool", eng)

            @blk.sync
            def _(eng):
                replay("sp", eng)
        self.ops = {e: [] for e in ENGS}
        self.phase += 1


class Ctx:
    pass


def build(NP, NO, TOPK, dbg=False, stop_after=None):
    NT = NP + NO
    nc = bass.Bass("TRN2", target_bir_lowering=False)
    g = Ctx()
    kind_s = "ExternalOutput" if dbg else "Internal"

    def din(name, shape, dt=F32):
        return nc.dram_tensor(name, list(shape), dt, kind="ExternalInput").ap()

    def dscr(name, shape, dt=BF16):
        return nc.dram_tensor(name, list(shape), dt, kind=kind_s).ap()

    x = din("x", [NT, D])
    g_mix = din("g_mix", [1, D])
    w_in = din("w_in", [D, IN_COLS])
    rope_tab = din("rope_tab", [NT, 192])
    ident_in = din("ident_in", [128, 128])
    q_gain = din("q_gain", [1, HD])
    k_gain = din("k_gain", [1, HD])
    pcols_in = din("pcols", [128, 84])
    wdu_in = din("w_decay_up", [64, RW])
    wau_in = din("w_aicl_up", [64, RW])
    wgu_in = din("w_gate_lora_up", [160, RW])
    cmats_in = din("cmats", [4, 128, 128])
    lnrow_in = din("lnrow", [2, RW])
    NG = NT // 128
    CMs = dscr("s_CM", [NG, 64, 16, 4, 128])
    TMs = dscr("s_TM", [NG, 128, 4, RW])
    WCs = dscr("s_WC", [64, 16, NT // 64], F32)
    GTs = dscr("s_GT", [RW, NO])
    BNs = dscr("s_BN", [RW, NO])
    w_out = din("w_out", [D, D])
    g_ffn = din("g_ffn", [1, D])
    wr_in = din("wr", [D, 20])
    br_in = din("br", [1, 20])
    weg = din("w_e_gate", [NEXP, D, DEXP])
    weu = din("w_e_up", [NEXP, D, DEXP])
    wed = din("w_e_down", [NEXP, DEXP, D])
    X1 = dscr("s_X1", [NO, D], F32)
    H2T = dscr("s_H2T", [D, NO])
    CMB = dscr("s_CMB", [NO, 16], F32)
    cmask_in = din("cmask", [128, 128])
    pbias_in = din("pbias", [128, 1])
    out = nc.dram_tensor("out", [NO, D], F32, kind="ExternalOutput").ap()
    YT = dscr("s_YT", [D, NO])
    if dbg:
        DBGM = dscr("dbg_mask", [NO // 128, 128, NT])


    QT = dscr("s_QT", [NH * HD, NO])
    KTs = dscr("s_KT", [NH * HD, NT])
    VS = dscr("s_V", [NT, DSA_W])
    QI = dscr("s_QI", [IH * IDD, NO])
    KI = dscr("s_KI", [128, NT])
    WI = dscr("s_WI", [NO, IH], F32)

    es = ExitStack()
    S = Sched(nc, es)
    S.stop = stop_after

    uniq = [0]

    def sb(st, name, shape, dt):
        uniq[0] += 1
        return st.enter_context(nc.sbuf_tensor("%s_%d" % (name, uniq[0]), list(shape), dt))

    def ps(st, name, shape, dt=F32):
        uniq[0] += 1
        return st.enter_context(nc.psum_tensor("%s_%d" % (name, uniq[0]), list(shape), dt))

    ident_f = sb(es, "ident_f", [128, 128], F32)
    ident = sb(es, "ident", [128, 128], BF16)
    t_ident = Tok("ident")
    S.dma("sp", lambda e: e.dma_start(out=ident_f[:], in_=ident_in), writes=[t_ident])
    S.op("dve", lambda e: e.tensor_copy(out=ident[:], in_=ident_f[:]), reads=[t_ident], writes=[t_ident])

    def phase_norm(st, hT, t_hT, tok0, ntok):
        gm = sb(st, "gm", [128, D], F32)
        t_gm = Tok()
        S.dma("sp", lambda e: e.dma_start(out=gm[:], in_=g_mix.broadcast_to([128, D])), writes=[t_gm])
        xt = [sb(st, "xt%d" % i, [128, D], F32) for i in range(2)]
        xn = [sb(st, "xn%d" % i, [128, D], BF16) for i in range(2)]
        junk = sb(st, "junk", [128, D], BF16)
        ss = [sb(st, "ss%d" % i, [128, 2], F32) for i in range(2)]
        pst = [ps(st, "pst%d" % i, [128, D], BF16) for i in range(2)]
        t_xt = [Tok() for _ in range(2)]
        t_xn = [Tok() for _ in range(2)]
        t_ss = [Tok() for _ in range(2)]
        t_ps = [Tok() for _ in range(2)]
        t_junk = Tok()
        for it in range(ntok // 128):
            b = it % 2
            r0 = tok0 + it * 128
            S.dma("sp", lambda e, b=b, r0=r0: e.dma_start(out=xt[b][:], in_=x[r0:r0 + 128, :]), writes=[t_xt[b]])
            S.op("act", lambda e, b=b: e.activation(out=junk[:], in_=xt[b][:], func=AF.Square,
                                                   accum_out=ss[b][:, 0:1]),
                 reads=[t_xt[b]], writes=[t_junk, t_ss[b]])
            S.op("act", lambda e, b=b: e.activation(out=ss[b][:, 1:2], in_=ss[b][:, 0:1], func=AF.Sqrt,
                                                   scale=1.0 / D, bias=NORM_EPS),
                 reads=[t_ss[b]], writes=[t_ss[b]])
            S.op("dve", lambda e, b=b: e.reciprocal(out=ss[b][:, 1:2], in_=ss[b][:, 1:2]),
                 reads=[t_ss[b]], writes=[t_ss[b]])
            S.op("dve", lambda e, b=b: e.scalar_tensor_tensor(out=xn[b][:], in0=xt[b][:], scalar=ss[b][:, 1:2],
                                                             in1=gm[:], op0=ALU.mult, op1=ALU.mult),
                 reads=[t_xt[b], t_ss[b], t_gm], writes=[t_xn[b]])
            for kt in range(KT):
                S.op("pe", lambda e, b=b, kt=kt: e.transpose(out=pst[b][:, kt * 128:(kt + 1) * 128],
                                                            in_=xn[b][:, kt * 128:(kt + 1) * 128],
                                                            identity=ident[:]),
                     reads=[t_xn[b], t_ident], writes=[t_ps[b]])
            c0 = it * 128
            S.op("act", lambda e, b=b, c0=c0: e.copy(out=hT[:, :, c0:c0 + 128],
                                                    in_=pst[b][:].rearrange("p (k t) -> p k t", k=KT)),
                 reads=[t_ps[b]], writes=[t_hT])

    def load_w(st_bufs, cols0, ncols):
        wst, t_wst, wbfs, t_wbfs, cnt = st_bufs
        i = cnt[0]
        cnt[0] += 1
        wb, tw = wbfs[i % 2], t_wbfs[i % 2]
        src = w_in[:, cols0:cols0 + ncols].rearrange("(k p) c -> p k c", p=128)
        S.dma("sp", lambda e: e.dma_start(out=wst[:, :, 0:ncols], in_=src), writes=[t_wst])
        eng = "pool" if i % 2 == 0 else "dve"
        S.op(eng, lambda e: e.tensor_copy(out=wb[:, :, 0:ncols], in_=wst[:, :, 0:ncols]),
             reads=[t_wst], writes=[tw])
        return wb, tw

    def phase_dsa_proj(st, hT, t_hT, tok0, ntok, own):
        wst = sb(st, "wst", [128, KT, 512], F32)
        wbfs = [sb(st, "wbf%d" % i, [128, KT, 512], BF16) for i in range(2)]
        wb = (wst, Tok(), wbfs, [Tok(), Tok()], [0])
        gq = sb(st, "gq", [128, HD], F32)
        gk = sb(st, "gk", [128, HD], F32)
        t_g = Tok()
        S.dma("sp", lambda e: e.dma_start(out=gq[:], in_=q_gain.broadcast_to([128, HD])), writes=[t_g])
        S.dma("sp", lambda e: e.dma_start(out=gk[:], in_=k_gain.broadcast_to([128, HD])), writes=[t_g])
        ntt = ntok // 128
        tabs = sb(st, "tabs", [128, ntt, 192], F32)
        t_tabs = Tok()
        S.dma("sp", lambda e: e.dma_start(out=tabs[:], in_=rope_tab[tok0:tok0 + ntok, :].rearrange(
            "(n p) c -> p n c", p=128)), writes=[t_tabs])
        pp = [ps(st, "pp%d" % i, [128, 512], F32) for i in range(2)]
        t_pp = [Tok(), Tok()]
        ptr_full = [ps(st, "ptr%d" % i, [128, 1024], BF16) for i in range(2)]
        ptr = [t_[:, 0:512] for t_ in ptr_full]
        t_ptr = [Tok(), Tok()]
        sq = sb(st, "sq", [128, 512], F32)
        xn = sb(st, "xnq", [128, 512], F32)
        ro = sb(st, "ro", [128, 512], BF16)
        tmp1 = sb(st, "tmp1", [128, 256], F32)
        tmp2 = sb(st, "tmp2", [128, 256], F32)
        ssq = sb(st, "ssq", [128, 8], F32)
        t_sq, t_xn, t_ro, t_t1, t_t2, t_ssq = Tok(), Tok(), Tok(), Tok(), Tok(), Tok()
        stg = [sb(st, "stg%d" % i, [128, 4, 512], BF16) for i in range(2)]
        t_stg = [Tok(), Tok()]
        vst = [sb(st, "vst%d" % i, [128, 512], BF16) for i in range(2)]
        t_vst = [Tok(), Tok()]
        wis = sb(st, "wis", [128, IH], F32)
        t_wis = Tok()
        cnt = [0]
        nstg = [0]

        def mm_tok(wtile, tw, ncols, it):
            i = cnt[0]
            cnt[0] += 1
            p, tp = pp[i % 2], t_pp[i % 2]
            for kt in range(KT):
                S.op("pe", lambda e, kt=kt, p=p: e.matmul(p[:, 0:ncols], lhsT=hT[:, kt, it * 128:(it + 1) * 128],
                                                       rhs=wtile[:, kt, 0:ncols], start=(kt == 0),
                                                       stop=(kt == KT - 1)),
                     reads=[t_hT, tw], writes=[tp])
            return p, tp

        def rope_ops(src, t_src, nh, hd, cos, sin, dst, t_dst):
            hf = hd // 2
            s3 = src.rearrange("p (h d) -> p h d", h=nh)
            d3 = dst.rearrange("p (h d) -> p h d", h=nh)
            cb = cos.unsqueeze(1).broadcast_to([128, nh, hf])
            sn = sin.unsqueeze(1).broadcast_to([128, nh, hf])
            a = tmp1[:, 0:nh * hf].rearrange("p (h d) -> p h d", h=nh)
            b = tmp2[:, 0:nh * hf].rearrange("p (h d) -> p h d", h=nh)
            x1, x2 = s3[:, :, 0:hf], s3[:, :, hf:hd]
            S.op("dve", lambda e: e.tensor_tensor(out=a, in0=x1, in1=cb, op=ALU.mult),
                 reads=[t_src, t_tabs], writes=[t_t1])
            S.op("pool", lambda e: e.tensor_tensor(out=b, in0=x2, in1=sn, op=ALU.mult),
                 reads=[t_src, t_tabs], writes=[t_t2])
            S.op("dve", lambda e: e.tensor_tensor(out=d3[:, :, 0:hf], in0=a, in1=b, op=ALU.subtract),
                 reads=[t_t1, t_t2], writes=[t_dst])
            S.op("dve", lambda e: e.tensor_tensor(out=a, in0=x2, in1=cb, op=ALU.mult),
                 reads=[t_src, t_tabs, t_dst], writes=[t_t1])
            S.op("pool", lambda e: e.tensor_tensor(out=b, in0=x1, in1=sn, op=ALU.mult),
                 reads=[t_src, t_tabs, t_dst], writes=[t_t2])
            S.op("dve", lambda e: e.tensor_tensor(out=d3[:, :, hf:hd], in0=a, in1=b, op=ALU.add),
                 reads=[t_t1, t_t2], writes=[t_dst])

        def qk_group(col0, gain, dstT, is_q):
            for half in range(2):
                wtile, tw = load_w(wb, col0 + half * 512, 512)
                for it in range(ntt):
                    p, tp = mm_tok(wtile, tw, 512, it)
                    S.op("act", lambda e, p=p: e.activation(out=sq[:], in_=p[:], func=AF.Square),
                         reads=[tp], writes=[t_sq])
                    S.op("dve", lambda e: e.tensor_reduce(out=ssq[:, 0:4], in_=sq[:].rearrange(
                        "p (h d) -> p h d", h=4), axis=AX.X, op=ALU.add), reads=[t_sq], writes=[t_ssq])
                    S.op("act", lambda e: e.activation(out=ssq[:, 4:8], in_=ssq[:, 0:4], func=AF.Sqrt,
                                                       scale=1.0 / HD, bias=NORM_EPS),
                         reads=[t_ssq], writes=[t_ssq])
                    S.op("dve", lambda e: e.reciprocal(out=ssq[:, 4:8], in_=ssq[:, 4:8]),
                         reads=[t_ssq], writes=[t_ssq])
                    S.op("dve", lambda e, p=p: e.tensor_tensor(
                        out=xn[:].rearrange("p (h d) -> p h d", h=4), in0=p[:].rearrange("p (h d) -> p h d", h=4),
                        in1=ssq[:, 4:8].unsqueeze(2).broadcast_to([128, 4, HD]), op=ALU.mult),
                         reads=[tp, t_ssq], writes=[t_xn])
                    S.op("pool", lambda e: e.tensor_tensor(
                        out=xn[:].rearrange("p (h d) -> p h d", h=4), in0=xn[:].rearrange("p (h d) -> p h d", h=4),
                        in1=gain[:].unsqueeze(1).broadcast_to([128, 4, HD]), op=ALU.mult),
                         reads=[t_xn, t_g], writes=[t_xn])
                    rope_ops(xn[:], t_xn, 4, HD, tabs[:, it, 0:64], tabs[:, it, 64:128], ro[:], t_ro)
                    j = nstg[0] // 4
                    sl = nstg[0] % 4
                    nstg[0] += 1
                    pt, tpt = ptr[it % 2], t_ptr[it % 2]
                    for h in range(4):
                        S.op("pe", lambda e, h=h, pt=pt: e.transpose(out=pt[:, h * 128:(h + 1) * 128],
                                                                  in_=ro[:, h * 128:(h + 1) * 128],
                                                                  identity=ident[:]),
                             reads=[t_ro, t_ident], writes=[tpt])
                    sg, tsg = stg[j % 2], t_stg[j % 2]
                    S.op("act", lambda e, pt=pt, sg=sg, sl=sl: e.copy(
                        out=sg[:, :, sl * 128:(sl + 1) * 128], in_=pt.rearrange("p (h t) -> p h t", h=4)),
                         reads=[tpt], writes=[tsg])
                    if sl == 3:
                        t0 = (it - 3) * 128 + (0 if is_q else tok0)
                        h0 = half * 4
                        dst = dstT.rearrange("(h d) t -> d h t", d=HD)[:, h0:h0 + 4, t0:t0 + 512]
                        S.dma("sp", lambda e, sg=sg, dst=dst: e.dma_start(out=dst, in_=sg[:]),
                              reads=[tsg], writes=[])

        qk_group(DSA_W, gk, KTs, False)
        if own:
            qk_group(0, gq, QT, True)
        for half in range(2):
            wtile, tw = load_w(wb, 2 * DSA_W + half * 512, 512)
            for it in range(ntt):
                p, tp = mm_tok(wtile, tw, 512, it)
                v, tv = vst[it % 2], t_vst[it % 2]
                S.op("act", lambda e, p=p, v=v: e.copy(out=v[:], in_=p[:]), reads=[tp], writes=[tv])
                r0 = tok0 + it * 128
                S.dma("sp", lambda e, v=v, r0=r0, half=half: e.dma_start(
                    out=VS[r0:r0 + 128, half * 512:(half + 1) * 512], in_=v[:]), reads=[tv], writes=[])
        wtile, tw = load_w(wb, 3 * DSA_W + IH * IDD, IDD + IH)
        for it in range(ntt):
            p, tp = mm_tok(wtile, tw, IDD + IH, it)
            S.op("act", lambda e, p=p: e.copy(out=xn[:, 0:IDD + IH], in_=p[:, 0:IDD + IH]), reads=[tp], writes=[t_xn])
            rope_ops(xn[:, 0:IDD], t_xn, 1, IDD, tabs[:, it, 128:160], tabs[:, it, 160:192], ro[:, 0:IDD], t_ro)
            S.op("pool", lambda e: e.tensor_copy(out=ro[:, IDD:2 * IDD], in_=ro[:, 0:IDD]),
                 reads=[t_ro], writes=[t_ro])
            if own:
                S.op("act", lambda e: e.mul(out=wis[:], in_=xn[:, IDD:IDD + IH], mul=1.0 / 32.0),
                     reads=[t_xn], writes=[t_wis])
                S.dma("sp", lambda e, it=it: e.dma_start(out=WI[it * 128:(it + 1) * 128, :], in_=wis[:]),
                      reads=[t_wis], writes=[])
            pt, tpt = ptr[it % 2], t_ptr[it % 2]
            S.op("pe", lambda e, pt=pt: e.transpose(out=pt[:, 0:128], in_=ro[:, 0:128], identity=ident[:]),
                 reads=[t_ro, t_ident], writes=[tpt])
            j = nstg[0] // 4
            sl = nstg[0] % 4
            nstg[0] += 1
            sg, tsg = stg[j % 2], t_stg[j % 2]
            S.op("act", lambda e, pt=pt, sg=sg, sl=sl: e.copy(out=sg[:, 0, sl * 128:(sl + 1) * 128],
                                                            in_=pt[:, 0:128]), reads=[tpt], writes=[tsg])
            if sl == 3:
                t0 = tok0 + (it - 3) * 128
                S.dma("sp", lambda e, sg=sg, t0=t0: e.dma_start(out=KI[:, t0:t0 + 512], in_=sg[:, 0, :]),
                      reads=[tsg], writes=[])
        if own:
            for half in range(2):
                wtile, tw = load_w(wb, 3 * DSA_W + half * 512, 512)
                for it in range(ntt):
                    p, tp = mm_tok(wtile, tw, 512, it)
                    S.op("act", lambda e, p=p: e.copy(out=xn[:], in_=p[:]), reads=[tp], writes=[t_xn])
                    rope_ops(xn[:], t_xn, 8, IDD, tabs[:, it, 128:160], tabs[:, it, 160:192], ro[:], t_ro)
                    pt, tpt = ptr[it % 2], t_ptr[it % 2]
                    for h in range(4):
                        S.op("pe", lambda e, h=h, pt=pt: e.transpose(out=pt[:, h * 128:(h + 1) * 128],
                                                                  in_=ro[:, h * 128:(h + 1) * 128],
                                                                  identity=ident[:]),
                             reads=[t_ro, t_ident], writes=[tpt])
                    j = nstg[0] // 4
                    sl = nstg[0] % 4
                    nstg[0] += 1
                    sg, tsg = stg[j % 2], t_stg[j % 2]
                    S.op("act", lambda e, pt=pt, sg=sg, sl=sl: e.copy(
                        out=sg[:, :, sl * 128:(sl + 1) * 128], in_=pt.rearrange("p (h t) -> p h t", h=4)),
                         reads=[tpt], writes=[tsg])
                    if sl == 3:
                        t0 = (it - 3) * 128
                        dst = QI.rearrange("(h d) t -> d h t", d=128)[:, half * 4:half * 4 + 4, t0:t0 + 512]
                        S.dma("sp", lambda e, sg=sg, dst=dst: e.dma_start(out=dst, in_=sg[:]),
                              reads=[tsg], writes=[])


    NQT = NO // 128
    mt_base = []
    acc_ = 0
    for qt in range(NQT):
        mt_base.append(acc_)
        acc_ += (NP + (qt + 1) * 128) // 128
    MT_TILES = acc_
    NBIS = 20

    def phase_index(st, maskT, t_maskT):
        qi = sb(st, "qi", [128, 8, NO], BF16)
        ki = sb(st, "ki", [128, NT], BF16)
        wi = sb(st, "wi", [128, NQT, IH], F32)
        cm = sb(st, "cm", [128, 128], F32)
        pb = sb(st, "pb", [128, 1], F32)
        t_in = Tok()
        S.dma("sp", lambda e: e.dma_start(out=qi[:], in_=QI.rearrange("(h d) t -> d h t", d=128)), writes=[t_in])
        S.dma("sp", lambda e: e.dma_start(out=ki[:], in_=KI), writes=[t_in])
        S.dma("sp", lambda e: e.dma_start(out=wi[:], in_=WI.rearrange("(n p) h -> p n h", p=128)), writes=[t_in])
        S.dma("sp", lambda e: e.dma_start(out=cm[:], in_=cmask_in), writes=[t_in])
        S.dma("sp", lambda e: e.dma_start(out=pb[:], in_=pbias_in), writes=[t_in])
        iscs = [sb(st, "isc%d" % i, [128, NT], F32) for i in range(2)]
        junks = [sb(st, "junkm%d" % i, [128, NT], BF16) for i in range(2)]
        sms = [sb(st, "sm%d" % i, [128, 8], F32) for i in range(2)]
        t_iscs, t_junks, t_sms = [Tok(), Tok()], [Tok(), Tok()], [Tok(), Tok()]
        rr = [sb(st, "rr%d" % i, [128, 512], F32) for i in range(4)]
        t_rr = [Tok() for _ in range(4)]
        iscB = [sb(st, "iscB%d" % i, [128, 512], F32) for i in range(2)]
        t_iscB = [Tok(), Tok()]
        pp = [ps(st, "pi%d" % i, [128, 512], F32) for i in range(4)]
        t_pp = [Tok() for _ in range(4)]
        pT_full = [ps(st, "pT%d" % i, [128, 1024], BF16) for i in range(2)]
        pT = [t_[:, 0:512] for t_ in pT_full]
        t_pT = [Tok(), Tok()]
        cnt = [0]
        NDV = 12

        def head_block(qt, kb, h, isc, t_isc, nk):
            w = min(512, nk - kb * 512)
            i = cnt[0]
            cnt[0] += 1
            p, tp = pp[i % 4], t_pp[i % 4]
            r, tr = rr[i % 4], t_rr[i % 4]
            pr = (h % 2) * 64
            S.op("pe", lambda e: e.matmul(p[:, 0:w], lhsT=qi[pr:pr + 64, h // 2, qt * 128:(qt + 1) * 128],
                                          rhs=ki[pr:pr + 64, kb * 512:kb * 512 + w], start=True, stop=True),
                 reads=[t_in], writes=[tp])
            S.op("act", lambda e: e.activation(out=r[:, 0:w], in_=p[:, 0:w], func=AF.Relu), reads=[tp], writes=[tr])
            dst = isc[:, kb * 512:kb * 512 + w]
            ws = wi[:, qt, h:h + 1]
            if h == 0:
                S.op("dve", lambda e: e.tensor_scalar(out=dst, in0=r[:, 0:w], scalar1=ws, scalar2=None, op0=ALU.mult),
                     reads=[tr, t_in], writes=[t_isc])
            elif h < NDV:
                S.op("dve", lambda e: e.scalar_tensor_tensor(out=dst, in0=r[:, 0:w], scalar=ws, in1=dst, op0=ALU.mult, op1=ALU.add),
                     reads=[tr, t_in, t_isc], writes=[t_isc])
            else:
                ib, tib = iscB[kb % 2], t_iscB[kb % 2]
                if h == NDV:
                    S.op("pool", lambda e: e.tensor_scalar(out=ib[:, 0:w], in0=r[:, 0:w], scalar1=ws, scalar2=None, op0=ALU.mult),
                         reads=[tr, t_in], writes=[tib])
                else:
                    S.op("pool", lambda e: e.tensor_scalar(out=r[:, 0:w], in0=r[:, 0:w], scalar1=ws, scalar2=None, op0=ALU.mult),
                         reads=[tr, t_in], writes=[tr])
                    S.op("pool", lambda e: e.tensor_tensor(out=ib[:, 0:w], in0=ib[:, 0:w], in1=r[:, 0:w], op=ALU.add),
                         reads=[tr, tib], writes=[tib])
                if h == IH - 1:
                    S.op("dve", lambda e: e.tensor_tensor(out=dst, in0=dst, in1=ib[:, 0:w], op=ALU.add),
                         reads=[tib, t_isc], writes=[t_isc])

        for q0 in range(0, NQT, 2):
            qts = [q_ for q_ in (q0, q0 + 1) if q_ < NQT]
            nks = [NP + (q_ + 1) * 128 for q_ in qts]
            for x_, qt in enumerate(qts):
                nk = nks[x_]
                nblk = (nk + 511) // 512
                for k0 in range(0, nblk, 2):
                    kbs = [k_ for k_ in (k0, k0 + 1) if k_ < nblk]
                    for h in range(IH):
                        for kb in kbs:
                            head_block(qt, kb, h, iscs[x_], t_iscs[x_], nk)
            def both(fn):
                for x_ in range(len(qts)):
                    fn(x_, iscs[x_], t_iscs[x_], junks[x_], t_junks[x_], sms[x_], t_sms[x_], nks[x_])
            both(lambda x_, isc, ti, junk, tj, sm, ts, nk: S.op("dve", lambda e: e.tensor_reduce(
                out=sm[:, 5:6], in_=isc[:, 0:nk], axis=AX.X, op=ALU.max), reads=[ti], writes=[ts]))
            both(lambda x_, isc, ti, junk, tj, sm, ts, nk: S.op("dve", lambda e: e.tensor_reduce(
                out=sm[:, 0:1], in_=isc[:, 0:nk], axis=AX.X, op=ALU.min), reads=[ti, ts], writes=[ts]))
            both(lambda x_, isc, ti, junk, tj, sm, ts, nk: S.op("dve", lambda e: e.tensor_tensor(
                out=sm[:, 1:2], in0=sm[:, 5:6], in1=sm[:, 0:1], op=ALU.subtract), reads=[ts], writes=[ts]))
            both(lambda x_, isc, ti, junk, tj, sm, ts, nk: S.op("dve", lambda e: e.tensor_tensor(
                out=isc[:, nk - 128:nk], in0=isc[:, nk - 128:nk], in1=cm[:], op=ALU.add), reads=[ti, t_in], writes=[ti]))
            if NP > 0:
                both(lambda x_, isc, ti, junk, tj, sm, ts, nk: S.op("dve", lambda e: e.tensor_scalar(
                    out=isc[:, 0:NP], in0=isc[:, 0:NP], scalar1=pb[:, 0:1], scalar2=None, op0=ALU.add),
                    reads=[ti, t_in], writes=[ti]))
            both(lambda x_, isc, ti, junk, tj, sm, ts, nk: S.op("dve", lambda e: e.tensor_scalar(
                out=sm[:, 1:2], in0=sm[:, 1:2], scalar1=1e-20, scalar2=None, op0=ALU.max), reads=[ts], writes=[ts]))
            both(lambda x_, isc, ti, junk, tj, sm, ts, nk: S.op("dve", lambda e: e.reciprocal(
                out=sm[:, 4:5], in_=sm[:, 1:2]), reads=[ts], writes=[ts]))
            both(lambda x_, isc, ti, junk, tj, sm, ts, nk: S.op("dve", lambda e: e.tensor_scalar(
                out=isc[:, 0:nk], in0=isc[:, 0:nk], scalar1=sm[:, 0:1], scalar2=sm[:, 4:5], op0=ALU.subtract, op1=ALU.mult),
                reads=[ti, ts], writes=[ti]))
            both(lambda x_, isc, ti, junk, tj, sm, ts, nk: S.op("dve", lambda e: e.memset(sm[:, 2:3], 0.5),
                                                               reads=[ts], writes=[ts]))
            for it in range(NBIS):
                last = (it == NBIS - 1)
                f = 0.5 ** (it + 1)
                both(lambda x_, isc, ti, junk, tj, sm, ts, nk: S.op("dve", lambda e: e.tensor_scalar(
                    out=junk[:, 0:nk], in0=isc[:, 0:nk], scalar1=sm[:, 2:3], scalar2=None, op0=ALU.is_ge, op1=ALU.add,
                    accum_out=sm[:, 3:4]), reads=[ti, ts, tj], writes=[tj, ts]))
                both(lambda x_, isc, ti, junk, tj, sm, ts, nk, last=last: S.op("dve", lambda e: e.tensor_scalar(
                    out=sm[:, 5:6], in0=sm[:, 3:4], scalar1=float(TOPK) - 0.5, scalar2=(-1.0 if last else -0.5),
                    op0=ALU.is_ge, op1=ALU.add), reads=[ts], writes=[ts]))
                both(lambda x_, isc, ti, junk, tj, sm, ts, nk, last=last, f=f: S.op("dve", lambda e: e.scalar_tensor_tensor(
                    out=(sm[:, 0:1] if last else sm[:, 2:3]), in0=sm[:, 5:6], scalar=f, in1=sm[:, 2:3], op0=ALU.mult,
                    op1=ALU.add), reads=[ts], writes=[ts]))
            both(lambda x_, isc, ti, junk, tj, sm, ts, nk: S.op("dve", lambda e: e.tensor_scalar(
                out=junk[:, 0:nk], in0=isc[:, 0:nk], scalar1=sm[:, 0:1], scalar2=None, op0=ALU.is_ge),
                reads=[ti, ts, tj], writes=[tj]))
            for x_, qt in enumerate(qts):
                nk = nks[x_]
                nkt = nk // 128
                junk, t_junk = junks[x_], t_junks[x_]
                if dbg:
                    S.dma("sp", lambda e, qt=qt, nk=nk, junk=junk: e.dma_start(out=DBGM[qt, :, 0:nk], in_=junk[:, 0:nk]), reads=[t_junk])
                k0 = 0
                g_ = 0
                while k0 < nkt:
                    n = min(4, nkt - k0)
                    p, tp = pT[g_ % 2], t_pT[g_ % 2]
                    g_ += 1
                    for j in range(n):
                        S.op("pe", lambda e, p=p, j=j, k0=k0, junk=junk: e.transpose(
                            out=p[:, j * 128:(j + 1) * 128], in_=junk[:, (k0 + j) * 128:(k0 + j + 1) * 128],
                            identity=ident[:]), reads=[t_junk, t_ident], writes=[tp])
                    b0 = (mt_base[qt] + k0) * 128
                    S.op("act", lambda e, p=p, n=n, b0=b0: e.activation(out=maskT[:, b0:b0 + n * 128], in_=p[:, 0:n * 128],
                                                                       func=AF.Identity, scale=30000.0, bias=-30000.0),
                         reads=[tp], writes=[t_maskT])
                    k0 += n

    def phase_attn(st, maskT, t_maskT):
        ones = sb(st, "ones", [128, 128], BF16)
        t_ones = Tok()
        S.op("dve", lambda e: e.memset(ones[:], 1.0), writes=[t_ones])
        NKT = NT // 128
        kth = [sb(st, "kth%d" % i, [128, NT], BF16) for i in range(2)]
        vh = [sb(st, "vh%d" % i, [128, NKT, 128], BF16) for i in range(2)]
        qth = [sb(st, "qth%d" % i, [128, NO], BF16) for i in range(2)]
        yth = [sb(st, "yth%d" % i, [128, NO], BF16) for i in range(2)]
        t_k = [Tok(), Tok()]
        t_y = [Tok(), Tok()]
        pS = [ps(st, "pS%d" % i, [128, 512], F32) for i in range(2)]
        t_pS = [Tok(), Tok()]
        pO = [ps(st, "pO%d" % i, [128, 512], F32) for i in range(2)]
        pD = [ps(st, "pD%d" % i, [128, 512], F32) for i in range(2)]
        t_pO = [Tok(), Tok()]
        P = [sb(st, "P%d" % i, [128, 512], BF16) for i in range(2)]
        Pm = [sb(st, "Pm%d" % i, [128, 512], BF16) for i in range(2)]
        t_P = [Tok(), Tok()]
        t_Pm = [Tok(), Tok()]
        rec = sb(st, "rec", [128, 128], F32)
        t_rec = Tok()
        g_ = [0]
        scale = float(HD) ** -0.5
        for h in range(NH):
            b = h % 2
            S.dma("sp", lambda e, b=b, h=h: e.dma_start(out=kth[b][:], in_=KTs[h * 128:(h + 1) * 128, :]),
                  writes=[t_k[b]])
            S.dma("sp", lambda e, b=b, h=h: e.dma_start(out=vh[b][:], in_=VS[:, h * 128:(h + 1) * 128].rearrange(
                "(k p) d -> p k d", p=128)), writes=[t_k[b]])
            S.dma("sp", lambda e, b=b, h=h: e.dma_start(out=qth[b][:], in_=QT[h * 128:(h + 1) * 128, :]),
                  writes=[t_k[b]])
            for qt in range(NQT):
                nkt = (NP + (qt + 1) * 128) // 128
                po, tpo = pO[qt % 2], t_pO[qt % 2]
                pd = pD[qt % 2]
                k0 = 0
                while k0 < nkt:
                    n = min(4, nkt - k0)
                    i = g_[0]
                    g_[0] += 1
                    p_s, tps = pS[i % 2], t_pS[i % 2]
                    b0 = (mt_base[qt] + k0) * 128
                    S.op("pe", lambda e, p_s=p_s, n=n, b0=b0: e.matmul(
                        p_s[:, 0:n * 128], lhsT=ident[:], rhs=maskT[:, b0:b0 + n * 128], start=True, stop=False),
                         reads=[t_ident, t_maskT], writes=[tps])
                    for j in range(n):
                        S.op("pe", lambda e, p_s=p_s, j=j, k0=k0, b=b, qt=qt, n=n: e.matmul(
                            p_s[:, j * 128:(j + 1) * 128], lhsT=kth[b][:, (k0 + j) * 128:(k0 + j + 1) * 128],
                            rhs=qth[b][:, qt * 128:(qt + 1) * 128], start=False, stop=(j == n - 1)),
                             reads=[t_k[b]], writes=[tps])
                    S.op("act", lambda e, p_s=p_s, n=n, i=i: e.activation(
                        out=Pm[i % 2][:, 0:n * 128], in_=p_s[:, 0:n * 128], func=AF.Exp, scale=scale),
                         reads=[tps], writes=[t_Pm[i % 2]])
                    for j in range(n):
                        first = (k0 + j == 0)
                        last = (k0 + j == nkt - 1)
                        S.op("pe", lambda e, po=po, j=j, k0=k0, b=b, i=i, first=first, last=last: e.matmul(
                            po[:, 0:128], lhsT=vh[b][:, k0 + j, :], rhs=Pm[i % 2][:, j * 128:(j + 1) * 128],
                            start=first, stop=last), reads=[t_k[b], t_Pm[i % 2]], writes=[tpo])
                        S.op("pe", lambda e, pd=pd, j=j, i=i, first=first, last=last: e.matmul(
                            pd[:, 0:128], lhsT=ones[:], rhs=Pm[i % 2][:, j * 128:(j + 1) * 128],
                            start=first, stop=last), reads=[t_ones, t_Pm[i % 2]], writes=[tpo])
                    k0 += n
                S.op("dve", lambda e, pd=pd: e.reciprocal(out=rec[:], in_=pd[:, 0:128]),
                     reads=[tpo], writes=[t_rec])
                S.op("dve", lambda e, po=po, b=b, qt=qt: e.tensor_tensor(
                    out=yth[b][:, qt * 128:(qt + 1) * 128], in0=po[:, 0:128], in1=rec[:], op=ALU.mult),
                     reads=[tpo, t_rec], writes=[t_y[b]])
            S.dma("sp", lambda e, b=b, h=h: e.dma_start(out=YT[h * 128:(h + 1) * 128, :], in_=yth[b][:]),
                  reads=[t_y[b]], writes=[])


    RO = DSA_COLS
    R_R, R_DW, R_K, R_V, R_DA, R_DG = RO, RO + 1024, RO + 1088, RO + 2112, RO + 3136, RO + 3200
    carry = sb(es, "carry", [128, 32], F32)
    t_carry = Tok()
    S.op("dve", lambda e: e.memset(carry[:], 0.0), writes=[t_carry])

    def phase_rwkv_prep(st, hT, t_hT, tok0, ntok, own):
        nb = ntok // 512
        wst = sb(st, "wstr", [128, KT, 512], F32)
        wbfs = [sb(st, "wbfr%d" % i, [128, KT, 512], BF16) for i in range(2)]
        wb = (wst, Tok(), wbfs, [Tok(), Tok()], [0])
        pc = sb(st, "pc", [128, 84], F32)
        t_pc = Tok()
        S.dma("sp", lambda e: e.dma_start(out=pc[:], in_=pcols_in), writes=[t_pc])
        lst = sb(st, "lst", [128, 1, RW], F32)
        wdu = sb(st, "wdu", [64, RW], BF16)
        wau = sb(st, "wau", [64, RW], BF16)
        wgu0 = sb(st, "wgu0", [128, RW], BF16)
        wgu1 = sb(st, "wgu1", [32, RW], BF16)
        cst = sb(st, "cst", [128, 128], F32)
        bones = sb(st, "bones", [128, 128], BF16)
        t_lw = Tok()
        S.dma("sp", lambda e: e.dma_start(out=cst[:], in_=cmats_in[0]), writes=[t_lw])
        S.op("dve", lambda e: e.tensor_copy(out=bones[:], in_=cst[:]), reads=[t_lw], writes=[t_lw])
        S.dma("sp", lambda e: e.dma_start(out=lst[0:64, 0, :], in_=wdu_in), reads=[t_lw], writes=[t_lw])
        S.op("dve", lambda e: e.tensor_copy(out=wdu[:], in_=lst[0:64, 0, :]), reads=[t_lw], writes=[t_lw])
        S.dma("sp", lambda e: e.dma_start(out=lst[0:64, 0, :], in_=wau_in), reads=[t_lw], writes=[t_lw])
        S.op("dve", lambda e: e.tensor_copy(out=wau[:], in_=lst[0:64, 0, :]), reads=[t_lw], writes=[t_lw])
        S.dma("sp", lambda e: e.dma_start(out=lst[:, 0, :], in_=wgu_in[0:128, :]), reads=[t_lw], writes=[t_lw])
        S.op("dve", lambda e: e.tensor_copy(out=wgu0[:], in_=lst[:, 0, :]), reads=[t_lw], writes=[t_lw])
        S.dma("sp", lambda e: e.dma_start(out=lst[0:32, 0, :], in_=wgu_in[128:160, :]), reads=[t_lw], writes=[t_lw])
        S.op("dve", lambda e: e.tensor_copy(out=wgu1[:], in_=lst[0:32, 0, :]), reads=[t_lw], writes=[t_lw])
        th_all = sb(st, "th_all", [64, ntok], BF16)
        da_all = sb(st, "da_all", [64, ntok], BF16)
        dg0_all = sb(st, "dg0_all", [128, ntok], BF16)
        dg1_all = sb(st, "dg1_all", [32, ntok], BF16)
        t_lora = Tok()
        pm = [ps(st, "pm%d" % i, [128, 512], F32) for i in range(3)]
        t_pm = [Tok() for _ in range(3)]
        px = [ps(st, "px%d" % i, [128, 512], F32) for i in range(2)]
        t_px = [Tok(), Tok()]
        ptT_full = ps(st, "ptT", [128, 1024], BF16)
        ptT = ptT_full[:, 0:512]
        t_ptT = Tok()
        nbuf = ["raw", "dsh", "r", "k", "v", "lw", "a", "kk", "kkn", "k2", "b", "cA", "cB", "e", "tmp"]
        B = {n: sb(st, "rb_" + n, [128, 512], F32) for n in nbuf}
        T = {n: Tok() for n in nbuf}
        hb = {n: sb(st, "rh_" + n, [128, 512], BF16) for n in ("sqb", "rkb", "tmk", "tmb", "vb", "gt", "bn")}
        TH = {n: Tok() for n in hb}
        cm = sb(st, "cmt", [128, 4, 512], BF16)
        t_cm = Tok()
        tmt = sb(st, "tmt", [128, 4, 128], BF16)
        t_tmt = Tok()
        wc = sb(st, "wc", [128, 8], F32)
        t_wc = Tok()
        mmc = [0]

        def mm_ch(wtile, tw, c0, ncol, tb):
            i = mmc[0]
            mmc[0] += 1
            p, tp = pm[i % 3], t_pm[i % 3]
            for kt in range(KT):
                S.op("pe", lambda e, kt=kt, p=p: e.matmul(p[0:ncol, :], lhsT=wtile[:, kt, c0:c0 + ncol],
                                                       rhs=hT[:, kt, tb * 512:(tb + 1) * 512], start=(kt == 0),
                                                       stop=(kt == KT - 1)), reads=[t_hT, tw], writes=[tp])
            return p, tp

        def shift(p, tp, n_, mu_col, cidx, dst, t_dst):
            raw, dsh = B["raw"], B["dsh"]
            S.op("act", lambda e: e.copy(out=raw[0:n_, :], in_=p[0:n_, :]), reads=[tp], writes=[T["raw"]])
            S.op("dve", lambda e: e.tensor_tensor(out=dsh[0:n_, 1:512], in0=raw[0:n_, 0:511], in1=raw[0:n_, 1:512],
                                                  op=ALU.subtract), reads=[T["raw"]], writes=[T["dsh"]])
            S.op("dve", lambda e: e.tensor_tensor(out=dsh[0:n_, 0:1], in0=carry[0:n_, cidx:cidx + 1],
                                                  in1=raw[0:n_, 0:1], op=ALU.subtract),
                 reads=[T["raw"], t_carry], writes=[T["dsh"]])
            S.op("pool", lambda e: e.tensor_copy(out=carry[0:n_, cidx:cidx + 1], in_=raw[0:n_, 511:512]),
                 reads=[T["raw"], T["dsh"]], writes=[t_carry])
            S.op("dve", lambda e: e.scalar_tensor_tensor(out=dst, in0=dsh[0:n_, :], scalar=pc[0:n_, mu_col:mu_col + 1],
                                                         in1=raw[0:n_, :], op0=ALU.mult, op1=ALU.add),
                 reads=[T["dsh"], T["raw"], t_pc], writes=[t_dst])

        wtile, tw = load_w(wb, R_DW, 64)
        for tb in range(nb):
            p, tp = mm_ch(wtile, tw, 0, 64, tb)
            shift(p, tp, 64, 80, 0, B["tmp"][0:64, :], T["tmp"])
            S.op("act", lambda e, tb=tb: e.activation(out=th_all[:, tb * 512:(tb + 1) * 512], in_=B["tmp"][0:64, :],
                                                     func=AF.Tanh), reads=[T["tmp"]], writes=[t_lora])
        wtile, tw = load_w(wb, R_DA, 64 + 160)
        for tb in range(nb):
            p, tp = mm_ch(wtile, tw, 0, 64, tb)
            shift(p, tp, 64, 81, 1, B["tmp"][0:64, :], T["tmp"])
            S.op("act", lambda e, tb=tb: e.copy(out=da_all[:, tb * 512:(tb + 1) * 512], in_=B["tmp"][0:64, :]),
                 reads=[T["tmp"]], writes=[t_lora])
            p, tp = mm_ch(wtile, tw, 64, 128, tb)
            shift(p, tp, 128, 82, 2, B["tmp"][:, :], T["tmp"])
            S.op("act", lambda e, tb=tb: e.activation(out=dg0_all[:, tb * 512:(tb + 1) * 512], in_=B["tmp"][:, :],
                                                     func=AF.Sigmoid), reads=[T["tmp"]], writes=[t_lora])
            p, tp = mm_ch(wtile, tw, 192, 32, tb)
            shift(p, tp, 32, 83, 3, B["tmp"][0:32, :], T["tmp"])
            S.op("act", lambda e, tb=tb: e.activation(out=dg1_all[:, tb * 512:(tb + 1) * 512], in_=B["tmp"][0:32, :],
                                                     func=AF.Sigmoid), reads=[T["tmp"]], writes=[t_lora])

        def v3(ap):
            return ap.rearrange("p (c t) -> p c t", t=64)

        for ct in range(8):
            wst_, t_wst, wbfs_, t_wbfs, cnt_ = wb
            i = cnt_[0]
            cnt_[0] += 1
            wtile, tw = wbfs_[i % 2], t_wbfs[i % 2]
            for j, c0 in enumerate((R_R, R_K, R_V)):
                src = w_in[:, c0 + ct * 128:c0 + (ct + 1) * 128].rearrange("(k p) c -> p k c", p=128)
                S.dma("sp", lambda e, src=src, j=j: e.dma_start(out=wst_[:, :, j * 128:(j + 1) * 128], in_=src),
                      writes=[t_wst])
            S.op("pool", lambda e, wtile=wtile: e.tensor_copy(out=wtile[:, :, 0:384], in_=wst_[:, :, 0:384]),
                 reads=[t_wst], writes=[tw])
            for tb in range(nb):
                t0 = tb * 512
                for j, nm in enumerate(("r", "k", "v")):
                    p, tp = mm_ch(wtile, tw, j * 128, 128, tb)
                    shift(p, tp, 128, j * 8 + ct, 4 + j * 8 + ct, B[nm][:], T[nm])
                cs = slice(ct * 128, (ct + 1) * 128)
                pz, tpz = px[0], t_px[0]
                S.op("pe", lambda e, pz=pz, cs=cs, t0=t0: e.matmul(pz[:], lhsT=wdu[:, cs], rhs=th_all[:, t0:t0 + 512],
                                                             start=True, stop=True), reads=[t_lw, t_lora], writes=[tpz])
                S.op("act", lambda e, pz=pz, ct=ct: e.activation(out=B["lw"][:], in_=pz[:], func=AF.Sigmoid,
                                                               bias=pc[:, 24 + ct:25 + ct]),
                     reads=[tpz, t_pc], writes=[T["lw"]])
                S.op("pool", lambda e: e.tensor_scalar(out=B["lw"][:], in0=B["lw"][:], scalar1=-0.6065306597126334,
                                                      scalar2=None, op0=ALU.mult), reads=[T["lw"]], writes=[T["lw"]])
                pa, tpa = px[1], t_px[1]
                S.op("pe", lambda e, pa=pa, cs=cs, t0=t0: e.matmul(pa[:], lhsT=wau[:, cs], rhs=da_all[:, t0:t0 + 512],
                                                             start=True, stop=True), reads=[t_lw, t_lora], writes=[tpa])
                S.op("act", lambda e, pa=pa, ct=ct: e.activation(out=B["a"][:], in_=pa[:], func=AF.Sigmoid,
                                                               bias=pc[:, 32 + ct:33 + ct]),
                     reads=[tpa, t_pc], writes=[T["a"]])
                if own:
                    pg, tpg = px[0], t_px[0]
                    S.op("pe", lambda e, pg=pg, cs=cs, t0=t0: e.matmul(pg[:], lhsT=wgu0[:, cs], rhs=dg0_all[:, t0:t0 + 512],
                                                                 start=True, stop=False), reads=[t_lw, t_lora], writes=[tpg])
                    S.op("pe", lambda e, pg=pg, cs=cs, t0=t0: e.matmul(pg[:], lhsT=wgu1[:, cs], rhs=dg1_all[:, t0:t0 + 512],
                                                                 start=False, stop=True), reads=[t_lw, t_lora], writes=[tpg])
                    S.op("act", lambda e, pg=pg: e.copy(out=hb["gt"][:], in_=pg[:]), reads=[tpg], writes=[TH["gt"]])
                    S.dma("sp", lambda e, cs=cs, t0=t0: e.dma_start(out=GTs[cs, t0:t0 + 512], in_=hb["gt"][:]),
                          reads=[TH["gt"]])
                S.op("dve", lambda e, ct=ct: e.tensor_scalar(out=B["kk"][:], in0=B["k"][:], scalar1=pc[:, 40 + ct:41 + ct],
                                                            scalar2=None, op0=ALU.mult),
                     reads=[T["k"], t_pc], writes=[T["kk"]])
                S.op("pool", lambda e: e.tensor_tensor(out=hb["sqb"][:], in0=B["kk"][:], in1=B["kk"][:], op=ALU.mult),
                     reads=[T["kk"]], writes=[TH["sqb"]])
                pss, tpss = px[1], t_px[1]
                S.op("pe", lambda e, pss=pss: e.matmul(pss[:], lhsT=bones[:], rhs=hb["sqb"][:], start=True, stop=True),
                     reads=[t_lw, TH["sqb"]], writes=[tpss])
                S.op("act", lambda e, pss=pss: e.activation(out=B["tmp"][:], in_=pss[:], func=AF.Sqrt),
                     reads=[tpss], writes=[T["tmp"]])
                S.op("dve", lambda e: e.tensor_scalar(out=B["tmp"][:], in0=B["tmp"][:], scalar1=1e-12, scalar2=None,
                                                      op0=ALU.max), reads=[T["tmp"]], writes=[T["tmp"]])
                S.op("dve", lambda e: e.reciprocal(out=B["tmp"][:], in_=B["tmp"][:]), reads=[T["tmp"]], writes=[T["tmp"]])
                S.op("dve", lambda e: e.tensor_tensor(out=B["kkn"][:], in0=B["kk"][:], in1=B["tmp"][:], op=ALU.mult),
                     reads=[T["kk"], T["tmp"]], writes=[T["kkn"]])
                S.op("pool", lambda e, ct=ct: e.tensor_scalar(out=B["k2"][:], in0=B["a"][:], scalar1=-1.0,
                                                             scalar2=pc[:, 48 + ct:49 + ct], op0=ALU.add, op1=ALU.mult),
                     reads=[T["a"], t_pc], writes=[T["k2"]])
                S.op("pool", lambda e: e.tensor_scalar(out=B["k2"][:], in0=B["k2"][:], scalar1=1.0, scalar2=None, op0=ALU.add),
                     reads=[T["k2"]], writes=[T["k2"]])
                S.op("pool", lambda e: e.tensor_tensor(out=B["k2"][:], in0=B["k2"][:], in1=B["k"][:], op=ALU.mult),
                     reads=[T["k2"], T["k"]], writes=[T["k2"]])
                S.op("pool", lambda e: e.tensor_tensor(out=B["b"][:], in0=B["kkn"][:], in1=B["a"][:], op=ALU.mult),
                     reads=[T["kkn"], T["a"]], writes=[T["b"]])
                if own:
                    S.op("dve", lambda e, ct=ct: e.scalar_tensor_tensor(out=hb["rkb"][:], in0=B["r"][:],
                                                                       scalar=pc[:, 56 + ct:57 + ct], in1=B["k2"][:],
                                                                       op0=ALU.mult, op1=ALU.mult),
                         reads=[T["r"], T["k2"], t_pc], writes=[TH["rkb"]])
                    prk, tprk = px[0], t_px[0]
                    S.op("pe", lambda e, prk=prk: e.matmul(prk[:], lhsT=bones[:], rhs=hb["rkb"][:], start=True, stop=True),
                         reads=[t_lw, TH["rkb"]], writes=[tprk])
                    S.op("dve", lambda e, prk=prk: e.tensor_tensor(out=hb["bn"][:], in0=prk[:], in1=B["v"][:], op=ALU.mult),
                         reads=[tprk, T["v"]], writes=[TH["bn"]])
                    S.dma("sp", lambda e, cs=cs, t0=t0: e.dma_start(out=BNs[cs, t0:t0 + 512], in_=hb["bn"][:]),
                          reads=[TH["bn"]])
                src, t_src = B["lw"], T["lw"]
                for si, sft in enumerate((1, 2, 4, 8, 16, 32)):
                    dn = "cA" if si % 2 == 0 else "cB"
                    dst_, t_dst_ = B[dn], T[dn]
                    S.op("pool", lambda e, src=src, dst_=dst_, sft=sft: e.tensor_copy(
                        out=v3(dst_[:])[:, :, 0:sft], in_=v3(src[:])[:, :, 0:sft]), reads=[t_src], writes=[t_dst_])
                    S.op("dve", lambda e, src=src, dst_=dst_, sft=sft: e.tensor_tensor(
                        out=v3(dst_[:])[:, :, sft:64], in0=v3(src[:])[:, :, sft:64], in1=v3(src[:])[:, :, 0:64 - sft],
                        op=ALU.add), reads=[t_src], writes=[t_dst_])
                    src, t_src = dst_, t_dst_
                cl, t_cl = src, t_src
                S.op("act", lambda e, cl=cl: e.activation(out=B["e"][:], in_=cl[:], func=AF.Exp), reads=[t_cl], writes=[T["e"]])
                S.op("dve", lambda e: e.tensor_tensor(out=cm[:, 3, :], in0=B["r"][:], in1=B["e"][:], op=ALU.mult),
                     reads=[T["r"], T["e"]], writes=[t_cm])
                S.op("pool", lambda e, cl=cl: e.tensor_tensor(out=B["tmp"][:], in0=cl[:], in1=B["lw"][:], op=ALU.subtract),
                     reads=[t_cl, T["lw"]], writes=[T["tmp"]])
                S.op("act", lambda e: e.activation(out=B["e"][:], in_=B["tmp"][:], func=AF.Exp),
                     reads=[T["tmp"]], writes=[T["e"]])
                S.op("dve", lambda e: e.tensor_tensor(out=cm[:, 2, :], in0=B["kkn"][:], in1=B["e"][:], op=ALU.mult),
                     reads=[T["kkn"], T["e"]], writes=[t_cm])
                S.op("act", lambda e, cl=cl: e.activation(out=B["e"][:], in_=cl[:], func=AF.Exp, scale=-1.0),
                     reads=[t_cl], writes=[T["e"]])
                S.op("dve", lambda e: e.tensor_tensor(out=cm[:, 1, :], in0=B["k2"][:], in1=B["e"][:], op=ALU.mult),
                     reads=[T["k2"], T["e"]], writes=[t_cm])
                S.op("pool", lambda e: e.tensor_tensor(out=cm[:, 0, :], in0=B["b"][:], in1=B["e"][:], op=ALU.mult),
                     reads=[T["b"], T["e"]], writes=[t_cm])
                S.op("dve", lambda e, cl=cl: e.tensor_tensor(out=v3(B["tmp"][:]), in0=v3(cl[:])[:, :, 63:64].broadcast_to([128, 8, 64]),
                                                            in1=v3(cl[:]), op=ALU.subtract), reads=[t_cl], writes=[T["tmp"]])
                S.op("act", lambda e: e.activation(out=B["e"][:], in_=B["tmp"][:], func=AF.Exp),
                     reads=[T["tmp"]], writes=[T["e"]])
                S.op("dve", lambda e: e.tensor_tensor(out=hb["tmk"][:], in0=B["k2"][:], in1=B["e"][:], op=ALU.mult),
                     reads=[T["k2"], T["e"]], writes=[TH["tmk"]])
                S.op("pool", lambda e: e.tensor_tensor(out=hb["tmb"][:], in0=B["b"][:], in1=B["e"][:], op=ALU.mult),
                     reads=[T["b"], T["e"]], writes=[TH["tmb"]])
                S.op("act", lambda e: e.copy(out=hb["vb"][:], in_=B["v"][:]), reads=[T["v"]], writes=[TH["vb"]])
                S.op("act", lambda e, cl=cl: e.activation(out=wc[:].unsqueeze(2), in_=v3(cl[:])[:, :, 63:64], func=AF.Exp),
                     reads=[t_cl], writes=[t_wc])
                g0 = (tok0 + t0) // 128
                for hh in range(2):
                    hd_ = 2 * ct + hh
                    for j in range(4):
                        dst = CMs[g0 + j, :, hd_, :, :]
                        srcap = cm[hh * 64:(hh + 1) * 64, :, j * 128:(j + 1) * 128]
                        S.dma("sp", lambda e, dst=dst, srcap=srcap: e.dma_start(out=dst, in_=srcap), reads=[t_cm])
                    c0_ = (tok0 + t0) // 64
                    S.dma("sp", lambda e, hh=hh, hd_=hd_, c0_=c0_: e.dma_start(
                        out=WCs[:, hd_, c0_:c0_ + 8], in_=wc[hh * 64:(hh + 1) * 64, :]), reads=[t_wc])
                for j in range(4):
                    srcs = (cm[:, 2, j * 128:(j + 1) * 128], hb["tmb"][:, j * 128:(j + 1) * 128],
                            hb["tmk"][:, j * 128:(j + 1) * 128], hb["vb"][:, j * 128:(j + 1) * 128])
                    for x_, sa in enumerate(srcs):
                        S.op("pe", lambda e, x_=x_, sa=sa: e.transpose(out=ptT[:, x_ * 128:(x_ + 1) * 128], in_=sa,
                                                                     identity=ident[:]),
                             reads=[t_cm, TH["tmb"], TH["tmk"], TH["vb"], t_ident], writes=[t_ptT])
                    S.op("act", lambda e: e.copy(out=tmt[:], in_=ptT.rearrange("p (x c) -> p x c", x=4)),
                         reads=[t_ptT], writes=[t_tmt])
                    S.dma("sp", lambda e, g0=g0, j=j, cs=cs: e.dma_start(out=TMs[g0 + j, :, :, cs], in_=tmt[:]),
                          reads=[t_tmt])


    NGP = NP // 128

    def phase_rwkv_scan(st):
        mk = sb(st, "mk", [128, 3, 128], F32)
        pc = sb(st, "pc2", [128, 84], F32)
        t_c = Tok()
        S.dma("sp", lambda e: e.dma_start(out=mk[:], in_=cmats_in[1:4].rearrange("m p c -> p m c")), writes=[t_c])
        S.dma("sp", lambda e: e.dma_start(out=pc[:], in_=pcols_in), writes=[t_c])
        cmg = [sb(st, "cmg%d" % i, [64, 16, 4, 128], BF16) for i in range(2)]
        tmg = [sb(st, "tmg%d" % i, [128, 4, RW], BF16) for i in range(2)]
        wcg = [sb(st, "wcg%d" % i, [64, 16, 2], F32) for i in range(2)]
        t_ld = [Tok(), Tok()]
        H = sb(st, "H", [64, 16, 64], BF16)
        t_H = [Tok() for _ in range(16)]
        S.op("dve", lambda e: e.memset(H[:], 0.0), writes=t_H)
        PB = [ps(st, "pb%d" % i, [128, 512], F32) for i in range(7)]
        t_PB = [Tok() for _ in range(7)]
        pTp = ps(st, "pTp", [128, 8, 128], BF16)
        t_pTp = Tok()
        pbc = [0]

        def bank():
            i = pbc[0] % 7
            pbc[0] += 1
            return PB[i], t_PB[i]
        names = ["LL", "G2", "Rb", "P0", "PT0", "P1", "PT1", "QpT", "AiT", "Kp", "M0", "M1"]
        NBUF = 4
        hk = [sb(st, "hk%d" % i, [64, 64], F32) for i in range(NBUF)]
        t_hk = [Tok() for _ in range(NBUF)]
        W = {n: [sb(st, "sw_%s%d" % (n, i), [128, 256], BF16) for i in range(NBUF)] for n in names}
        TW = {n: [Tok() for _ in range(NBUF)] for n in names}
        lqk = [sb(st, "lqk%d" % i, [128, 128], F32) for i in range(NBUF)]
        t_lqk = [Tok() for _ in range(NBUF)]
        ysb = sb(st, "ysb", [64, 2, RW], F32)
        t_ysb = Tok()
        ynb = sb(st, "ynb", [64, 2, RW], BF16)
        st1 = sb(st, "st1", [64, 2, 16], F32)
        st2 = sb(st, "st2", [64, 2, 16], F32)
        dd = sb(st, "dd", [64, 2, RW], F32)
        sq2 = sb(st, "sq2", [64, 2, RW], F32)
        t_post = Tok()
        bng = sb(st, "bng", [128, 8, 128], BF16)
        gtg = sb(st, "gtg", [128, 8, 128], BF16)
        t_bg = Tok()
        fin = sb(st, "fin", [128, 8, 128], F32)
        yrt = sb(st, "yrt", [128, 8, 128], BF16)
        t_fin, t_yrt = Tok(), Tok()
        engs = ("dve", "act")
        for g_ in range(NG):
            b = g_ % 2
            own_g = g_ >= NGP
            S.dma("sp", lambda e, b=b, g_=g_: e.dma_start(out=cmg[b][:], in_=CMs[g_]), writes=[t_ld[b]])
            S.dma("sp", lambda e, b=b, g_=g_: e.dma_start(out=tmg[b][:], in_=TMs[g_]), writes=[t_ld[b]])
            S.dma("sp", lambda e, b=b, g_=g_: e.dma_start(out=wcg[b][:], in_=WCs[:, :, 2 * g_:2 * g_ + 2]), writes=[t_ld[b]])
            LVL = int(os.environ.get("SCAN_LEVEL", "9"))

            def head_gen(h, b=b, g_=g_, own_g=own_g):
                u = h % NBUF
                hs = slice(h * 64, (h + 1) * 64)
                bT, kT, kkT, qT = (cmg[b][:, h, x_, :] for x_ in range(4))
                KKt, Bh, Kh, V = (tmg[b][:, x_, hs] for x_ in range(4))
                LL, G2, Rb = W["LL"][u], W["G2"][u], W["Rb"][u]
                p1, tp1 = bank()
                S.op("pe", lambda e, p1=p1, kkT=kkT, b=b, h=h: e.matmul(p1[:, 0:256], lhsT=kkT,
                     rhs=cmg[b][:, h, 0:2, :].rearrange("p x t -> p (x t)"), start=True, stop=True), reads=[t_ld[b]], writes=[tp1])
                S.op("dve", lambda e, p1=p1, LL=LL: e.tensor_tensor(out=LL[:].rearrange("p (x c) -> p x c", x=2),
                     in0=p1[:, 0:256].rearrange("p (x c) -> p x c", x=2),
                     in1=mk[:, 0, :].unsqueeze(1).broadcast_to([128, 2, 128]), op=ALU.mult), reads=[tp1, t_c], writes=[TW["LL"][u]])
                p2, tp2 = bank()
                S.op("pe", lambda e, p2=p2, bT=bT, b=b, h=h: e.matmul(p2[:, 0:256], lhsT=bT,
                     rhs=cmg[b][:, h, 2:4, :].rearrange("p x t -> p (x t)"), start=True, stop=True), reads=[t_ld[b]], writes=[tp2])
                S.op("dve", lambda e, p2=p2, G2=G2: e.tensor_tensor(out=G2[:].rearrange("p (x c) -> p x c", x=2),
                     in0=p2[:, 0:256].rearrange("p (x c) -> p x c", x=2), in1=mk[:, 1:3, :], op=ALU.mult),
                     reads=[tp2, t_c], writes=[TW["G2"][u]])
                p3, tp3 = bank()
                S.op("pe", lambda e, p3=p3, kT=kT, qT=qT: e.matmul(p3[:, 0:128], lhsT=kT, rhs=qT, start=True, stop=True),
                     reads=[t_ld[b]], writes=[tp3])
                S.op("dve", lambda e, p3=p3, u=u: e.tensor_tensor(out=lqk[u][:], in0=p3[:, 0:128], in1=mk[:, 2, :], op=ALU.mult),
                     reads=[tp3, t_c], writes=[t_lqk[u]])
                yield
                S.op("act", lambda e, Rb=Rb, KKt=KKt: e.copy(out=Rb[:, 0:64], in_=KKt), reads=[t_ld[b]], writes=[TW["Rb"][u]])
                S.op("act", lambda e, Rb=Rb, LL=LL: e.copy(out=Rb[:, 64:192], in_=LL[:, 128:256]), reads=[TW["LL"][u]], writes=[TW["Rb"][u]])
                Pc, tPc = LL[:, 0:128], TW["LL"][u]
                PTc, tPTc = G2[:, 0:128], TW["G2"][u]
                if LVL < 3:
                    return
                for k_ in range(6):
                    yield
                    if k_ > 0:
                        nP, tnP = W["P%d" % (k_ % 2)][u], TW["P%d" % (k_ % 2)][u]
                        nPT, tnPT = W["PT%d" % (k_ % 2)][u], TW["PT%d" % (k_ % 2)][u]
                        pa, tpa = bank()
                        pb_, tpb = bank()
                        S.op("pe", lambda e, pa=pa, PTc=PTc, Pc=Pc: e.matmul(pa[:, 0:128], lhsT=PTc, rhs=Pc, start=True, stop=True),
                             reads=[tPc, tPTc], writes=[tpa])
                        S.op("pe", lambda e, pb_=pb_, PTc=PTc, Pc=Pc: e.matmul(pb_[:, 0:128], lhsT=Pc, rhs=PTc, start=True, stop=True),
                             reads=[tPc, tPTc], writes=[tpb])
                        S.op("act", lambda e, pa=pa, nP=nP: e.copy(out=nP[:, 0:128], in_=pa[:, 0:128]), reads=[tpa], writes=[tnP])
                        S.op("dve", lambda e, pb_=pb_, nPT=nPT: e.tensor_copy(out=nPT[:, 0:128], in_=pb_[:, 0:128]), reads=[tpb], writes=[tnPT])
                        Pc, tPc, PTc, tPTc = nP[:, 0:128], tnP, nPT[:, 0:128], tnPT
                    pr_, tpr = bank()
                    S.op("pe", lambda e, pr_=pr_, PTc=PTc, Rb=Rb: e.matmul(pr_[:, 0:192], lhsT=PTc, rhs=Rb[:, 0:192], start=True, stop=True),
                         reads=[tPTc, TW["Rb"][u]], writes=[tpr])
                    S.op("dve", lambda e, pr_=pr_, Rb=Rb, k_=k_: e.tensor_tensor(out=Rb[:, 0:192], in0=Rb[:, 0:192], in1=pr_[:, 0:192],
                         op=(ALU.subtract if k_ == 0 else ALU.add)), reads=[tpr, TW["Rb"][u]], writes=[TW["Rb"][u]])
                if LVL < 4:
                    return
                yield
                E, F = Rb[:, 0:64], Rb[:, 64:192]
                LqbT = G2[:, 128:256]
                QpT, AiT, Kp, M0, M1 = W["QpT"][u], W["AiT"][u], W["Kp"][u], W["M0"][u], W["M1"][u]
                pq, tpq = bank()
                S.op("pe", lambda e, pq=pq, E=E, LqbT=LqbT: e.matmul(pq[0:64, 0:128], lhsT=E, rhs=LqbT, start=True, stop=True),
                     reads=[TW["Rb"][u], TW["G2"][u]], writes=[tpq])
                S.op("dve", lambda e, pq=pq, QpT=QpT, qT=qT: e.tensor_tensor(out=QpT[0:64, 0:128], in0=qT, in1=pq[0:64, 0:128], op=ALU.subtract),
                     reads=[tpq, t_ld[b]], writes=[TW["QpT"][u]])
                yield
                pa2, tpa2 = bank()
                S.op("pe", lambda e, pa2=pa2, F=F, LqbT=LqbT: e.matmul(pa2[:, 0:128], lhsT=F, rhs=LqbT, start=True, stop=True),
                     reads=[TW["Rb"][u], TW["G2"][u]], writes=[tpa2])
                S.op("dve", lambda e, pa2=pa2, AiT=AiT, u=u: e.tensor_tensor(out=AiT[:, 0:128], in0=lqk[u][:], in1=pa2[:, 0:128], op=ALU.subtract),
                     reads=[tpa2, t_lqk[u]], writes=[TW["AiT"][u]])
                yield
                pk, tpk = bank()
                S.op("pe", lambda e, pk=pk, F=F, Bh=Bh: e.matmul(pk[:, 0:64], lhsT=F, rhs=Bh, start=True, stop=True),
                     reads=[TW["Rb"][u], t_ld[b]], writes=[tpk])
                S.op("dve", lambda e, pk=pk, Kp=Kp, Kh=Kh: e.tensor_tensor(out=Kp[:, 0:64], in0=Kh, in1=pk[:, 0:64], op=ALU.subtract),
                     reads=[tpk, t_ld[b]], writes=[TW["Kp"][u]])
                yield
                for c in range(2):
                    Mc, tMc = (M0, TW["M0"][u]) if c == 0 else (M1, TW["M1"][u])
                    rs = slice(c * 64, (c + 1) * 64)
                    pm_, tpm = bank()
                    S.op("pe", lambda e, pm_=pm_, rs=rs, Bh=Bh, Rb=Rb, b=b, h=h: e.matmul(pm_[0:64, 0:64], lhsT=Rb[rs, 0:64],
                         rhs=tmg[b][rs, 1, h * 64:(h + 1) * 64], start=True, stop=True), reads=[TW["Rb"][u], t_ld[b]], writes=[tpm])
                    S.op("dve", lambda e, pm_=pm_, Mc=Mc, c=c, b=b, h=h: e.scalar_tensor_tensor(out=Mc[0:64, 0:64], in0=ident_f[0:64, 0:64],
                         scalar=wcg[b][:, h, c:c + 1], in1=pm_[0:64, 0:64], op0=ALU.mult, op1=ALU.subtract),
                         reads=[tpm, t_ld[b], t_ident], writes=[tMc])
                if LVL < 5:
                    return
                for c in range(2):
                    Mc, tMc = (M0, TW["M0"][u]) if c == 0 else (M1, TW["M1"][u])
                    rs = slice(c * 64, (c + 1) * 64)
                    yield
                    if own_g:
                        py, tpy = bank()
                        py2, tpy2 = bank()
                        S.op("pe", lambda e, py=py, QpT=QpT, rs=rs, h=h: e.matmul(py[0:64, 0:64], lhsT=QpT[0:64, rs], rhs=H[:, h, :],
                             start=True, stop=True), reads=[TW["QpT"][u], t_H[h]], writes=[tpy])
                        S.op("pe", lambda e, py2=py2, AiT=AiT, rs=rs, b=b, h=h: e.matmul(py2[0:64, 0:64], lhsT=AiT[rs, rs],
                             rhs=tmg[b][rs, 3, h * 64:(h + 1) * 64], start=True, stop=True), reads=[TW["AiT"][u], t_ld[b]], writes=[tpy2])
                        S.op("act", lambda e, py=py, c=c, hs=hs: e.copy(out=ysb[:, c, hs], in_=py[0:64, 0:64]), reads=[tpy], writes=[t_ysb])
                        S.op("dve", lambda e, py2=py2, c=c, hs=hs: e.tensor_tensor(out=ysb[:, c, hs], in0=ysb[:, c, hs], in1=py2[0:64, 0:64],
                                                                                  op=ALU.add), reads=[tpy2, t_ysb], writes=[t_ysb])
                    ph, tph = bank()
                    ph2, tph2 = bank()
                    S.op("pe", lambda e, ph2=ph2, Kp=Kp, rs=rs, b=b, h=h: e.matmul(ph2[0:64, 0:64], lhsT=Kp[rs, 0:64],
                         rhs=tmg[b][rs, 3, h * 64:(h + 1) * 64], start=True, stop=True), reads=[TW["Kp"][u], t_ld[b]], writes=[tph2])
                    S.op("pe", lambda e, ph=ph, Mc=Mc, h=h: e.matmul(ph[0:64, 0:64], lhsT=Mc[0:64, 0:64], rhs=H[:, h, :], start=True, stop=True),
                         reads=[tMc, t_H[h]], writes=[tph])
                    S.op("act", lambda e, ph2=ph2, u=u: e.copy(out=hk[u][:], in_=ph2[0:64, 0:64]), reads=[tph2], writes=[t_hk[u]])
                    S.op("dve", lambda e, ph=ph, h=h, u=u: e.tensor_tensor(out=H[:, h, :], in0=hk[u][:], in1=ph[0:64, 0:64], op=ALU.add),
                         reads=[tph, t_hk[u]], writes=[t_H[h]])

            if LVL >= 2:
                gens = [head_gen(h) for h in range(16)]
                active = []
                nxt = 0
                while nxt < 16 or active:
                    while nxt < 16 and len(active) < NBUF:
                        active.append(gens[nxt])
                        nxt += 1
                    for gi in list(active):
                        try:
                            next(gi)
                        except StopIteration:
                            active.remove(gi)
            if own_g and LVL >= 6:
                go = g_ - NGP
                y4 = lambda ap: ap.rearrange("p c (h v) -> p c h v", v=64)
                S.dma("sp", lambda e, go=go: e.dma_start(out=bng[:], in_=BNs[:, go * 128:(go + 1) * 128].rearrange("(c p) t -> p c t", p=128)), writes=[t_bg])
                S.dma("sp", lambda e, go=go: e.dma_start(out=gtg[:], in_=GTs[:, go * 128:(go + 1) * 128].rearrange("(c p) t -> p c t", p=128)), writes=[t_bg])
                S.op("dve", lambda e: e.tensor_reduce(out=st1[:], in_=y4(ysb[:]), axis=AX.X, op=ALU.add), reads=[t_ysb], writes=[t_post])
                S.op("dve", lambda e: e.tensor_scalar(out=st1[:], in0=st1[:], scalar1=1.0 / 64, scalar2=None, op0=ALU.mult), reads=[t_post], writes=[t_post])
                S.op("dve", lambda e: e.tensor_tensor(out=y4(dd[:]), in0=y4(ysb[:]), in1=st1[:].unsqueeze(3).broadcast_to([64, 2, 16, 64]),
                                                      op=ALU.subtract), reads=[t_ysb, t_post], writes=[t_post])
                S.op("pool", lambda e: e.tensor_tensor(out=sq2[:], in0=dd[:], in1=dd[:], op=ALU.mult), reads=[t_post], writes=[t_post])
                S.op("dve", lambda e: e.tensor_reduce(out=st2[:], in_=y4(sq2[:]), axis=AX.X, op=ALU.add), reads=[t_post], writes=[t_post])
                S.op("act", lambda e: e.activation(out=st2[:], in_=st2[:], func=AF.Sqrt, scale=1.0 / 64, bias=GN_EPS), reads=[t_post], writes=[t_post])
                S.op("dve", lambda e: e.reciprocal(out=st2[:], in_=st2[:]), reads=[t_post], writes=[t_post])
                S.op("dve", lambda e: e.tensor_tensor(out=y4(ynb[:]), in0=y4(dd[:]), in1=st2[:].unsqueeze(3).broadcast_to([64, 2, 16, 64]),
                                                      op=ALU.mult), reads=[t_post], writes=[t_post])
                for c in range(2):
                    for ct in range(8):
                        S.op("pe", lambda e, c=c, ct=ct: e.transpose(out=pTp[:, ct, c * 64:(c + 1) * 64], in_=ynb[:, c, ct * 128:(ct + 1) * 128],
                                                                  identity=ident[0:64, 0:64]), reads=[t_post, t_ident], writes=[t_pTp])
                S.op("dve", lambda e: e.tensor_tensor(out=fin[:], in0=pTp[:], in1=pc[:, 64:72].unsqueeze(2).broadcast_to([128, 8, 128]), op=ALU.mult),
                     reads=[t_pTp, t_c], writes=[t_fin])
                S.op("pool", lambda e: e.tensor_tensor(out=fin[:], in0=fin[:], in1=pc[:, 72:80].unsqueeze(2).broadcast_to([128, 8, 128]), op=ALU.add),
                     reads=[t_fin, t_c], writes=[t_fin])
                S.op("pool", lambda e: e.tensor_tensor(out=fin[:], in0=fin[:], in1=bng[:], op=ALU.add), reads=[t_fin, t_bg], writes=[t_fin])
                S.op("dve", lambda e: e.tensor_tensor(out=yrt[:], in0=fin[:], in1=gtg[:], op=ALU.mult), reads=[t_fin, t_bg], writes=[t_yrt])
                S.dma("sp", lambda e, go=go: e.dma_start(out=YT[1024:2048, go * 128:(go + 1) * 128].rearrange("(c p) t -> p c t", p=128), in_=yrt[:]),
                      reads=[t_yrt])


    def phase_outproj(st):
        yT = sb(st, "yT", [128, KT, NO], BF16)
        t_yT = Tok()
        S.dma("sp", lambda e: e.dma_start(out=yT[:], in_=YT.rearrange("(k p) t -> p k t", p=128)), writes=[t_yT])
        wo = sb(st, "wo", [128, KT, D], BF16)
        wst = sb(st, "wsto", [128, KT, 512], F32)
        t_wst, t_wo = Tok(), Tok()
        for cb in range(4):
            S.dma("sp", lambda e, cb=cb: e.dma_start(out=wst[:], in_=w_out[:, cb * 512:(cb + 1) * 512].rearrange(
                "(k p) c -> p k c", p=128)), writes=[t_wst])
            S.op("pool" if cb % 2 else "dve", lambda e, cb=cb: e.tensor_copy(out=wo[:, :, cb * 512:(cb + 1) * 512], in_=wst[:]),
                 reads=[t_wst], writes=[t_wo])
        wrs = sb(st, "wrs", [128, KT, 20], F32)
        wrb = sb(st, "wrb", [128, KT, 20], BF16)
        brb = sb(st, "brb", [128, 20], F32)
        gf = sb(st, "gf", [128, D], F32)
        t_r = Tok()
        S.dma("sp", lambda e: e.dma_start(out=wrs[:], in_=wr_in.rearrange("(k p) c -> p k c", p=128)), writes=[t_r])
        S.dma("sp", lambda e: e.dma_start(out=brb[:], in_=br_in.broadcast_to([128, 20])), writes=[t_r])
        S.dma("sp", lambda e: e.dma_start(out=gf[:], in_=g_ffn.broadcast_to([128, D])), writes=[t_r])
        S.op("dve", lambda e: e.tensor_copy(out=wrb[:], in_=wrs[:]), reads=[t_r], writes=[t_r])
        _xt = sb(st, "xo", [128, D], F32)
        _x1 = sb(st, "x1", [128, D], F32)
        xt = [_xt, _xt]
        x1 = [_x1, _x1]
        xn = sb(st, "xno", [128, D], BF16)
        junk = xn
        h2t = sb(st, "h2t", [128, KT, 128], BF16)
        ss = sb(st, "sso", [128, 2], F32)
        _a, _b = Tok(), Tok()
        t_xt, t_x1 = [_a, _a], [_b, _b]
        t_xn, t_h2t, t_ss = Tok(), Tok(), Tok()
        t_junk = t_xn
        po = [ps(st, "po%d" % i, [128, 512], F32) for i in range(4)]
        t_po = [Tok() for _ in range(4)]
        pst = ps(st, "psto", [128, D], BF16)
        t_pst = Tok()
        pl = ps(st, "pl", [128, 512], F32)
        t_pl = Tok()
        R = sb(st, "rt", [128, 96], F32)
        t_R = Tok()
        for it in range(NO // 128):
            b = it % 2
            r0 = NP + it * 128
            S.dma("sp", lambda e, b=b, r0=r0: e.dma_start(out=xt[b][:], in_=x[r0:r0 + 128, :]), writes=[t_xt[b]])
            for cb in range(4):
                for kt in range(KT):
                    S.op("pe", lambda e, cb=cb, kt=kt, it=it: e.matmul(po[cb][:], lhsT=yT[:, kt, it * 128:(it + 1) * 128],
                                                                       rhs=wo[:, kt, cb * 512:(cb + 1) * 512], start=(kt == 0),
                                                                       stop=(kt == KT - 1)), reads=[t_yT, t_wo], writes=[t_po[cb]])
                S.op("dve", lambda e, cb=cb, b=b: e.tensor_tensor(out=x1[b][:, cb * 512:(cb + 1) * 512], in0=po[cb][:],
                                                                 in1=xt[b][:, cb * 512:(cb + 1) * 512], op=ALU.add),
                     reads=[t_po[cb], t_xt[b]], writes=[t_x1[b]])
            S.dma("sp", lambda e, b=b, it=it: e.dma_start(out=X1[it * 128:(it + 1) * 128, :], in_=x1[b][:]), reads=[t_x1[b]])
            S.op("act", lambda e, b=b: e.activation(out=junk[:], in_=x1[b][:], func=AF.Square, accum_out=ss[:, 0:1]),
                 reads=[t_x1[b]], writes=[t_junk, t_ss])
            S.op("act", lambda e: e.activation(out=ss[:, 1:2], in_=ss[:, 0:1], func=AF.Sqrt, scale=1.0 / D, bias=NORM_EPS),
                 reads=[t_ss], writes=[t_ss])
            S.op("dve", lambda e: e.reciprocal(out=ss[:, 1:2], in_=ss[:, 1:2]), reads=[t_ss], writes=[t_ss])
            S.op("dve", lambda e, b=b: e.scalar_tensor_tensor(out=xn[:], in0=x1[b][:], scalar=ss[:, 1:2], in1=gf[:],
                                                             op0=ALU.mult, op1=ALU.mult), reads=[t_x1[b], t_ss, t_r], writes=[t_xn])
            for kt in range(KT):
                S.op("pe", lambda e, kt=kt: e.transpose(out=pst[:, kt * 128:(kt + 1) * 128], in_=xn[:, kt * 128:(kt + 1) * 128],
                                                       identity=ident[:]), reads=[t_xn, t_ident], writes=[t_pst])
            S.op("act", lambda e: e.copy(out=h2t[:], in_=pst[:].rearrange("p (k t) -> p k t", k=KT)), reads=[t_pst], writes=[t_h2t])
            S.dma("sp", lambda e, it=it: e.dma_start(out=H2T[:, it * 128:(it + 1) * 128].rearrange("(k p) t -> p k t", p=128),
                                                        in_=h2t[:]), reads=[t_h2t])
            for kt in range(KT):
                S.op("pe", lambda e, kt=kt: e.matmul(pl[:, 0:20], lhsT=h2t[:, kt, :], rhs=wrb[:, kt, :], start=(kt == 0),
                                                     stop=(kt == KT - 1)), reads=[t_h2t, t_r], writes=[t_pl])
            lg, gmx, ge, gs, gm = R[:, 0:20], R[:, 20:21], R[:, 21:25], R[:, 25:26], R[:, 26:30]
            tmp16, sel, m1, ee, t4 = R[:, 30:46], R[:, 46:50], R[:, 50:51], R[:, 51:55], R[:, 55:59]
            m2, mk2, ws, cmb = R[:, 59:60], R[:, 60:64], R[:, 64:65], R[:, 65:81]
            ngm, nm1 = R[:, 81:82], R[:, 82:83]
            def rop(eng, fn):
                S.op(eng, fn, reads=[t_R, t_pl, t_r], writes=[t_R])
            rop("dve", lambda e: e.tensor_tensor(out=lg, in0=pl[:, 0:20], in1=brb[:], op=ALU.add))
            rop("dve", lambda e: e.tensor_reduce(out=gmx, in_=R[:, 0:4], axis=AX.X, op=ALU.max))
            rop("dve", lambda e: e.tensor_scalar(out=ngm, in0=gmx, scalar1=-1.0, scalar2=None, op0=ALU.mult))
            rop("act", lambda e: e.activation(out=ge, in_=R[:, 0:4], func=AF.Exp, bias=ngm, accum_out=gs))
            rop("dve", lambda e: e.tensor_scalar(out=gm, in0=R[:, 0:4], scalar1=gmx, scalar2=None, op0=ALU.is_ge))
            rop("dve", lambda e: e.tensor_tensor(out=tmp16.rearrange("p (g j) -> p g j", g=4),
                                                 in0=R[:, 4:20].rearrange("p (g j) -> p g j", g=4),
                                                 in1=gm.unsqueeze(2).broadcast_to([128, 4, 4]), op=ALU.mult))
            rop("dve", lambda e: e.tensor_reduce(out=sel, in_=tmp16.rearrange("p (g j) -> p j g", g=4), axis=AX.X, op=ALU.add))
            rop("dve", lambda e: e.tensor_reduce(out=m1, in_=sel, axis=AX.X, op=ALU.max))
            rop("dve", lambda e: e.tensor_scalar(out=nm1, in0=m1, scalar1=-1.0, scalar2=None, op0=ALU.mult))
            rop("act", lambda e: e.activation(out=ee, in_=sel, func=AF.Exp, bias=nm1))
            rop("dve", lambda e: e.tensor_scalar(out=t4, in0=sel, scalar1=m1, scalar2=-1e30, op0=ALU.is_ge, op1=ALU.mult))
            rop("dve", lambda e: e.tensor_tensor(out=t4, in0=t4, in1=sel, op=ALU.add))
            rop("dve", lambda e: e.tensor_reduce(out=m2, in_=t4, axis=AX.X, op=ALU.max))
            rop("dve", lambda e: e.tensor_scalar(out=mk2, in0=sel, scalar1=m2, scalar2=None, op0=ALU.is_ge))
            rop("dve", lambda e: e.tensor_tensor(out=ee, in0=ee, in1=mk2, op=ALU.mult))
            rop("dve", lambda e: e.tensor_reduce(out=ws, in_=ee, axis=AX.X, op=ALU.add))
            rop("dve", lambda e: e.tensor_tensor(out=ws, in0=ws, in1=gs, op=ALU.mult))
            rop("dve", lambda e: e.reciprocal(out=ws, in_=ws))
            rop("dve", lambda e: e.tensor_scalar(out=ee, in0=ee, scalar1=ws, scalar2=None, op0=ALU.mult))
            rop("dve", lambda e: e.tensor_tensor(out=cmb.rearrange("p (g j) -> p g j", g=4),
                                                 in0=gm.unsqueeze(2).broadcast_to([128, 4, 4]),
                                                 in1=ee.unsqueeze(1).broadcast_to([128, 4, 4]), op=ALU.mult))
            S.dma("sp", lambda e, it=it: e.dma_start(out=CMB[it * 128:(it + 1) * 128, :], in_=cmb), reads=[t_R])

    def phase_moe(st):
        NB = NO // 512
        wg = [sb(st, "wg%d" % i, [128, KT, 512], BF16) for i in range(2)]
        wu = [sb(st, "wu%d" % i, [128, KT, 512], BF16) for i in range(2)]
        wd = [sb(st, "wd%d" % i, [128, 4, D], BF16) for i in range(2)]
        t_w = [[Tok(), Tok()] for _ in range(3)]
        h2 = sb(st, "h2m", [128, KT, 512], BF16)
        t_h2 = Tok()
        acc = sb(st, "accm", [128, 4, D], F32)
        t_acc = Tok()
        cmb = sb(st, "cmbm", [128, 4, 16], F32)
        t_cmb = Tok()
        xres = sb(st, "xres", [128, D], F32)
        t_xres = Tok()
        he = [sb(st, "he%d" % i, [128, 512], BF16) for i in range(4)]
        t_he = [Tok() for _ in range(4)]
        sg = [sb(st, "sgm%d" % i, [128, 512], F32) for i in range(2)]
        t_sg = [Tok(), Tok()]
        pg = [ps(st, "pg%d" % i, [128, 512], F32) for i in range(2)]
        pu = [ps(st, "pu%d" % i, [128, 512], F32) for i in range(2)]
        py = [ps(st, "py%d" % i, [128, 512], F32) for i in range(4)]
        t_pg, t_pu, t_py = [Tok(), Tok()], [Tok(), Tok()], [Tok() for _ in range(4)]
        seq = [(blk, ex) for blk in range(NB) for ex in range(NEXP)]

        def load(i):
            b = i % 2
            ex = seq[i][1]
            S.dma("pool", lambda e: e.dma_start(out=wg[b][:], in_=weg[ex].rearrange("(k p) c -> p k c", p=128)),
                  writes=[t_w[0][b]])
            S.dma("pool", lambda e: e.dma_start(out=wu[b][:], in_=weu[ex].rearrange("(k p) c -> p k c", p=128)),
                  writes=[t_w[1][b]])
            S.dma("pool", lambda e: e.dma_start(out=wd[b][:], in_=wed[ex].rearrange("(k p) c -> p k c", p=128)),
                  writes=[t_w[2][b]])
        load(0)
        for i, (blk, ex) in enumerate(seq):
            b = i % 2
            if ex == 0:
                S.dma("sp", lambda e, blk=blk: e.dma_start(out=h2[:], in_=H2T[:, blk * 512:(blk + 1) * 512].rearrange(
                    "(k p) t -> p k t", p=128)), writes=[t_h2])
                S.dma("sp", lambda e, blk=blk: e.dma_start(out=cmb[:], in_=CMB[blk * 512:(blk + 1) * 512, :].rearrange(
                    "(n p) c -> p n c", p=128)), writes=[t_cmb])
            if i + 1 < len(seq):
                load(i + 1)
            for ft in range(4):
                fb = ft % 2
                for kt in range(KT):
                    S.op("pe", lambda e, kt=kt, ft=ft, fb=fb, b=b: e.matmul(pg[fb][:], lhsT=wg[b][:, kt, ft * 128:(ft + 1) * 128],
                                                                           rhs=h2[:, kt, :], start=(kt == 0), stop=(kt == KT - 1)),
                         reads=[t_w[0][b], t_h2], writes=[t_pg[fb]])
                for kt in range(KT):
                    S.op("pe", lambda e, kt=kt, ft=ft, fb=fb, b=b: e.matmul(pu[fb][:], lhsT=wu[b][:, kt, ft * 128:(ft + 1) * 128],
                                                                           rhs=h2[:, kt, :], start=(kt == 0), stop=(kt == KT - 1)),
                         reads=[t_w[1][b], t_h2], writes=[t_pu[fb]])
                S.op("act", lambda e, fb=fb: e.activation(out=sg[fb][:], in_=pg[fb][:], func=AF.Silu), reads=[t_pg[fb]], writes=[t_sg[fb]])
                S.op("dve", lambda e, fb=fb, ft=ft: e.tensor_tensor(out=he[ft][:], in0=sg[fb][:], in1=pu[fb][:], op=ALU.mult),
                     reads=[t_sg[fb], t_pu[fb]], writes=[t_he[ft]])
            for tt in range(4):
                for cb in range(4):
                    for ft in range(4):
                        S.op("pe", lambda e, tt=tt, cb=cb, ft=ft, b=b: e.matmul(py[cb][:], lhsT=he[ft][:, tt * 128:(tt + 1) * 128],
                                                                               rhs=wd[b][:, ft, cb * 512:(cb + 1) * 512],
                                                                               start=(ft == 0), stop=(ft == 3)),
                             reads=[t_he[ft], t_w[2][b]], writes=[t_py[cb]])
                    dst = acc[:, tt, cb * 512:(cb + 1) * 512]
                    if ex == 0:
                        S.op("dve", lambda e, dst=dst, cb=cb, tt=tt, ex=ex: e.tensor_scalar(
                            out=dst, in0=py[cb][:], scalar1=cmb[:, tt, ex:ex + 1], scalar2=None, op0=ALU.mult),
                             reads=[t_py[cb], t_cmb], writes=[t_acc])
                    else:
                        S.op("dve", lambda e, dst=dst, cb=cb, tt=tt, ex=ex: e.scalar_tensor_tensor(
                            out=dst, in0=py[cb][:], scalar=cmb[:, tt, ex:ex + 1], in1=dst, op0=ALU.mult, op1=ALU.add),
                             reads=[t_py[cb], t_cmb, t_acc], writes=[t_acc])
            if ex == NEXP - 1:
                for tt in range(4):
                    r0 = blk * 512 + tt * 128
                    S.dma("sp", lambda e, r0=r0: e.dma_start(out=xres[:], in_=X1[r0:r0 + 128, :]), writes=[t_xres])
                    S.op("dve", lambda e, tt=tt: e.tensor_tensor(out=acc[:, tt, :], in0=acc[:, tt, :], in1=xres[:], op=ALU.add),
                         reads=[t_xres, t_acc], writes=[t_acc])
                    S.dma("sp", lambda e, r0=r0, tt=tt: e.dma_start(out=out[r0:r0 + 128, :], in_=acc[:, tt, :]), reads=[t_acc])

    for (tok0, ntok, own) in ((0, NP, False), (NP, NO, True)):
        with ExitStack() as st:
            hT = sb(st, "hT", [128, KT, ntok], BF16)
            t_hT = Tok("hT")
            with ExitStack() as st2:
                phase_norm(st2, hT, t_hT, tok0, ntok)
                S.barrier()
                S.emit()
            with ExitStack() as st2:
                phase_dsa_proj(st2, hT, t_hT, tok0, ntok, own)
                S.barrier()
                S.emit()
            with ExitStack() as st2:
                phase_rwkv_prep(st2, hT, t_hT, tok0, ntok, own)
                S.barrier()
                S.emit()

    with ExitStack() as st:
        phase_rwkv_scan(st)
        S.barrier()
        S.emit()
    with ExitStack() as st:
        maskT = sb(st, "maskT", [128, MT_TILES * 128], BF16)
        t_maskT = Tok()
        with ExitStack() as st2:
            phase_index(st2, maskT, t_maskT)
            S.barrier()
            S.emit()
        with ExitStack() as st2:
            phase_attn(st2, maskT, t_maskT)
            S.barrier()
            S.emit()

    with ExitStack() as st:
        phase_outproj(st)
        S.barrier()
        S.emit()
    with ExitStack() as st:
        phase_moe(st)
        S.barrier()
        S.emit()
    es.close()
    return nc


def rope_tables(pos):
    pos = pos.astype(np.float32)
    tabs = np.zeros((pos.shape[0], 192), np.float32)
    inv128 = (10000.0 ** (-np.arange(64, dtype=np.float32) * (2.0 / 128))).astype(np.float32)
    inv64 = (10000.0 ** (-np.arange(32, dtype=np.float32) * (2.0 / 64))).astype(np.float32)
    a = pos[:, None] * inv128[None, :]
    tabs[:, 0:64] = np.cos(a)
    tabs[:, 64:128] = np.sin(a)
    a = pos[:, None] * inv64[None, :]
    tabs[:, 128:160] = np.cos(a)
    tabs[:, 160:192] = np.sin(a)
    return tabs


def pcols_np(inp):
    pc = np.zeros((128, 84), np.float32)
    sm = inp["rwkv_shift_mix"].reshape(-1)
    vecs = [sm[0:1024], sm[1088:2112], sm[2112:3136], inp["w0"].reshape(-1), inp["a0"].reshape(-1),
            inp["k_k"].reshape(-1), inp["k_a"].reshape(-1), inp["r_k"].reshape(-1), inp["ln_x_w"].reshape(-1),
            inp["ln_x_b"].reshape(-1)]
    for j, v in enumerate(vecs):
        pc[:, j * 8:(j + 1) * 8] = v.reshape(8, 128).T
    pc[0:64, 80] = sm[1024:1088]
    pc[0:64, 81] = sm[3136:3200]
    pc[:, 82] = sm[3200:3328]
    pc[0:32, 83] = sm[3328:3360]
    return pc


def cmats_np():
    m = np.zeros((4, 128, 128), np.float32)
    for blk in range(2):
        o = blk * 64
        m[0, o:o + 64, o:o + 64] = 1.0
        m[1, o:o + 64, o:o + 64] = np.tril(np.ones((64, 64), np.float32), -1)
        m[2, o:o + 64, o:o + 64] = np.triu(np.ones((64, 64), np.float32), 1)
        m[3, o:o + 64, o:o + 64] = np.triu(np.ones((64, 64), np.float32), 0)
    return m


def cmask_np():
    m = np.zeros((128, 128), np.float32)
    m[0:64, 64:128] = -1e30
    return m


def kernel(**inputs):
    NP_, NO_ = 2048, 2048
    inp = {k: np.asarray(v) for k, v in inputs.items()}
    xfull = inp["x"]
    B = xfull.shape[0]
    nc = build(NP_, NO_, 256)
    p0 = {k: v[0] for k, v in inp.items() if k != "x"}
    shared = dict(g_mix=inp["g_mix"], w_in=p0["w_in"], ident_in=np.eye(128, dtype=np.float32), q_gain=inp["q_gain"],
                  k_gain=inp["k_gain"], cmask=cmask_np(), pcols=pcols_np(p0), cmats=cmats_np(),
                  lnrow=np.stack([p0["ln_x_w"], p0["ln_x_b"]]), w_decay_up=p0["w_decay_up"], w_aicl_up=p0["w_aicl_up"],
                  w_gate_lora_up=p0["w_gate_lora_up"], w_out=p0["w_out"], g_ffn=inp["g_ffn"],
                  wr=np.ascontiguousarray(np.concatenate([p0["w_route_group"], p0["w_route_expert"]], axis=1)),
                  br=np.ascontiguousarray(np.concatenate([inp["b_route_group"], inp["b_route_expert"]], axis=1)),
                  w_e_gate=p0["w_e_gate"], w_e_up=p0["w_e_up"], w_e_down=p0["w_e_down"])
    in_maps = []
    for c in range(2 * B):
        b, s_ = c // 2, c % 2
        if s_ == 0:
            xs = np.concatenate([np.zeros((NP_, D), np.float32), xfull[b, :NO_]], 0)
            pos = np.concatenate([np.zeros(NP_), np.arange(NO_)])
            pb = np.full((128, 1), -1e30, np.float32)
        else:
            xs = np.ascontiguousarray(xfull[b])
            pos = np.arange(NP_ + NO_)
            pb = np.zeros((128, 1), np.float32)
        m = dict(shared)
        m.update(x=xs, rope_tab=rope_tables(pos), pbias=pb)
        in_maps.append(m)
    res = run_bass_kernel_spmd(nc, in_maps, core_ids=list(range(2 * B)))
    outp = np.zeros_like(xfull)
    for c in range(2 * B):
        b, s_ = c // 2, c % 2
        outp[b, s_ * NO_:(s_ + 1) * NO_] = res.results[c]["out"]
    return outp
```

```python
import os
import numpy as np
import ml_dtypes
from contextlib import ExitStack

import concourse.bass as bass
import concourse.mybir as mybir
from concourse.bass_utils import run_bass_kernel_spmd

F32 = mybir.dt.float32
BF16 = mybir.dt.bfloat16
ALU = mybir.AluOpType
AF = mybir.ActivationFunctionType
AX = mybir.AxisListType

D = 2048
KT = 16
DSA_W = 1024
NH = 8
HD = 128
IH = 16
IDD = 64
RW = 1024
RH = 16
RN = 64
NEXP = 16
DEXP = 512
DSA_COLS = 4176
RWKV_COLS = 3360
IN_COLS = 7536
NORM_EPS = 1e-6
GN_EPS = 64e-5
CHUNK = 64

ENGS = ("pe", "act", "dve", "pool", "sp")
DMAQ = ("sp", "act", "pool")
NSLOT = 24
FUSE_WAIT = True


class Tok:
    __slots__ = ("name", "w", "r")

    def __init__(self, name=""):
        self.name = name
        self.w = None
        self.r = []


class Sched:
    def __init__(self, nc, es):
        self.nc = nc
        self.esem = {e: es.enter_context(nc.semaphore("es_" + e)) for e in ENGS}
        self.ebase = {e: 0 for e in ENGS}
        self.dsem = {q: [es.enter_context(nc.semaphore("ds_%s_%d" % (q, i))) for i in range(NSLOT)]
                     for q in DMAQ}
        self.dcnt = {q: [0] * NSLOT for q in DMAQ}
        self.dn = {q: 0 for q in DMAQ}
        self.waited = {e: {} for e in ENGS}
        self.ops = {e: [] for e in ENGS}
        self.phase = 0
        self.stop = None

    def _deps(self, eng, reads, writes, is_dma):
        raw, other = set(), set()
        for t in reads:
            if t.w is not None:
                raw.add(t.w)
        for t in writes:
            if t.w is not None:
                other.add(t.w)
            for r in t.r:
                other.add(r)
        deps = set()
        for ev in raw:
            if ev[0] == "e" and ev[3] != self.phase:
                continue
            if ev[0] == "e" and ev[1] == eng and eng == "pe" and not is_dma:
                continue
            deps.add(ev)
        for ev in other:
            if ev[0] == "e" and ev[3] != self.phase:
                continue
            if ev[0] == "e" and ev[1] == eng and eng == "pe" and not is_dma:
                continue
            deps.add(ev)
        return deps

    def op(self, eng, fn, reads=(), writes=()):
        idx = len(self.ops[eng])
        deps = self._deps(eng, reads, writes, False)
        ev = ("e", eng, idx, self.phase)
        for t in reads:
            t.r.append(ev)
        for t in writes:
            t.w = ev
            t.r = []
        self.ops[eng].append(dict(fn=fn, deps=deps, ms=False, dma=None))

    def dma(self, q, fn, reads=(), writes=()):
        deps = self._deps(q, reads, writes, True)
        n = self.dn[q]
        self.dn[q] += 1
        slot = n % NSLOT
        if self.dcnt[q][slot] > 0:
            deps.add(("d", q, slot, self.dcnt[q][slot]))
        self.dcnt[q][slot] += 16
        ev = ("d", q, slot, self.dcnt[q][slot])
        for t in reads:
            t.r.append(ev)
        for t in writes:
            t.w = ev
            t.r = []
        self.ops[q].append(dict(fn=fn, deps=deps, ms=False, dma=(q, slot)))

    def barrier(self):
        evs = []
        for e in ENGS:
            for i in range(len(self.ops[e]) - 1, -1, -1):
                if self.ops[e][i]["dma"] is None and self.ops[e][i]["fn"] is not None:
                    evs.append(("e", e, i, self.phase))
                    break
        for q in DMAQ:
            for s in range(NSLOT):
                if self.dcnt[q][s] > 0:
                    evs.append(("d", q, s, self.dcnt[q][s]))
        for e in ENGS:
            deps = set(ev for ev in evs if not (ev[0] == "e" and ev[1] == e))
            self.ops[e].append(dict(fn=None, deps=deps, ms=False, dma=None))

    def emit(self):
        nc = self.nc
        if self.stop is not None and self.phase >= self.stop:
            self.ops = {e: [] for e in ENGS}
            self.phase += 1
            return
        for e in ENGS:
            for o in self.ops[e]:
                for ev in o["deps"]:
                    if ev[0] == "e":
                        self.ops[ev[1]][ev[2]]["ms"] = True
        msv = {}
        for e in ENGS:
            c = self.ebase[e]
            arr = []
            for o in self.ops[e]:
                if o["ms"]:
                    c += 1
                arr.append(c)
            msv[e] = arr
            self.ebase[e] = c
        handles = {"pe": "tensor", "act": "scalar", "dve": "vector", "pool": "gpsimd", "sp": "sync"}

        def replay(e, eng):
            wd = self.waited[e]
            for i, o in enumerate(self.ops[e]):
                need = {}
                for ev in o["deps"]:
                    if ev[0] == "e":
                        sem, val, key = self.esem[ev[1]], msv[ev[1]][ev[2]], ("e", ev[1])
                    else:
                        sem, val, key = self.dsem[ev[1]][ev[2]], ev[3], ("d", ev[1], ev[2])
                    if wd.get(key, 0) >= val:
                        continue
                    if key not in need or need[key][1] < val:
                        need[key] = (sem, val)
                waits = [need[k] for k in sorted(need, key=str)]
                for k in need:
                    wd[k] = need[k][1]
                fused = None
                if FUSE_WAIT and o["fn"] is not None and waits:
                    fused = waits.pop()
                for sem, val in waits:
                    eng.wait_ge(sem, val)
                if o["fn"] is None:
                    continue
                ins = o["fn"](eng)
                if fused is not None:
                    ins._wait_ge(fused[0], fused[1])
                if o["dma"] is not None:
                    ins.then_inc(self.dsem[o["dma"][0]][o["dma"][1]], 16)
                elif o["ms"]:
                    ins.then_inc(self.esem[e], 1)

        with nc.Block() as blk:
            @blk.tensor
            def _(eng):
                replay("pe", eng)

            @blk.scalar
            def _(eng):
                replay("act", eng)

            @blk.vector
            def _(eng):
                replay("dve", eng)

            @blk.gpsimd
            def _(eng):
                replay("pool", eng)

            @blk.sync
            def _(eng):
                replay("sp", eng)
        self.ops = {e: [] for e in ENGS}
        self.phase += 1


class Ctx:
    pass


def build(NP, NO, TOPK, dbg=False, stop_after=None):
    NT = NP + NO
    nc = bass.Bass("TRN2", target_bir_lowering=False)
    g = Ctx()
    kind_s = "ExternalOutput" if dbg else "Internal"

    def din(name, shape, dt=F32):
        return nc.dram_tensor(name, list(shape), dt, kind="ExternalInput").ap()

    def dscr(name, shape, dt=BF16):
        return nc.dram_tensor(name, list(shape), dt, kind=kind_s).ap()

    x = din("x", [NT, D])
    g_mix = din("g_mix", [1, D])
    w_in = din("w_in", [D, IN_COLS])
    rope_tab = din("rope_tab", [NT, 192])
    ident_in = din("ident_in", [128, 128])
    q_gain = din("q_gain", [1, HD])
    k_gain = din("k_gain", [1, HD])
    pcols_in = din("pcols", [128, 84])
    wdu_in = din("w_decay_up", [64, RW])
    wau_in = din("w_aicl_up", [64, RW])
    wgu_in = din("w_gate_lora_up", [160, RW])
    cmats_in = din("cmats", [4, 128, 128])
    lnrow_in = din("lnrow", [2, RW])
    NG = NT // 128
    CMs = dscr("s_CM", [NG, 64, 16, 4, 128])
    TMs = dscr("s_TM", [NG, 128, 4, RW])
    WCs = dscr("s_WC", [64, 16, NT // 64], F32)
    GTs = dscr("s_GT", [RW, NO])
    BNs = dscr("s_BN", [RW, NO])
    w_out = din("w_out", [D, D])
    g_ffn = din("g_ffn", [1, D])
    wr_in = din("wr", [D, 20])
    br_in = din("br", [1, 20])
    weg = din("w_e_gate", [NEXP, D, DEXP])
    weu = din("w_e_up", [NEXP, D, DEXP])
    wed = din("w_e_down", [NEXP, DEXP, D])
    X1 = dscr("s_X1", [NO, D], F32)
    H2T = dscr("s_H2T", [D, NO])
    CMB = dscr("s_CMB", [NO, 16], F32)
    cmask_in = din("cmask", [128, 128])
    pbias_in = din("pbias", [128, 1])
    out = nc.dram_tensor("out", [NO, D], F32, kind="ExternalOutput").ap()
    YT = dscr("s_YT", [D, NO])
    if dbg:
        DBGM = dscr("dbg_mask", [NO // 128, 128, NT])


    QT = dscr("s_QT", [NH * HD, NO])
    KTs = dscr("s_KT", [NH * HD, NT])
    VS = dscr("s_V", [NT, DSA_W])
    QI = dscr("s_QI", [IH * IDD, NO])
    KI = dscr("s_KI", [128, NT])
    WI = dscr("s_WI", [NO, IH], F32)

    es = ExitStack()
    S = Sched(nc, es)
    S.stop = stop_after

    uniq = [0]

    def sb(st, name, shape, dt):
        uniq[0] += 1
        return st.enter_context(nc.sbuf_tensor("%s_%d" % (name, uniq[0]), list(shape), dt))

    def ps(st, name, shape, dt=F32):
        uniq[0] += 1
        return st.enter_context(nc.psum_tensor("%s_%d" % (name, uniq[0]), list(shape), dt))

    ident_f = sb(es, "ident_f", [128, 128], F32)
    ident = sb(es, "ident", [128, 128], BF16)
    t_ident = Tok("ident")
    S.dma("sp", lambda e: e.dma_start(out=ident_f[:], in_=ident_in), writes=[t_ident])
    S.op("dve", lambda e: e.tensor_copy(out=ident[:], in_=ident_f[:]), reads=[t_ident], writes=[t_ident])

    def phase_norm(st, hT, t_hT, tok0, ntok):
        gm = sb(st, "gm", [128, D], F32)
        t_gm = Tok()
        S.dma("sp", lambda e: e.dma_start(out=gm[:], in_=g_mix.broadcast_to([128, D])), writes=[t_gm])
        xt = [sb(st, "xt%d" % i, [128, D], F32) for i in range(2)]
        xn = [sb(st, "xn%d" % i, [128, D], BF16) for i in range(2)]
        junk = sb(st, "junk", [128, D], BF16)
        ss = [sb(st, "ss%d" % i, [128, 2], F32) for i in range(2)]
        pst = [ps(st, "pst%d" % i, [128, D], BF16) for i in range(2)]
        t_xt = [Tok() for _ in range(2)]
        t_xn = [Tok() for _ in range(2)]
        t_ss = [Tok() for _ in range(2)]
        t_ps = [Tok() for _ in range(2)]
        t_junk = Tok()
        for it in range(ntok // 128):
            b = it % 2
            r0 = tok0 + it * 128
            S.dma("sp", lambda e, b=b, r0=r0: e.dma_start(out=xt[b][:], in_=x[r0:r0 + 128, :]), writes=[t_xt[b]])
            S.op("act", lambda e, b=b: e.activation(out=junk[:], in_=xt[b][:], func=AF.Square,
                                                   accum_out=ss[b][:, 0:1]),
                 reads=[t_xt[b]], writes=[t_junk, t_ss[b]])
            S.op("act", lambda e, b=b: e.activation(out=ss[b][:, 1:2], in_=ss[b][:, 0:1], func=AF.Sqrt,
                                                   scale=1.0 / D, bias=NORM_EPS),
                 reads=[t_ss[b]], writes=[t_ss[b]])
            S.op("dve", lambda e, b=b: e.reciprocal(out=ss[b][:, 1:2], in_=ss[b][:, 1:2]),
                 reads=[t_ss[b]], writes=[t_ss[b]])
            S.op("dve", lambda e, b=b: e.scalar_tensor_tensor(out=xn[b][:], in0=xt[b][:], scalar=ss[b][:, 1:2],
                                                             in1=gm[:], op0=ALU.mult, op1=ALU.mult),
                 reads=[t_xt[b], t_ss[b], t_gm], writes=[t_xn[b]])
            for kt in range(KT):
                S.op("pe", lambda e, b=b, kt=kt: e.transpose(out=pst[b][:, kt * 128:(kt + 1) * 128],
                                                            in_=xn[b][:, kt * 128:(kt + 1) * 128],
                                                            identity=ident[:]),
                     reads=[t_xn[b], t_ident], writes=[t_ps[b]])
            c0 = it * 128
            S.op("act", lambda e, b=b, c0=c0: e.copy(out=hT[:, :, c0:c0 + 128],
                                                    in_=pst[b][:].rearrange("p (k t) -> p k t", k=KT)),
                 reads=[t_ps[b]], writes=[t_hT])

    def load_w(st_bufs, cols0, ncols):
        wst, t_wst, wbfs, t_wbfs, cnt = st_bufs
        i = cnt[0]
        cnt[0] += 1
        wb, tw = wbfs[i % 2], t_wbfs[i % 2]
        src = w_in[:, cols0:cols0 + ncols].rearrange("(k p) c -> p k c", p=128)
        S.dma("sp", lambda e: e.dma_start(out=wst[:, :, 0:ncols], in_=src), writes=[t_wst])
        if i % 2 == 0:
            S.op("act", lambda e: e.copy(out=wb[:, :, 0:ncols], in_=wst[:, :, 0:ncols]), reads=[t_wst], writes=[tw])
        else:
            S.op("dve", lambda e: e.tensor_copy(out=wb[:, :, 0:ncols], in_=wst[:, :, 0:ncols]), reads=[t_wst], writes=[tw])
        return wb, tw

    def phase_dsa_proj(st, hT, t_hT, tok0, ntok, own):
        wst = sb(st, "wst", [128, KT, 512], F32)
        wbfs = [sb(st, "wbf%d" % i, [128, KT, 512], BF16) for i in range(2)]
        wb = (wst, Tok(), wbfs, [Tok(), Tok()], [0])
        gq = sb(st, "gq", [128, HD], F32)
        gk = sb(st, "gk", [128, HD], F32)
        t_g = Tok()
        S.dma("sp", lambda e: e.dma_start(out=gq[:], in_=q_gain.broadcast_to([128, HD])), writes=[t_g])
        S.dma("sp", lambda e: e.dma_start(out=gk[:], in_=k_gain.broadcast_to([128, HD])), writes=[t_g])
        ntt = ntok // 128
        tabs = sb(st, "tabs", [128, ntt, 192], F32)
        t_tabs = Tok()
        S.dma("sp", lambda e: e.dma_start(out=tabs[:], in_=rope_tab[tok0:tok0 + ntok, :].rearrange(
            "(n p) c -> p n c", p=128)), writes=[t_tabs])
        pp = [ps(st, "pp%d" % i, [128, 512], F32) for i in range(2)]
        t_pp = [Tok(), Tok()]
        ptr_full = [ps(st, "ptr%d" % i, [128, 1024], BF16) for i in range(2)]
        ptr = [t_[:, 0:512] for t_ in ptr_full]
        t_ptr = [Tok(), Tok()]
        sq = sb(st, "sq", [128, 512], F32)
        xn = sb(st, "xnq", [128, 512], F32)
        ro = sb(st, "ro", [128, 512], BF16)
        tmp1 = sb(st, "tmp1", [128, 256], F32)
        tmp2 = sb(st, "tmp2", [128, 256], F32)
        ssq = sb(st, "ssq", [128, 8], F32)
        t_sq, t_xn, t_ro, t_t1, t_t2, t_ssq = Tok(), Tok(), Tok(), Tok(), Tok(), Tok()
        stg = [sb(st, "stg%d" % i, [128, 4, 512], BF16) for i in range(2)]
        t_stg = [Tok(), Tok()]
        vst = [sb(st, "vst%d" % i, [128, 512], BF16) for i in range(2)]
        t_vst = [Tok(), Tok()]
        wis = sb(st, "wis", [128, IH], F32)
        t_wis = Tok()
        cnt = [0]
        nstg = [0]

        def mm_tok(wtile, tw, ncols, it):
            i = cnt[0]
            cnt[0] += 1
            p, tp = pp[i % 2], t_pp[i % 2]
            for kt in range(KT):
                S.op("pe", lambda e, kt=kt, p=p: e.matmul(p[:, 0:ncols], lhsT=hT[:, kt, it * 128:(it + 1) * 128],
                                                       rhs=wtile[:, kt, 0:ncols], start=(kt == 0),
                                                       stop=(kt == KT - 1)),
                     reads=[t_hT, tw], writes=[tp])
            return p, tp

        def rope_ops(src, t_src, nh, hd, cos, sin, dst, t_dst):
            hf = hd // 2
            s3 = src.rearrange("p (h d) -> p h d", h=nh)
            d3 = dst.rearrange("p (h d) -> p h d", h=nh)
            cb = cos.unsqueeze(1).broadcast_to([128, nh, hf])
            sn = sin.unsqueeze(1).broadcast_to([128, nh, hf])
            a = tmp1[:, 0:nh * hf].rearrange("p (h d) -> p h d", h=nh)
            b = tmp2[:, 0:nh * hf].rearrange("p (h d) -> p h d", h=nh)
            x1, x2 = s3[:, :, 0:hf], s3[:, :, hf:hd]
            S.op("dve", lambda e: e.tensor_tensor(out=a, in0=x1, in1=cb, op=ALU.mult),
                 reads=[t_src, t_tabs], writes=[t_t1])
            S.op("dve", lambda e: e.tensor_tensor(out=b, in0=x2, in1=sn, op=ALU.mult),
                 reads=[t_src, t_tabs], writes=[t_t2])
            S.op("dve", lambda e: e.tensor_tensor(out=d3[:, :, 0:hf], in0=a, in1=b, op=ALU.subtract),
                 reads=[t_t1, t_t2], writes=[t_dst])
            S.op("dve", lambda e: e.tensor_tensor(out=a, in0=x2, in1=cb, op=ALU.mult),
                 reads=[t_src, t_tabs, t_dst], writes=[t_t1])
            S.op("dve", lambda e: e.tensor_tensor(out=b, in0=x1, in1=sn, op=ALU.mult),
                 reads=[t_src, t_tabs, t_dst], writes=[t_t2])
            S.op("dve", lambda e: e.tensor_tensor(out=d3[:, :, hf:hd], in0=a, in1=b, op=ALU.add),
                 reads=[t_t1, t_t2], writes=[t_dst])

        def qk_group(col0, gain, dstT, is_q):
            for half in range(2):
                wtile, tw = load_w(wb, col0 + half * 512, 512)
                for it in range(ntt):
                    p, tp = mm_tok(wtile, tw, 512, it)
                    S.op("act", lambda e, p=p: e.activation(out=sq[:], in_=p[:], func=AF.Square),
                         reads=[tp], writes=[t_sq])
                    S.op("dve", lambda e: e.tensor_reduce(out=ssq[:, 0:4], in_=sq[:].rearrange(
                        "p (h d) -> p h d", h=4), axis=AX.X, op=ALU.add), reads=[t_sq], writes=[t_ssq])
                    S.op("act", lambda e: e.activation(out=ssq[:, 4:8], in_=ssq[:, 0:4], func=AF.Sqrt,
                                                       scale=1.0 / HD, bias=NORM_EPS),
                         reads=[t_ssq], writes=[t_ssq])
                    S.op("dve", lambda e: e.reciprocal(out=ssq[:, 4:8], in_=ssq[:, 4:8]),
                         reads=[t_ssq], writes=[t_ssq])
                    S.op("dve", lambda e, p=p: e.tensor_tensor(
                        out=xn[:].rearrange("p (h d) -> p h d", h=4), in0=p[:].rearrange("p (h d) -> p h d", h=4),
                        in1=ssq[:, 4:8].unsqueeze(2).broadcast_to([128, 4, HD]), op=ALU.mult),
                         reads=[tp, t_ssq], writes=[t_xn])
                    S.op("dve", lambda e: e.tensor_tensor(
                        out=xn[:].rearrange("p (h d) -> p h d", h=4), in0=xn[:].rearrange("p (h d) -> p h d", h=4),
                        in1=gain[:].unsqueeze(1).broadcast_to([128, 4, HD]), op=ALU.mult),
                         reads=[t_xn, t_g], writes=[t_xn])
                    rope_ops(xn[:], t_xn, 4, HD, tabs[:, it, 0:64], tabs[:, it, 64:128], ro[:], t_ro)
                    j = nstg[0] // 4
                    sl = nstg[0] % 4
                    nstg[0] += 1
                    pt, tpt = ptr[it % 2], t_ptr[it % 2]
                    for h in range(4):
                        S.op("pe", lambda e, h=h, pt=pt: e.transpose(out=pt[:, h * 128:(h + 1) * 128],
                                                                  in_=ro[:, h * 128:(h + 1) * 128],
                                                                  identity=ident[:]),
                             reads=[t_ro, t_ident], writes=[tpt])
                    sg, tsg = stg[j % 2], t_stg[j % 2]
                    S.op("act", lambda e, pt=pt, sg=sg, sl=sl: e.copy(
                        out=sg[:, :, sl * 128:(sl + 1) * 128], in_=pt.rearrange("p (h t) -> p h t", h=4)),
                         reads=[tpt], writes=[tsg])
                    if sl == 3:
                        t0 = (it - 3) * 128 + (0 if is_q else tok0)
                        h0 = half * 4
                        dst = dstT.rearrange("(h d) t -> d h t", d=HD)[:, h0:h0 + 4, t0:t0 + 512]
                        S.dma("sp", lambda e, sg=sg, dst=dst: e.dma_start(out=dst, in_=sg[:]),
                              reads=[tsg], writes=[])

        qk_group(DSA_W, gk, KTs, False)
        if own:
            qk_group(0, gq, QT, True)
        for half in range(2):
            wtile, tw = load_w(wb, 2 * DSA_W + half * 512, 512)
            for it in range(ntt):
                p, tp = mm_tok(wtile, tw, 512, it)
                v, tv = vst[it % 2], t_vst[it % 2]
                S.op("act", lambda e, p=p, v=v: e.copy(out=v[:], in_=p[:]), reads=[tp], writes=[tv])
                r0 = tok0 + it * 128
                S.dma("sp", lambda e, v=v, r0=r0, half=half: e.dma_start(
                    out=VS[r0:r0 + 128, half * 512:(half + 1) * 512], in_=v[:]), reads=[tv], writes=[])
        wtile, tw = load_w(wb, 3 * DSA_W + IH * IDD, IDD + IH)
        for it in range(ntt):
            p, tp = mm_tok(wtile, tw, IDD + IH, it)
            S.op("act", lambda e, p=p: e.copy(out=xn[:, 0:IDD + IH], in_=p[:, 0:IDD + IH]), reads=[tp], writes=[t_xn])
            rope_ops(xn[:, 0:IDD], t_xn, 1, IDD, tabs[:, it, 128:160], tabs[:, it, 160:192], ro[:, 0:IDD], t_ro)
            S.op("act", lambda e: e.copy(out=ro[:, IDD:2 * IDD], in_=ro[:, 0:IDD]),
                 reads=[t_ro], writes=[t_ro])
            if own:
                S.op("act", lambda e: e.mul(out=wis[:], in_=xn[:, IDD:IDD + IH], mul=1.0 / 32.0),
                     reads=[t_xn], writes=[t_wis])
                S.dma("sp", lambda e, it=it: e.dma_start(out=WI[it * 128:(it + 1) * 128, :], in_=wis[:]),
                      reads=[t_wis], writes=[])
            pt, tpt = ptr[it % 2], t_ptr[it % 2]
            S.op("pe", lambda e, pt=pt: e.transpose(out=pt[:, 0:128], in_=ro[:, 0:128], identity=ident[:]),
                 reads=[t_ro, t_ident], writes=[tpt])
            j = nstg[0] // 4
            sl = nstg[0] % 4
            nstg[0] += 1
            sg, tsg = stg[j % 2], t_stg[j % 2]
            S.op("act", lambda e, pt=pt, sg=sg, sl=sl: e.copy(out=sg[:, 0, sl * 128:(sl + 1) * 128],
                                                            in_=pt[:, 0:128]), reads=[tpt], writes=[tsg])
            if sl == 3:
                t0 = tok0 + (it - 3) * 128
                S.dma("sp", lambda e, sg=sg, t0=t0: e.dma_start(out=KI[:, t0:t0 + 512], in_=sg[:, 0, :]),
                      reads=[tsg], writes=[])
        if own:
            for half in range(2):
                wtile, tw = load_w(wb, 3 * DSA_W + half * 512, 512)
                for it in range(ntt):
                    p, tp = mm_tok(wtile, tw, 512, it)
                    S.op("act", lambda e, p=p: e.copy(out=xn[:], in_=p[:]), reads=[tp], writes=[t_xn])
                    rope_ops(xn[:], t_xn, 8, IDD, tabs[:, it, 128:160], tabs[:, it, 160:192], ro[:], t_ro)
                    pt, tpt = ptr[it % 2], t_ptr[it % 2]
                    for h in range(4):
                        S.op("pe", lambda e, h=h, pt=pt: e.transpose(out=pt[:, h * 128:(h + 1) * 128],
                                                                  in_=ro[:, h * 128:(h + 1) * 128],
                                                                  identity=ident[:]),
                             reads=[t_ro, t_ident], writes=[tpt])
                    j = nstg[0] // 4
                    sl = nstg[0] % 4
                    nstg[0] += 1
                    sg, tsg = stg[j % 2], t_stg[j % 2]
                    S.op("act", lambda e, pt=pt, sg=sg, sl=sl: e.copy(
                        out=sg[:, :, sl * 128:(sl + 1) * 128], in_=pt.rearrange("p (h t) -> p h t", h=4)),
                         reads=[tpt], writes=[tsg])
                    if sl == 3:
                        t0 = (it - 3) * 128
                        dst = QI.rearrange("(h d) t -> d h t", d=128)[:, half * 4:half * 4 + 4, t0:t0 + 512]
                        S.dma("sp", lambda e, sg=sg, dst=dst: e.dma_start(out=dst, in_=sg[:]),
                              reads=[tsg], writes=[])


    NQT = NO // 128
    mt_base = []
    acc_ = 0
    for qt in range(NQT):
        mt_base.append(acc_)
        acc_ += (NP + (qt + 1) * 128) // 128
    MT_TILES = acc_
    NBIS = 20

    def phase_index(st, maskT, t_maskT):
        qi = sb(st, "qi", [128, 8, NO], BF16)
        ki = sb(st, "ki", [128, NT], BF16)
        wi = sb(st, "wi", [128, NQT, IH], F32)
        cm = sb(st, "cm", [128, 128], F32)
        pb = sb(st, "pb", [128, 1], F32)
        t_in = Tok()
        S.dma("sp", lambda e: e.dma_start(out=qi[:], in_=QI.rearrange("(h d) t -> d h t", d=128)), writes=[t_in])
        S.dma("sp", lambda e: e.dma_start(out=ki[:], in_=KI), writes=[t_in])
        S.dma("sp", lambda e: e.dma_start(out=wi[:], in_=WI.rearrange("(n p) h -> p n h", p=128)), writes=[t_in])
        S.dma("sp", lambda e: e.dma_start(out=cm[:], in_=cmask_in), writes=[t_in])
        S.dma("sp", lambda e: e.dma_start(out=pb[:], in_=pbias_in), writes=[t_in])
        iscs = [sb(st, "isc%d" % i, [128, NT], F32) for i in range(2)]
        junks = [sb(st, "junkm%d" % i, [128, NT], BF16) for i in range(2)]
        sms = [sb(st, "sm%d" % i, [128, 8], F32) for i in range(2)]
        t_iscs, t_junks, t_sms = [Tok(), Tok()], [Tok(), Tok()], [Tok(), Tok()]
        rr = [sb(st, "rr%d" % i, [128, 512], F32) for i in range(4)]
        t_rr = [Tok() for _ in range(4)]
        iscB = [sb(st, "iscB%d" % i, [128, 512], F32) for i in range(2)]
        t_iscB = [Tok(), Tok()]
        pp = [ps(st, "pi%d" % i, [128, 512], F32) for i in range(4)]
        t_pp = [Tok() for _ in range(4)]
        pT_full = [ps(st, "pT%d" % i, [128, 1024], BF16) for i in range(2)]
        pT = [t_[:, 0:512] for t_ in pT_full]
        t_pT = [Tok(), Tok()]
        cnt = [0]
        NDV = 16

        def head_block(qt, kb, h, isc, t_isc, nk):
            w = min(512, nk - kb * 512)
            i = cnt[0]
            cnt[0] += 1
            p, tp = pp[i % 4], t_pp[i % 4]
            r, tr = rr[i % 4], t_rr[i % 4]
            pr = (h % 2) * 64
            S.op("pe", lambda e: e.matmul(p[:, 0:w], lhsT=qi[pr:pr + 64, h // 2, qt * 128:(qt + 1) * 128],
                                          rhs=ki[pr:pr + 64, kb * 512:kb * 512 + w], start=True, stop=True),
                 reads=[t_in], writes=[tp])
            S.op("act", lambda e: e.activation(out=r[:, 0:w], in_=p[:, 0:w], func=AF.Relu), reads=[tp], writes=[tr])
            dst = isc[:, kb * 512:kb * 512 + w]
            ws = wi[:, qt, h:h + 1]
            if h == 0:
                S.op("dve", lambda e: e.tensor_scalar(out=dst, in0=r[:, 0:w], scalar1=ws, scalar2=None, op0=ALU.mult),
                     reads=[tr, t_in], writes=[t_isc])
            elif h < NDV:
                S.op("dve", lambda e: e.scalar_tensor_tensor(out=dst, in0=r[:, 0:w], scalar=ws, in1=dst, op0=ALU.mult, op1=ALU.add),
                     reads=[tr, t_in, t_isc], writes=[t_isc])
            else:
                ib, tib = iscB[kb % 2], t_iscB[kb % 2]
                if h == NDV:
                    S.op("pool", lambda e: e.tensor_scalar(out=ib[:, 0:w], in0=r[:, 0:w], scalar1=ws, scalar2=None, op0=ALU.mult),
                         reads=[tr, t_in], writes=[tib])
                else:
                    S.op("pool", lambda e: e.tensor_scalar(out=r[:, 0:w], in0=r[:, 0:w], scalar1=ws, scalar2=None, op0=ALU.mult),
                         reads=[tr, t_in], writes=[tr])
                    S.op("pool", lambda e: e.tensor_tensor(out=ib[:, 0:w], in0=ib[:, 0:w], in1=r[:, 0:w], op=ALU.add),
                         reads=[tr, tib], writes=[tib])
                if h == IH - 1:
                    S.op("dve", lambda e: e.tensor_tensor(out=dst, in0=dst, in1=ib[:, 0:w], op=ALU.add),
                         reads=[tib, t_isc], writes=[t_isc])

        for q0 in range(0, NQT, 2):
            qts = [q_ for q_ in (q0, q0 + 1) if q_ < NQT]
            nks = [NP + (q_ + 1) * 128 for q_ in qts]
            for x_, qt in enumerate(qts):
                nk = nks[x_]
                nblk = (nk + 511) // 512
                for k0 in range(0, nblk, 2):
                    kbs = [k_ for k_ in (k0, k0 + 1) if k_ < nblk]
                    for h in range(IH):
                        for kb in kbs:
                            head_block(qt, kb, h, iscs[x_], t_iscs[x_], nk)
            def both(fn):
                for x_ in range(len(qts)):
                    fn(x_, iscs[x_], t_iscs[x_], junks[x_], t_junks[x_], sms[x_], t_sms[x_], nks[x_])
            both(lambda x_, isc, ti, junk, tj, sm, ts, nk: S.op("dve", lambda e: e.tensor_reduce(
                out=sm[:, 5:6], in_=isc[:, 0:nk], axis=AX.X, op=ALU.max), reads=[ti], writes=[ts]))
            both(lambda x_, isc, ti, junk, tj, sm, ts, nk: S.op("dve", lambda e: e.tensor_reduce(
                out=sm[:, 0:1], in_=isc[:, 0:nk], axis=AX.X, op=ALU.min), reads=[ti, ts], writes=[ts]))
            both(lambda x_, isc, ti, junk, tj, sm, ts, nk: S.op("dve", lambda e: e.tensor_tensor(
                out=sm[:, 1:2], in0=sm[:, 5:6], in1=sm[:, 0:1], op=ALU.subtract), reads=[ts], writes=[ts]))
            both(lambda x_, isc, ti, junk, tj, sm, ts, nk: S.op("dve", lambda e: e.tensor_tensor(
                out=isc[:, nk - 128:nk], in0=isc[:, nk - 128:nk], in1=cm[:], op=ALU.add), reads=[ti, t_in], writes=[ti]))
            if NP > 0:
                both(lambda x_, isc, ti, junk, tj, sm, ts, nk: S.op("dve", lambda e: e.tensor_scalar(
                    out=isc[:, 0:NP], in0=isc[:, 0:NP], scalar1=pb[:, 0:1], scalar2=None, op0=ALU.add),
                    reads=[ti, t_in], writes=[ti]))
            both(lambda x_, isc, ti, junk, tj, sm, ts, nk: S.op("dve", lambda e: e.tensor_scalar(
                out=sm[:, 1:2], in0=sm[:, 1:2], scalar1=1e-20, scalar2=None, op0=ALU.max), reads=[ts], writes=[ts]))
            both(lambda x_, isc, ti, junk, tj, sm, ts, nk: S.op("dve", lambda e: e.reciprocal(
                out=sm[:, 4:5], in_=sm[:, 1:2]), reads=[ts], writes=[ts]))
            both(lambda x_, isc, ti, junk, tj, sm, ts, nk: S.op("dve", lambda e: e.tensor_scalar(
                out=isc[:, 0:nk], in0=isc[:, 0:nk], scalar1=sm[:, 0:1], scalar2=sm[:, 4:5], op0=ALU.subtract, op1=ALU.mult),
                reads=[ti, ts], writes=[ti]))
            both(lambda x_, isc, ti, junk, tj, sm, ts, nk: S.op("dve", lambda e: e.memset(sm[:, 2:3], 0.5),
                                                               reads=[ts], writes=[ts]))
            for it in range(NBIS):
                last = (it == NBIS - 1)
                f = 0.5 ** (it + 1)
                both(lambda x_, isc, ti, junk, tj, sm, ts, nk: S.op("dve", lambda e: e.tensor_scalar(
                    out=junk[:, 0:nk], in0=isc[:, 0:nk], scalar1=sm[:, 2:3], scalar2=None, op0=ALU.is_ge, op1=ALU.add,
                    accum_out=sm[:, 3:4]), reads=[ti, ts, tj], writes=[tj, ts]))
                both(lambda x_, isc, ti, junk, tj, sm, ts, nk, last=last: S.op("dve", lambda e: e.tensor_scalar(
                    out=sm[:, 5:6], in0=sm[:, 3:4], scalar1=float(TOPK) - 0.5, scalar2=(-1.0 if last else -0.5),
                    op0=ALU.is_ge, op1=ALU.add), reads=[ts], writes=[ts]))
                both(lambda x_, isc, ti, junk, tj, sm, ts, nk, last=last, f=f: S.op("dve", lambda e: e.scalar_tensor_tensor(
                    out=(sm[:, 0:1] if last else sm[:, 2:3]), in0=sm[:, 5:6], scalar=f, in1=sm[:, 2:3], op0=ALU.mult,
                    op1=ALU.add), reads=[ts], writes=[ts]))
            both(lambda x_, isc, ti, junk, tj, sm, ts, nk: S.op("dve", lambda e: e.tensor_scalar(
                out=junk[:, 0:nk], in0=isc[:, 0:nk], scalar1=sm[:, 0:1], scalar2=None, op0=ALU.is_ge),
                reads=[ti, ts, tj], writes=[tj]))
            for x_, qt in enumerate(qts):
                nk = nks[x_]
                nkt = nk // 128
                junk, t_junk = junks[x_], t_junks[x_]
                if dbg:
                    S.dma("sp", lambda e, qt=qt, nk=nk, junk=junk: e.dma_start(out=DBGM[qt, :, 0:nk], in_=junk[:, 0:nk]), reads=[t_junk])
                k0 = 0
                g_ = 0
                while k0 < nkt:
                    n = min(4, nkt - k0)
                    p, tp = pT[g_ % 2], t_pT[g_ % 2]
                    g_ += 1
                    for j in range(n):
                        S.op("pe", lambda e, p=p, j=j, k0=k0, junk=junk: e.transpose(
                            out=p[:, j * 128:(j + 1) * 128], in_=junk[:, (k0 + j) * 128:(k0 + j + 1) * 128],
                            identity=ident[:]), reads=[t_junk, t_ident], writes=[tp])
                    b0 = (mt_base[qt] + k0) * 128
                    S.op("act", lambda e, p=p, n=n, b0=b0: e.activation(out=maskT[:, b0:b0 + n * 128], in_=p[:, 0:n * 128],
                                                                       func=AF.Identity, scale=30000.0, bias=-30000.0),
                         reads=[tp], writes=[t_maskT])
                    k0 += n

    def phase_attn(st, maskT, t_maskT):
        ones = sb(st, "ones", [128, 128], BF16)
        t_ones = Tok()
        S.op("dve", lambda e: e.memset(ones[:], 1.0), writes=[t_ones])
        NKT = NT // 128
        kth = [sb(st, "kth%d" % i, [128, NT], BF16) for i in range(2)]
        vh = [sb(st, "vh%d" % i, [128, NKT, 128], BF16) for i in range(2)]
        qth = [sb(st, "qth%d" % i, [128, NO], BF16) for i in range(2)]
        yth = [sb(st, "yth%d" % i, [128, NO], BF16) for i in range(2)]
        t_k = [Tok(), Tok()]
        t_y = [Tok(), Tok()]
        pS = [ps(st, "pS%d" % i, [128, 512], F32) for i in range(2)]
        t_pS = [Tok(), Tok()]
        pO = [ps(st, "pO%d" % i, [128, 512], F32) for i in range(2)]
        pD = [ps(st, "pD%d" % i, [128, 512], F32) for i in range(2)]
        t_pO = [Tok(), Tok()]
        P = [sb(st, "P%d" % i, [128, 512], BF16) for i in range(2)]
        Pm = [sb(st, "Pm%d" % i, [128, 512], BF16) for i in range(2)]
        t_P = [Tok(), Tok()]
        t_Pm = [Tok(), Tok()]
        rec = sb(st, "rec", [128, 128], F32)
        t_rec = Tok()
        g_ = [0]
        scale = float(HD) ** -0.5
        for h in range(NH):
            b = h % 2
            S.dma("sp", lambda e, b=b, h=h: e.dma_start(out=kth[b][:], in_=KTs[h * 128:(h + 1) * 128, :]),
                  writes=[t_k[b]])
            S.dma("sp", lambda e, b=b, h=h: e.dma_start(out=vh[b][:], in_=VS[:, h * 128:(h + 1) * 128].rearrange(
                "(k p) d -> p k d", p=128)), writes=[t_k[b]])
            S.dma("sp", lambda e, b=b, h=h: e.dma_start(out=qth[b][:], in_=QT[h * 128:(h + 1) * 128, :]),
                  writes=[t_k[b]])
            for qt in range(NQT):
                nkt = (NP + (qt + 1) * 128) // 128
                po, tpo = pO[qt % 2], t_pO[qt % 2]
                pd = pD[qt % 2]
                k0 = 0
                while k0 < nkt:
                    n = min(4, nkt - k0)
                    i = g_[0]
                    g_[0] += 1
                    p_s, tps = pS[i % 2], t_pS[i % 2]
                    b0 = (mt_base[qt] + k0) * 128
                    S.op("pe", lambda e, p_s=p_s, n=n, b0=b0: e.matmul(
                        p_s[:, 0:n * 128], lhsT=ident[:], rhs=maskT[:, b0:b0 + n * 128], start=True, stop=False),
                         reads=[t_ident, t_maskT], writes=[tps])
                    for j in range(n):
                        S.op("pe", lambda e, p_s=p_s, j=j, k0=k0, b=b, qt=qt, n=n: e.matmul(
                            p_s[:, j * 128:(j + 1) * 128], lhsT=kth[b][:, (k0 + j) * 128:(k0 + j + 1) * 128],
                            rhs=qth[b][:, qt * 128:(qt + 1) * 128], start=False, stop=(j == n - 1)),
                             reads=[t_k[b]], writes=[tps])
                    S.op("act", lambda e, p_s=p_s, n=n, i=i: e.activation(
                        out=Pm[i % 2][:, 0:n * 128], in_=p_s[:, 0:n * 128], func=AF.Exp, scale=scale),
                         reads=[tps], writes=[t_Pm[i % 2]])
                    for j in range(n):
                        first = (k0 + j == 0)
                        last = (k0 + j == nkt - 1)
                        S.op("pe", lambda e, po=po, j=j, k0=k0, b=b, i=i, first=first, last=last: e.matmul(
                            po[:, 0:128], lhsT=vh[b][:, k0 + j, :], rhs=Pm[i % 2][:, j * 128:(j + 1) * 128],
                            start=first, stop=last), reads=[t_k[b], t_Pm[i % 2]], writes=[tpo])
                        S.op("pe", lambda e, pd=pd, j=j, i=i, first=first, last=last: e.matmul(
                            pd[:, 0:128], lhsT=ones[:], rhs=Pm[i % 2][:, j * 128:(j + 1) * 128],
                            start=first, stop=last), reads=[t_ones, t_Pm[i % 2]], writes=[tpo])
                    k0 += n
                S.op("dve", lambda e, pd=pd: e.reciprocal(out=rec[:], in_=pd[:, 0:128]),
                     reads=[tpo], writes=[t_rec])
                S.op("dve", lambda e, po=po, b=b, qt=qt: e.tensor_tensor(
                    out=yth[b][:, qt * 128:(qt + 1) * 128], in0=po[:, 0:128], in1=rec[:], op=ALU.mult),
                     reads=[tpo, t_rec], writes=[t_y[b]])
            S.dma("sp", lambda e, b=b, h=h: e.dma_start(out=YT[h * 128:(h + 1) * 128, :], in_=yth[b][:]),
                  reads=[t_y[b]], writes=[])


    RO = DSA_COLS
    R_R, R_DW, R_K, R_V, R_DA, R_DG = RO, RO + 1024, RO + 1088, RO + 2112, RO + 3136, RO + 3200
    carry = sb(es, "carry", [128, 32], F32)
    t_carry = Tok()
    S.op("dve", lambda e: e.memset(carry[:], 0.0), writes=[t_carry])

    def phase_rwkv_prep(st, hT, t_hT, tok0, ntok, own):
        nb = ntok // 512
        wst = sb(st, "wstr", [128, KT, 512], F32)
        wbfs = [sb(st, "wbfr%d" % i, [128, KT, 512], BF16) for i in range(2)]
        wb = (wst, Tok(), wbfs, [Tok(), Tok()], [0])
        pc = sb(st, "pc", [128, 84], F32)
        t_pc = Tok()
        S.dma("sp", lambda e: e.dma_start(out=pc[:], in_=pcols_in), writes=[t_pc])
        omk = sb(st, "omk", [128, 8], F32)
        S.op("dve", lambda e: e.tensor_scalar(out=omk[:], in0=pc[:, 48:56], scalar1=-1.0, scalar2=1.0, op0=ALU.mult, op1=ALU.add),
             reads=[t_pc], writes=[t_pc])
        lst = sb(st, "lst", [128, 1, RW], F32)
        wdu = sb(st, "wdu", [64, RW], BF16)
        wau = sb(st, "wau", [64, RW], BF16)
        wgu0 = sb(st, "wgu0", [128, RW], BF16)
        wgu1 = sb(st, "wgu1", [32, RW], BF16)
        cst = sb(st, "cst", [128, 128], F32)
        bones = sb(st, "bones", [128, 128], BF16)
        t_lw = Tok()
        S.dma("sp", lambda e: e.dma_start(out=cst[:], in_=cmats_in[0]), writes=[t_lw])
        S.op("dve", lambda e: e.tensor_copy(out=bones[:], in_=cst[:]), reads=[t_lw], writes=[t_lw])
        S.dma("sp", lambda e: e.dma_start(out=lst[0:64, 0, :], in_=wdu_in), reads=[t_lw], writes=[t_lw])
        S.op("dve", lambda e: e.tensor_copy(out=wdu[:], in_=lst[0:64, 0, :]), reads=[t_lw], writes=[t_lw])
        S.dma("sp", lambda e: e.dma_start(out=lst[0:64, 0, :], in_=wau_in), reads=[t_lw], writes=[t_lw])
        S.op("dve", lambda e: e.tensor_copy(out=wau[:], in_=lst[0:64, 0, :]), reads=[t_lw], writes=[t_lw])
        S.dma("sp", lambda e: e.dma_start(out=lst[:, 0, :], in_=wgu_in[0:128, :]), reads=[t_lw], writes=[t_lw])
        S.op("dve", lambda e: e.tensor_copy(out=wgu0[:], in_=lst[:, 0, :]), reads=[t_lw], writes=[t_lw])
        S.dma("sp", lambda e: e.dma_start(out=lst[0:32, 0, :], in_=wgu_in[128:160, :]), reads=[t_lw], writes=[t_lw])
        S.op("dve", lambda e: e.tensor_copy(out=wgu1[:], in_=lst[0:32, 0, :]), reads=[t_lw], writes=[t_lw])
        th_all = sb(st, "th_all", [64, ntok], BF16)
        da_all = sb(st, "da_all", [64, ntok], BF16)
        dg0_all = sb(st, "dg0_all", [128, ntok], BF16)
        dg1_all = sb(st, "dg1_all", [32, ntok], BF16)
        t_lora = Tok()
        pm = [ps(st, "pm%d" % i, [128, 512], F32) for i in range(3)]
        t_pm = [Tok() for _ in range(3)]
        px = [ps(st, "px%d" % i, [128, 512], F32) for i in range(2)]
        t_px = [Tok(), Tok()]
        ptT_full = ps(st, "ptT", [128, 1024], BF16)
        ptT = ptT_full[:, 0:512]
        t_ptT = Tok()
        nbuf = ["raw", "dsh", "r", "k", "v", "lw", "a", "kk", "kkn", "k2", "b", "cA", "cB", "e", "tmp"]
        B = {n: sb(st, "rb_" + n, [128, 512], F32) for n in nbuf}
        T = {n: Tok() for n in nbuf}
        hb = {n: sb(st, "rh_" + n, [128, 512], BF16) for n in ("sqb", "rkb", "tmk", "tmb", "vb", "gt", "bn")}
        TH = {n: Tok() for n in hb}
        cm = sb(st, "cmt", [128, 4, 512], BF16)
        t_cm = Tok()
        tmt = sb(st, "tmt", [128, 4, 128], BF16)
        t_tmt = Tok()
        wc = sb(st, "wc", [128, 8], F32)
        t_wc = Tok()
        mmc = [0]

        def mm_ch(wtile, tw, c0, ncol, tb):
            i = mmc[0]
            mmc[0] += 1
            p, tp = pm[i % 3], t_pm[i % 3]
            for kt in range(KT):
                S.op("pe", lambda e, kt=kt, p=p: e.matmul(p[0:ncol, :], lhsT=wtile[:, kt, c0:c0 + ncol],
                                                       rhs=hT[:, kt, tb * 512:(tb + 1) * 512], start=(kt == 0),
                                                       stop=(kt == KT - 1)), reads=[t_hT, tw], writes=[tp])
            return p, tp

        def shift(p, tp, n_, mu_col, cidx, dst, t_dst):
            raw, dsh = B["raw"], B["dsh"]
            S.op("act", lambda e: e.copy(out=raw[0:n_, :], in_=p[0:n_, :]), reads=[tp], writes=[T["raw"]])
            S.op("dve", lambda e: e.tensor_tensor(out=dsh[0:n_, 1:512], in0=raw[0:n_, 0:511], in1=raw[0:n_, 1:512],
                                                  op=ALU.subtract), reads=[T["raw"]], writes=[T["dsh"]])
            S.op("dve", lambda e: e.tensor_tensor(out=dsh[0:n_, 0:1], in0=carry[0:n_, cidx:cidx + 1],
                                                  in1=raw[0:n_, 0:1], op=ALU.subtract),
                 reads=[T["raw"], t_carry], writes=[T["dsh"]])
            S.op("act", lambda e: e.copy(out=carry[0:n_, cidx:cidx + 1], in_=raw[0:n_, 511:512]),
                 reads=[T["raw"], T["dsh"]], writes=[t_carry])
            S.op("dve", lambda e: e.scalar_tensor_tensor(out=dst, in0=dsh[0:n_, :], scalar=pc[0:n_, mu_col:mu_col + 1],
                                                         in1=raw[0:n_, :], op0=ALU.mult, op1=ALU.add),
                 reads=[T["dsh"], T["raw"], t_pc], writes=[t_dst])

        wtile, tw = load_w(wb, R_DW, 64)
        for tb in range(nb):
            p, tp = mm_ch(wtile, tw, 0, 64, tb)
            shift(p, tp, 64, 80, 0, B["tmp"][0:64, :], T["tmp"])
            S.op("act", lambda e, tb=tb: e.activation(out=th_all[:, tb * 512:(tb + 1) * 512], in_=B["tmp"][0:64, :],
                                                     func=AF.Tanh), reads=[T["tmp"]], writes=[t_lora])
        wtile, tw = load_w(wb, R_DA, 64 + 160)
        for tb in range(nb):
            p, tp = mm_ch(wtile, tw, 0, 64, tb)
            shift(p, tp, 64, 81, 1, B["tmp"][0:64, :], T["tmp"])
            S.op("act", lambda e, tb=tb: e.copy(out=da_all[:, tb * 512:(tb + 1) * 512], in_=B["tmp"][0:64, :]),
                 reads=[T["tmp"]], writes=[t_lora])
            p, tp = mm_ch(wtile, tw, 64, 128, tb)
            shift(p, tp, 128, 82, 2, B["tmp"][:, :], T["tmp"])
            S.op("act", lambda e, tb=tb: e.activation(out=dg0_all[:, tb * 512:(tb + 1) * 512], in_=B["tmp"][:, :],
                                                     func=AF.Sigmoid), reads=[T["tmp"]], writes=[t_lora])
            p, tp = mm_ch(wtile, tw, 192, 32, tb)
            shift(p, tp, 32, 83, 3, B["tmp"][0:32, :], T["tmp"])
            S.op("act", lambda e, tb=tb: e.activation(out=dg1_all[:, tb * 512:(tb + 1) * 512], in_=B["tmp"][0:32, :],
                                                     func=AF.Sigmoid), reads=[T["tmp"]], writes=[t_lora])

        def v3(ap):
            return ap.rearrange("p (c t) -> p c t", t=64)

        for ct in range(8):
            wst_, t_wst, wbfs_, t_wbfs, cnt_ = wb
            i = cnt_[0]
            cnt_[0] += 1
            wtile, tw = wbfs_[i % 2], t_wbfs[i % 2]
            for j, c0 in enumerate((R_R, R_K, R_V)):
                src = w_in[:, c0 + ct * 128:c0 + (ct + 1) * 128].rearrange("(k p) c -> p k c", p=128)
                S.dma("sp", lambda e, src=src, j=j: e.dma_start(out=wst_[:, :, j * 128:(j + 1) * 128], in_=src),
                      writes=[t_wst])
            S.op("act", lambda e, wtile=wtile: e.copy(out=wtile[:, :, 0:384], in_=wst_[:, :, 0:384]),
                 reads=[t_wst], writes=[tw])
            for tb in range(nb):
                t0 = tb * 512
                for j, nm in enumerate(("r", "k", "v")):
                    p, tp = mm_ch(wtile, tw, j * 128, 128, tb)
                    shift(p, tp, 128, j * 8 + ct, 4 + j * 8 + ct, B[nm][:], T[nm])
                cs = slice(ct * 128, (ct + 1) * 128)
                pz, tpz = px[0], t_px[0]
                S.op("pe", lambda e, pz=pz, cs=cs, t0=t0: e.matmul(pz[:], lhsT=wdu[:, cs], rhs=th_all[:, t0:t0 + 512],
                                                             start=True, stop=True), reads=[t_lw, t_lora], writes=[tpz])
                S.op("act", lambda e, pz=pz, ct=ct: e.activation(out=B["lw"][:], in_=pz[:], func=AF.Sigmoid,
                                                               bias=pc[:, 24 + ct:25 + ct]),
                     reads=[tpz, t_pc], writes=[T["lw"]])
                S.op("act", lambda e: e.mul(out=B["lw"][:], in_=B["lw"][:], mul=-0.6065306597126334),
                     reads=[T["lw"]], writes=[T["lw"]])
                pa, tpa = px[1], t_px[1]
                S.op("pe", lambda e, pa=pa, cs=cs, t0=t0: e.matmul(pa[:], lhsT=wau[:, cs], rhs=da_all[:, t0:t0 + 512],
                                                             start=True, stop=True), reads=[t_lw, t_lora], writes=[tpa])
                S.op("act", lambda e, pa=pa, ct=ct: e.activation(out=B["a"][:], in_=pa[:], func=AF.Sigmoid,
                                                               bias=pc[:, 32 + ct:33 + ct]),
                     reads=[tpa, t_pc], writes=[T["a"]])
                if own:
                    pg, tpg = px[0], t_px[0]
                    S.op("pe", lambda e, pg=pg, cs=cs, t0=t0: e.matmul(pg[:], lhsT=wgu0[:, cs], rhs=dg0_all[:, t0:t0 + 512],
                                                                 start=True, stop=False), reads=[t_lw, t_lora], writes=[tpg])
                    S.op("pe", lambda e, pg=pg, cs=cs, t0=t0: e.matmul(pg[:], lhsT=wgu1[:, cs], rhs=dg1_all[:, t0:t0 + 512],
                                                                 start=False, stop=True), reads=[t_lw, t_lora], writes=[tpg])
                    S.op("act", lambda e, pg=pg: e.copy(out=hb["gt"][:], in_=pg[:]), reads=[tpg], writes=[TH["gt"]])
                    S.dma("sp", lambda e, cs=cs, t0=t0: e.dma_start(out=GTs[cs, t0:t0 + 512], in_=hb["gt"][:]),
                          reads=[TH["gt"]])
                S.op("dve", lambda e, ct=ct: e.tensor_scalar(out=B["kk"][:], in0=B["k"][:], scalar1=pc[:, 40 + ct:41 + ct],
                                                            scalar2=None, op0=ALU.mult),
                     reads=[T["k"], t_pc], writes=[T["kk"]])
                S.op("act", lambda e: e.activation(out=hb["sqb"][:], in_=B["kk"][:], func=AF.Square),
                     reads=[T["kk"]], writes=[TH["sqb"]])
                pss, tpss = px[1], t_px[1]
                S.op("pe", lambda e, pss=pss: e.matmul(pss[:], lhsT=bones[:], rhs=hb["sqb"][:], start=True, stop=True),
                     reads=[t_lw, TH["sqb"]], writes=[tpss])
                S.op("act", lambda e, pss=pss: e.activation(out=B["tmp"][:], in_=pss[:], func=AF.Sqrt),
                     reads=[tpss], writes=[T["tmp"]])
                S.op("dve", lambda e: e.tensor_scalar(out=B["tmp"][:], in0=B["tmp"][:], scalar1=1e-12, scalar2=None,
                                                      op0=ALU.max), reads=[T["tmp"]], writes=[T["tmp"]])
                S.op("dve", lambda e: e.reciprocal(out=B["tmp"][:], in_=B["tmp"][:]), reads=[T["tmp"]], writes=[T["tmp"]])
                S.op("dve", lambda e: e.tensor_tensor(out=B["kkn"][:], in0=B["kk"][:], in1=B["tmp"][:], op=ALU.mult),
                     reads=[T["kk"], T["tmp"]], writes=[T["kkn"]])
                S.op("act", lambda e, ct=ct: e.activation(out=B["k2"][:], in_=B["a"][:], func=AF.Identity,
                                                         scale=pc[:, 48 + ct:49 + ct], bias=omk[:, ct:ct + 1]),
                     reads=[T["a"], t_pc], writes=[T["k2"]])
                S.op("dve", lambda e: e.tensor_tensor(out=B["k2"][:], in0=B["k2"][:], in1=B["k"][:], op=ALU.mult),
                     reads=[T["k2"], T["k"]], writes=[T["k2"]])
                S.op("dve", lambda e: e.tensor_tensor(out=B["b"][:], in0=B["kkn"][:], in1=B["a"][:], op=ALU.mult),
                     reads=[T["kkn"], T["a"]], writes=[T["b"]])
                if own:
                    S.op("dve", lambda e, ct=ct: e.scalar_tensor_tensor(out=hb["rkb"][:], in0=B["r"][:],
                                                                       scalar=pc[:, 56 + ct:57 + ct], in1=B["k2"][:],
                                                                       op0=ALU.mult, op1=ALU.mult),
                         reads=[T["r"], T["k2"], t_pc], writes=[TH["rkb"]])
                    prk, tprk = px[0], t_px[0]
                    S.op("pe", lambda e, prk=prk: e.matmul(prk[:], lhsT=bones[:], rhs=hb["rkb"][:], start=True, stop=True),
                         reads=[t_lw, TH["rkb"]], writes=[tprk])
                    S.op("dve", lambda e, prk=prk: e.tensor_tensor(out=hb["bn"][:], in0=prk[:], in1=B["v"][:], op=ALU.mult),
                         reads=[tprk, T["v"]], writes=[TH["bn"]])
                    S.dma("sp", lambda e, cs=cs, t0=t0: e.dma_start(out=BNs[cs, t0:t0 + 512], in_=hb["bn"][:]),
                          reads=[TH["bn"]])
                src, t_src = B["lw"], T["lw"]
                for si, sft in enumerate((1, 2, 4, 8, 16, 32)):
                    dn = "cA" if si % 2 == 0 else "cB"
                    dst_, t_dst_ = B[dn], T[dn]
                    S.op("act", lambda e, src=src, dst_=dst_, sft=sft: e.copy(
                        out=v3(dst_[:])[:, :, 0:sft], in_=v3(src[:])[:, :, 0:sft]), reads=[t_src], writes=[t_dst_])
                    S.op("dve", lambda e, src=src, dst_=dst_, sft=sft: e.tensor_tensor(
                        out=v3(dst_[:])[:, :, sft:64], in0=v3(src[:])[:, :, sft:64], in1=v3(src[:])[:, :, 0:64 - sft],
                        op=ALU.add), reads=[t_src], writes=[t_dst_])
                    src, t_src = dst_, t_dst_
                cl, t_cl = src, t_src
                S.op("act", lambda e, cl=cl: e.activation(out=B["e"][:], in_=cl[:], func=AF.Exp), reads=[t_cl], writes=[T["e"]])
                S.op("dve", lambda e: e.tensor_tensor(out=cm[:, 3, :], in0=B["r"][:], in1=B["e"][:], op=ALU.mult),
                     reads=[T["r"], T["e"]], writes=[t_cm])
                S.op("dve", lambda e, cl=cl: e.tensor_tensor(out=B["tmp"][:], in0=cl[:], in1=B["lw"][:], op=ALU.subtract),
                     reads=[t_cl, T["lw"]], writes=[T["tmp"]])
                S.op("act", lambda e: e.activation(out=B["e"][:], in_=B["tmp"][:], func=AF.Exp),
                     reads=[T["tmp"]], writes=[T["e"]])
                S.op("dve", lambda e: e.tensor_tensor(out=cm[:, 2, :], in0=B["kkn"][:], in1=B["e"][:], op=ALU.mult),
                     reads=[T["kkn"], T["e"]], writes=[t_cm])
                S.op("act", lambda e, cl=cl: e.activation(out=B["e"][:], in_=cl[:], func=AF.Exp, scale=-1.0),
                     reads=[t_cl], writes=[T["e"]])
                S.op("dve", lambda e: e.tensor_tensor(out=cm[:, 1, :], in0=B["k2"][:], in1=B["e"][:], op=ALU.mult),
                     reads=[T["k2"], T["e"]], writes=[t_cm])
                S.op("dve", lambda e: e.tensor_tensor(out=cm[:, 0, :], in0=B["b"][:], in1=B["e"][:], op=ALU.mult),
                     reads=[T["b"], T["e"]], writes=[t_cm])
                S.op("dve", lambda e, cl=cl: e.tensor_tensor(out=v3(B["tmp"][:]), in0=v3(cl[:])[:, :, 63:64].broadcast_to([128, 8, 64]),
                                                            in1=v3(cl[:]), op=ALU.subtract), reads=[t_cl], writes=[T["tmp"]])
                S.op("act", lambda e: e.activation(out=B["e"][:], in_=B["tmp"][:], func=AF.Exp),
                     reads=[T["tmp"]], writes=[T["e"]])
                S.op("dve", lambda e: e.tensor_tensor(out=hb["tmk"][:], in0=B["k2"][:], in1=B["e"][:], op=ALU.mult),
                     reads=[T["k2"], T["e"]], writes=[TH["tmk"]])
                S.op("dve", lambda e: e.tensor_tensor(out=hb["tmb"][:], in0=B["b"][:], in1=B["e"][:], op=ALU.mult),
                     reads=[T["b"], T["e"]], writes=[TH["tmb"]])
                S.op("act", lambda e: e.copy(out=hb["vb"][:], in_=B["v"][:]), reads=[T["v"]], writes=[TH["vb"]])
                S.op("act", lambda e, cl=cl: e.activation(out=wc[:].unsqueeze(2), in_=v3(cl[:])[:, :, 63:64], func=AF.Exp),
                     reads=[t_cl], writes=[t_wc])
                g0 = (tok0 + t0) // 128
                for hh in range(2):
                    hd_ = 2 * ct + hh
                    for j in range(4):
                        dst = CMs[g0 + j, :, hd_, :, :]
                        srcap = cm[hh * 64:(hh + 1) * 64, :, j * 128:(j + 1) * 128]
                        S.dma("sp", lambda e, dst=dst, srcap=srcap: e.dma_start(out=dst, in_=srcap), reads=[t_cm])
                    c0_ = (tok0 + t0) // 64
                    S.dma("sp", lambda e, hh=hh, hd_=hd_, c0_=c0_: e.dma_start(
                        out=WCs[:, hd_, c0_:c0_ + 8], in_=wc[hh * 64:(hh + 1) * 64, :]), reads=[t_wc])
                for j in range(4):
                    srcs = (cm[:, 2, j * 128:(j + 1) * 128], hb["tmb"][:, j * 128:(j + 1) * 128],
                            hb["tmk"][:, j * 128:(j + 1) * 128], hb["vb"][:, j * 128:(j + 1) * 128])
                    for x_, sa in enumerate(srcs):
                        S.op("pe", lambda e, x_=x_, sa=sa: e.transpose(out=ptT[:, x_ * 128:(x_ + 1) * 128], in_=sa,
                                                                     identity=ident[:]),
                             reads=[t_cm, TH["tmb"], TH["tmk"], TH["vb"], t_ident], writes=[t_ptT])
                    S.op("act", lambda e: e.copy(out=tmt[:], in_=ptT.rearrange("p (x c) -> p x c", x=4)),
                         reads=[t_ptT], writes=[t_tmt])
                    S.dma("sp", lambda e, g0=g0, j=j, cs=cs: e.dma_start(out=TMs[g0 + j, :, :, cs], in_=tmt[:]),
                          reads=[t_tmt])


    NGP = NP // 128

    def phase_rwkv_scan(st):
        mk = sb(st, "mk", [128, 3, 128], F32)
        pc = sb(st, "pc2", [128, 84], F32)
        t_c = Tok()
        S.dma("sp", lambda e: e.dma_start(out=mk[:], in_=cmats_in[1:4].rearrange("m p c -> p m c")), writes=[t_c])
        S.dma("sp", lambda e: e.dma_start(out=pc[:], in_=pcols_in), writes=[t_c])
        cmg = [sb(st, "cmg%d" % i, [64, 16, 4, 128], BF16) for i in range(2)]
        tmg = [sb(st, "tmg%d" % i, [128, 4, RW], BF16) for i in range(2)]
        wcg = [sb(st, "wcg%d" % i, [64, 16, 2], F32) for i in range(2)]
        t_ld = [Tok(), Tok()]
        H = sb(st, "H", [64, 16, 64], BF16)
        t_H = [Tok() for _ in range(16)]
        S.op("dve", lambda e: e.memset(H[:], 0.0), writes=t_H)
        PB = [ps(st, "pb%d" % i, [128, 512], F32) for i in range(7)]
        t_PB = [Tok() for _ in range(7)]
        pTp = ps(st, "pTp", [128, 8, 128], BF16)
        t_pTp = Tok()
        pbc = [0]

        def bank():
            i = pbc[0] % 7
            pbc[0] += 1
            return PB[i], t_PB[i]
        names = ["LL", "G2", "Rb", "P0", "PT0", "P1", "PT1", "QpT", "AiT", "Kp", "M0", "M1"]
        NBUF = 4
        hk = [sb(st, "hk%d" % i, [64, 64], F32) for i in range(NBUF)]
        t_hk = [Tok() for _ in range(NBUF)]
        W = {n: [sb(st, "sw_%s%d" % (n, i), [128, 256], BF16) for i in range(NBUF)] for n in names}
        TW = {n: [Tok() for _ in range(NBUF)] for n in names}
        lqk = [sb(st, "lqk%d" % i, [128, 128], F32) for i in range(NBUF)]
        t_lqk = [Tok() for _ in range(NBUF)]
        ysb = sb(st, "ysb", [64, 2, RW], F32)
        t_ysb = Tok()
        ynb = sb(st, "ynb", [64, 2, RW], BF16)
        st1 = sb(st, "st1", [64, 2, 16], F32)
        st2 = sb(st, "st2", [64, 2, 16], F32)
        dd = sb(st, "dd", [64, 2, RW], F32)
        sq2 = sb(st, "sq2", [64, 2, RW], F32)
        t_post = Tok()
        bng = sb(st, "bng", [128, 8, 128], BF16)
        gtg = sb(st, "gtg", [128, 8, 128], BF16)
        t_bg = Tok()
        fin = sb(st, "fin", [128, 8, 128], F32)
        yrt = sb(st, "yrt", [128, 8, 128], BF16)
        t_fin, t_yrt = Tok(), Tok()
        engs = ("dve", "act")
        for g_ in range(NG):
            b = g_ % 2
            own_g = g_ >= NGP
            S.dma("sp", lambda e, b=b, g_=g_: e.dma_start(out=cmg[b][:], in_=CMs[g_]), writes=[t_ld[b]])
            S.dma("sp", lambda e, b=b, g_=g_: e.dma_start(out=tmg[b][:], in_=TMs[g_]), writes=[t_ld[b]])
            S.dma("sp", lambda e, b=b, g_=g_: e.dma_start(out=wcg[b][:], in_=WCs[:, :, 2 * g_:2 * g_ + 2]), writes=[t_ld[b]])
            LVL = int(os.environ.get("SCAN_LEVEL", "9"))

            def head_gen(h, b=b, g_=g_, own_g=own_g):
                u = h % NBUF
                hs = slice(h * 64, (h + 1) * 64)
                bT, kT, kkT, qT = (cmg[b][:, h, x_, :] for x_ in range(4))
                KKt, Bh, Kh, V = (tmg[b][:, x_, hs] for x_ in range(4))
                LL, G2, Rb = W["LL"][u], W["G2"][u], W["Rb"][u]
                p1, tp1 = bank()
                S.op("pe", lambda e, p1=p1, kkT=kkT, b=b, h=h: e.matmul(p1[:, 0:256], lhsT=kkT,
                     rhs=cmg[b][:, h, 0:2, :].rearrange("p x t -> p (x t)"), start=True, stop=True), reads=[t_ld[b]], writes=[tp1])
                S.op("dve", lambda e, p1=p1, LL=LL: e.tensor_tensor(out=LL[:].rearrange("p (x c) -> p x c", x=2),
                     in0=p1[:, 0:256].rearrange("p (x c) -> p x c", x=2),
                     in1=mk[:, 0, :].unsqueeze(1).broadcast_to([128, 2, 128]), op=ALU.mult), reads=[tp1, t_c], writes=[TW["LL"][u]])
                p2, tp2 = bank()
                S.op("pe", lambda e, p2=p2, bT=bT, b=b, h=h: e.matmul(p2[:, 0:256], lhsT=bT,
                     rhs=cmg[b][:, h, 2:4, :].rearrange("p x t -> p (x t)"), start=True, stop=True), reads=[t_ld[b]], writes=[tp2])
                S.op("dve", lambda e, p2=p2, G2=G2: e.tensor_tensor(out=G2[:].rearrange("p (x c) -> p x c", x=2),
                     in0=p2[:, 0:256].rearrange("p (x c) -> p x c", x=2), in1=mk[:, 1:3, :], op=ALU.mult),
                     reads=[tp2, t_c], writes=[TW["G2"][u]])
                p3, tp3 = bank()
                S.op("pe", lambda e, p3=p3, kT=kT, qT=qT: e.matmul(p3[:, 0:128], lhsT=kT, rhs=qT, start=True, stop=True),
                     reads=[t_ld[b]], writes=[tp3])
                S.op("dve", lambda e, p3=p3, u=u: e.tensor_tensor(out=lqk[u][:], in0=p3[:, 0:128], in1=mk[:, 2, :], op=ALU.mult),
                     reads=[tp3, t_c], writes=[t_lqk[u]])
                yield
                S.op("act", lambda e, Rb=Rb, KKt=KKt: e.copy(out=Rb[:, 0:64], in_=KKt), reads=[t_ld[b]], writes=[TW["Rb"][u]])
                S.op("act", lambda e, Rb=Rb, LL=LL: e.copy(out=Rb[:, 64:192], in_=LL[:, 128:256]), reads=[TW["LL"][u]], writes=[TW["Rb"][u]])
                Pc, tPc = LL[:, 0:128], TW["LL"][u]
                PTc, tPTc = G2[:, 0:128], TW["G2"][u]
                if LVL < 3:
                    return
                for k_ in range(6):
                    yield
                    if k_ > 0:
                        nP, tnP = W["P%d" % (k_ % 2)][u], TW["P%d" % (k_ % 2)][u]
                        nPT, tnPT = W["PT%d" % (k_ % 2)][u], TW["PT%d" % (k_ % 2)][u]
                        pa, tpa = bank()
                        pb_, tpb = bank()
                        S.op("pe", lambda e, pa=pa, PTc=PTc, Pc=Pc: e.matmul(pa[:, 0:128], lhsT=PTc, rhs=Pc, start=True, stop=True),
                             reads=[tPc, tPTc], writes=[tpa])
                        S.op("pe", lambda e, pb_=pb_, PTc=PTc, Pc=Pc: e.matmul(pb_[:, 0:128], lhsT=Pc, rhs=PTc, start=True, stop=True),
                             reads=[tPc, tPTc], writes=[tpb])
                        S.op("act", lambda e, pa=pa, nP=nP: e.copy(out=nP[:, 0:128], in_=pa[:, 0:128]), reads=[tpa], writes=[tnP])
                        S.op("act", lambda e, pb_=pb_, nPT=nPT: e.copy(out=nPT[:, 0:128], in_=pb_[:, 0:128]), reads=[tpb], writes=[tnPT])
                        Pc, tPc, PTc, tPTc = nP[:, 0:128], tnP, nPT[:, 0:128], tnPT
                    pr_, tpr = bank()
                    S.op("pe", lambda e, pr_=pr_, PTc=PTc, Rb=Rb: e.matmul(pr_[:, 0:192], lhsT=PTc, rhs=Rb[:, 0:192], start=True, stop=True),
                         reads=[tPTc, TW["Rb"][u]], writes=[tpr])
                    S.op("dve", lambda e, pr_=pr_, Rb=Rb, k_=k_: e.tensor_tensor(out=Rb[:, 0:192], in0=Rb[:, 0:192], in1=pr_[:, 0:192],
                         op=(ALU.subtract if k_ == 0 else ALU.add)), reads=[tpr, TW["Rb"][u]], writes=[TW["Rb"][u]])
                if LVL < 4:
                    return
                yield
                E, F = Rb[:, 0:64], Rb[:, 64:192]
                LqbT = G2[:, 128:256]
                QpT, AiT, Kp, M0, M1 = W["QpT"][u], W["AiT"][u], W["Kp"][u], W["M0"][u], W["M1"][u]
                pq, tpq = bank()
                S.op("pe", lambda e, pq=pq, E=E, LqbT=LqbT: e.matmul(pq[0:64, 0:128], lhsT=E, rhs=LqbT, start=True, stop=True),
                     reads=[TW["Rb"][u], TW["G2"][u]], writes=[tpq])
                S.op("dve", lambda e, pq=pq, QpT=QpT, qT=qT: e.tensor_tensor(out=QpT[0:64, 0:128], in0=qT, in1=pq[0:64, 0:128], op=ALU.subtract),
                     reads=[tpq, t_ld[b]], writes=[TW["QpT"][u]])
                yield
                pa2, tpa2 = bank()
                S.op("pe", lambda e, pa2=pa2, F=F, LqbT=LqbT: e.matmul(pa2[:, 0:128], lhsT=F, rhs=LqbT, start=True, stop=True),
                     reads=[TW["Rb"][u], TW["G2"][u]], writes=[tpa2])
                S.op("dve", lambda e, pa2=pa2, AiT=AiT, u=u: e.tensor_tensor(out=AiT[:, 0:128], in0=lqk[u][:], in1=pa2[:, 0:128], op=ALU.subtract),
                     reads=[tpa2, t_lqk[u]], writes=[TW["AiT"][u]])
                yield
                pk, tpk = bank()
                S.op("pe", lambda e, pk=pk, F=F, Bh=Bh: e.matmul(pk[:, 0:64], lhsT=F, rhs=Bh, start=True, stop=True),
                     reads=[TW["Rb"][u], t_ld[b]], writes=[tpk])
                S.op("dve", lambda e, pk=pk, Kp=Kp, Kh=Kh: e.tensor_tensor(out=Kp[:, 0:64], in0=Kh, in1=pk[:, 0:64], op=ALU.subtract),
                     reads=[tpk, t_ld[b]], writes=[TW["Kp"][u]])
                yield
                for c in range(2):
                    Mc, tMc = (M0, TW["M0"][u]) if c == 0 else (M1, TW["M1"][u])
                    rs = slice(c * 64, (c + 1) * 64)
                    pm_, tpm = bank()
                    S.op("pe", lambda e, pm_=pm_, rs=rs, Bh=Bh, Rb=Rb, b=b, h=h: e.matmul(pm_[0:64, 0:64], lhsT=Rb[rs, 0:64],
                         rhs=tmg[b][rs, 1, h * 64:(h + 1) * 64], start=True, stop=True), reads=[TW["Rb"][u], t_ld[b]], writes=[tpm])
                    S.op("dve", lambda e, pm_=pm_, Mc=Mc, c=c, b=b, h=h: e.scalar_tensor_tensor(out=Mc[0:64, 0:64], in0=ident_f[0:64, 0:64],
                         scalar=wcg[b][:, h, c:c + 1], in1=pm_[0:64, 0:64], op0=ALU.mult, op1=ALU.subtract),
                         reads=[tpm, t_ld[b], t_ident], writes=[tMc])
                if LVL < 5:
                    return
                for c in range(2):
                    Mc, tMc = (M0, TW["M0"][u]) if c == 0 else (M1, TW["M1"][u])
                    rs = slice(c * 64, (c + 1) * 64)
                    yield
                    if own_g:
                        py, tpy = bank()
                        py2, tpy2 = bank()
                        S.op("pe", lambda e, py=py, QpT=QpT, rs=rs, h=h: e.matmul(py[0:64, 0:64], lhsT=QpT[0:64, rs], rhs=H[:, h, :],
                             start=True, stop=True), reads=[TW["QpT"][u], t_H[h]], writes=[tpy])
                        S.op("pe", lambda e, py2=py2, AiT=AiT, rs=rs, b=b, h=h: e.matmul(py2[0:64, 0:64], lhsT=AiT[rs, rs],
                             rhs=tmg[b][rs, 3, h * 64:(h + 1) * 64], start=True, stop=True), reads=[TW["AiT"][u], t_ld[b]], writes=[tpy2])
                        S.op("act", lambda e, py=py, c=c, hs=hs: e.copy(out=ysb[:, c, hs], in_=py[0:64, 0:64]), reads=[tpy], writes=[t_ysb])
                        S.op("dve", lambda e, py2=py2, c=c, hs=hs: e.tensor_tensor(out=ysb[:, c, hs], in0=ysb[:, c, hs], in1=py2[0:64, 0:64],
                                                                                  op=ALU.add), reads=[tpy2, t_ysb], writes=[t_ysb])
                    ph, tph = bank()
                    ph2, tph2 = bank()
                    S.op("pe", lambda e, ph2=ph2, Kp=Kp, rs=rs, b=b, h=h: e.matmul(ph2[0:64, 0:64], lhsT=Kp[rs, 0:64],
                         rhs=tmg[b][rs, 3, h * 64:(h + 1) * 64], start=True, stop=True), reads=[TW["Kp"][u], t_ld[b]], writes=[tph2])
                    S.op("pe", lambda e, ph=ph, Mc=Mc, h=h: e.matmul(ph[0:64, 0:64], lhsT=Mc[0:64, 0:64], rhs=H[:, h, :], start=True, stop=True),
                         reads=[tMc, t_H[h]], writes=[tph])
                    S.op("act", lambda e, ph2=ph2, u=u: e.copy(out=hk[u][:], in_=ph2[0:64, 0:64]), reads=[tph2], writes=[t_hk[u]])
                    S.op("dve", lambda e, ph=ph, h=h, u=u: e.tensor_tensor(out=H[:, h, :], in0=hk[u][:], in1=ph[0:64, 0:64], op=ALU.add),
                         reads=[tph, t_hk[u]], writes=[t_H[h]])

            if LVL >= 2:
                gens = [head_gen(h) for h in range(16)]
                active = []
                nxt = 0
                while nxt < 16 or active:
                    while nxt < 16 and len(active) < NBUF:
                        active.append(gens[nxt])
                        nxt += 1
                    for gi in list(active):
                        try:
                            next(gi)
                        except StopIteration:
                            active.remove(gi)
            if own_g and LVL >= 6:
                go = g_ - NGP
                y4 = lambda ap: ap.rearrange("p c (h v) -> p c h v", v=64)
                S.dma("sp", lambda e, go=go: e.dma_start(out=bng[:], in_=BNs[:, go * 128:(go + 1) * 128].rearrange("(c p) t -> p c t", p=128)), writes=[t_bg])
                S.dma("sp", lambda e, go=go: e.dma_start(out=gtg[:], in_=GTs[:, go * 128:(go + 1) * 128].rearrange("(c p) t -> p c t", p=128)), writes=[t_bg])
                S.op("dve", lambda e: e.tensor_reduce(out=st1[:], in_=y4(ysb[:]), axis=AX.X, op=ALU.add), reads=[t_ysb], writes=[t_post])
                S.op("dve", lambda e: e.tensor_scalar(out=st1[:], in0=st1[:], scalar1=1.0 / 64, scalar2=None, op0=ALU.mult), reads=[t_post], writes=[t_post])
                S.op("dve", lambda e: e.tensor_tensor(out=y4(dd[:]), in0=y4(ysb[:]), in1=st1[:].unsqueeze(3).broadcast_to([64, 2, 16, 64]),
                                                      op=ALU.subtract), reads=[t_ysb, t_post], writes=[t_post])
                S.op("dve", lambda e: e.tensor_tensor(out=sq2[:], in0=dd[:], in1=dd[:], op=ALU.mult), reads=[t_post], writes=[t_post])
                S.op("dve", lambda e: e.tensor_reduce(out=st2[:], in_=y4(sq2[:]), axis=AX.X, op=ALU.add), reads=[t_post], writes=[t_post])
                S.op("act", lambda e: e.activation(out=st2[:], in_=st2[:], func=AF.Sqrt, scale=1.0 / 64, bias=GN_EPS), reads=[t_post], writes=[t_post])
                S.op("dve", lambda e: e.reciprocal(out=st2[:], in_=st2[:]), reads=[t_post], writes=[t_post])
                S.op("dve", lambda e: e.tensor_tensor(out=y4(ynb[:]), in0=y4(dd[:]), in1=st2[:].unsqueeze(3).broadcast_to([64, 2, 16, 64]),
                                                      op=ALU.mult), reads=[t_post], writes=[t_post])
                for c in range(2):
                    for ct in range(8):
                        S.op("pe", lambda e, c=c, ct=ct: e.transpose(out=pTp[:, ct, c * 64:(c + 1) * 64], in_=ynb[:, c, ct * 128:(ct + 1) * 128],
                                                                  identity=ident[0:64, 0:64]), reads=[t_post, t_ident], writes=[t_pTp])
                S.op("dve", lambda e: e.tensor_tensor(out=fin[:], in0=pTp[:], in1=pc[:, 64:72].unsqueeze(2).broadcast_to([128, 8, 128]), op=ALU.mult),
                     reads=[t_pTp, t_c], writes=[t_fin])
                S.op("dve", lambda e: e.tensor_tensor(out=fin[:], in0=fin[:], in1=pc[:, 72:80].unsqueeze(2).broadcast_to([128, 8, 128]), op=ALU.add),
                     reads=[t_fin, t_c], writes=[t_fin])
                S.op("dve", lambda e: e.tensor_tensor(out=fin[:], in0=fin[:], in1=bng[:], op=ALU.add), reads=[t_fin, t_bg], writes=[t_fin])
                S.op("dve", lambda e: e.tensor_tensor(out=yrt[:], in0=fin[:], in1=gtg[:], op=ALU.mult), reads=[t_fin, t_bg], writes=[t_yrt])
                S.dma("sp", lambda e, go=go: e.dma_start(out=YT[1024:2048, go * 128:(go + 1) * 128].rearrange("(c p) t -> p c t", p=128), in_=yrt[:]),
                      reads=[t_yrt])


    def phase_outproj(st):
        yT = sb(st, "yT", [128, KT, NO], BF16)
        t_yT = Tok()
        S.dma("sp", lambda e: e.dma_start(out=yT[:], in_=YT.rearrange("(k p) t -> p k t", p=128)), writes=[t_yT])
        wo = sb(st, "wo", [128, KT, D], BF16)
        wst = sb(st, "wsto", [128, KT, 512], F32)
        t_wst, t_wo = Tok(), Tok()
        for cb in range(4):
            S.dma("sp", lambda e, cb=cb: e.dma_start(out=wst[:], in_=w_out[:, cb * 512:(cb + 1) * 512].rearrange(
                "(k p) c -> p k c", p=128)), writes=[t_wst])
            if cb % 2:
                S.op("act", lambda e, cb=cb: e.copy(out=wo[:, :, cb * 512:(cb + 1) * 512], in_=wst[:]), reads=[t_wst], writes=[t_wo])
            else:
                S.op("dve", lambda e, cb=cb: e.tensor_copy(out=wo[:, :, cb * 512:(cb + 1) * 512], in_=wst[:]), reads=[t_wst], writes=[t_wo])
        wrs = sb(st, "wrs", [128, KT, 20], F32)
        wrb = sb(st, "wrb", [128, KT, 20], BF16)
        brb = sb(st, "brb", [128, 20], F32)
        gf = sb(st, "gf", [128, D], F32)
        t_r = Tok()
        S.dma("sp", lambda e: e.dma_start(out=wrs[:], in_=wr_in.rearrange("(k p) c -> p k c", p=128)), writes=[t_r])
        S.dma("sp", lambda e: e.dma_start(out=brb[:], in_=br_in.broadcast_to([128, 20])), writes=[t_r])
        S.dma("sp", lambda e: e.dma_start(out=gf[:], in_=g_ffn.broadcast_to([128, D])), writes=[t_r])
        S.op("dve", lambda e: e.tensor_copy(out=wrb[:], in_=wrs[:]), reads=[t_r], writes=[t_r])
        _xt = sb(st, "xo", [128, D], F32)
        _x1 = sb(st, "x1", [128, D], F32)
        xt = [_xt, _xt]
        x1 = [_x1, _x1]
        xn = sb(st, "xno", [128, D], BF16)
        junk = xn
        h2t = sb(st, "h2t", [128, KT, 128], BF16)
        ss = sb(st, "sso", [128, 2], F32)
        _a, _b = Tok(), Tok()
        t_xt, t_x1 = [_a, _a], [_b, _b]
        t_xn, t_h2t, t_ss = Tok(), Tok(), Tok()
        t_junk = t_xn
        po = [ps(st, "po%d" % i, [128, 512], F32) for i in range(4)]
        t_po = [Tok() for _ in range(4)]
        pst = ps(st, "psto", [128, D], BF16)
        t_pst = Tok()
        pl = ps(st, "pl", [128, 512], F32)
        t_pl = Tok()
        R = sb(st, "rt", [128, 96], F32)
        t_R = Tok()
        for it in range(NO // 128):
            b = it % 2
            r0 = NP + it * 128
            S.dma("sp", lambda e, b=b, r0=r0: e.dma_start(out=xt[b][:], in_=x[r0:r0 + 128, :]), writes=[t_xt[b]])
            for cb in range(4):
                for kt in range(KT):
                    S.op("pe", lambda e, cb=cb, kt=kt, it=it: e.matmul(po[cb][:], lhsT=yT[:, kt, it * 128:(it + 1) * 128],
                                                                       rhs=wo[:, kt, cb * 512:(cb + 1) * 512], start=(kt == 0),
                                                                       stop=(kt == KT - 1)), reads=[t_yT, t_wo], writes=[t_po[cb]])
                S.op("dve", lambda e, cb=cb, b=b: e.tensor_tensor(out=x1[b][:, cb * 512:(cb + 1) * 512], in0=po[cb][:],
                                                                 in1=xt[b][:, cb * 512:(cb + 1) * 512], op=ALU.add),
                     reads=[t_po[cb], t_xt[b]], writes=[t_x1[b]])
            S.dma("sp", lambda e, b=b, it=it: e.dma_start(out=X1[it * 128:(it + 1) * 128, :], in_=x1[b][:]), reads=[t_x1[b]])
            S.op("act", lambda e, b=b: e.activation(out=junk[:], in_=x1[b][:], func=AF.Square, accum_out=ss[:, 0:1]),
                 reads=[t_x1[b]], writes=[t_junk, t_ss])
            S.op("act", lambda e: e.activation(out=ss[:, 1:2], in_=ss[:, 0:1], func=AF.Sqrt, scale=1.0 / D, bias=NORM_EPS),
                 reads=[t_ss], writes=[t_ss])
            S.op("dve", lambda e: e.reciprocal(out=ss[:, 1:2], in_=ss[:, 1:2]), reads=[t_ss], writes=[t_ss])
            S.op("dve", lambda e, b=b: e.scalar_tensor_tensor(out=xn[:], in0=x1[b][:], scalar=ss[:, 1:2], in1=gf[:],
                                                             op0=ALU.mult, op1=ALU.mult), reads=[t_x1[b], t_ss, t_r], writes=[t_xn])
            for kt in range(KT):
                S.op("pe", lambda e, kt=kt: e.transpose(out=pst[:, kt * 128:(kt + 1) * 128], in_=xn[:, kt * 128:(kt + 1) * 128],
                                                       identity=ident[:]), reads=[t_xn, t_ident], writes=[t_pst])
            S.op("act", lambda e: e.copy(out=h2t[:], in_=pst[:].rearrange("p (k t) -> p k t", k=KT)), reads=[t_pst], writes=[t_h2t])
            S.dma("sp", lambda e, it=it: e.dma_start(out=H2T[:, it * 128:(it + 1) * 128].rearrange("(k p) t -> p k t", p=128),
                                                        in_=h2t[:]), reads=[t_h2t])
            for kt in range(KT):
                S.op("pe", lambda e, kt=kt: e.matmul(pl[:, 0:20], lhsT=h2t[:, kt, :], rhs=wrb[:, kt, :], start=(kt == 0),
                                                     stop=(kt == KT - 1)), reads=[t_h2t, t_r], writes=[t_pl])
            lg, gmx, ge, gs, gm = R[:, 0:20], R[:, 20:21], R[:, 21:25], R[:, 25:26], R[:, 26:30]
            tmp16, sel, m1, ee, t4 = R[:, 30:46], R[:, 46:50], R[:, 50:51], R[:, 51:55], R[:, 55:59]
            m2, mk2, ws, cmb = R[:, 59:60], R[:, 60:64], R[:, 64:65], R[:, 65:81]
            ngm, nm1 = R[:, 81:82], R[:, 82:83]
            def rop(eng, fn):
                S.op(eng, fn, reads=[t_R, t_pl, t_r], writes=[t_R])
            rop("dve", lambda e: e.tensor_tensor(out=lg, in0=pl[:, 0:20], in1=brb[:], op=ALU.add))
            rop("dve", lambda e: e.tensor_reduce(out=gmx, in_=R[:, 0:4], axis=AX.X, op=ALU.max))
            rop("dve", lambda e: e.tensor_scalar(out=ngm, in0=gmx, scalar1=-1.0, scalar2=None, op0=ALU.mult))
            rop("act", lambda e: e.activation(out=ge, in_=R[:, 0:4], func=AF.Exp, bias=ngm, accum_out=gs))
            rop("dve", lambda e: e.tensor_scalar(out=gm, in0=R[:, 0:4], scalar1=gmx, scalar2=None, op0=ALU.is_ge))
            rop("dve", lambda e: e.tensor_tensor(out=tmp16.rearrange("p (g j) -> p g j", g=4),
                                                 in0=R[:, 4:20].rearrange("p (g j) -> p g j", g=4),
                                                 in1=gm.unsqueeze(2).broadcast_to([128, 4, 4]), op=ALU.mult))
            rop("dve", lambda e: e.tensor_reduce(out=sel, in_=tmp16.rearrange("p (g j) -> p j g", g=4), axis=AX.X, op=ALU.add))
            rop("dve", lambda e: e.tensor_reduce(out=m1, in_=sel, axis=AX.X, op=ALU.max))
            rop("dve", lambda e: e.tensor_scalar(out=nm1, in0=m1, scalar1=-1.0, scalar2=None, op0=ALU.mult))
            rop("act", lambda e: e.activation(out=ee, in_=sel, func=AF.Exp, bias=nm1))
            rop("dve", lambda e: e.tensor_scalar(out=t4, in0=sel, scalar1=m1, scalar2=-1e30, op0=ALU.is_ge, op1=ALU.mult))
            rop("dve", lambda e: e.tensor_tensor(out=t4, in0=t4, in1=sel, op=ALU.add))
            rop("dve", lambda e: e.tensor_reduce(out=m2, in_=t4, axis=AX.X, op=ALU.max))
            rop("dve", lambda e: e.tensor_scalar(out=mk2, in0=sel, scalar1=m2, scalar2=None, op0=ALU.is_ge))
            rop("dve", lambda e: e.tensor_tensor(out=ee, in0=ee, in1=mk2, op=ALU.mult))
            rop("dve", lambda e: e.tensor_reduce(out=ws, in_=ee, axis=AX.X, op=ALU.add))
            rop("dve", lambda e: e.tensor_tensor(out=ws, in0=ws, in1=gs, op=ALU.mult))
            rop("dve", lambda e: e.reciprocal(out=ws, in_=ws))
            rop("dve", lambda e: e.tensor_scalar(out=ee, in0=ee, scalar1=ws, scalar2=None, op0=ALU.mult))
            rop("dve", lambda e: e.tensor_tensor(out=cmb.rearrange("p (g j) -> p g j", g=4),
                                                 in0=gm.unsqueeze(2).broadcast_to([128, 4, 4]),
                                                 in1=ee.unsqueeze(1).broadcast_to([128, 4, 4]), op=ALU.mult))
            S.dma("sp", lambda e, it=it: e.dma_start(out=CMB[it * 128:(it + 1) * 128, :], in_=cmb), reads=[t_R])

    def phase_moe(st):
        NB = NO // 512
        wg = [sb(st, "wg%d" % i, [128, KT, 512], BF16) for i in range(2)]
        wu = [sb(st, "wu%d" % i, [128, KT, 512], BF16) for i in range(2)]
        wd = [sb(st, "wd%d" % i, [128, 4, D], BF16) for i in range(2)]
        t_w = [[Tok(), Tok()] for _ in range(3)]
        h2 = sb(st, "h2m", [128, KT, 512], BF16)
        t_h2 = Tok()
        acc = sb(st, "accm", [128, 4, D], F32)
        t_acc = Tok()
        cmb = sb(st, "cmbm", [128, 4, 16], F32)
        t_cmb = Tok()
        xres = sb(st, "xres", [128, D], F32)
        t_xres = Tok()
        he = [sb(st, "he%d" % i, [128, 512], BF16) for i in range(4)]
        t_he = [Tok() for _ in range(4)]
        sg = [sb(st, "sgm%d" % i, [128, 512], F32) for i in range(2)]
        t_sg = [Tok(), Tok()]
        pg = [ps(st, "pg%d" % i, [128, 512], F32) for i in range(2)]
        pu = [ps(st, "pu%d" % i, [128, 512], F32) for i in range(2)]
        py = [ps(st, "py%d" % i, [128, 512], F32) for i in range(4)]
        t_pg, t_pu, t_py = [Tok(), Tok()], [Tok(), Tok()], [Tok() for _ in range(4)]
        seq = [(blk, ex) for blk in range(NB) for ex in range(NEXP)]

        def load(i):
            b = i % 2
            ex = seq[i][1]
            S.dma("pool", lambda e: e.dma_start(out=wg[b][:], in_=weg[ex].rearrange("(k p) c -> p k c", p=128)),
                  writes=[t_w[0][b]])
            S.dma("pool", lambda e: e.dma_start(out=wu[b][:], in_=weu[ex].rearrange("(k p) c -> p k c", p=128)),
                  writes=[t_w[1][b]])
            S.dma("pool", lambda e: e.dma_start(out=wd[b][:], in_=wed[ex].rearrange("(k p) c -> p k c", p=128)),
                  writes=[t_w[2][b]])
        load(0)
        for i, (blk, ex) in enumerate(seq):
            b = i % 2
            if ex == 0:
                S.dma("sp", lambda e, blk=blk: e.dma_start(out=h2[:], in_=H2T[:, blk * 512:(blk + 1) * 512].rearrange(
                    "(k p) t -> p k t", p=128)), writes=[t_h2])
                S.dma("sp", lambda e, blk=blk: e.dma_start(out=cmb[:], in_=CMB[blk * 512:(blk + 1) * 512, :].rearrange(
                    "(n p) c -> p n c", p=128)), writes=[t_cmb])
            if i + 1 < len(seq):
                load(i + 1)
            for ft in range(4):
                fb = ft % 2
                for kt in range(KT):
                    S.op("pe", lambda e, kt=kt, ft=ft, fb=fb, b=b: e.matmul(pg[fb][:], lhsT=wg[b][:, kt, ft * 128:(ft + 1) * 128],
                                                                           rhs=h2[:, kt, :], start=(kt == 0), stop=(kt == KT - 1)),
                         reads=[t_w[0][b], t_h2], writes=[t_pg[fb]])
                for kt in range(KT):
                    S.op("pe", lambda e, kt=kt, ft=ft, fb=fb, b=b: e.matmul(pu[fb][:], lhsT=wu[b][:, kt, ft * 128:(ft + 1) * 128],
                                                                           rhs=h2[:, kt, :], start=(kt == 0), stop=(kt == KT - 1)),
                         reads=[t_w[1][b], t_h2], writes=[t_pu[fb]])
                S.op("act", lambda e, fb=fb: e.activation(out=sg[fb][:], in_=pg[fb][:], func=AF.Silu), reads=[t_pg[fb]], writes=[t_sg[fb]])
                S.op("dve", lambda e, fb=fb, ft=ft: e.tensor_tensor(out=he[ft][:], in0=sg[fb][:], in1=pu[fb][:], op=ALU.mult),
                     reads=[t_sg[fb], t_pu[fb]], writes=[t_he[ft]])
            for tt in range(4):
                for cb in range(4):
                    for ft in range(4):
                        S.op("pe", lambda e, tt=tt, cb=cb, ft=ft, b=b: e.matmul(py[cb][:], lhsT=he[ft][:, tt * 128:(tt + 1) * 128],
                                                                               rhs=wd[b][:, ft, cb * 512:(cb + 1) * 512],
                                                                               start=(ft == 0), stop=(ft == 3)),
                             reads=[t_he[ft], t_w[2][b]], writes=[t_py[cb]])
                    dst = acc[:, tt, cb * 512:(cb + 1) * 512]
                    if ex == 0:
                        S.op("dve", lambda e, dst=dst, cb=cb, tt=tt, ex=ex: e.tensor_scalar(
                            out=dst, in0=py[cb][:], scalar1=cmb[:, tt, ex:ex + 1], scalar2=None, op0=ALU.mult),
                             reads=[t_py[cb], t_cmb], writes=[t_acc])
                    else:
                        S.op("dve", lambda e, dst=dst, cb=cb, tt=tt, ex=ex: e.scalar_tensor_tensor(
                            out=dst, in0=py[cb][:], scalar=cmb[:, tt, ex:ex + 1], in1=dst, op0=ALU.mult, op1=ALU.add),
                             reads=[t_py[cb], t_cmb, t_acc], writes=[t_acc])
            if ex == NEXP - 1:
                for tt in range(4):
                    r0 = blk * 512 + tt * 128
                    S.dma("sp", lambda e, r0=r0: e.dma_start(out=xres[:], in_=X1[r0:r0 + 128, :]), writes=[t_xres])
                    S.op("dve", lambda e, tt=tt: e.tensor_tensor(out=acc[:, tt, :], in0=acc[:, tt, :], in1=xres[:], op=ALU.add),
                         reads=[t_xres, t_acc], writes=[t_acc])
                    S.dma("sp", lambda e, r0=r0, tt=tt: e.dma_start(out=out[r0:r0 + 128, :], in_=acc[:, tt, :]), reads=[t_acc])

    for (tok0, ntok, own) in ((0, NP, False), (NP, NO, True)):
        with ExitStack() as st:
            hT = sb(st, "hT", [128, KT, ntok], BF16)
            t_hT = Tok("hT")
            with ExitStack() as st2:
                phase_norm(st2, hT, t_hT, tok0, ntok)
                S.barrier()
                S.emit()
            with ExitStack() as st2:
                phase_dsa_proj(st2, hT, t_hT, tok0, ntok, own)
                S.barrier()
                S.emit()
            with ExitStack() as st2:
                phase_rwkv_prep(st2, hT, t_hT, tok0, ntok, own)
                S.barrier()
                S.emit()

    with ExitStack() as st:
        phase_rwkv_scan(st)
        S.barrier()
        S.emit()
    with ExitStack() as st:
        maskT = sb(st, "maskT", [128, MT_TILES * 128], BF16)
        t_maskT = Tok()
        with ExitStack() as st2:
            phase_index(st2, maskT, t_maskT)
            S.barrier()
            S.emit()
        with ExitStack() as st2:
            phase_attn(st2, maskT, t_maskT)
            S.barrier()
            S.emit()

    with ExitStack() as st:
        phase_outproj(st)
        S.barrier()
        S.emit()
    with ExitStack() as st:
        phase_moe(st)
        S.barrier()
        S.emit()
    es.close()
    return nc


def rope_tables(pos):
    pos = pos.astype(np.float32)
    tabs = np.zeros((pos.shape[0], 192), np.float32)
    inv128 = (10000.0 ** (-np.arange(64, dtype=np.float32) * (2.0 / 128))).astype(np.float32)
    inv64 = (10000.0 ** (-np.arange(32, dtype=np.float32) * (2.0 / 64))).astype(np.float32)
    a = pos[:, None] * inv128[None, :]
    tabs[:, 0:64] = np.cos(a)
    tabs[:, 64:128] = np.sin(a)
    a = pos[:, None] * inv64[None, :]
    tabs[:, 128:160] = np.cos(a)
    tabs[:, 160:192] = np.sin(a)
    return tabs


def pcols_np(inp):
    pc = np.zeros((128, 84), np.float32)
    sm = inp["rwkv_shift_mix"].reshape(-1)
    vecs = [sm[0:1024], sm[1088:2112], sm[2112:3136], inp["w0"].reshape(-1), inp["a0"].reshape(-1),
            inp["k_k"].reshape(-1), inp["k_a"].reshape(-1), inp["r_k"].reshape(-1), inp["ln_x_w"].reshape(-1),
            inp["ln_x_b"].reshape(-1)]
    for j, v in enumerate(vecs):
        pc[:, j * 8:(j + 1) * 8] = v.reshape(8, 128).T
    pc[0:64, 80] = sm[1024:1088]
    pc[0:64, 81] = sm[3136:3200]
    pc[:, 82] = sm[3200:3328]
    pc[0:32, 83] = sm[3328:3360]
    return pc


def cmats_np():
    m = np.zeros((4, 128, 128), np.float32)
    for blk in range(2):
        o = blk * 64
        m[0, o:o + 64, o:o + 64] = 1.0
        m[1, o:o + 64, o:o + 64] = np.tril(np.ones((64, 64), np.float32), -1)
        m[2, o:o + 64, o:o + 64] = np.triu(np.ones((64, 64), np.float32), 1)
        m[3, o:o + 64, o:o + 64] = np.triu(np.ones((64, 64), np.float32), 0)
    return m


def cmask_np():
    m = np.zeros((128, 128), np.float32)
    m[0:64, 64:128] = -1e30
    return m


def kernel(**inputs):
    NP_, NO_ = 2048, 2048
    inp = {k: np.asarray(v) for k, v in inputs.items()}
    xfull = inp["x"]
    B = xfull.shape[0]
    nc = build(NP_, NO_, 256)
    p0 = {k: v[0] for k, v in inp.items() if k != "x"}
    shared = dict(g_mix=inp["g_mix"], w_in=p0["w_in"], ident_in=np.eye(128, dtype=np.float32), q_gain=inp["q_gain"],
                  k_gain=inp["k_gain"], cmask=cmask_np(), pcols=pcols_np(p0), cmats=cmats_np(),
                  lnrow=np.stack([p0["ln_x_w"], p0["ln_x_b"]]), w_decay_up=p0["w_decay_up"], w_aicl_up=p0["w_aicl_up"],
                  w_gate_lora_up=p0["w_gate_lora_up"], w_out=p0["w_out"], g_ffn=inp["g_ffn"],
                  wr=np.ascontiguousarray(np.concatenate([p0["w_route_group"], p0["w_route_expert"]], axis=1)),
                  br=np.ascontiguousarray(np.concatenate([inp["b_route_group"], inp["b_route_expert"]], axis=1)),
                  w_e_gate=p0["w_e_gate"], w_e_up=p0["w_e_up"], w_e_down=p0["w_e_down"])
    in_maps = []
    for c in range(2 * B):
        b, s_ = c // 2, c % 2
        if s_ == 0:
            xs = np.concatenate([np.zeros((NP_, D), np.float32), xfull[b, :NO_]], 0)
            pos = np.concatenate([np.zeros(NP_), np.arange(NO_)])
            pb = np.full((128, 1), -1e30, np.float32)
        else:
            xs = np.ascontiguousarray(xfull[b])
            pos = np.arange(NP_ + NO_)
            pb = np.zeros((128, 1), np.float32)
        m = dict(shared)
        m.update(x=xs, rope_tab=rope_tables(pos), pbias=pb)
        in_maps.append(m)
    res = run_bass_kernel_spmd(nc, in_maps, core_ids=list(range(2 * B)))
    outp = np.zeros_like(xfull)
    for c in range(2 * B):
        b, s_ = c // 2, c % 2
        outp[b, s_ * NO_:(s_ + 1) * NO_] = res.results[c]["out"]
    return outp
```

```python
import os
import numpy as np
import ml_dtypes
from contextlib import ExitStack

import concourse.bass as bass
import concourse.mybir as mybir
from concourse.bass_utils import run_bass_kernel_spmd

F32 = mybir.dt.float32
BF16 = mybir.dt.bfloat16
ALU = mybir.AluOpType
AF = mybir.ActivationFunctionType
AX = mybir.AxisListType

D = 2048
KT = 16
DSA_W = 1024
NH = 8
HD = 128
IH = 16
IDD = 64
RW = 1024
RH = 16
RN = 64
NEXP = 16
DEXP = 512
DSA_COLS = 4176
RWKV_COLS = 3360
IN_COLS = 7536
NORM_EPS = 1e-6
GN_EPS = 64e-5
CHUNK = 64

ENGS = ("pe", "act", "dve", "pool", "sp")
DMAQ = ("sp", "act", "pool")
NSLOT = 24
FUSE_WAIT = True


class Tok:
    __slots__ = ("name", "w", "r")

    def __init__(self, name=""):
        self.name = name
        self.w = None
        self.r = []


class Sched:
    def __init__(self, nc, es):
        self.nc = nc
        self.esem = {e: es.enter_context(nc.semaphore("es_" + e)) for e in ENGS}
        self.ebase = {e: 0 for e in ENGS}
        self.dsem = {q: [es.enter_context(nc.semaphore("ds_%s_%d" % (q, i))) for i in range(NSLOT)]
                     for q in DMAQ}
        self.dcnt = {q: [0] * NSLOT for q in DMAQ}
        self.dn = {q: 0 for q in DMAQ}
        self.waited = {e: {} for e in ENGS}
        self.ops = {e: [] for e in ENGS}
        self.phase = 0
        self.stop = None

    def _deps(self, eng, reads, writes, is_dma):
        raw, other = set(), set()
        for t in reads:
            if t.w is not None:
                raw.add(t.w)
        for t in writes:
            if t.w is not None:
                other.add(t.w)
            for r in t.r:
                other.add(r)
        deps = set()
        for ev in raw:
            if ev[0] == "e" and ev[3] != self.phase:
                continue
            if ev[0] == "e" and ev[1] == eng and eng == "pe" and not is_dma:
                continue
            deps.add(ev)
        for ev in other:
            if ev[0] == "e" and ev[3] != self.phase:
                continue
            if ev[0] == "e" and ev[1] == eng and eng == "pe" and not is_dma:
                continue
            deps.add(ev)
        return deps

    def op(self, eng, fn, reads=(), writes=()):
        idx = len(self.ops[eng])
        deps = self._deps(eng, reads, writes, False)
        ev = ("e", eng, idx, self.phase)
        for t in reads:
            t.r.append(ev)
        for t in writes:
            t.w = ev
            t.r = []
        self.ops[eng].append(dict(fn=fn, deps=deps, ms=False, dma=None))

    def dma(self, q, fn, reads=(), writes=()):
        deps = self._deps(q, reads, writes, True)
        n = self.dn[q]
        self.dn[q] += 1
        slot = n % NSLOT
        if self.dcnt[q][slot] > 0:
            deps.add(("d", q, slot, self.dcnt[q][slot]))
        self.dcnt[q][slot] += 16
        ev = ("d", q, slot, self.dcnt[q][slot])
        for t in reads:
            t.r.append(ev)
        for t in writes:
            t.w = ev
            t.r = []
        self.ops[q].append(dict(fn=fn, deps=deps, ms=False, dma=(q, slot)))

    def barrier(self):
        evs = []
        for e in ENGS:
            for i in range(len(self.ops[e]) - 1, -1, -1):
                if self.ops[e][i]["dma"] is None and self.ops[e][i]["fn"] is not None:
                    evs.append(("e", e, i, self.phase))
                    break
        for q in DMAQ:
            for s in range(NSLOT):
                if self.dcnt[q][s] > 0:
                    evs.append(("d", q, s, self.dcnt[q][s]))
        for e in ENGS:
            deps = set(ev for ev in evs if not (ev[0] == "e" and ev[1] == e))
            self.ops[e].append(dict(fn=None, deps=deps, ms=False, dma=None))

    def emit(self):
        nc = self.nc
        if self.stop is not None and self.phase >= self.stop:
            self.ops = {e: [] for e in ENGS}
            self.phase += 1
            return
        for e in ENGS:
            for o in self.ops[e]:
                for ev in o["deps"]:
                    if ev[0] == "e":
                        self.ops[ev[1]][ev[2]]["ms"] = True
        msv = {}
        for e in ENGS:
            c = self.ebase[e]
            arr = []
            for o in self.ops[e]:
                if o["ms"]:
                    c += 1
                arr.append(c)
            msv[e] = arr
            self.ebase[e] = c
        handles = {"pe": "tensor", "act": "scalar", "dve": "vector", "pool": "gpsimd", "sp": "sync"}

        def replay(e, eng):
            wd = self.waited[e]
            for i, o in enumerate(self.ops[e]):
                need = {}
                for ev in o["deps"]:
                    if ev[0] == "e":
                        sem, val, key = self.esem[ev[1]], msv[ev[1]][ev[2]], ("e", ev[1])
                    else:
                        sem, val, key = self.dsem[ev[1]][ev[2]], ev[3], ("d", ev[1], ev[2])
                    if wd.get(key, 0) >= val:
                        continue
                    if key not in need or need[key][1] < val:
                        need[key] = (sem, val)
                waits = [need[k] for k in sorted(need, key=str)]
                for k in need:
                    wd[k] = need[k][1]
                fused = None
                if FUSE_WAIT and o["fn"] is not None and waits:
                    fused = waits.pop()
                for sem, val in waits:
                    eng.wait_ge(sem, val)
                if o["fn"] is None:
                    continue
                ins = o["fn"](eng)
                if fused is not None:
                    ins._wait_ge(fused[0], fused[1])
                if o["dma"] is not None:
                    ins.then_inc(self.dsem[o["dma"][0]][o["dma"][1]], 16)
                elif o["ms"]:
                    ins.then_inc(self.esem[e], 1)

        with nc.Block() as blk:
            @blk.tensor
            def _(eng):
                replay("pe", eng)

            @blk.scalar
            def _(eng):
                replay("act", eng)

            @blk.vector
            def _(eng):
                replay("dve", eng)

            @blk.gpsimd
            def _(eng):
                replay("pool", eng)

            @blk.sync
            def _(eng):
                replay("sp", eng)
        self.ops = {e: [] for e in ENGS}
        self.phase += 1


class Ctx:
    pass


def build(NP, NO, TOPK, dbg=False, stop_after=None):
    NT = NP + NO
    nc = bass.Bass("TRN2", target_bir_lowering=False)
    g = Ctx()
    kind_s = "ExternalOutput" if dbg else "Internal"

    def din(name, shape, dt=F32):
        return nc.dram_tensor(name, list(shape), dt, kind="ExternalInput").ap()

    def dscr(name, shape, dt=BF16):
        return nc.dram_tensor(name, list(shape), dt, kind=kind_s).ap()

    x = din("x", [NT, D])
    g_mix = din("g_mix", [1, D])
    w_in = din("w_in", [D, IN_COLS])
    rope_tab = din("rope_tab", [NT, 192])
    ident_in = din("ident_in", [128, 128])
    q_gain = din("q_gain", [1, HD])
    k_gain = din("k_gain", [1, HD])
    pcols_in = din("pcols", [128, 84])
    wdu_in = din("w_decay_up", [64, RW])
    wau_in = din("w_aicl_up", [64, RW])
    wgu_in = din("w_gate_lora_up", [160, RW])
    cmats_in = din("cmats", [4, 128, 128])
    lnrow_in = din("lnrow", [2, RW])
    NG = NT // 128
    CMs = dscr("s_CM", [NG, 64, 16, 4, 128])
    TMs = dscr("s_TM", [NG, 128, 4, RW])
    WCs = dscr("s_WC", [64, 16, NT // 64], F32)
    GTs = dscr("s_GT", [RW, NO])
    BNs = dscr("s_BN", [RW, NO])
    w_out = din("w_out", [D, D])
    g_ffn = din("g_ffn", [1, D])
    wr_in = din("wr", [D, 20])
    br_in = din("br", [1, 20])
    weg = din("w_e_gate", [NEXP, D, DEXP])
    weu = din("w_e_up", [NEXP, D, DEXP])
    wed = din("w_e_down", [NEXP, DEXP, D])
    X1 = dscr("s_X1", [NO, D], F32)
    H2T = dscr("s_H2T", [D, NO])
    CMB = dscr("s_CMB", [NO, 16], F32)
    cmask_in = din("cmask", [128, 128])
    pbias_in = din("pbias", [128, 1])
    out = nc.dram_tensor("out", [NO, D], F32, kind="ExternalOutput").ap()
    YT = dscr("s_YT", [D, NO])
    if dbg:
        DBGM = dscr("dbg_mask", [NO // 128, 128, NT])


    QT = dscr("s_QT", [NH * HD, NO])
    KTs = dscr("s_KT", [NH * HD, NT])
    VS = dscr("s_V", [NT, DSA_W])
    QI = dscr("s_QI", [IH * IDD, NO])
    KI = dscr("s_KI", [128, NT])
    WI = dscr("s_WI", [NO, IH], F32)

    es = ExitStack()
    S = Sched(nc, es)
    S.stop = stop_after

    uniq = [0]

    def sb(st, name, shape, dt):
        uniq[0] += 1
        return st.enter_context(nc.sbuf_tensor("%s_%d" % (name, uniq[0]), list(shape), dt))

    def ps(st, name, shape, dt=F32):
        uniq[0] += 1
        return st.enter_context(nc.psum_tensor("%s_%d" % (name, uniq[0]), list(shape), dt))

    ident_f = sb(es, "ident_f", [128, 128], F32)
    ident = sb(es, "ident", [128, 128], BF16)
    t_ident = Tok("ident")
    S.dma("sp", lambda e: e.dma_start(out=ident_f[:], in_=ident_in), writes=[t_ident])
    S.op("dve", lambda e: e.tensor_copy(out=ident[:], in_=ident_f[:]), reads=[t_ident], writes=[t_ident])

    def phase_norm(st, hT, t_hT, tok0, ntok):
        gm = sb(st, "gm", [128, D], F32)
        t_gm = Tok()
        S.dma("sp", lambda e: e.dma_start(out=gm[:], in_=g_mix.broadcast_to([128, D])), writes=[t_gm])
        xt = [sb(st, "xt%d" % i, [128, D], F32) for i in range(2)]
        xn = [sb(st, "xn%d" % i, [128, D], BF16) for i in range(2)]
        junk = sb(st, "junk", [128, D], BF16)
        ss = [sb(st, "ss%d" % i, [128, 2], F32) for i in range(2)]
        pst = [ps(st, "pst%d" % i, [128, D], BF16) for i in range(2)]
        t_xt = [Tok() for _ in range(2)]
        t_xn = [Tok() for _ in range(2)]
        t_ss = [Tok() for _ in range(2)]
        t_ps = [Tok() for _ in range(2)]
        t_junk = Tok()
        for it in range(ntok // 128):
            b = it % 2
            r0 = tok0 + it * 128
            S.dma("sp", lambda e, b=b, r0=r0: e.dma_start(out=xt[b][:], in_=x[r0:r0 + 128, :]), writes=[t_xt[b]])
            S.op("act", lambda e, b=b: e.activation(out=junk[:], in_=xt[b][:], func=AF.Square,
                                                   accum_out=ss[b][:, 0:1]),
                 reads=[t_xt[b]], writes=[t_junk, t_ss[b]])
            S.op("act", lambda e, b=b: e.activation(out=ss[b][:, 1:2], in_=ss[b][:, 0:1], func=AF.Sqrt,
                                                   scale=1.0 / D, bias=NORM_EPS),
                 reads=[t_ss[b]], writes=[t_ss[b]])
            S.op("dve", lambda e, b=b: e.reciprocal(out=ss[b][:, 1:2], in_=ss[b][:, 1:2]),
                 reads=[t_ss[b]], writes=[t_ss[b]])
            S.op("dve", lambda e, b=b: e.scalar_tensor_tensor(out=xn[b][:], in0=xt[b][:], scalar=ss[b][:, 1:2],
                                                             in1=gm[:], op0=ALU.mult, op1=ALU.mult),
                 reads=[t_xt[b], t_ss[b], t_gm], writes=[t_xn[b]])
            for kt in range(KT):
                S.op("pe", lambda e, b=b, kt=kt: e.transpose(out=pst[b][:, kt * 128:(kt + 1) * 128],
                                                            in_=xn[b][:, kt * 128:(kt + 1) * 128],
                                                            identity=ident[:]),
                     reads=[t_xn[b], t_ident], writes=[t_ps[b]])
            c0 = it * 128
            S.op("act", lambda e, b=b, c0=c0: e.copy(out=hT[:, :, c0:c0 + 128],
                                                    in_=pst[b][:].rearrange("p (k t) -> p k t", k=KT)),
                 reads=[t_ps[b]], writes=[t_hT])

    def load_w(st_bufs, cols0, ncols):
        wst, t_wst, wbfs, t_wbfs, cnt = st_bufs
        i = cnt[0]
        cnt[0] += 1
        wb, tw = wbfs[i % 2], t_wbfs[i % 2]
        src = w_in[:, cols0:cols0 + ncols].rearrange("(k p) c -> p k c", p=128)
        S.dma("sp", lambda e: e.dma_start(out=wst[:, :, 0:ncols], in_=src), writes=[t_wst])
        if i % 2 == 0:
            S.op("act", lambda e: e.copy(out=wb[:, :, 0:ncols], in_=wst[:, :, 0:ncols]), reads=[t_wst], writes=[tw])
        else:
            S.op("dve", lambda e: e.tensor_copy(out=wb[:, :, 0:ncols], in_=wst[:, :, 0:ncols]), reads=[t_wst], writes=[tw])
        return wb, tw

    def phase_dsa_proj(st, hT, t_hT, tok0, ntok, own):
        wst = sb(st, "wst", [128, KT, 512], F32)
        wbfs = [sb(st, "wbf%d" % i, [128, KT, 512], BF16) for i in range(2)]
        wb = (wst, Tok(), wbfs, [Tok(), Tok()], [0])
        gq = sb(st, "gq", [128, HD], F32)
        gk = sb(st, "gk", [128, HD], F32)
        t_g = Tok()
        S.dma("sp", lambda e: e.dma_start(out=gq[:], in_=q_gain.broadcast_to([128, HD])), writes=[t_g])
        S.dma("sp", lambda e: e.dma_start(out=gk[:], in_=k_gain.broadcast_to([128, HD])), writes=[t_g])
        ntt = ntok // 128
        tabs = sb(st, "tabs", [128, ntt, 192], F32)
        t_tabs = Tok()
        S.dma("sp", lambda e: e.dma_start(out=tabs[:], in_=rope_tab[tok0:tok0 + ntok, :].rearrange(
            "(n p) c -> p n c", p=128)), writes=[t_tabs])
        pp = [ps(st, "pp%d" % i, [128, 512], F32) for i in range(2)]
        t_pp = [Tok(), Tok()]
        ptr_full = [ps(st, "ptr%d" % i, [128, 1024], BF16) for i in range(2)]
        ptr = [t_[:, 0:512] for t_ in ptr_full]
        t_ptr = [Tok(), Tok()]
        sq = sb(st, "sq", [128, 512], F32)
        xn = sb(st, "xnq", [128, 512], F32)
        ro = sb(st, "ro", [128, 512], BF16)
        tmp1 = sb(st, "tmp1", [128, 256], F32)
        tmp2 = sb(st, "tmp2", [128, 256], F32)
        ssq = sb(st, "ssq", [128, 8], F32)
        t_sq, t_xn, t_ro, t_t1, t_t2, t_ssq = Tok(), Tok(), Tok(), Tok(), Tok(), Tok()
        stg = [sb(st, "stg%d" % i, [128, 4, 512], BF16) for i in range(2)]
        t_stg = [Tok(), Tok()]
        vst = [sb(st, "vst%d" % i, [128, 512], BF16) for i in range(2)]
        t_vst = [Tok(), Tok()]
        wis = sb(st, "wis", [128, IH], F32)
        t_wis = Tok()
        cnt = [0]
        nstg = [0]

        def mm_tok(wtile, tw, ncols, it):
            i = cnt[0]
            cnt[0] += 1
            p, tp = pp[i % 2], t_pp[i % 2]
            for kt in range(KT):
                S.op("pe", lambda e, kt=kt, p=p: e.matmul(p[:, 0:ncols], lhsT=hT[:, kt, it * 128:(it + 1) * 128],
                                                       rhs=wtile[:, kt, 0:ncols], start=(kt == 0),
                                                       stop=(kt == KT - 1)),
                     reads=[t_hT, tw], writes=[tp])
            return p, tp

        def rope_ops(src, t_src, nh, hd, cos, sin, dst, t_dst):
            hf = hd // 2
            s3 = src.rearrange("p (h d) -> p h d", h=nh)
            d3 = dst.rearrange("p (h d) -> p h d", h=nh)
            cb = cos.unsqueeze(1).broadcast_to([128, nh, hf])
            sn = sin.unsqueeze(1).broadcast_to([128, nh, hf])
            a = tmp1[:, 0:nh * hf].rearrange("p (h d) -> p h d", h=nh)
            b = tmp2[:, 0:nh * hf].rearrange("p (h d) -> p h d", h=nh)
            x1, x2 = s3[:, :, 0:hf], s3[:, :, hf:hd]
            S.op("dve", lambda e: e.tensor_tensor(out=a, in0=x1, in1=cb, op=ALU.mult),
                 reads=[t_src, t_tabs], writes=[t_t1])
            S.op("dve", lambda e: e.tensor_tensor(out=b, in0=x2, in1=sn, op=ALU.mult),
                 reads=[t_src, t_tabs], writes=[t_t2])
            S.op("dve", lambda e: e.tensor_tensor(out=d3[:, :, 0:hf], in0=a, in1=b, op=ALU.subtract),
                 reads=[t_t1, t_t2], writes=[t_dst])
            S.op("dve", lambda e: e.tensor_tensor(out=a, in0=x2, in1=cb, op=ALU.mult),
                 reads=[t_src, t_tabs, t_dst], writes=[t_t1])
            S.op("dve", lambda e: e.tensor_tensor(out=b, in0=x1, in1=sn, op=ALU.mult),
                 reads=[t_src, t_tabs, t_dst], writes=[t_t2])
            S.op("dve", lambda e: e.tensor_tensor(out=d3[:, :, hf:hd], in0=a, in1=b, op=ALU.add),
                 reads=[t_t1, t_t2], writes=[t_dst])

        def qk_group(col0, gain, dstT, is_q):
            for half in range(2):
                wtile, tw = load_w(wb, col0 + half * 512, 512)
                for it in range(ntt):
                    p, tp = mm_tok(wtile, tw, 512, it)
                    S.op("act", lambda e, p=p: e.activation(out=sq[:], in_=p[:], func=AF.Square),
                         reads=[tp], writes=[t_sq])
                    S.op("dve", lambda e: e.tensor_reduce(out=ssq[:, 0:4], in_=sq[:].rearrange(
                        "p (h d) -> p h d", h=4), axis=AX.X, op=ALU.add), reads=[t_sq], writes=[t_ssq])
                    S.op("act", lambda e: e.activation(out=ssq[:, 4:8], in_=ssq[:, 0:4], func=AF.Sqrt,
                                                       scale=1.0 / HD, bias=NORM_EPS),
                         reads=[t_ssq], writes=[t_ssq])
                    S.op("dve", lambda e: e.reciprocal(out=ssq[:, 4:8], in_=ssq[:, 4:8]),
                         reads=[t_ssq], writes=[t_ssq])
                    S.op("dve", lambda e, p=p: e.tensor_tensor(
                        out=xn[:].rearrange("p (h d) -> p h d", h=4), in0=p[:].rearrange("p (h d) -> p h d", h=4),
                        in1=ssq[:, 4:8].unsqueeze(2).broadcast_to([128, 4, HD]), op=ALU.mult),
                         reads=[tp, t_ssq], writes=[t_xn])
                    S.op("dve", lambda e: e.tensor_tensor(
                        out=xn[:].rearrange("p (h d) -> p h d", h=4), in0=xn[:].rearrange("p (h d) -> p h d", h=4),
                        in1=gain[:].unsqueeze(1).broadcast_to([128, 4, HD]), op=ALU.mult),
                         reads=[t_xn, t_g], writes=[t_xn])
                    rope_ops(xn[:], t_xn, 4, HD, tabs[:, it, 0:64], tabs[:, it, 64:128], ro[:], t_ro)
                    j = nstg[0] // 4
                    sl = nstg[0] % 4
                    nstg[0] += 1
                    pt, tpt = ptr[it % 2], t_ptr[it % 2]
                    for h in range(4):
                        S.op("pe", lambda e, h=h, pt=pt: e.transpose(out=pt[:, h * 128:(h + 1) * 128],
                                                                  in_=ro[:, h * 128:(h + 1) * 128],
                                                                  identity=ident[:]),
                             reads=[t_ro, t_ident], writes=[tpt])
                    sg, tsg = stg[j % 2], t_stg[j % 2]
                    S.op("act", lambda e, pt=pt, sg=sg, sl=sl: e.copy(
                        out=sg[:, :, sl * 128:(sl + 1) * 128], in_=pt.rearrange("p (h t) -> p h t", h=4)),
                         reads=[tpt], writes=[tsg])
                    if sl == 3:
                        t0 = (it - 3) * 128 + (0 if is_q else tok0)
                        h0 = half * 4
                        dst = dstT.rearrange("(h d) t -> d h t", d=HD)[:, h0:h0 + 4, t0:t0 + 512]
                        S.dma("sp", lambda e, sg=sg, dst=dst: e.dma_start(out=dst, in_=sg[:]),
                              reads=[tsg], writes=[])

        qk_group(DSA_W, gk, KTs, False)
        if own:
            qk_group(0, gq, QT, True)
        for half in range(2):
            wtile, tw = load_w(wb, 2 * DSA_W + half * 512, 512)
            for it in range(ntt):
                p, tp = mm_tok(wtile, tw, 512, it)
                v, tv = vst[it % 2], t_vst[it % 2]
                S.op("act", lambda e, p=p, v=v: e.copy(out=v[:], in_=p[:]), reads=[tp], writes=[tv])
                r0 = tok0 + it * 128
                S.dma("sp", lambda e, v=v, r0=r0, half=half: e.dma_start(
                    out=VS[r0:r0 + 128, half * 512:(half + 1) * 512], in_=v[:]), reads=[tv], writes=[])
        wtile, tw = load_w(wb, 3 * DSA_W + IH * IDD, IDD + IH)
        for it in range(ntt):
            p, tp = mm_tok(wtile, tw, IDD + IH, it)
            S.op("act", lambda e, p=p: e.copy(out=xn[:, 0:IDD + IH], in_=p[:, 0:IDD + IH]), reads=[tp], writes=[t_xn])
            rope_ops(xn[:, 0:IDD], t_xn, 1, IDD, tabs[:, it, 128:160], tabs[:, it, 160:192], ro[:, 0:IDD], t_ro)
            S.op("act", lambda e: e.copy(out=ro[:, IDD:2 * IDD], in_=ro[:, 0:IDD]),
                 reads=[t_ro], writes=[t_ro])
            if own:
                S.op("act", lambda e: e.mul(out=wis[:], in_=xn[:, IDD:IDD + IH], mul=1.0 / 32.0),
                     reads=[t_xn], writes=[t_wis])
                S.dma("sp", lambda e, it=it: e.dma_start(out=WI[it * 128:(it + 1) * 128, :], in_=wis[:]),
                      reads=[t_wis], writes=[])
            pt, tpt = ptr[it % 2], t_ptr[it % 2]
            S.op("pe", lambda e, pt=pt: e.transpose(out=pt[:, 0:128], in_=ro[:, 0:128], identity=ident[:]),
                 reads=[t_ro, t_ident], writes=[tpt])
            j = nstg[0] // 4
            sl = nstg[0] % 4
            nstg[0] += 1
            sg, tsg = stg[j % 2], t_stg[j % 2]
            S.op("act", lambda e, pt=pt, sg=sg, sl=sl: e.copy(out=sg[:, 0, sl * 128:(sl + 1) * 128],
                                                            in_=pt[:, 0:128]), reads=[tpt], writes=[tsg])
            if sl == 3:
                t0 = tok0 + (it - 3) * 128
                S.dma("sp", lambda e, sg=sg, t0=t0: e.dma_start(out=KI[:, t0:t0 + 512], in_=sg[:, 0, :]),
                      reads=[tsg], writes=[])
        if own:
            for half in range(2):
                wtile, tw = load_w(wb, 3 * DSA_W + half * 512, 512)
                for it in range(ntt):
                    p, tp = mm_tok(wtile, tw, 512, it)
                    S.op("act", lambda e, p=p: e.copy(out=xn[:], in_=p[:]), reads=[tp], writes=[t_xn])
                    rope_ops(xn[:], t_xn, 8, IDD, tabs[:, it, 128:160], tabs[:, it, 160:192], ro[:], t_ro)
                    pt, tpt = ptr[it % 2], t_ptr[it % 2]
                    for h in range(4):
                        S.op("pe", lambda e, h=h, pt=pt: e.transpose(out=pt[:, h * 128:(h + 1) * 128],
                                                                  in_=ro[:, h * 128:(h + 1) * 128],
                                                                  identity=ident[:]),
                             reads=[t_ro, t_ident], writes=[tpt])
                    j = nstg[0] // 4
                    sl = nstg[0] % 4
                    nstg[0] += 1
                    sg, tsg = stg[j % 2], t_stg[j % 2]
                    S.op("act", lambda e, pt=pt, sg=sg, sl=sl: e.copy(
                        out=sg[:, :, sl * 128:(sl + 1) * 128], in_=pt.rearrange("p (h t) -> p h t", h=4)),
                         reads=[tpt], writes=[tsg])
                    if sl == 3:
                        t0 = (it - 3) * 128
                        dst = QI.rearrange("(h d) t -> d h t", d=128)[:, half * 4:half * 4 + 4, t0:t0 + 512]
                        S.dma("sp", lambda e, sg=sg, dst=dst: e.dma_start(out=dst, in_=sg[:]),
                              reads=[tsg], writes=[])


    NQT = NO // 128
    mt_base = []
    acc_ = 0
    for qt in range(NQT):
        mt_base.append(acc_)
        acc_ += (NP + (qt + 1) * 128) // 128
    MT_TILES = acc_
    NBIS = 20

    def phase_index(st, maskT, t_maskT):
        qi = sb(st, "qi", [128, 8, NO], BF16)
        ki = sb(st, "ki", [128, NT], BF16)
        wi = sb(st, "wi", [128, NQT, IH], F32)
        cm = sb(st, "cm", [128, 128], F32)
        pb = sb(st, "pb", [128, 1], F32)
        t_in = Tok()
        S.dma("sp", lambda e: e.dma_start(out=qi[:], in_=QI.rearrange("(h d) t -> d h t", d=128)), writes=[t_in])
        S.dma("sp", lambda e: e.dma_start(out=ki[:], in_=KI), writes=[t_in])
        S.dma("sp", lambda e: e.dma_start(out=wi[:], in_=WI.rearrange("(n p) h -> p n h", p=128)), writes=[t_in])
        S.dma("sp", lambda e: e.dma_start(out=cm[:], in_=cmask_in), writes=[t_in])
        S.dma("sp", lambda e: e.dma_start(out=pb[:], in_=pbias_in), writes=[t_in])
        iscs = [sb(st, "isc%d" % i, [128, NT], F32) for i in range(2)]
        junks = [sb(st, "junkm%d" % i, [128, NT], BF16) for i in range(2)]
        sms = [sb(st, "sm%d" % i, [128, 8], F32) for i in range(2)]
        t_iscs, t_junks, t_sms = [Tok(), Tok()], [Tok(), Tok()], [Tok(), Tok()]
        rr = [sb(st, "rr%d" % i, [128, 512], F32) for i in range(4)]
        t_rr = [Tok() for _ in range(4)]
        iscB = [sb(st, "iscB%d" % i, [128, 512], F32) for i in range(2)]
        t_iscB = [Tok(), Tok()]
        pp = [ps(st, "pi%d" % i, [128, 512], F32) for i in range(4)]
        t_pp = [Tok() for _ in range(4)]
        pT_full = [ps(st, "pT%d" % i, [128, 1024], BF16) for i in range(2)]
        pT = [t_[:, 0:512] for t_ in pT_full]
        t_pT = [Tok(), Tok()]
        cnt = [0]
        NDV = 16

        def head_block(qt, kb, h, isc, t_isc, nk):
            w = min(512, nk - kb * 512)
            i = cnt[0]
            cnt[0] += 1
            p, tp = pp[i % 4], t_pp[i % 4]
            r, tr = rr[i % 4], t_rr[i % 4]
            pr = (h % 2) * 64
            S.op("pe", lambda e: e.matmul(p[:, 0:w], lhsT=qi[pr:pr + 64, h // 2, qt * 128:(qt + 1) * 128],
                                          rhs=ki[pr:pr + 64, kb * 512:kb * 512 + w], start=True, stop=True),
                 reads=[t_in], writes=[tp])
            S.op("act", lambda e: e.activation(out=r[:, 0:w], in_=p[:, 0:w], func=AF.Relu), reads=[tp], writes=[tr])
            dst = isc[:, kb * 512:kb * 512 + w]
            ws = wi[:, qt, h:h + 1]
            if h == 0:
                S.op("dve", lambda e: e.tensor_scalar(out=dst, in0=r[:, 0:w], scalar1=ws, scalar2=None, op0=ALU.mult),
                     reads=[tr, t_in], writes=[t_isc])
            elif h < NDV:
                S.op("dve", lambda e: e.scalar_tensor_tensor(out=dst, in0=r[:, 0:w], scalar=ws, in1=dst, op0=ALU.mult, op1=ALU.add),
                     reads=[tr, t_in, t_isc], writes=[t_isc])
            else:
                ib, tib = iscB[kb % 2], t_iscB[kb % 2]
                if h == NDV:
                    S.op("pool", lambda e: e.tensor_scalar(out=ib[:, 0:w], in0=r[:, 0:w], scalar1=ws, scalar2=None, op0=ALU.mult),
                         reads=[tr, t_in], writes=[tib])
                else:
                    S.op("pool", lambda e: e.tensor_scalar(out=r[:, 0:w], in0=r[:, 0:w], scalar1=ws, scalar2=None, op0=ALU.mult),
                         reads=[tr, t_in], writes=[tr])
                    S.op("pool", lambda e: e.tensor_tensor(out=ib[:, 0:w], in0=ib[:, 0:w], in1=r[:, 0:w], op=ALU.add),
                         reads=[tr, tib], writes=[tib])
                if h == IH - 1:
                    S.op("dve", lambda e: e.tensor_tensor(out=dst, in0=dst, in1=ib[:, 0:w], op=ALU.add),
                         reads=[tib, t_isc], writes=[t_isc])

        for q0 in range(0, NQT, 2):
            qts = [q_ for q_ in (q0, q0 + 1) if q_ < NQT]
            nks = [NP + (q_ + 1) * 128 for q_ in qts]
            for x_, qt in enumerate(qts):
                nk = nks[x_]
                nblk = (nk + 511) // 512
                for k0 in range(0, nblk, 2):
                    kbs = [k_ for k_ in (k0, k0 + 1) if k_ < nblk]
                    for h in range(IH):
                        for kb in kbs:
                            head_block(qt, kb, h, iscs[x_], t_iscs[x_], nk)
            def both(fn):
                for x_ in range(len(qts)):
                    fn(x_, iscs[x_], t_iscs[x_], junks[x_], t_junks[x_], sms[x_], t_sms[x_], nks[x_])
            both(lambda x_, isc, ti, junk, tj, sm, ts, nk: S.op("dve", lambda e: e.tensor_reduce(
                out=sm[:, 5:6], in_=isc[:, 0:nk], axis=AX.X, op=ALU.max), reads=[ti], writes=[ts]))
            both(lambda x_, isc, ti, junk, tj, sm, ts, nk: S.op("dve", lambda e: e.tensor_reduce(
                out=sm[:, 0:1], in_=isc[:, 0:nk], axis=AX.X, op=ALU.min), reads=[ti, ts], writes=[ts]))
            both(lambda x_, isc, ti, junk, tj, sm, ts, nk: S.op("dve", lambda e: e.tensor_tensor(
                out=sm[:, 1:2], in0=sm[:, 5:6], in1=sm[:, 0:1], op=ALU.subtract), reads=[ts], writes=[ts]))
            both(lambda x_, isc, ti, junk, tj, sm, ts, nk: S.op("dve", lambda e: e.tensor_tensor(
                out=isc[:, nk - 128:nk], in0=isc[:, nk - 128:nk], in1=cm[:], op=ALU.add), reads=[ti, t_in], writes=[ti]))
            if NP > 0:
                both(lambda x_, isc, ti, junk, tj, sm, ts, nk: S.op("dve", lambda e: e.tensor_scalar(
                    out=isc[:, 0:NP], in0=isc[:, 0:NP], scalar1=pb[:, 0:1], scalar2=None, op0=ALU.add),
                    reads=[ti, t_in], writes=[ti]))
            both(lambda x_, isc, ti, junk, tj, sm, ts, nk: S.op("dve", lambda e: e.tensor_scalar(
                out=sm[:, 1:2], in0=sm[:, 1:2], scalar1=1e-20, scalar2=None, op0=ALU.max), reads=[ts], writes=[ts]))
            both(lambda x_, isc, ti, junk, tj, sm, ts, nk: S.op("dve", lambda e: e.reciprocal(
                out=sm[:, 4:5], in_=sm[:, 1:2]), reads=[ts], writes=[ts]))
            both(lambda x_, isc, ti, junk, tj, sm, ts, nk: S.op("dve", lambda e: e.tensor_scalar(
                out=isc[:, 0:nk], in0=isc[:, 0:nk], scalar1=sm[:, 0:1], scalar2=sm[:, 4:5], op0=ALU.subtract, op1=ALU.mult),
                reads=[ti, ts], writes=[ti]))
            both(lambda x_, isc, ti, junk, tj, sm, ts, nk: S.op("dve", lambda e: e.memset(sm[:, 2:3], -0.5),
                                                               reads=[ts], writes=[ts]))
            for it in range(NBIS):
                last = (it == NBIS - 1)
                f = 0.5 ** (it + 1)
                both(lambda x_, isc, ti, junk, tj, sm, ts, nk: S.op("act", lambda e: e.activation(
                    out=junk[:, 0:nk], in_=isc[:, 0:nk], func=AF.Sign, bias=sm[:, 2:3], accum_out=sm[:, 3:4]),
                    reads=[ti, ts, tj], writes=[tj, ts]))
                both(lambda x_, isc, ti, junk, tj, sm, ts, nk, last=last: S.op("dve", lambda e: e.tensor_scalar(
                    out=sm[:, 5:6], in0=sm[:, 3:4], scalar1=float(2 * TOPK - nk - 1), scalar2=(0.0 if last else -0.5),
                    op0=ALU.is_lt, op1=ALU.add), reads=[ts], writes=[ts]))
                both(lambda x_, isc, ti, junk, tj, sm, ts, nk, last=last, f=f: S.op("dve", lambda e: e.scalar_tensor_tensor(
                    out=(sm[:, 0:1] if last else sm[:, 2:3]), in0=sm[:, 5:6], scalar=f, in1=sm[:, 2:3], op0=ALU.mult,
                    op1=ALU.add), reads=[ts], writes=[ts]))
            both(lambda x_, isc, ti, junk, tj, sm, ts, nk: S.op("dve", lambda e: e.tensor_scalar(
                out=junk[:, 0:nk], in0=isc[:, 0:nk], scalar1=sm[:, 0:1], scalar2=0.0, op0=ALU.add, op1=ALU.is_ge),
                reads=[ti, ts, tj], writes=[tj]))
            for x_, qt in enumerate(qts):
                nk = nks[x_]
                nkt = nk // 128
                junk, t_junk = junks[x_], t_junks[x_]
                if dbg:
                    S.dma("sp", lambda e, qt=qt, nk=nk, junk=junk: e.dma_start(out=DBGM[qt, :, 0:nk], in_=junk[:, 0:nk]), reads=[t_junk])
                k0 = 0
                g_ = 0
                while k0 < nkt:
                    n = min(4, nkt - k0)
                    p, tp = pT[g_ % 2], t_pT[g_ % 2]
                    g_ += 1
                    for j in range(n):
                        S.op("pe", lambda e, p=p, j=j, k0=k0, junk=junk: e.transpose(
                            out=p[:, j * 128:(j + 1) * 128], in_=junk[:, (k0 + j) * 128:(k0 + j + 1) * 128],
                            identity=ident[:]), reads=[t_junk, t_ident], writes=[tp])
                    b0 = (mt_base[qt] + k0) * 128
                    S.op("act", lambda e, p=p, n=n, b0=b0: e.activation(out=maskT[:, b0:b0 + n * 128], in_=p[:, 0:n * 128],
                                                                       func=AF.Identity, scale=30000.0, bias=-30000.0),
                         reads=[tp], writes=[t_maskT])
                    k0 += n

    def phase_attn(st, maskT, t_maskT):
        ones = sb(st, "ones", [128, 128], BF16)
        t_ones = Tok()
        S.op("dve", lambda e: e.memset(ones[:], 1.0), writes=[t_ones])
        NKT = NT // 128
        kth = [sb(st, "kth%d" % i, [128, NT], BF16) for i in range(2)]
        vh = [sb(st, "vh%d" % i, [128, NKT, 128], BF16) for i in range(2)]
        qth = [sb(st, "qth%d" % i, [128, NO], BF16) for i in range(2)]
        yth = [sb(st, "yth%d" % i, [128, NO], BF16) for i in range(2)]
        t_k = [Tok(), Tok()]
        t_y = [Tok(), Tok()]
        pS = [ps(st, "pS%d" % i, [128, 512], F32) for i in range(2)]
        t_pS = [Tok(), Tok()]
        pO = [ps(st, "pO%d" % i, [128, 512], F32) for i in range(2)]
        pD = [ps(st, "pD%d" % i, [128, 512], F32) for i in range(2)]
        t_pO = [Tok(), Tok()]
        P = [sb(st, "P%d" % i, [128, 512], BF16) for i in range(2)]
        Pm = [sb(st, "Pm%d" % i, [128, 512], BF16) for i in range(2)]
        t_P = [Tok(), Tok()]
        t_Pm = [Tok(), Tok()]
        rec = sb(st, "rec", [128, 128], F32)
        t_rec = Tok()
        g_ = [0]
        scale = float(HD) ** -0.5
        for h in range(NH):
            b = h % 2
            S.dma("sp", lambda e, b=b, h=h: e.dma_start(out=kth[b][:], in_=KTs[h * 128:(h + 1) * 128, :]),
                  writes=[t_k[b]])
            S.dma("sp", lambda e, b=b, h=h: e.dma_start(out=vh[b][:], in_=VS[:, h * 128:(h + 1) * 128].rearrange(
                "(k p) d -> p k d", p=128)), writes=[t_k[b]])
            S.dma("sp", lambda e, b=b, h=h: e.dma_start(out=qth[b][:], in_=QT[h * 128:(h + 1) * 128, :]),
                  writes=[t_k[b]])
            for qt in range(NQT):
                nkt = (NP + (qt + 1) * 128) // 128
                po, tpo = pO[qt % 2], t_pO[qt % 2]
                pd = pD[qt % 2]
                k0 = 0
                while k0 < nkt:
                    n = min(4, nkt - k0)
                    i = g_[0]
                    g_[0] += 1
                    p_s, tps = pS[i % 2], t_pS[i % 2]
                    b0 = (mt_base[qt] + k0) * 128
                    S.op("pe", lambda e, p_s=p_s, n=n, b0=b0: e.matmul(
                        p_s[:, 0:n * 128], lhsT=ident[:], rhs=maskT[:, b0:b0 + n * 128], start=True, stop=False),
                         reads=[t_ident, t_maskT], writes=[tps])
                    for j in range(n):
                        S.op("pe", lambda e, p_s=p_s, j=j, k0=k0, b=b, qt=qt, n=n: e.matmul(
                            p_s[:, j * 128:(j + 1) * 128], lhsT=kth[b][:, (k0 + j) * 128:(k0 + j + 1) * 128],
                            rhs=qth[b][:, qt * 128:(qt + 1) * 128], start=False, stop=(j == n - 1)),
                             reads=[t_k[b]], writes=[tps])
                    S.op("act", lambda e, p_s=p_s, n=n, i=i: e.activation(
                        out=Pm[i % 2][:, 0:n * 128], in_=p_s[:, 0:n * 128], func=AF.Exp, scale=scale),
                         reads=[tps], writes=[t_Pm[i % 2]])
                    for j in range(n):
                        first = (k0 + j == 0)
                        last = (k0 + j == nkt - 1)
                        S.op("pe", lambda e, po=po, j=j, k0=k0, b=b, i=i, first=first, last=last: e.matmul(
                            po[:, 0:128], lhsT=vh[b][:, k0 + j, :], rhs=Pm[i % 2][:, j * 128:(j + 1) * 128],
                            start=first, stop=last), reads=[t_k[b], t_Pm[i % 2]], writes=[tpo])
                        S.op("pe", lambda e, pd=pd, j=j, i=i, first=first, last=last: e.matmul(
                            pd[:, 0:128], lhsT=ones[:], rhs=Pm[i % 2][:, j * 128:(j + 1) * 128],
                            start=first, stop=last), reads=[t_ones, t_Pm[i % 2]], writes=[tpo])
                    k0 += n
                S.op("dve", lambda e, pd=pd: e.reciprocal(out=rec[:], in_=pd[:, 0:128]),
                     reads=[tpo], writes=[t_rec])
                S.op("dve", lambda e, po=po, b=b, qt=qt: e.tensor_tensor(
                    out=yth[b][:, qt * 128:(qt + 1) * 128], in0=po[:, 0:128], in1=rec[:], op=ALU.mult),
                     reads=[tpo, t_rec], writes=[t_y[b]])
            S.dma("sp", lambda e, b=b, h=h: e.dma_start(out=YT[h * 128:(h + 1) * 128, :], in_=yth[b][:]),
                  reads=[t_y[b]], writes=[])


    RO = DSA_COLS
    R_R, R_DW, R_K, R_V, R_DA, R_DG = RO, RO + 1024, RO + 1088, RO + 2112, RO + 3136, RO + 3200
    carry = sb(es, "carry", [128, 32], F32)
    t_carry = Tok()
    S.op("dve", lambda e: e.memset(carry[:], 0.0), writes=[t_carry])

    def phase_rwkv_prep(st, hT, t_hT, tok0, ntok, own):
        nb = ntok // 512
        wst = sb(st, "wstr", [128, KT, 512], F32)
        wbfs = [sb(st, "wbfr%d" % i, [128, KT, 512], BF16) for i in range(2)]
        wb = (wst, Tok(), wbfs, [Tok(), Tok()], [0])
        pc = sb(st, "pc", [128, 84], F32)
        t_pc = Tok()
        S.dma("sp", lambda e: e.dma_start(out=pc[:], in_=pcols_in), writes=[t_pc])
        omk = sb(st, "omk", [128, 8], F32)
        S.op("dve", lambda e: e.tensor_scalar(out=omk[:], in0=pc[:, 48:56], scalar1=-1.0, scalar2=1.0, op0=ALU.mult, op1=ALU.add),
             reads=[t_pc], writes=[t_pc])
        lst = sb(st, "lst", [128, 1, RW], F32)
        wdu = sb(st, "wdu", [64, RW], BF16)
        wau = sb(st, "wau", [64, RW], BF16)
        wgu0 = sb(st, "wgu0", [128, RW], BF16)
        wgu1 = sb(st, "wgu1", [32, RW], BF16)
        cst = sb(st, "cst", [128, 128], F32)
        bones = sb(st, "bones", [128, 128], BF16)
        t_lw = Tok()
        S.dma("sp", lambda e: e.dma_start(out=cst[:], in_=cmats_in[0]), writes=[t_lw])
        S.op("dve", lambda e: e.tensor_copy(out=bones[:], in_=cst[:]), reads=[t_lw], writes=[t_lw])
        S.dma("sp", lambda e: e.dma_start(out=lst[0:64, 0, :], in_=wdu_in), reads=[t_lw], writes=[t_lw])
        S.op("dve", lambda e: e.tensor_copy(out=wdu[:], in_=lst[0:64, 0, :]), reads=[t_lw], writes=[t_lw])
        S.dma("sp", lambda e: e.dma_start(out=lst[0:64, 0, :], in_=wau_in), reads=[t_lw], writes=[t_lw])
        S.op("dve", lambda e: e.tensor_copy(out=wau[:], in_=lst[0:64, 0, :]), reads=[t_lw], writes=[t_lw])
        S.dma("sp", lambda e: e.dma_start(out=lst[:, 0, :], in_=wgu_in[0:128, :]), reads=[t_lw], writes=[t_lw])
        S.op("dve", lambda e: e.tensor_copy(out=wgu0[:], in_=lst[:, 0, :]), reads=[t_lw], writes=[t_lw])
        S.dma("sp", lambda e: e.dma_start(out=lst[0:32, 0, :], in_=wgu_in[128:160, :]), reads=[t_lw], writes=[t_lw])
        S.op("dve", lambda e: e.tensor_copy(out=wgu1[:], in_=lst[0:32, 0, :]), reads=[t_lw], writes=[t_lw])
        th_all = sb(st, "th_all", [64, ntok], BF16)
        da_all = sb(st, "da_all", [64, ntok], BF16)
        dg0_all = sb(st, "dg0_all", [128, ntok], BF16)
        dg1_all = sb(st, "dg1_all", [32, ntok], BF16)
        t_lora = Tok()
        pm = [ps(st, "pm%d" % i, [128, 512], F32) for i in range(3)]
        t_pm = [Tok() for _ in range(3)]
        px = [ps(st, "px%d" % i, [128, 512], F32) for i in range(2)]
        t_px = [Tok(), Tok()]
        ptT_full = ps(st, "ptT", [128, 1024], BF16)
        ptT = ptT_full[:, 0:512]
        t_ptT = Tok()
        nbuf = ["raw", "dsh", "r", "k", "v", "lw", "a", "kk", "kkn", "k2", "b", "cA", "cB", "e", "tmp"]
        B = {n: sb(st, "rb_" + n, [128, 512], F32) for n in nbuf}
        T = {n: Tok() for n in nbuf}
        hb = {n: sb(st, "rh_" + n, [128, 512], BF16) for n in ("sqb", "rkb", "tmk", "tmb", "vb", "gt", "bn")}
        TH = {n: Tok() for n in hb}
        cm = sb(st, "cmt", [128, 4, 512], BF16)
        t_cm = Tok()
        tmt = sb(st, "tmt", [128, 4, 128], BF16)
        t_tmt = Tok()
        wc = sb(st, "wc", [128, 8], F32)
        t_wc = Tok()
        mmc = [0]

        def mm_ch(wtile, tw, c0, ncol, tb):
            i = mmc[0]
            mmc[0] += 1
            p, tp = pm[i % 3], t_pm[i % 3]
            for kt in range(KT):
                S.op("pe", lambda e, kt=kt, p=p: e.matmul(p[0:ncol, :], lhsT=wtile[:, kt, c0:c0 + ncol],
                                                       rhs=hT[:, kt, tb * 512:(tb + 1) * 512], start=(kt == 0),
                                                       stop=(kt == KT - 1)), reads=[t_hT, tw], writes=[tp])
            return p, tp

        def shift(p, tp, n_, mu_col, cidx, dst, t_dst):
            raw, dsh = B["raw"], B["dsh"]
            S.op("act", lambda e: e.copy(out=raw[0:n_, :], in_=p[0:n_, :]), reads=[tp], writes=[T["raw"]])
            S.op("dve", lambda e: e.tensor_tensor(out=dsh[0:n_, 1:512], in0=raw[0:n_, 0:511], in1=raw[0:n_, 1:512],
                                                  op=ALU.subtract), reads=[T["raw"]], writes=[T["dsh"]])
            S.op("dve", lambda e: e.tensor_tensor(out=dsh[0:n_, 0:1], in0=carry[0:n_, cidx:cidx + 1],
                                                  in1=raw[0:n_, 0:1], op=ALU.subtract),
                 reads=[T["raw"], t_carry], writes=[T["dsh"]])
            S.op("act", lambda e: e.copy(out=carry[0:n_, cidx:cidx + 1], in_=raw[0:n_, 511:512]),
                 reads=[T["raw"], T["dsh"]], writes=[t_carry])
            S.op("dve", lambda e: e.scalar_tensor_tensor(out=dst, in0=dsh[0:n_, :], scalar=pc[0:n_, mu_col:mu_col + 1],
                                                         in1=raw[0:n_, :], op0=ALU.mult, op1=ALU.add),
                 reads=[T["dsh"], T["raw"], t_pc], writes=[t_dst])

        wtile, tw = load_w(wb, R_DW, 64)
        for tb in range(nb):
            p, tp = mm_ch(wtile, tw, 0, 64, tb)
            shift(p, tp, 64, 80, 0, B["tmp"][0:64, :], T["tmp"])
            S.op("act", lambda e, tb=tb: e.activation(out=th_all[:, tb * 512:(tb + 1) * 512], in_=B["tmp"][0:64, :],
                                                     func=AF.Tanh), reads=[T["tmp"]], writes=[t_lora])
        wtile, tw = load_w(wb, R_DA, 64 + 160)
        for tb in range(nb):
            p, tp = mm_ch(wtile, tw, 0, 64, tb)
            shift(p, tp, 64, 81, 1, B["tmp"][0:64, :], T["tmp"])
            S.op("act", lambda e, tb=tb: e.copy(out=da_all[:, tb * 512:(tb + 1) * 512], in_=B["tmp"][0:64, :]),
                 reads=[T["tmp"]], writes=[t_lora])
            p, tp = mm_ch(wtile, tw, 64, 128, tb)
            shift(p, tp, 128, 82, 2, B["tmp"][:, :], T["tmp"])
            S.op("act", lambda e, tb=tb: e.activation(out=dg0_all[:, tb * 512:(tb + 1) * 512], in_=B["tmp"][:, :],
                                                     func=AF.Sigmoid), reads=[T["tmp"]], writes=[t_lora])
            p, tp = mm_ch(wtile, tw, 192, 32, tb)
            shift(p, tp, 32, 83, 3, B["tmp"][0:32, :], T["tmp"])
            S.op("act", lambda e, tb=tb: e.activation(out=dg1_all[:, tb * 512:(tb + 1) * 512], in_=B["tmp"][0:32, :],
                                                     func=AF.Sigmoid), reads=[T["tmp"]], writes=[t_lora])

        def v3(ap):
            return ap.rearrange("p (c t) -> p c t", t=64)

        for ct in range(8):
            wst_, t_wst, wbfs_, t_wbfs, cnt_ = wb
            i = cnt_[0]
            cnt_[0] += 1
            wtile, tw = wbfs_[i % 2], t_wbfs[i % 2]
            for j, c0 in enumerate((R_R, R_K, R_V)):
                src = w_in[:, c0 + ct * 128:c0 + (ct + 1) * 128].rearrange("(k p) c -> p k c", p=128)
                S.dma("sp", lambda e, src=src, j=j: e.dma_start(out=wst_[:, :, j * 128:(j + 1) * 128], in_=src),
                      writes=[t_wst])
            S.op("act", lambda e, wtile=wtile: e.copy(out=wtile[:, :, 0:384], in_=wst_[:, :, 0:384]),
                 reads=[t_wst], writes=[tw])
            for tb in range(nb):
                t0 = tb * 512
                for j, nm in enumerate(("r", "k", "v")):
                    p, tp = mm_ch(wtile, tw, j * 128, 128, tb)
                    shift(p, tp, 128, j * 8 + ct, 4 + j * 8 + ct, B[nm][:], T[nm])
                cs = slice(ct * 128, (ct + 1) * 128)
                pz, tpz = px[0], t_px[0]
                S.op("pe", lambda e, pz=pz, cs=cs, t0=t0: e.matmul(pz[:], lhsT=wdu[:, cs], rhs=th_all[:, t0:t0 + 512],
                                                             start=True, stop=True), reads=[t_lw, t_lora], writes=[tpz])
                S.op("act", lambda e, pz=pz, ct=ct: e.activation(out=B["lw"][:], in_=pz[:], func=AF.Sigmoid,
                                                               bias=pc[:, 24 + ct:25 + ct]),
                     reads=[tpz, t_pc], writes=[T["lw"]])
                S.op("act", lambda e: e.mul(out=B["lw"][:], in_=B["lw"][:], mul=-0.6065306597126334),
                     reads=[T["lw"]], writes=[T["lw"]])
                pa, tpa = px[1], t_px[1]
                S.op("pe", lambda e, pa=pa, cs=cs, t0=t0: e.matmul(pa[:], lhsT=wau[:, cs], rhs=da_all[:, t0:t0 + 512],
                                                             start=True, stop=True), reads=[t_lw, t_lora], writes=[tpa])
                S.op("act", lambda e, pa=pa, ct=ct: e.activation(out=B["a"][:], in_=pa[:], func=AF.Sigmoid,
                                                               bias=pc[:, 32 + ct:33 + ct]),
                     reads=[tpa, t_pc], writes=[T["a"]])
                if own:
                    pg, tpg = px[0], t_px[0]
                    S.op("pe", lambda e, pg=pg, cs=cs, t0=t0: e.matmul(pg[:], lhsT=wgu0[:, cs], rhs=dg0_all[:, t0:t0 + 512],
                                                                 start=True, stop=False), reads=[t_lw, t_lora], writes=[tpg])
                    S.op("pe", lambda e, pg=pg, cs=cs, t0=t0: e.matmul(pg[:], lhsT=wgu1[:, cs], rhs=dg1_all[:, t0:t0 + 512],
                                                                 start=False, stop=True), reads=[t_lw, t_lora], writes=[tpg])
                    S.op("act", lambda e, pg=pg: e.copy(out=hb["gt"][:], in_=pg[:]), reads=[tpg], writes=[TH["gt"]])
                    S.dma("sp", lambda e, cs=cs, t0=t0: e.dma_start(out=GTs[cs, t0:t0 + 512], in_=hb["gt"][:]),
                          reads=[TH["gt"]])
                S.op("dve", lambda e, ct=ct: e.tensor_scalar(out=B["kk"][:], in0=B["k"][:], scalar1=pc[:, 40 + ct:41 + ct],
                                                            scalar2=None, op0=ALU.mult),
                     reads=[T["k"], t_pc], writes=[T["kk"]])
                S.op("act", lambda e: e.activation(out=hb["sqb"][:], in_=B["kk"][:], func=AF.Square),
                     reads=[T["kk"]], writes=[TH["sqb"]])
                pss, tpss = px[1], t_px[1]
                S.op("pe", lambda e, pss=pss: e.matmul(pss[:], lhsT=bones[:], rhs=hb["sqb"][:], start=True, stop=True),
                     reads=[t_lw, TH["sqb"]], writes=[tpss])
                S.op("act", lambda e, pss=pss: e.activation(out=B["tmp"][:], in_=pss[:], func=AF.Sqrt),
                     reads=[tpss], writes=[T["tmp"]])
                S.op("dve", lambda e: e.tensor_scalar(out=B["tmp"][:], in0=B["tmp"][:], scalar1=1e-12, scalar2=None,
                                                      op0=ALU.max), reads=[T["tmp"]], writes=[T["tmp"]])
                S.op("dve", lambda e: e.reciprocal(out=B["tmp"][:], in_=B["tmp"][:]), reads=[T["tmp"]], writes=[T["tmp"]])
                S.op("dve", lambda e: e.tensor_tensor(out=B["kkn"][:], in0=B["kk"][:], in1=B["tmp"][:], op=ALU.mult),
                     reads=[T["kk"], T["tmp"]], writes=[T["kkn"]])
                S.op("act", lambda e, ct=ct: e.activation(out=B["k2"][:], in_=B["a"][:], func=AF.Identity,
                                                         scale=pc[:, 48 + ct:49 + ct], bias=omk[:, ct:ct + 1]),
                     reads=[T["a"], t_pc], writes=[T["k2"]])
                S.op("dve", lambda e: e.tensor_tensor(out=B["k2"][:], in0=B["k2"][:], in1=B["k"][:], op=ALU.mult),
                     reads=[T["k2"], T["k"]], writes=[T["k2"]])
                S.op("dve", lambda e: e.tensor_tensor(out=B["b"][:], in0=B["kkn"][:], in1=B["a"][:], op=ALU.mult),
                     reads=[T["kkn"], T["a"]], writes=[T["b"]])
                if own:
                    S.op("dve", lambda e, ct=ct: e.scalar_tensor_tensor(out=hb["rkb"][:], in0=B["r"][:],
                                                                       scalar=pc[:, 56 + ct:57 + ct], in1=B["k2"][:],
                                                                       op0=ALU.mult, op1=ALU.mult),
                         reads=[T["r"], T["k2"], t_pc], writes=[TH["rkb"]])
                    prk, tprk = px[0], t_px[0]
                    S.op("pe", lambda e, prk=prk: e.matmul(prk[:], lhsT=bones[:], rhs=hb["rkb"][:], start=True, stop=True),
                         reads=[t_lw, TH["rkb"]], writes=[tprk])
                    S.op("dve", lambda e, prk=prk: e.tensor_tensor(out=hb["bn"][:], in0=prk[:], in1=B["v"][:], op=ALU.mult),
                         reads=[tprk, T["v"]], writes=[TH["bn"]])
                    S.dma("sp", lambda e, cs=cs, t0=t0: e.dma_start(out=BNs[cs, t0:t0 + 512], in_=hb["bn"][:]),
                          reads=[TH["bn"]])
                src, t_src = B["lw"], T["lw"]
                for si, sft in enumerate((1, 2, 4, 8, 16, 32)):
                    dn = "cA" if si % 2 == 0 else "cB"
                    dst_, t_dst_ = B[dn], T[dn]
                    S.op("act", lambda e, src=src, dst_=dst_, sft=sft: e.copy(
                        out=v3(dst_[:])[:, :, 0:sft], in_=v3(src[:])[:, :, 0:sft]), reads=[t_src], writes=[t_dst_])
                    S.op("dve", lambda e, src=src, dst_=dst_, sft=sft: e.tensor_tensor(
                        out=v3(dst_[:])[:, :, sft:64], in0=v3(src[:])[:, :, sft:64], in1=v3(src[:])[:, :, 0:64 - sft],
                        op=ALU.add), reads=[t_src], writes=[t_dst_])
                    src, t_src = dst_, t_dst_
                cl, t_cl = src, t_src
                S.op("act", lambda e, cl=cl: e.activation(out=B["e"][:], in_=cl[:], func=AF.Exp), reads=[t_cl], writes=[T["e"]])
                S.op("dve", lambda e: e.tensor_tensor(out=cm[:, 3, :], in0=B["r"][:], in1=B["e"][:], op=ALU.mult),
                     reads=[T["r"], T["e"]], writes=[t_cm])
                S.op("dve", lambda e, cl=cl: e.tensor_tensor(out=B["tmp"][:], in0=cl[:], in1=B["lw"][:], op=ALU.subtract),
                     reads=[t_cl, T["lw"]], writes=[T["tmp"]])
                S.op("act", lambda e: e.activation(out=B["e"][:], in_=B["tmp"][:], func=AF.Exp),
                     reads=[T["tmp"]], writes=[T["e"]])
                S.op("dve", lambda e: e.tensor_tensor(out=cm[:, 2, :], in0=B["kkn"][:], in1=B["e"][:], op=ALU.mult),
                     reads=[T["kkn"], T["e"]], writes=[t_cm])
                S.op("act", lambda e, cl=cl: e.activation(out=B["e"][:], in_=cl[:], func=AF.Exp, scale=-1.0),
                     reads=[t_cl], writes=[T["e"]])
                S.op("dve", lambda e: e.tensor_tensor(out=cm[:, 1, :], in0=B["k2"][:], in1=B["e"][:], op=ALU.mult),
                     reads=[T["k2"], T["e"]], writes=[t_cm])
                S.op("dve", lambda e: e.tensor_tensor(out=cm[:, 0, :], in0=B["b"][:], in1=B["e"][:], op=ALU.mult),
                     reads=[T["b"], T["e"]], writes=[t_cm])
                S.op("dve", lambda e, cl=cl: e.tensor_tensor(out=v3(B["tmp"][:]), in0=v3(cl[:])[:, :, 63:64].broadcast_to([128, 8, 64]),
                                                            in1=v3(cl[:]), op=ALU.subtract), reads=[t_cl], writes=[T["tmp"]])
                S.op("act", lambda e: e.activation(out=B["e"][:], in_=B["tmp"][:], func=AF.Exp),
                     reads=[T["tmp"]], writes=[T["e"]])
                S.op("dve", lambda e: e.tensor_tensor(out=hb["tmk"][:], in0=B["k2"][:], in1=B["e"][:], op=ALU.mult),
                     reads=[T["k2"], T["e"]], writes=[TH["tmk"]])
                S.op("dve", lambda e: e.tensor_tensor(out=hb["tmb"][:], in0=B["b"][:], in1=B["e"][:], op=ALU.mult),
                     reads=[T["b"], T["e"]], writes=[TH["tmb"]])
                S.op("act", lambda e: e.copy(out=hb["vb"][:], in_=B["v"][:]), reads=[T["v"]], writes=[TH["vb"]])
                S.op("act", lambda e, cl=cl: e.activation(out=wc[:].unsqueeze(2), in_=v3(cl[:])[:, :, 63:64], func=AF.Exp),
                     reads=[t_cl], writes=[t_wc])
                g0 = (tok0 + t0) // 128
                for hh in range(2):
                    hd_ = 2 * ct + hh
                    for j in range(4):
                        dst = CMs[g0 + j, :, hd_, :, :]
                        srcap = cm[hh * 64:(hh + 1) * 64, :, j * 128:(j + 1) * 128]
                        S.dma("sp", lambda e, dst=dst, srcap=srcap: e.dma_start(out=dst, in_=srcap), reads=[t_cm])
                    c0_ = (tok0 + t0) // 64
                    S.dma("sp", lambda e, hh=hh, hd_=hd_, c0_=c0_: e.dma_start(
                        out=WCs[:, hd_, c0_:c0_ + 8], in_=wc[hh * 64:(hh + 1) * 64, :]), reads=[t_wc])
                for j in range(4):
                    srcs = (cm[:, 2, j * 128:(j + 1) * 128], hb["tmb"][:, j * 128:(j + 1) * 128],
                            hb["tmk"][:, j * 128:(j + 1) * 128], hb["vb"][:, j * 128:(j + 1) * 128])
                    for x_, sa in enumerate(srcs):
                        S.op("pe", lambda e, x_=x_, sa=sa: e.transpose(out=ptT[:, x_ * 128:(x_ + 1) * 128], in_=sa,
                                                                     identity=ident[:]),
                             reads=[t_cm, TH["tmb"], TH["tmk"], TH["vb"], t_ident], writes=[t_ptT])
                    S.op("act", lambda e: e.copy(out=tmt[:], in_=ptT.rearrange("p (x c) -> p x c", x=4)),
                         reads=[t_ptT], writes=[t_tmt])
                    S.dma("sp", lambda e, g0=g0, j=j, cs=cs: e.dma_start(out=TMs[g0 + j, :, :, cs], in_=tmt[:]),
                          reads=[t_tmt])


    NGP = NP // 128

    def phase_rwkv_scan(st):
        mk = sb(st, "mk", [128, 3, 128], F32)
        pc = sb(st, "pc2", [128, 84], F32)
        t_c = Tok()
        S.dma("sp", lambda e: e.dma_start(out=mk[:], in_=cmats_in[1:4].rearrange("m p c -> p m c")), writes=[t_c])
        S.dma("sp", lambda e: e.dma_start(out=pc[:], in_=pcols_in), writes=[t_c])
        cmg = [sb(st, "cmg%d" % i, [64, 16, 4, 128], BF16) for i in range(2)]
        tmg = [sb(st, "tmg%d" % i, [128, 4, RW], BF16) for i in range(2)]
        wcg = [sb(st, "wcg%d" % i, [64, 16, 2], F32) for i in range(2)]
        t_ld = [Tok(), Tok()]
        H = sb(st, "H", [64, 16, 64], BF16)
        t_H = [Tok() for _ in range(16)]
        S.op("dve", lambda e: e.memset(H[:], 0.0), writes=t_H)
        PB = [ps(st, "pb%d" % i, [128, 512], F32) for i in range(7)]
        t_PB = [Tok() for _ in range(7)]
        pTp = ps(st, "pTp", [128, 8, 128], BF16)
        t_pTp = Tok()
        pbc = [0]

        def bank():
            i = pbc[0] % 7
            pbc[0] += 1
            return PB[i], t_PB[i]
        names = ["LL", "G2", "Rb", "P0", "PT0", "P1", "PT1", "QpT", "AiT", "Kp", "M0", "M1"]
        NBUF = 4
        hk = [sb(st, "hk%d" % i, [64, 64], F32) for i in range(NBUF)]
        t_hk = [Tok() for _ in range(NBUF)]
        W = {n: [sb(st, "sw_%s%d" % (n, i), [128, 256], BF16) for i in range(NBUF)] for n in names}
        TW = {n: [Tok() for _ in range(NBUF)] for n in names}
        lqk = [sb(st, "lqk%d" % i, [128, 128], F32) for i in range(NBUF)]
        t_lqk = [Tok() for _ in range(NBUF)]
        ysb = sb(st, "ysb", [64, 2, RW], F32)
        t_ysb = Tok()
        ynb = sb(st, "ynb", [64, 2, RW], BF16)
        st1 = sb(st, "st1", [64, 2, 16], F32)
        st2 = sb(st, "st2", [64, 2, 16], F32)
        dd = sb(st, "dd", [64, 2, RW], F32)
        sq2 = sb(st, "sq2", [64, 2, RW], F32)
        t_post = Tok()
        bng = sb(st, "bng", [128, 8, 128], BF16)
        gtg = sb(st, "gtg", [128, 8, 128], BF16)
        t_bg = Tok()
        fin = sb(st, "fin", [128, 8, 128], F32)
        yrt = sb(st, "yrt", [128, 8, 128], BF16)
        t_fin, t_yrt = Tok(), Tok()
        engs = ("dve", "act")
        for g_ in range(NG):
            b = g_ % 2
            own_g = g_ >= NGP
            S.dma("sp", lambda e, b=b, g_=g_: e.dma_start(out=cmg[b][:], in_=CMs[g_]), writes=[t_ld[b]])
            S.dma("sp", lambda e, b=b, g_=g_: e.dma_start(out=tmg[b][:], in_=TMs[g_]), writes=[t_ld[b]])
            S.dma("sp", lambda e, b=b, g_=g_: e.dma_start(out=wcg[b][:], in_=WCs[:, :, 2 * g_:2 * g_ + 2]), writes=[t_ld[b]])
            LVL = int(os.environ.get("SCAN_LEVEL", "9"))

            def head_gen(h, b=b, g_=g_, own_g=own_g):
                u = h % NBUF
                hs = slice(h * 64, (h + 1) * 64)
                bT, kT, kkT, qT = (cmg[b][:, h, x_, :] for x_ in range(4))
                KKt, Bh, Kh, V = (tmg[b][:, x_, hs] for x_ in range(4))
                LL, G2, Rb = W["LL"][u], W["G2"][u], W["Rb"][u]
                p1, tp1 = bank()
                S.op("pe", lambda e, p1=p1, kkT=kkT, b=b, h=h: e.matmul(p1[:, 0:256], lhsT=kkT,
                     rhs=cmg[b][:, h, 0:2, :].rearrange("p x t -> p (x t)"), start=True, stop=True), reads=[t_ld[b]], writes=[tp1])
                S.op("dve", lambda e, p1=p1, LL=LL: e.tensor_tensor(out=LL[:].rearrange("p (x c) -> p x c", x=2),
                     in0=p1[:, 0:256].rearrange("p (x c) -> p x c", x=2),
                     in1=mk[:, 0, :].unsqueeze(1).broadcast_to([128, 2, 128]), op=ALU.mult), reads=[tp1, t_c], writes=[TW["LL"][u]])
                p2, tp2 = bank()
                S.op("pe", lambda e, p2=p2, bT=bT, b=b, h=h: e.matmul(p2[:, 0:256], lhsT=bT,
                     rhs=cmg[b][:, h, 2:4, :].rearrange("p x t -> p (x t)"), start=True, stop=True), reads=[t_ld[b]], writes=[tp2])
                S.op("dve", lambda e, p2=p2, G2=G2: e.tensor_tensor(out=G2[:].rearrange("p (x c) -> p x c", x=2),
                     in0=p2[:, 0:256].rearrange("p (x c) -> p x c", x=2), in1=mk[:, 1:3, :], op=ALU.mult),
                     reads=[tp2, t_c], writes=[TW["G2"][u]])
                p3, tp3 = bank()
                S.op("pe", lambda e, p3=p3, kT=kT, qT=qT: e.matmul(p3[:, 0:128], lhsT=kT, rhs=qT, start=True, stop=True),
                     reads=[t_ld[b]], writes=[tp3])
                S.op("dve", lambda e, p3=p3, u=u: e.tensor_tensor(out=lqk[u][:], in0=p3[:, 0:128], in1=mk[:, 2, :], op=ALU.mult),
                     reads=[tp3, t_c], writes=[t_lqk[u]])
                yield
                S.op("act", lambda e, Rb=Rb, KKt=KKt: e.copy(out=Rb[:, 0:64], in_=KKt), reads=[t_ld[b]], writes=[TW["Rb"][u]])
                S.op("act", lambda e, Rb=Rb, LL=LL: e.copy(out=Rb[:, 64:192], in_=LL[:, 128:256]), reads=[TW["LL"][u]], writes=[TW["Rb"][u]])
                Pc, tPc = LL[:, 0:128], TW["LL"][u]
                PTc, tPTc = G2[:, 0:128], TW["G2"][u]
                if LVL < 3:
                    return
                for k_ in range(6):
                    yield
                    if k_ > 0:
                        nP, tnP = W["P%d" % (k_ % 2)][u], TW["P%d" % (k_ % 2)][u]
                        nPT, tnPT = W["PT%d" % (k_ % 2)][u], TW["PT%d" % (k_ % 2)][u]
                        pa, tpa = bank()
                        pb_, tpb = bank()
                        S.op("pe", lambda e, pa=pa, PTc=PTc, Pc=Pc: e.matmul(pa[:, 0:128], lhsT=PTc, rhs=Pc, start=True, stop=True),
                             reads=[tPc, tPTc], writes=[tpa])
                        S.op("pe", lambda e, pb_=pb_, PTc=PTc, Pc=Pc: e.matmul(pb_[:, 0:128], lhsT=Pc, rhs=PTc, start=True, stop=True),
                             reads=[tPc, tPTc], writes=[tpb])
                        S.op("act", lambda e, pa=pa, nP=nP: e.copy(out=nP[:, 0:128], in_=pa[:, 0:128]), reads=[tpa], writes=[tnP])
                        S.op("act", lambda e, pb_=pb_, nPT=nPT: e.copy(out=nPT[:, 0:128], in_=pb_[:, 0:128]), reads=[tpb], writes=[tnPT])
                        Pc, tPc, PTc, tPTc = nP[:, 0:128], tnP, nPT[:, 0:128], tnPT
                    pr_, tpr = bank()
                    S.op("pe", lambda e, pr_=pr_, PTc=PTc, Rb=Rb: e.matmul(pr_[:, 0:192], lhsT=PTc, rhs=Rb[:, 0:192], start=True, stop=True),
                         reads=[tPTc, TW["Rb"][u]], writes=[tpr])
                    S.op("dve", lambda e, pr_=pr_, Rb=Rb, k_=k_: e.tensor_tensor(out=Rb[:, 0:192], in0=Rb[:, 0:192], in1=pr_[:, 0:192],
                         op=(ALU.subtract if k_ == 0 else ALU.add)), reads=[tpr, TW["Rb"][u]], writes=[TW["Rb"][u]])
                if LVL < 4:
                    return
                yield
                E, F = Rb[:, 0:64], Rb[:, 64:192]
                LqbT = G2[:, 128:256]
                QpT, AiT, Kp, M0, M1 = W["QpT"][u], W["AiT"][u], W["Kp"][u], W["M0"][u], W["M1"][u]
                pq, tpq = bank()
                S.op("pe", lambda e, pq=pq, E=E, LqbT=LqbT: e.matmul(pq[0:64, 0:128], lhsT=E, rhs=LqbT, start=True, stop=True),
                     reads=[TW["Rb"][u], TW["G2"][u]], writes=[tpq])
                S.op("dve", lambda e, pq=pq, QpT=QpT, qT=qT: e.tensor_tensor(out=QpT[0:64, 0:128], in0=qT, in1=pq[0:64, 0:128], op=ALU.subtract),
                     reads=[tpq, t_ld[b]], writes=[TW["QpT"][u]])
                yield
                pa2, tpa2 = bank()
                S.op("pe", lambda e, pa2=pa2, F=F, LqbT=LqbT: e.matmul(pa2[:, 0:128], lhsT=F, rhs=LqbT, start=True, stop=True),
                     reads=[TW["Rb"][u], TW["G2"][u]], writes=[tpa2])
                S.op("dve", lambda e, pa2=pa2, AiT=AiT, u=u: e.tensor_tensor(out=AiT[:, 0:128], in0=lqk[u][:], in1=pa2[:, 0:128], op=ALU.subtract),
                     reads=[tpa2, t_lqk[u]], writes=[TW["AiT"][u]])
                yield
                pk, tpk = bank()
                S.op("pe", lambda e, pk=pk, F=F, Bh=Bh: e.matmul(pk[:, 0:64], lhsT=F, rhs=Bh, start=True, stop=True),
                     reads=[TW["Rb"][u], t_ld[b]], writes=[tpk])
                S.op("dve", lambda e, pk=pk, Kp=Kp, Kh=Kh: e.tensor_tensor(out=Kp[:, 0:64], in0=Kh, in1=pk[:, 0:64], op=ALU.subtract),
                     reads=[tpk, t_ld[b]], writes=[TW["Kp"][u]])
                yield
                for c in range(2):
                    Mc, tMc = (M0, TW["M0"][u]) if c == 0 else (M1, TW["M1"][u])
                    rs = slice(c * 64, (c + 1) * 64)
                    pm_, tpm = bank()
                    S.op("pe", lambda e, pm_=pm_, rs=rs, Bh=Bh, Rb=Rb, b=b, h=h: e.matmul(pm_[0:64, 0:64], lhsT=Rb[rs, 0:64],
                         rhs=tmg[b][rs, 1, h * 64:(h + 1) * 64], start=True, stop=True), reads=[TW["Rb"][u], t_ld[b]], writes=[tpm])
                    S.op("dve", lambda e, pm_=pm_, Mc=Mc, c=c, b=b, h=h: e.scalar_tensor_tensor(out=Mc[0:64, 0:64], in0=ident_f[0:64, 0:64],
                         scalar=wcg[b][:, h, c:c + 1], in1=pm_[0:64, 0:64], op0=ALU.mult, op1=ALU.subtract),
                         reads=[tpm, t_ld[b], t_ident], writes=[tMc])
                if LVL < 5:
                    return
                for c in range(2):
                    Mc, tMc = (M0, TW["M0"][u]) if c == 0 else (M1, TW["M1"][u])
                    rs = slice(c * 64, (c + 1) * 64)
                    yield
                    if own_g:
                        py, tpy = bank()
                        py2, tpy2 = bank()
                        S.op("pe", lambda e, py=py, QpT=QpT, rs=rs, h=h: e.matmul(py[0:64, 0:64], lhsT=QpT[0:64, rs], rhs=H[:, h, :],
                             start=True, stop=True), reads=[TW["QpT"][u], t_H[h]], writes=[tpy])
                        S.op("pe", lambda e, py2=py2, AiT=AiT, rs=rs, b=b, h=h: e.matmul(py2[0:64, 0:64], lhsT=AiT[rs, rs],
                             rhs=tmg[b][rs, 3, h * 64:(h + 1) * 64], start=True, stop=True), reads=[TW["AiT"][u], t_ld[b]], writes=[tpy2])
                        S.op("act", lambda e, py=py, c=c, hs=hs: e.copy(out=ysb[:, c, hs], in_=py[0:64, 0:64]), reads=[tpy], writes=[t_ysb])
                        S.op("dve", lambda e, py2=py2, c=c, hs=hs: e.tensor_tensor(out=ysb[:, c, hs], in0=ysb[:, c, hs], in1=py2[0:64, 0:64],
                                                                                  op=ALU.add), reads=[tpy2, t_ysb], writes=[t_ysb])
                    ph, tph = bank()
                    ph2, tph2 = bank()
                    S.op("pe", lambda e, ph2=ph2, Kp=Kp, rs=rs, b=b, h=h: e.matmul(ph2[0:64, 0:64], lhsT=Kp[rs, 0:64],
                         rhs=tmg[b][rs, 3, h * 64:(h + 1) * 64], start=True, stop=True), reads=[TW["Kp"][u], t_ld[b]], writes=[tph2])
                    S.op("pe", lambda e, ph=ph, Mc=Mc, h=h: e.matmul(ph[0:64, 0:64], lhsT=Mc[0:64, 0:64], rhs=H[:, h, :], start=True, stop=True),
                         reads=[tMc, t_H[h]], writes=[tph])
                    S.op("act", lambda e, ph2=ph2, u=u: e.copy(out=hk[u][:], in_=ph2[0:64, 0:64]), reads=[tph2], writes=[t_hk[u]])
                    S.op("dve", lambda e, ph=ph, h=h, u=u: e.tensor_tensor(out=H[:, h, :], in0=hk[u][:], in1=ph[0:64, 0:64], op=ALU.add),
                         reads=[tph, t_hk[u]], writes=[t_H[h]])

            if LVL >= 2:
                gens = [head_gen(h) for h in range(16)]
                active = []
                nxt = 0
                while nxt < 16 or active:
                    while nxt < 16 and len(active) < NBUF:
                        active.append(gens[nxt])
                        nxt += 1
                    for gi in list(active):
                        try:
                            next(gi)
                        except StopIteration:
                            active.remove(gi)
            if own_g and LVL >= 6:
                go = g_ - NGP
                y4 = lambda ap: ap.rearrange("p c (h v) -> p c h v", v=64)
                S.dma("sp", lambda e, go=go: e.dma_start(out=bng[:], in_=BNs[:, go * 128:(go + 1) * 128].rearrange("(c p) t -> p c t", p=128)), writes=[t_bg])
                S.dma("sp", lambda e, go=go: e.dma_start(out=gtg[:], in_=GTs[:, go * 128:(go + 1) * 128].rearrange("(c p) t -> p c t", p=128)), writes=[t_bg])
                S.op("dve", lambda e: e.tensor_reduce(out=st1[:], in_=y4(ysb[:]), axis=AX.X, op=ALU.add), reads=[t_ysb], writes=[t_post])
                S.op("dve", lambda e: e.tensor_scalar(out=st1[:], in0=st1[:], scalar1=1.0 / 64, scalar2=None, op0=ALU.mult), reads=[t_post], writes=[t_post])
                S.op("dve", lambda e: e.tensor_tensor(out=y4(dd[:]), in0=y4(ysb[:]), in1=st1[:].unsqueeze(3).broadcast_to([64, 2, 16, 64]),
                                                      op=ALU.subtract), reads=[t_ysb, t_post], writes=[t_post])
                S.op("dve", lambda e: e.tensor_tensor(out=sq2[:], in0=dd[:], in1=dd[:], op=ALU.mult), reads=[t_post], writes=[t_post])
                S.op("dve", lambda e: e.tensor_reduce(out=st2[:], in_=y4(sq2[:]), axis=AX.X, op=ALU.add), reads=[t_post], writes=[t_post])
                S.op("act", lambda e: e.activation(out=st2[:], in_=st2[:], func=AF.Sqrt, scale=1.0 / 64, bias=GN_EPS), reads=[t_post], writes=[t_post])
                S.op("dve", lambda e: e.reciprocal(out=st2[:], in_=st2[:]), reads=[t_post], writes=[t_post])
                S.op("dve", lambda e: e.tensor_tensor(out=y4(ynb[:]), in0=y4(dd[:]), in1=st2[:].unsqueeze(3).broadcast_to([64, 2, 16, 64]),
                                                      op=ALU.mult), reads=[t_post], writes=[t_post])
                for c in range(2):
                    for ct in range(8):
                        S.op("pe", lambda e, c=c, ct=ct: e.transpose(out=pTp[:, ct, c * 64:(c + 1) * 64], in_=ynb[:, c, ct * 128:(ct + 1) * 128],
                                                                  identity=ident[0:64, 0:64]), reads=[t_post, t_ident], writes=[t_pTp])
                S.op("dve", lambda e: e.tensor_tensor(out=fin[:], in0=pTp[:], in1=pc[:, 64:72].unsqueeze(2).broadcast_to([128, 8, 128]), op=ALU.mult),
                     reads=[t_pTp, t_c], writes=[t_fin])
                S.op("dve", lambda e: e.tensor_tensor(out=fin[:], in0=fin[:], in1=pc[:, 72:80].unsqueeze(2).broadcast_to([128, 8, 128]), op=ALU.add),
                     reads=[t_fin, t_c], writes=[t_fin])
                S.op("dve", lambda e: e.tensor_tensor(out=fin[:], in0=fin[:], in1=bng[:], op=ALU.add), reads=[t_fin, t_bg], writes=[t_fin])
                S.op("dve", lambda e: e.tensor_tensor(out=yrt[:], in0=fin[:], in1=gtg[:], op=ALU.mult), reads=[t_fin, t_bg], writes=[t_yrt])
                S.dma("sp", lambda e, go=go: e.dma_start(out=YT[1024:2048, go * 128:(go + 1) * 128].rearrange("(c p) t -> p c t", p=128), in_=yrt[:]),
                      reads=[t_yrt])


    def phase_outproj(st):
        yT = sb(st, "yT", [128, KT, NO], BF16)
        t_yT = Tok()
        S.dma("sp", lambda e: e.dma_start(out=yT[:], in_=YT.rearrange("(k p) t -> p k t", p=128)), writes=[t_yT])
        wo = sb(st, "wo", [128, KT, D], BF16)
        wst = sb(st, "wsto", [128, KT, 512], F32)
        t_wst, t_wo = Tok(), Tok()
        for cb in range(4):
            S.dma("sp", lambda e, cb=cb: e.dma_start(out=wst[:], in_=w_out[:, cb * 512:(cb + 1) * 512].rearrange(
                "(k p) c -> p k c", p=128)), writes=[t_wst])
            if cb % 2:
                S.op("act", lambda e, cb=cb: e.copy(out=wo[:, :, cb * 512:(cb + 1) * 512], in_=wst[:]), reads=[t_wst], writes=[t_wo])
            else:
                S.op("dve", lambda e, cb=cb: e.tensor_copy(out=wo[:, :, cb * 512:(cb + 1) * 512], in_=wst[:]), reads=[t_wst], writes=[t_wo])
        wrs = sb(st, "wrs", [128, KT, 20], F32)
        wrb = sb(st, "wrb", [128, KT, 20], BF16)
        brb = sb(st, "brb", [128, 20], F32)
        gf = sb(st, "gf", [128, D], F32)
        t_r = Tok()
        S.dma("sp", lambda e: e.dma_start(out=wrs[:], in_=wr_in.rearrange("(k p) c -> p k c", p=128)), writes=[t_r])
        S.dma("sp", lambda e: e.dma_start(out=brb[:], in_=br_in.broadcast_to([128, 20])), writes=[t_r])
        S.dma("sp", lambda e: e.dma_start(out=gf[:], in_=g_ffn.broadcast_to([128, D])), writes=[t_r])
        S.op("dve", lambda e: e.tensor_copy(out=wrb[:], in_=wrs[:]), reads=[t_r], writes=[t_r])
        _xt = sb(st, "xo", [128, D], F32)
        _x1 = sb(st, "x1", [128, D], F32)
        xt = [_xt, _xt]
        x1 = [_x1, _x1]
        xn = sb(st, "xno", [128, D], BF16)
        junk = xn
        h2t = sb(st, "h2t", [128, KT, 128], BF16)
        ss = sb(st, "sso", [128, 2], F32)
        _a, _b = Tok(), Tok()
        t_xt, t_x1 = [_a, _a], [_b, _b]
        t_xn, t_h2t, t_ss = Tok(), Tok(), Tok()
        t_junk = t_xn
        po = [ps(st, "po%d" % i, [128, 512], F32) for i in range(4)]
        t_po = [Tok() for _ in range(4)]
        pst = ps(st, "psto", [128, D], BF16)
        t_pst = Tok()
        pl = ps(st, "pl", [128, 512], F32)
        t_pl = Tok()
        R = sb(st, "rt", [128, 96], F32)
        t_R = Tok()
        for it in range(NO // 128):
            b = it % 2
            r0 = NP + it * 128
            S.dma("sp", lambda e, b=b, r0=r0: e.dma_start(out=xt[b][:], in_=x[r0:r0 + 128, :]), writes=[t_xt[b]])
            for cb in range(4):
                for kt in range(KT):
                    S.op("pe", lambda e, cb=cb, kt=kt, it=it: e.matmul(po[cb][:], lhsT=yT[:, kt, it * 128:(it + 1) * 128],
                                                                       rhs=wo[:, kt, cb * 512:(cb + 1) * 512], start=(kt == 0),
                                                                       stop=(kt == KT - 1)), reads=[t_yT, t_wo], writes=[t_po[cb]])
                S.op("dve", lambda e, cb=cb, b=b: e.tensor_tensor(out=x1[b][:, cb * 512:(cb + 1) * 512], in0=po[cb][:],
                                                                 in1=xt[b][:, cb * 512:(cb + 1) * 512], op=ALU.add),
                     reads=[t_po[cb], t_xt[b]], writes=[t_x1[b]])
            S.dma("sp", lambda e, b=b, it=it: e.dma_start(out=X1[it * 128:(it + 1) * 128, :], in_=x1[b][:]), reads=[t_x1[b]])
            S.op("act", lambda e, b=b: e.activation(out=junk[:], in_=x1[b][:], func=AF.Square, accum_out=ss[:, 0:1]),
                 reads=[t_x1[b]], writes=[t_junk, t_ss])
            S.op("act", lambda e: e.activation(out=ss[:, 1:2], in_=ss[:, 0:1], func=AF.Sqrt, scale=1.0 / D, bias=NORM_EPS),
                 reads=[t_ss], writes=[t_ss])
            S.op("dve", lambda e: e.reciprocal(out=ss[:, 1:2], in_=ss[:, 1:2]), reads=[t_ss], writes=[t_ss])
            S.op("dve", lambda e, b=b: e.scalar_tensor_tensor(out=xn[:], in0=x1[b][:], scalar=ss[:, 1:2], in1=gf[:],
                                                             op0=ALU.mult, op1=ALU.mult), reads=[t_x1[b], t_ss, t_r], writes=[t_xn])
            for kt in range(KT):
                S.op("pe", lambda e, kt=kt: e.transpose(out=pst[:, kt * 128:(kt + 1) * 128], in_=xn[:, kt * 128:(kt + 1) * 128],
                                                       identity=ident[:]), reads=[t_xn, t_ident], writes=[t_pst])
            S.op("act", lambda e: e.copy(out=h2t[:], in_=pst[:].rearrange("p (k t) -> p k t", k=KT)), reads=[t_pst], writes=[t_h2t])
            S.dma("sp", lambda e, it=it: e.dma_start(out=H2T[:, it * 128:(it + 1) * 128].rearrange("(k p) t -> p k t", p=128),
                                                        in_=h2t[:]), reads=[t_h2t])
            for kt in range(KT):
                S.op("pe", lambda e, kt=kt: e.matmul(pl[:, 0:20], lhsT=h2t[:, kt, :], rhs=wrb[:, kt, :], start=(kt == 0),
                                                     stop=(kt == KT - 1)), reads=[t_h2t, t_r], writes=[t_pl])
            lg, gmx, ge, gs, gm = R[:, 0:20], R[:, 20:21], R[:, 21:25], R[:, 25:26], R[:, 26:30]
            tmp16, sel, m1, ee, t4 = R[:, 30:46], R[:, 46:50], R[:, 50:51], R[:, 51:55], R[:, 55:59]
            m2, mk2, ws, cmb = R[:, 59:60], R[:, 60:64], R[:, 64:65], R[:, 65:81]
            ngm, nm1 = R[:, 81:82], R[:, 82:83]
            def rop(eng, fn):
                S.op(eng, fn, reads=[t_R, t_pl, t_r], writes=[t_R])
            rop("dve", lambda e: e.tensor_tensor(out=lg, in0=pl[:, 0:20], in1=brb[:], op=ALU.add))
            rop("dve", lambda e: e.tensor_reduce(out=gmx, in_=R[:, 0:4], axis=AX.X, op=ALU.max))
            rop("dve", lambda e: e.tensor_scalar(out=ngm, in0=gmx, scalar1=-1.0, scalar2=None, op0=ALU.mult))
            rop("act", lambda e: e.activation(out=ge, in_=R[:, 0:4], func=AF.Exp, bias=ngm, accum_out=gs))
            rop("dve", lambda e: e.tensor_scalar(out=gm, in0=R[:, 0:4], scalar1=gmx, scalar2=None, op0=ALU.is_ge))
            rop("dve", lambda e: e.tensor_tensor(out=tmp16.rearrange("p (g j) -> p g j", g=4),
                                                 in0=R[:, 4:20].rearrange("p (g j) -> p g j", g=4),
                                                 in1=gm.unsqueeze(2).broadcast_to([128, 4, 4]), op=ALU.mult))
            rop("dve", lambda e: e.tensor_reduce(out=sel, in_=tmp16.rearrange("p (g j) -> p j g", g=4), axis=AX.X, op=ALU.add))
            rop("dve", lambda e: e.tensor_reduce(out=m1, in_=sel, axis=AX.X, op=ALU.max))
            rop("dve", lambda e: e.tensor_scalar(out=nm1, in0=m1, scalar1=-1.0, scalar2=None, op0=ALU.mult))
            rop("act", lambda e: e.activation(out=ee, in_=sel, func=AF.Exp, bias=nm1))
            rop("dve", lambda e: e.tensor_scalar(out=t4, in0=sel, scalar1=m1, scalar2=-1e30, op0=ALU.is_ge, op1=ALU.mult))
            rop("dve", lambda e: e.tensor_tensor(out=t4, in0=t4, in1=sel, op=ALU.add))
            rop("dve", lambda e: e.tensor_reduce(out=m2, in_=t4, axis=AX.X, op=ALU.max))
            rop("dve", lambda e: e.tensor_scalar(out=mk2, in0=sel, scalar1=m2, scalar2=None, op0=ALU.is_ge))
            rop("dve", lambda e: e.tensor_tensor(out=ee, in0=ee, in1=mk2, op=ALU.mult))
            rop("dve", lambda e: e.tensor_reduce(out=ws, in_=ee, axis=AX.X, op=ALU.add))
            rop("dve", lambda e: e.tensor_tensor(out=ws, in0=ws, in1=gs, op=ALU.mult))
            rop("dve", lambda e: e.reciprocal(out=ws, in_=ws))
            rop("dve", lambda e: e.tensor_scalar(out=ee, in0=ee, scalar1=ws, scalar2=None, op0=ALU.mult))
            rop("dve", lambda e: e.tensor_tensor(out=cmb.rearrange("p (g j) -> p g j", g=4),
                                                 in0=gm.unsqueeze(2).broadcast_to([128, 4, 4]),
                                                 in1=ee.unsqueeze(1).broadcast_to([128, 4, 4]), op=ALU.mult))
            S.dma("sp", lambda e, it=it: e.dma_start(out=CMB[it * 128:(it + 1) * 128, :], in_=cmb), reads=[t_R])

    def phase_moe(st):
        NB = NO // 512
        wg = [sb(st, "wg%d" % i, [128, KT, 512], BF16) for i in range(2)]
        wu = [sb(st, "wu%d" % i, [128, KT, 512], BF16) for i in range(2)]
        wd = [sb(st, "wd%d" % i, [128, 4, D], BF16) for i in range(2)]
        t_w = [[Tok(), Tok()] for _ in range(3)]
        h2 = sb(st, "h2m", [128, KT, 512], BF16)
        t_h2 = Tok()
        acc = sb(st, "accm", [128, 4, D], F32)
        t_acc = Tok()
        cmb = sb(st, "cmbm", [128, 4, 16], F32)
        t_cmb = Tok()
        xres = sb(st, "xres", [128, D], F32)
        t_xres = Tok()
        he = [sb(st, "he%d" % i, [128, 512], BF16) for i in range(4)]
        t_he = [Tok() for _ in range(4)]
        sg = [sb(st, "sgm%d" % i, [128, 512], F32) for i in range(2)]
        t_sg = [Tok(), Tok()]
        pg = [ps(st, "pg%d" % i, [128, 512], F32) for i in range(2)]
        pu = [ps(st, "pu%d" % i, [128, 512], F32) for i in range(2)]
        py = [ps(st, "py%d" % i, [128, 512], F32) for i in range(4)]
        t_pg, t_pu, t_py = [Tok(), Tok()], [Tok(), Tok()], [Tok() for _ in range(4)]
        seq = [(blk, ex) for blk in range(NB) for ex in range(NEXP)]

        def load(i):
            b = i % 2
            ex = seq[i][1]
            S.dma("pool", lambda e: e.dma_start(out=wg[b][:], in_=weg[ex].rearrange("(k p) c -> p k c", p=128)),
                  writes=[t_w[0][b]])
            S.dma("pool", lambda e: e.dma_start(out=wu[b][:], in_=weu[ex].rearrange("(k p) c -> p k c", p=128)),
                  writes=[t_w[1][b]])
            S.dma("pool", lambda e: e.dma_start(out=wd[b][:], in_=wed[ex].rearrange("(k p) c -> p k c", p=128)),
                  writes=[t_w[2][b]])
        load(0)
        for i, (blk, ex) in enumerate(seq):
            b = i % 2
            if ex == 0:
                S.dma("sp", lambda e, blk=blk: e.dma_start(out=h2[:], in_=H2T[:, blk * 512:(blk + 1) * 512].rearrange(
                    "(k p) t -> p k t", p=128)), writes=[t_h2])
                S.dma("sp", lambda e, blk=blk: e.dma_start(out=cmb[:], in_=CMB[blk * 512:(blk + 1) * 512, :].rearrange(
                    "(n p) c -> p n c", p=128)), writes=[t_cmb])
            if i + 1 < len(seq):
                load(i + 1)
            for ft in range(4):
                fb = ft % 2
                for kt in range(KT):
                    S.op("pe", lambda e, kt=kt, ft=ft, fb=fb, b=b: e.matmul(pg[fb][:], lhsT=wg[b][:, kt, ft * 128:(ft + 1) * 128],
                                                                           rhs=h2[:, kt, :], start=(kt == 0), stop=(kt == KT - 1)),
                         reads=[t_w[0][b], t_h2], writes=[t_pg[fb]])
                for kt in range(KT):
                    S.op("pe", lambda e, kt=kt, ft=ft, fb=fb, b=b: e.matmul(pu[fb][:], lhsT=wu[b][:, kt, ft * 128:(ft + 1) * 128],
                                                                           rhs=h2[:, kt, :], start=(kt == 0), stop=(kt == KT - 1)),
                         reads=[t_w[1][b], t_h2], writes=[t_pu[fb]])
                S.op("act", lambda e, fb=fb: e.activation(out=sg[fb][:], in_=pg[fb][:], func=AF.Silu), reads=[t_pg[fb]], writes=[t_sg[fb]])
                S.op("dve", lambda e, fb=fb, ft=ft: e.tensor_tensor(out=he[ft][:], in0=sg[fb][:], in1=pu[fb][:], op=ALU.mult),
                     reads=[t_sg[fb], t_pu[fb]], writes=[t_he[ft]])
            for tt in range(4):
                for cb in range(4):
                    for ft in range(4):
                        S.op("pe", lambda e, tt=tt, cb=cb, ft=ft, b=b: e.matmul(py[cb][:], lhsT=he[ft][:, tt * 128:(tt + 1) * 128],
                                                                               rhs=wd[b][:, ft, cb * 512:(cb + 1) * 512],
                                                                               start=(ft == 0), stop=(ft == 3)),
                             reads=[t_he[ft], t_w[2][b]], writes=[t_py[cb]])
                    dst = acc[:, tt, cb * 512:(cb + 1) * 512]
                    if ex == 0:
                        S.op("dve", lambda e, dst=dst, cb=cb, tt=tt, ex=ex: e.tensor_scalar(
                            out=dst, in0=py[cb][:], scalar1=cmb[:, tt, ex:ex + 1], scalar2=None, op0=ALU.mult),
                             reads=[t_py[cb], t_cmb], writes=[t_acc])
                    else:
                        S.op("dve", lambda e, dst=dst, cb=cb, tt=tt, ex=ex: e.scalar_tensor_tensor(
                            out=dst, in0=py[cb][:], scalar=cmb[:, tt, ex:ex + 1], in1=dst, op0=ALU.mult, op1=ALU.add),
                             reads=[t_py[cb], t_cmb, t_acc], writes=[t_acc])
            if ex == NEXP - 1:
                for tt in range(4):
                    r0 = blk * 512 + tt * 128
                    S.dma("sp", lambda e, r0=r0: e.dma_start(out=xres[:], in_=X1[r0:r0 + 128, :]), writes=[t_xres])
                    S.op("dve", lambda e, tt=tt: e.tensor_tensor(out=acc[:, tt, :], in0=acc[:, tt, :], in1=xres[:], op=ALU.add),
                         reads=[t_xres, t_acc], writes=[t_acc])
                    S.dma("sp", lambda e, r0=r0, tt=tt: e.dma_start(out=out[r0:r0 + 128, :], in_=acc[:, tt, :]), reads=[t_acc])

    for (tok0, ntok, own) in ((0, NP, False), (NP, NO, True)):
        with ExitStack() as st:
            hT = sb(st, "hT", [128, KT, ntok], BF16)
            t_hT = Tok("hT")
            with ExitStack() as st2:
                phase_norm(st2, hT, t_hT, tok0, ntok)
                S.barrier()
                S.emit()
            with ExitStack() as st2:
                phase_dsa_proj(st2, hT, t_hT, tok0, ntok, own)
                S.barrier()
                S.emit()
            with ExitStack() as st2:
                phase_rwkv_prep(st2, hT, t_hT, tok0, ntok, own)
                S.barrier()
                S.emit()

    with ExitStack() as st:
        phase_rwkv_scan(st)
        S.barrier()
        S.emit()
    with ExitStack() as st:
        maskT = sb(st, "maskT", [128, MT_TILES * 128], BF16)
        t_maskT = Tok()
        with ExitStack() as st2:
            phase_index(st2, maskT, t_maskT)
            S.barrier()
            S.emit()
        with ExitStack() as st2:
            phase_attn(st2, maskT, t_maskT)
            S.barrier()
            S.emit()

    with ExitStack() as st:
        phase_outproj(st)
        S.barrier()
        S.emit()
    with ExitStack() as st:
        phase_moe(st)
        S.barrier()
        S.emit()
    es.close()
    return nc


def rope_tables(pos):
    pos = pos.astype(np.float32)
    tabs = np.zeros((pos.shape[0], 192), np.float32)
    inv128 = (10000.0 ** (-np.arange(64, dtype=np.float32) * (2.0 / 128))).astype(np.float32)
    inv64 = (10000.0 ** (-np.arange(32, dtype=np.float32) * (2.0 / 64))).astype(np.float32)
    a = pos[:, None] * inv128[None, :]
    tabs[:, 0:64] = np.cos(a)
    tabs[:, 64:128] = np.sin(a)
    a = pos[:, None] * inv64[None, :]
    tabs[:, 128:160] = np.cos(a)
    tabs[:, 160:192] = np.sin(a)
    return tabs


def pcols_np(inp):
    pc = np.zeros((128, 84), np.float32)
    sm = inp["rwkv_shift_mix"].reshape(-1)
    vecs = [sm[0:1024], sm[1088:2112], sm[2112:3136], inp["w0"].reshape(-1), inp["a0"].reshape(-1),
            inp["k_k"].reshape(-1), inp["k_a"].reshape(-1), inp["r_k"].reshape(-1), inp["ln_x_w"].reshape(-1),
            inp["ln_x_b"].reshape(-1)]
    for j, v in enumerate(vecs):
        pc[:, j * 8:(j + 1) * 8] = v.reshape(8, 128).T
    pc[0:64, 80] = sm[1024:1088]
    pc[0:64, 81] = sm[3136:3200]
    pc[:, 82] = sm[3200:3328]
    pc[0:32, 83] = sm[3328:3360]
    return pc


def cmats_np():
    m = np.zeros((4, 128, 128), np.float32)
    for blk in range(2):
        o = blk * 64
        m[0, o:o + 64, o:o + 64] = 1.0
        m[1, o:o + 64, o:o + 64] = np.tril(np.ones((64, 64), np.float32), -1)
        m[2, o:o + 64, o:o + 64] = np.triu(np.ones((64, 64), np.float32), 1)
        m[3, o:o + 64, o:o + 64] = np.triu(np.ones((64, 64), np.float32), 0)
    return m


def cmask_np():
    m = np.zeros((128, 128), np.float32)
    m[0:64, 64:128] = -1e30
    return m


def kernel(**inputs):
    NP_, NO_ = 2048, 2048
    inp = {k: np.asarray(v) for k, v in inputs.items()}
    xfull = inp["x"]
    B = xfull.shape[0]
    nc = build(NP_, NO_, 256)
    p0 = {k: v[0] for k, v in inp.items() if k != "x"}
    shared = dict(g_mix=inp["g_mix"], w_in=p0["w_in"], ident_in=np.eye(128, dtype=np.float32), q_gain=inp["q_gain"],
                  k_gain=inp["k_gain"], cmask=cmask_np(), pcols=pcols_np(p0), cmats=cmats_np(),
                  lnrow=np.stack([p0["ln_x_w"], p0["ln_x_b"]]), w_decay_up=p0["w_decay_up"], w_aicl_up=p0["w_aicl_up"],
                  w_gate_lora_up=p0["w_gate_lora_up"], w_out=p0["w_out"], g_ffn=inp["g_ffn"],
                  wr=np.ascontiguousarray(np.concatenate([p0["w_route_group"], p0["w_route_expert"]], axis=1)),
                  br=np.ascontiguousarray(np.concatenate([inp["b_route_group"], inp["b_route_expert"]], axis=1)),
                  w_e_gate=p0["w_e_gate"], w_e_up=p0["w_e_up"], w_e_down=p0["w_e_down"])
    in_maps = []
    for c in range(2 * B):
        b, s_ = c // 2, c % 2
        if s_ == 0:
            xs = np.concatenate([np.zeros((NP_, D), np.float32), xfull[b, :NO_]], 0)
            pos = np.concatenate([np.zeros(NP_), np.arange(NO_)])
            pb = np.full((128, 1), -1e30, np.float32)
        else:
            xs = np.ascontiguousarray(xfull[b])
            pos = np.arange(NP_ + NO_)
            pb = np.zeros((128, 1), np.float32)
        m = dict(shared)
        m.update(x=xs, rope_tab=rope_tables(pos), pbias=pb)
        in_maps.append(m)
    res = run_bass_kernel_spmd(nc, in_maps, core_ids=list(range(2 * B)))
    outp = np.zeros_like(xfull)
    for c in range(2 * B):
        b, s_ = c // 2, c % 2
        outp[b, s_ * NO_:(s_ + 1) * NO_] = res.results[c]["out"]
    return outp
```
